# Optimizing a Trainium2 kernel written in Bass

```python
import math
import jax
import jax.numpy as jnp
from jax import lax
import numpy as np

D_MODEL = 2048
BATCH = 4
SEQ = 2048
DEPTH = 1

D_PLE = 256
MIX_WIDTH = D_MODEL
DIFF_WIDTH = MIX_WIDTH // 2
DIFF_HEAD_DIM = 64
DIFF_V_DIM = 2 * DIFF_HEAD_DIM
DIFF_HEADS = DIFF_WIDTH // DIFF_V_DIM
DIFF_QK_WIDTH = DIFF_HEADS * 2 * DIFF_HEAD_DIM
RWKV_WIDTH = MIX_WIDTH - DIFF_WIDTH
RWKV_HEAD_SIZE = 64
RWKV_HEADS = RWKV_WIDTH // RWKV_HEAD_SIZE
LORA_DECAY = 64
LORA_AAA = 64
LORA_GATE = 160
RWKV_PROJ = 3 * RWKV_WIDTH + LORA_DECAY + LORA_AAA + LORA_GATE
O_RWKV = 2 * DIFF_QK_WIDTH + DIFF_WIDTH
W_IN_COLS = O_RWKV + RWKV_PROJ
REL_BUCKETS = 32
REL_MAX_DIST = 128
Q_BLOCK = 128
N_GROUPS = 4
EXPERTS_PER_GROUP = 8
N_EXPERTS = N_GROUPS * EXPERTS_PER_GROUP
TOP_K_EXPERTS = 2
D_EXPERT = D_MODEL // 4
LN_EPS = 1e-5
RWKV_GN_EPS = 64e-5
NEG_INF = -1e30
DEEPNORM_ALPHA = (2 * DEPTH) ** 0.25
DEEPNORM_BETA = (8 * DEPTH) ** -0.25

kernel_name = 'hybrid_diffattn_rwkv7_hmoe'


def layer_norm(x, g, b):
    xf = x.astype(jnp.float32)
    mu = xf.mean(-1, keepdims=True)
    var = jnp.square(xf - mu).mean(-1, keepdims=True)
    return ((xf - mu) * lax.rsqrt(var + LN_EPS) * g + b).astype(x.dtype)


def rms_norm(x, g):
    xf = x.astype(jnp.float32)
    return xf * lax.rsqrt(jnp.square(xf).mean(-1, keepdims=True) + LN_EPS) * g


def t5_bucket(rel):
    n = jnp.maximum(rel, 0)
    max_exact = REL_BUCKETS // 2
    nf = jnp.maximum(n, 1).astype(jnp.float32)
    large = max_exact + (jnp.log(nf / max_exact) / math.log(REL_MAX_DIST / max_exact)
                         * (REL_BUCKETS - max_exact)).astype(jnp.int32)
    large = jnp.minimum(large, REL_BUCKETS - 1)
    return jnp.where(n < max_exact, n, large)


def diff_attention(q, k, v, rel_bias, lam, subln_g, lambda_init):
    B, S = q.shape[0], q.shape[1]
    nb = S // Q_BLOCK
    q_blocks = q.reshape(B, nb, Q_BLOCK, DIFF_HEADS, 2, DIFF_HEAD_DIM).swapaxes(0, 1)
    k_pos = jnp.arange(S, dtype=jnp.int32)
    vf = v.astype(jnp.float32)
    scale = DIFF_HEAD_DIM ** -0.5

    def one_block(args):
        qb, bi = args
        q_pos = bi * Q_BLOCK + jnp.arange(Q_BLOCK, dtype=jnp.int32)
        rel = q_pos[:, None] - k_pos[None, :]
        bias = jnp.moveaxis(rel_bias[t5_bucket(rel)], -1, 0).astype(jnp.float32)
        logits = jnp.einsum('bqhcd,bkhcd->bchqk', qb, k).astype(jnp.float32) * scale + bias[None, None]
        logits = jnp.where(rel[None, None, None] >= 0, logits, NEG_INF)
        probs = jax.nn.softmax(logits, axis=-1)
        attn = probs[:, 0] - lam * probs[:, 1]
        return jnp.einsum('bhqk,bkhd->bqhd', attn, vf)

    out = lax.map(one_block, (q_blocks, jnp.arange(nb, dtype=jnp.int32)))
    out = out.swapaxes(0, 1).reshape(B, S, DIFF_HEADS, DIFF_V_DIM)
    out = rms_norm(out, subln_g) * (1.0 - lambda_init)
    return out.reshape(B, S, DIFF_WIDTH).astype(q.dtype)


def _wkv7_step(state, inp):
    r, w, k, v, a, b = inp
    sa = jnp.einsum('bhij,bhj->bhi', state, a)
    state = state * w[:, :, None, :] + sa[..., None] * b[:, :, None, :] + v[..., None] * k[:, :, None, :]
    y = jnp.einsum('bhij,bhj->bhi', state, r)
    return state, y


def rwkv7_time_mix(proj, mu, w0, w2, a0, a2, g2, k_k, k_a, r_k, lnx_g, lnx_b):
    B, S = proj.shape[0], proj.shape[1]
    H, N = RWKV_HEADS, RWKV_HEAD_SIZE
    prev = jnp.pad(proj, ((0, 0), (1, 0), (0, 0)))[:, :-1]
    xs = proj + (prev - proj) * mu
    cuts = [RWKV_WIDTH, 2 * RWKV_WIDTH, 3 * RWKV_WIDTH,
            3 * RWKV_WIDTH + LORA_DECAY, 3 * RWKV_WIDTH + LORA_DECAY + LORA_AAA]
    r, k, v, xw, xa, xg = jnp.split(xs, cuts, axis=-1)
    w = -jax.nn.softplus(-(w0 + jnp.tanh(xw) @ w2)) - 0.5
    decay = jnp.exp(-jnp.exp(w.astype(jnp.float32)))
    a = jax.nn.sigmoid(a0 + xa @ a2)
    g = jax.nn.sigmoid(xg) @ g2
    kk = (k * k_k).reshape(B, S, H, N).astype(jnp.float32)
    kk = kk / jnp.maximum(jnp.linalg.norm(kk, axis=-1, keepdims=True), 1e-12)
    k = k * (1.0 + (a - 1.0) * k_a)
    rh = r.reshape(B, S, H, N).astype(jnp.float32)
    kh = k.reshape(B, S, H, N).astype(jnp.float32)
    vh = v.reshape(B, S, H, N).astype(jnp.float32)
    wh = decay.reshape(B, S, H, N)
    ah = a.reshape(B, S, H, N).astype(jnp.float32)
    xs_seq = tuple(jnp.moveaxis(t, 1, 0) for t in (rh, wh, kh, vh, -kk, kk * ah))
    state0 = jnp.zeros((B, H, N, N), jnp.float32)
    _, y = lax.scan(_wkv7_step, state0, xs_seq)
    y = jnp.moveaxis(y, 0, 1)
    y_mu = y.mean(-1, keepdims=True)
    y_var = jnp.square(y - y_mu).mean(-1, keepdims=True)
    y = (y - y_mu) * lax.rsqrt(y_var + RWKV_GN_EPS) * lnx_g.reshape(H, N) + lnx_b.reshape(H, N)
    bonus = jnp.sum(rh * kh * r_k, axis=-1, keepdims=True) * vh
    return ((y + bonus).reshape(B, S, RWKV_WIDTH) * g).astype(proj.dtype)


def hier_moe(h, gw, gb, ew, eb, w_gate, w_up, w_down):
    B, S, D = h.shape
    t = h.reshape(B * S, D)
    gprob = jax.nn.softmax((t @ gw + gb).astype(jnp.float32), axis=-1)
    gtop, gidx = lax.top_k(gprob, 1)
    elog = (t @ ew + eb).astype(jnp.float32).reshape(-1, N_GROUPS, EXPERTS_PER_GROUP)
    elog = jnp.take_along_axis(elog, gidx[:, :, None], axis=1)[:, 0]
    eprob = jax.nn.softmax(elog, axis=-1)
    etop, eidx = lax.top_k(eprob, TOP_K_EXPERTS)
    etop = etop / etop.sum(-1, keepdims=True)
    w_local = jnp.sum(jax.nn.one_hot(eidx, EXPERTS_PER_GROUP, dtype=jnp.float32) * etop[..., None], axis=1) * gtop
    gsel = jax.nn.one_hot(gidx[:, 0], N_GROUPS, dtype=jnp.float32)
    out = jnp.zeros_like(t)
    for gi in range(N_GROUPS):
        wg = (w_local * gsel[:, gi:gi + 1]).astype(t.dtype)
        hid = jax.nn.silu(jnp.einsum('td,edf->tef', t, w_gate[gi])) * jnp.einsum('td,edf->tef', t, w_up[gi])
        out = out + jnp.einsum('tef,efd->td', hid * wg[:, :, None], w_down[gi])
    return out.reshape(B, S, D)


def setup_inputs(seed: int = 0) -> dict:
    key = jax.random.key(seed)
    ks = iter(jax.random.split(key, 64))
    L, D = DEPTH, D_MODEL
    sd = D ** -0.5

    def nrm(shape, s):
        return s * jax.random.normal(next(ks), shape, jnp.float32)

    def unif(shape, lo, hi):
        return jax.random.uniform(next(ks), shape, jnp.float32, minval=lo, maxval=hi)

    x = nrm((BATCH, SEQ, D), 1.0)
    p = nrm((L, BATCH, SEQ, D_PLE), 1.0)
    ln_in_g = 1.0 + nrm((D,), 0.02)
    ln_in_b = nrm((D,), 0.02)
    rel_bias = nrm((REL_BUCKETS, DIFF_HEADS), 0.2)
    w_in = jnp.concatenate([
        nrm((L, D, 2 * DIFF_QK_WIDTH), sd),
        nrm((L, D, DIFF_WIDTH), sd * DEEPNORM_BETA),
        nrm((L, D, 2 * RWKV_WIDTH), sd),
        nrm((L, D, RWKV_WIDTH), sd * DEEPNORM_BETA),
        nrm((L, D, LORA_DECAY + LORA_AAA + LORA_GATE), sd),
    ], axis=-1)
    diff_lam_q1 = nrm((L, DIFF_HEAD_DIM), 0.1)
    diff_lam_k1 = nrm((L, DIFF_HEAD_DIM), 0.1)
    diff_lam_q2 = nrm((L, DIFF_HEAD_DIM), 0.1)
    diff_lam_k2 = nrm((L, DIFF_HEAD_DIM), 0.1)
    diff_subln_g = 1.0 + nrm((L, DIFF_V_DIM), 0.02)
    rwkv_mu = unif((L, RWKV_PROJ), 0.0, 1.0)
    rwkv_w0 = unif((L, RWKV_WIDTH), -6.0, 1.0)
    rwkv_w2 = nrm((L, LORA_DECAY, RWKV_WIDTH), 0.1)
    rwkv_a0 = nrm((L, RWKV_WIDTH), 0.1)
    rwkv_a2 = nrm((L, LORA_AAA, RWKV_WIDTH), 0.5 * LORA_AAA ** -0.5)
    rwkv_g2 = nrm((L, LORA_GATE, RWKV_WIDTH), LORA_GATE ** -0.5)
    rwkv_k_k = 0.85 + nrm((L, RWKV_WIDTH), 0.02)
    rwkv_k_a = 1.0 + nrm((L, RWKV_WIDTH), 0.02)
    rwkv_r_k = nrm((L, RWKV_HEADS, RWKV_HEAD_SIZE), 0.1)
    rwkv_lnx_g = 1.0 + nrm((L, RWKV_WIDTH), 0.02)
    rwkv_lnx_b = nrm((L, RWKV_WIDTH), 0.02)
    w_out = nrm((L, MIX_WIDTH, D), MIX_WIDTH ** -0.5 * DEEPNORM_BETA)
    ln1_g = 1.0 + nrm((L, D), 0.02)
    ln1_b = nrm((L, D), 0.02)
    router_group_w = nrm((L, D, N_GROUPS), sd)
    router_group_b = nrm((L, N_GROUPS), 0.01)
    router_expert_w = nrm((L, D, N_EXPERTS), sd)
    router_expert_b = nrm((L, N_EXPERTS), 0.01)
    moe_w_gate = nrm((L, N_GROUPS, EXPERTS_PER_GROUP, D, D_EXPERT), sd)
    moe_w_up = nrm((L, N_GROUPS, EXPERTS_PER_GROUP, D, D_EXPERT), sd)
    moe_w_down = nrm((L, N_GROUPS, EXPERTS_PER_GROUP, D_EXPERT, D), D_EXPERT ** -0.5 * DEEPNORM_BETA)
    ln2_g = 1.0 + nrm((L, D), 0.02)
    ln2_b = nrm((L, D), 0.02)
    ple_w = nrm((L, D_PLE, D), D_PLE ** -0.5)
    ple_norm_g = 1.0 + nrm((L, D), 0.02)
    ple_gate_w = nrm((L, D, D), sd)
    return {'x': x, 'p': p, 'ln_in_g': ln_in_g, 'ln_in_b': ln_in_b, 'rel_bias': rel_bias,
            'w_in': w_in, 'diff_lam_q1': diff_lam_q1, 'diff_lam_k1': diff_lam_k1,
            'diff_lam_q2': diff_lam_q2, 'diff_lam_k2': diff_lam_k2, 'diff_subln_g': diff_subln_g,
            'rwkv_mu': rwkv_mu, 'rwkv_w0': rwkv_w0, 'rwkv_w2': rwkv_w2, 'rwkv_a0': rwkv_a0,
            'rwkv_a2': rwkv_a2, 'rwkv_g2': rwkv_g2, 'rwkv_k_k': rwkv_k_k, 'rwkv_k_a': rwkv_k_a,
            'rwkv_r_k': rwkv_r_k, 'rwkv_lnx_g': rwkv_lnx_g, 'rwkv_lnx_b': rwkv_lnx_b,
            'w_out': w_out, 'ln1_g': ln1_g, 'ln1_b': ln1_b,
            'router_group_w': router_group_w, 'router_group_b': router_group_b,
            'router_expert_w': router_expert_w, 'router_expert_b': router_expert_b,
            'moe_w_gate': moe_w_gate, 'moe_w_up': moe_w_up, 'moe_w_down': moe_w_down,
            'ln2_g': ln2_g, 'ln2_b': ln2_b, 'ple_w': ple_w, 'ple_norm_g': ple_norm_g,
            'ple_gate_w': ple_gate_w}


def reference(x, p, ln_in_g, ln_in_b, rel_bias, w_in, diff_lam_q1, diff_lam_k1, diff_lam_q2,
              diff_lam_k2, diff_subln_g, rwkv_mu, rwkv_w0, rwkv_w2, rwkv_a0, rwkv_a2, rwkv_g2,
              rwkv_k_k, rwkv_k_a, rwkv_r_k, rwkv_lnx_g, rwkv_lnx_b, w_out, ln1_g, ln1_b,
              router_group_w, router_group_b, router_expert_w, router_expert_b,
              moe_w_gate, moe_w_up, moe_w_down, ln2_g, ln2_b, ple_w, ple_norm_g, ple_gate_w):
    B, S = x.shape[0], x.shape[1]
    h = layer_norm(x, ln_in_g, ln_in_b)
    for i in range(DEPTH):
        proj = h @ w_in[i]
        q = proj[..., :DIFF_QK_WIDTH].reshape(B, S, DIFF_HEADS, 2, DIFF_HEAD_DIM)
        k = proj[..., DIFF_QK_WIDTH:2 * DIFF_QK_WIDTH].reshape(B, S, DIFF_HEADS, 2, DIFF_HEAD_DIM)
        v = proj[..., 2 * DIFF_QK_WIDTH:O_RWKV].reshape(B, S, DIFF_HEADS, DIFF_V_DIM)
        lambda_init = 0.8 - 0.6 * math.exp(-0.3 * i)
        lam = (jnp.exp(jnp.sum(diff_lam_q1[i].astype(jnp.float32) * diff_lam_k1[i]))
               - jnp.exp(jnp.sum(diff_lam_q2[i].astype(jnp.float32) * diff_lam_k2[i]))
               + lambda_init)
        y_diff = diff_attention(q, k, v, rel_bias, lam, diff_subln_g[i], lambda_init)
        y_rwkv = rwkv7_time_mix(proj[..., O_RWKV:], rwkv_mu[i], rwkv_w0[i], rwkv_w2[i], rwkv_a0[i],
                                rwkv_a2[i], rwkv_g2[i], rwkv_k_k[i], rwkv_k_a[i], rwkv_r_k[i],
                                rwkv_lnx_g[i], rwkv_lnx_b[i])
        mix = jnp.concatenate([y_diff, y_rwkv], axis=-1) @ w_out[i]
        h = layer_norm(DEEPNORM_ALPHA * h + mix, ln1_g[i], ln1_b[i])
        ffn = hier_moe(h, router_group_w[i], router_group_b[i], router_expert_w[i], router_expert_b[i],
                       moe_w_gate[i], moe_w_up[i], moe_w_down[i])
        h = layer_norm(DEEPNORM_ALPHA * h + ffn, ln2_g[i], ln2_b[i])
        ple = rms_norm(p[i] @ ple_w[i], ple_norm_g[i]).astype(h.dtype)
        h = h + jax.nn.sigmoid(h @ ple_gate_w[i]) * ple
    return h
```

```python
import math
from contextlib import ExitStack
import numpy as np
import concourse.bass as bass
import concourse.mybir as mybir
from concourse.bass_utils import run_bass_kernel_spmd

F32 = mybir.dt.float32
BF16 = mybir.dt.bfloat16
AF = mybir.ActivationFunctionType
ALU = mybir.AluOpType
AX = mybir.AxisListType
ENGS = ("sync", "scalar", "vector", "gpsimd", "tensor")
ALPHA = 2.0 ** 0.25
LAMBDA_INIT = 0.8 - 0.6
NEG = -1.0e30
EM05 = math.exp(-0.5)
NHEADS = 8
NPAIRS = 8
CUT = 0
NEXP = 32
OUTSEL = tuple(range(8))


class Buf:
    __slots__ = ("name", "w", "r", "dsem", "dcnt")

    def __init__(self, name):
        self.name = name
        self.w = None
        self.r = []
        self.dsem = None
        self.dcnt = 0


class Prog:
    def __init__(self, nc, stack):
        self.nc = nc
        self.stack = stack
        self.ops = {e: [] for e in ENGS}
        self.sem = {e: stack.enter_context(nc.semaphore("c_" + e)) for e in ("scalar", "vector", "gpsimd", "tensor")}
        self.cnt = {e: 0 for e in ENGS}
        self.seen = {e: {} for e in ENGS}
        self.semobj = {}
        self.nbuf = 0
        self.disabled = False
        self.dbufs = []

    def buf(self, name=None):
        self.nbuf += 1
        return Buf(name or ("b%d" % self.nbuf))

    def _deps(self, eng, reads, writes):
        deps = []
        for b in reads:
            if b.w is not None:
                deps.append(b.w)
        for b in writes:
            if b.w is not None:
                deps.append(b.w)
            deps.extend(b.r)
        waits = {}
        for (sid, val, src) in deps:
            if src == "tensor" and eng == "tensor":
                continue
            if self.seen[eng].get(sid, 0) >= val:
                continue
            if waits.get(sid, 0) < val:
                waits[sid] = val
        for sid, val in waits.items():
            self.seen[eng][sid] = val
        return [(self.semobj[sid], val) for sid, val in waits.items()]

    def op(self, eng, fn, reads=(), writes=()):
        if self.disabled:
            return None
        waits = self._deps(eng, reads, writes)
        self.cnt[eng] += 1
        sem = self.sem[eng]
        self.semobj[id(sem)] = sem
        tok = (id(sem), self.cnt[eng], eng)
        self.ops[eng].append((waits, fn, (sem, 1)))
        for b in reads:
            b.r.append(tok)
        for b in writes:
            b.w = tok
            b.r = []
        return tok

    def dma(self, eng, fn, sbuf_buf, reads=(), writes=()):
        if self.disabled:
            return None
        waits = self._deps(eng, reads, writes)
        if sbuf_buf.dsem is None:
            sbuf_buf.dsem = self.stack.enter_context(self.nc.semaphore("d_" + sbuf_buf.name))
            self.semobj[id(sbuf_buf.dsem)] = sbuf_buf.dsem
            self.dbufs.append(sbuf_buf)
        sbuf_buf.dcnt += 16
        tok = (id(sbuf_buf.dsem), sbuf_buf.dcnt, "dma")
        self.ops[eng].append((waits, fn, (sbuf_buf.dsem, 16)))
        for b in reads:
            b.r.append(tok)
        for b in writes:
            b.w = tok
            b.r = []
        return tok

    def wait_all(self, eng, bufs):
        if self.disabled:
            return
        waits = self._deps(eng, bufs, bufs)
        self.ops[eng].append((waits, None, None))

    def barrier(self):
        toks = []
        for x in ("scalar", "vector", "gpsimd", "tensor"):
            if self.cnt[x] > 0:
                self.semobj[id(self.sem[x])] = self.sem[x]
                toks.append((id(self.sem[x]), self.cnt[x]))
        for b in self.dbufs:
            toks.append((id(b.dsem), b.dcnt))
        for eng in ENGS:
            waits = []
            for sid, val in toks:
                if self.seen[eng].get(sid, 0) >= val:
                    continue
                self.seen[eng][sid] = val
                waits.append((self.semobj[sid], val))
            if waits:
                self.ops[eng].append((waits, None, None))

    def flush(self):
        self.barrier()
        nc = self.nc
        ops = self.ops
        self.ops = {e: [] for e in ENGS}

        def replay(lst, e):
            for waits, fn, inc in lst:
                for sem, val in waits:
                    e.wait_ge(sem, val)
                if fn is not None:
                    ins = fn(e)
                    ins.then_inc(inc[0], inc[1])

        with nc.Block() as block:
            @block.sync
            def _(e):
                replay(ops["sync"], e)

            @block.scalar
            def _(e):
                replay(ops["scalar"], e)

            @block.vector
            def _(e):
                replay(ops["vector"], e)

            @block.gpsimd
            def _(e):
                replay(ops["gpsimd"], e)

            @block.tensor
            def _(e):
                replay(ops["tensor"], e)


class T:
    def __init__(self, P, stack, name, shape, dtype, psum=False, nb=1):
        nc = P.nc
        if psum:
            self.t = stack.enter_context(nc.psum_tensor("t_" + name, shape, dtype))
        else:
            self.t = stack.enter_context(nc.sbuf_tensor("t_" + name, shape, dtype))
        self.b = [P.buf(name + "_%d" % i) for i in range(nb)]
        self.B = self.b[0]

    def __getitem__(self, k):
        return self.t[k]


class _Stop(Exception):
    pass


def build(dbg=None, stop=99):
    nc = bass.Bass("TRN2", target_bir_lowering=False)
    D = 2048
    dram = {}

    def din(name, shape):
        dram[name] = nc.dram_tensor(name, list(shape), F32, kind="ExternalInput").ap()
        return dram[name]

    xs = din("xs", [2048, D])
    p_in = din("p", [1024, 256])
    flag_d = din("flag", [128, 1])
    ln_in_g = din("ln_in_g", [D]); ln_in_b = din("ln_in_b", [D])
    rel_bias = din("rel_bias", [32, 8])
    w_in = din("w_in", [D, 6432])
    lamq1 = din("diff_lam_q1", [64]); lamk1 = din("diff_lam_k1", [64])
    lamq2 = din("diff_lam_q2", [64]); lamk2 = din("diff_lam_k2", [64])
    subln_g = din("diff_subln_g", [128])
    rwkv_mu = din("rwkv_mu", [3360])
    rwkv_w0 = din("rwkv_w0", [1024]); rwkv_w2 = din("rwkv_w2", [64, 1024])
    rwkv_a0 = din("rwkv_a0", [1024]); rwkv_a2 = din("rwkv_a2", [64, 1024])
    rwkv_g2 = din("rwkv_g2", [160, 1024])
    rwkv_k_k = din("rwkv_k_k", [1024]); rwkv_k_a = din("rwkv_k_a", [1024])
    rwkv_r_k = din("rwkv_r_k", [1024])
    lnx_g = din("rwkv_lnx_g", [1024]); lnx_b = din("rwkv_lnx_b", [1024])
    w_out = din("w_out", [D, D])
    ln1_g = din("ln1_g", [D]); ln1_b = din("ln1_b", [D])
    rgw = din("router_group_w", [D, 4]); rgb = din("router_group_b", [4])
    rew = din("router_expert_w", [D, 32]); reb = din("router_expert_b", [32])
    nE = 32 if stop >= 6 else 1
    wg_d = din("moe_w_gate", [nE, D, 512]); wu_d = din("moe_w_up", [nE, D, 512]); wd_d = din("moe_w_down", [nE, 512, D])
    ln2_g = din("ln2_g", [D]); ln2_b = din("ln2_b", [D])
    ple_w = din("ple_w", [256, D]); ple_ng = din("ple_norm_g", [D]); ple_gw = din("ple_gate_w", [D, D])
    cmat = din("cmat", [8, 128, 128])
    oh_d = din("onehot", [33, 383])
    out_d = nc.dram_tensor("out", [1024, D], F32, kind="ExternalOutput").ap()
    dbg_d = None
    if dbg:
        dbg_d = nc.dram_tensor("dbg", list(dbg), F32, kind="ExternalOutput").ap()
    trep_d = nc.dram_tensor("trep", [8, 128, 383], F32, kind="Internal").ap()

    stA = ExitStack()
    with ExitStack() as st0:
      try:
        P = Prog(nc, st0)
        stB = st0
        yT = T(P, stB, "yT", [128, 16, 1024], BF16, nb=16)
        cst = T(P, st0, "cst", [128, 8, 128], F32)
        cstb = T(P, st0, "cstb", [128, 8, 128], BF16)
        flag = T(P, st0, "flag", [128, 1], F32)
        pbias = T(P, st0, "pbias", [128, 1], F32)
        ps = [T(P, st0, "ps%d" % i, [128, 512], F32, psum=True) for i in range(8)]
        IDENT, TRI_I, TRI_E, TRI_R, UT_S, UT_I, LT_S, BLK1 = range(8)
        identb = cstb[:, IDENT, :]

        global EPSC
        EPSC = T(P, st0, "epsc", [128, 4], F32)
        P.op("vector", lambda e: e.memset(EPSC[:, 0:1], 1e-5), writes=[EPSC.B])
        P.op("vector", lambda e: e.memset(EPSC[:, 1:2], 64e-5), writes=[EPSC.B])
        P.op("vector", lambda e: e.memset(EPSC[:, 2:3], 1e-24), writes=[EPSC.B])
        P.op("vector", lambda e: e.memset(EPSC[:, 3:4], 0.0), writes=[EPSC.B])
        P.dma("sync", lambda e: e.dma_start(out=cst[:], in_=cmat.rearrange("m p n -> p m n")), cst.B, writes=[cst.B])
        P.dma("sync", lambda e: e.dma_start(out=flag[:], in_=flag_d), flag.B, writes=[flag.B])
        P.op("vector", lambda e: e.tensor_copy(out=cstb[:], in_=cst[:]), reads=[cst.B], writes=[cstb.B])
        P.op("vector", lambda e: e.tensor_scalar(out=pbias[:], in0=flag[:], scalar1=-1.0, scalar2=-NEG, op0=ALU.add, op1=ALU.mult),
             reads=[flag.B], writes=[pbias.B])

        dbg_bufs = []

        def dump(stk, slot, ap, bufs, width, parts=128):
            if not dbg:
                return
            tmp = T(P, stk, "dbg%d" % slot, [128, width], F32)
            P.op("vector", lambda e: e.tensor_copy(out=tmp[0:parts, :], in_=ap), reads=bufs, writes=[tmp.B])
            P.dma("sync", lambda e: e.dma_start(out=dbg_d[slot, 0:parts, 0:width], in_=tmp[0:parts, :]), tmp.B, reads=[tmp.B])
            dbg_bufs.append(tmp.B)

        def mm(out, lhsT, rhs, start, stop, reads, wB, tp=None, sgc=False):
            kw = {}
            if tp is not None:
                kw["tile_position"] = tp
            if sgc:
                kw["skip_group_check"] = True
            P.op("tensor", lambda e: e.matmul(out=out, lhsT=lhsT, rhs=rhs, start=start, stop=stop, **kw), reads=reads, writes=[wB])

        def V_(fn, reads, writes):
            P.op("vector", fn, reads=reads, writes=writes)

        def A_(fn, reads, writes):
            P.op("scalar", fn, reads=reads, writes=writes)

        def small_load(dst_ap, dstB, src_ap):
            P.dma("sync", lambda e: e.dma_start(out=dst_ap, in_=src_ap, allow_slow_non_contiguous=True), dstB, writes=[dstB])

        w_in_v = w_in.rearrange("(kc p) n -> p kc n", p=128)

        def proj_fm(wt, c0, M, tb, pst):
            for kc in range(16):
                mm(pst[0:M, 0:512], wt[:, kc, c0:c0 + M], hT0[:, kc, tb * 512:(tb + 1) * 512], kc == 0, kc == 15,
                   [wt.B] + hT0.b[tb * 4:(tb + 1) * 4], pst.B)

        def shiftmix(pst, M, tb, R, mucol, omucol, muB, out_ap, outB, tmp):
            if tb == 0:
                V_(lambda e: e.memset(R[:], 0.0), [], [R.B])
            else:
                V_(lambda e: e.tensor_copy(out=R[:, 0:1], in_=R[:, 512:513]), [R.B], [R.B])
            if tb < 2:
                A_(lambda e: e.activation(out=R[0:M, 1:513], in_=pst[0:M, 0:512], func=AF.Copy, scale=flag[0:M, 0:1]), [pst.B, flag.B, R.B], [R.B])
            else:
                A_(lambda e: e.activation(out=R[0:M, 1:513], in_=pst[0:M, 0:512], func=AF.Copy), [pst.B, R.B], [R.B])
            V_(lambda e: e.tensor_scalar(out=tmp[0:M, :], in0=R[0:M, 1:513], scalar1=omucol, scalar2=None, op0=ALU.mult), [R.B, muB], [tmp.B])
            V_(lambda e: e.scalar_tensor_tensor(out=out_ap, in0=R[0:M, 0:512], scalar=mucol, in1=tmp[0:M, :], op0=ALU.mult, op1=ALU.add),
               [R.B, muB, tmp.B], [outB])

        mul_ = T(P, st0, "mul", [128, 8], F32)
        murkv = T(P, st0, "murkv", [128, 48], F32)
        st0.enter_context(stA)
        hT0 = T(P, stA, "hT0", [128, 16, 2048], BF16, nb=16)
        txw = T(P, stA, "txw", [64, 2048], BF16)
        xaT = T(P, stA, "xaT", [64, 2048], BF16)
        sxa = T(P, stA, "sxa", [128, 2048], BF16)
        sxb = T(P, stA, "sxb", [32, 2048], BF16)
        V_(lambda e: e.memset(mul_[:], 0.0), [], [mul_.B])
        small_load(mul_[0:64, 0:1], mul_.B, rwkv_mu[3072:3136].rearrange("(p a) -> p a", a=1))
        small_load(mul_[0:64, 1:2], mul_.B, rwkv_mu[3136:3200].rearrange("(p a) -> p a", a=1))
        small_load(mul_[0:128, 2:3], mul_.B, rwkv_mu[3200:3328].rearrange("(p a) -> p a", a=1))
        small_load(mul_[0:32, 3:4], mul_.B, rwkv_mu[3328:3360].rearrange("(p a) -> p a", a=1))
        V_(lambda e: e.tensor_scalar(out=mul_[:, 4:8], in0=mul_[:, 0:4], scalar1=-1.0, scalar2=1.0, op0=ALU.mult, op1=ALU.add), [mul_.B], [mul_.B])
        small_load(murkv[:, 0:24], murkv.B, rwkv_mu[0:3072].rearrange("(a p) -> p a", p=128))
        V_(lambda e: e.tensor_scalar(out=murkv[:, 24:48], in0=murkv[:, 0:24], scalar1=-1.0, scalar2=1.0, op0=ALU.mult, op1=ALU.add), [murkv.B], [murkv.B])

        with ExitStack() as st:
            gbc = T(P, st, "gbc", [128, D], F32); bbc = T(P, st, "bbc", [128, D], F32)
            P.dma("sync", lambda e: e.dma_start(out=gbc[:], in_=ln_in_g.partition_broadcast(128)), gbc.B, writes=[gbc.B])
            P.dma("sync", lambda e: e.dma_start(out=bbc[:], in_=ln_in_b.partition_broadcast(128)), bbc.B, writes=[bbc.B])
            wl = T(P, st, "wl", [128, 16, 288], BF16)
            P.dma("gpsimd", lambda e: e.dma_start(out=wl[:], in_=w_in_v[:, :, 6144:6432]), wl.B, writes=[wl.B])
            xt = [T(P, st, "xt%d" % i, [128, D], F32) for i in range(2)]
            xn = [T(P, st, "xn%d" % i, [128, D], F32) for i in range(2)]
            hb = [T(P, st, "hb%d" % i, [128, D], BF16) for i in range(2)]
            stt = [T(P, st, "stt%d" % i, [128, 4, 6], F32) for i in range(2)]
            mv = [T(P, st, "mv%d" % i, [128, 4], F32) for i in range(2)]
            for i in range(16):
                s = i % 2
                X = xt[s]
                P.dma("sync", lambda e, X=X, i=i: e.dma_start(out=X[:], in_=xs[i * 128:(i + 1) * 128, :]), X.B, writes=[X.B])
                ln_tile(P, X, xn[s], stt[s], mv[s], gbc, bbc, hb[s], None)
                transpose_to_fm(P, hb[s], hT0, i, ps, identb, cstb.B)
            Rl = [T(P, st, "Rl%d" % i, [128, 513], F32) for i in range(4)]
            tmpl = T(P, st, "tmpl", [128, 512], F32)
            xsl = T(P, st, "xsl", [128, 512], F32)
            groups = [(0, 64, txw, AF.Tanh), (64, 64, xaT, AF.Copy), (128, 128, sxa, AF.Sigmoid), (256, 32, sxb, AF.Sigmoid)]
            for tb in range(4):
                for gi, (c0, M, dst, fn_) in enumerate(groups):
                    pst = ps[2 + (gi % 2)]
                    proj_fm(wl, c0, M, tb, pst)
                    shiftmix(pst, M, tb, Rl[gi], mul_[0:M, gi:gi + 1], mul_[0:M, 4 + gi:5 + gi], mul_.B, xsl[0:M, :], xsl.B, tmpl)
                    A_(lambda e, dst=dst, M=M, fn_=fn_, tb=tb: e.activation(out=dst[0:M, tb * 512:(tb + 1) * 512], in_=xsl[0:M, :], func=fn_),
                       [xsl.B], [dst.B])
            dump(st, 0, hT0[:, 3, 0:1024], hT0.b, 1024)
            dump(st, 1, txw[0:64, 1024:2048], [txw.B], 1024, 64)
            if stop == 1:
                P.wait_all("sync", dbg_bufs)
            P.flush()
            if stop == 1:
                P.disabled = True

        with ExitStack() as st:
            rb = T(P, st, "rb", [33, 8], F32)
            oh = T(P, st, "oh", [33, 383], F32)
            P.dma("sync", lambda e: e.dma_start(out=rb[0:32, :], in_=rel_bias), rb.B, writes=[rb.B])
            V_(lambda e: e.memset(rb[32:33, :], NEG), [], [rb.B])
            P.dma("sync", lambda e: e.dma_start(out=oh[:], in_=oh_d), oh.B, writes=[oh.B])
            rbh = T(P, st, "rbh", [33, 128], F32)
            treps = T(P, st, "treps", [128, 383], F32)
            BN = T(P, st, "BN", [128, 8, 256], F32, nb=8)
            farb = T(P, st, "farb", [128, 16], F32)
            drb = [P.buf("drb%d" % h) for h in range(8)]
            for h in range(8):
                V_(lambda e, h=h: e.tensor_copy(out=rbh[:], in_=rb[:, h:h + 1].to_broadcast([33, 128])), [rb.B], [rbh.B])
                mm(ps[0][:, 0:383], rbh[:], oh[:], True, True, [rbh.B, oh.B], ps[0].B)
                V_(lambda e: e.tensor_copy(out=treps[:], in_=ps[0][:, 0:383]), [ps[0].B], [treps.B])
                V_(lambda e, h=h: e.tensor_copy(out=farb[:, h:h + 1], in_=treps[:, 382:383]), [treps.B], [farb.B])
                P.dma("sync", lambda e, h=h: e.dma_start(out=trep_d[h], in_=treps[:]), treps.B, reads=[treps.B], writes=[drb[h]])
                skew = bass.AP(tensor=trep_d.tensor, offset=h * 128 * 383 + 127, ap=[[382, 128], [1, 256]])
                P.dma("sync", lambda e, h=h, skew=skew: e.dma_start(out=BN[:, h, :], in_=skew), BN.b[h], reads=[drb[h]], writes=[BN.b[h]])
            V_(lambda e: e.tensor_scalar(out=farb[:, 8:16], in0=farb[:, 0:8], scalar1=pbias[:, 0:1], scalar2=None, op0=ALU.add), [farb.B, pbias.B], [farb.B])
            if CUT == 1:
                P.disabled = True
            lq = T(P, st, "lq", [1, 4, 64], F32)
            for j, src in enumerate((lamq1, lamk1, lamq2, lamk2)):
                P.dma("sync", lambda e, j=j, src=src: e.dma_start(out=lq[0:1, j, :], in_=src.rearrange("(a n) -> a n", a=1)), lq.B, writes=[lq.B])
            lt = T(P, st, "lt", [1, 8], F32)
            lpr = T(P, st, "lpr", [1, 2, 64], F32)
            V_(lambda e: e.tensor_tensor(out=lpr[0:1, 0, :], in0=lq[0:1, 0, :], in1=lq[0:1, 1, :], op=ALU.mult), [lq.B], [lpr.B])
            V_(lambda e: e.tensor_tensor(out=lpr[0:1, 1, :], in0=lq[0:1, 2, :], in1=lq[0:1, 3, :], op=ALU.mult), [lq.B], [lpr.B])
            V_(lambda e: e.reduce_sum(out=lt[0:1, 0:2], in_=lpr[0:1, :, :], axis=AX.X), [lpr.B], [lt.B])
            A_(lambda e: e.activation(out=lt[0:1, 2:4], in_=lt[0:1, 0:2], func=AF.Exp), [lt.B], [lt.B])
            V_(lambda e: e.tensor_tensor(out=lt[0:1, 4:5], in0=lt[0:1, 3:4], in1=lt[0:1, 2:3], op=ALU.subtract), [lt.B], [lt.B])
            V_(lambda e: e.tensor_scalar(out=lt[0:1, 5:6], in0=lt[0:1, 4:5], scalar1=-LAMBDA_INIT, scalar2=None, op0=ALU.add), [lt.B], [lt.B])
            ones1 = T(P, st, "ones1", [1, 128], F32)
            V_(lambda e: e.memset(ones1[:], 1.0), [], [ones1.B])
            nlam = T(P, st, "nlam", [128, 1], F32)
            mm(ps[1][:, 0:1], ones1[0:1, :], lt[0:1, 5:6], True, True, [ones1.B, lt.B], ps[1].B)
            V_(lambda e: e.tensor_copy(out=nlam[:], in_=ps[1][:, 0:1]), [ps[1].B], [nlam.B])
            gsub = T(P, st, "gsub", [128, 128], F32)
            P.dma("sync", lambda e: e.dma_start(out=gsub[:], in_=subln_g.partition_broadcast(128)), gsub.B, writes=[gsub.B])
            V_(lambda e: e.tensor_scalar(out=gsub[:], in0=gsub[:], scalar1=1.0 - LAMBDA_INIT, scalar2=None, op0=ALU.mult), [gsub.B], [gsub.B])
            if CUT == 2:
                P.disabled = True

            WQ = [T(P, st, "WQ%d" % i, [128, 16, 128], BF16) for i in range(2)]
            WK = [T(P, st, "WK%d" % i, [128, 16, 128], BF16) for i in range(2)]
            WV = [T(P, st, "WV%d" % i, [128, 16, 128], BF16) for i in range(2)]
            qT = T(P, st, "qT", [128, 1024], BF16)
            kT = T(P, st, "kT", [128, 2048], BF16)
            Va = T(P, st, "Va", [128, 16, 129], BF16)
            V_(lambda e: e.memset(Va[:, :, 128:129], 1.0), [], [Va.B])
            E = [[T(P, st, "E%d%d" % (m, j), [128, 512], BF16) for j in range(2)] for m in range(2)]
            TMP = [T(P, st, "TMPa%d" % m, [128, 256], F32) for m in range(2)]
            rc = T(P, st, "rc", [128, 4], F32)
            o2 = T(P, st, "o2", [128, 128], F32)
            oo = T(P, st, "oo", [128, 128], F32)
            junk = T(P, st, "junk", [128, 128], F32)
            ss = T(P, st, "ss", [128, 2], F32)
            yb = T(P, st, "yb", [128, 128], BF16)

            def load_head(h):
                s = h % 2
                for W_, c0 in ((WQ[s], h * 128), (WK[s], 1024 + h * 128), (WV[s], 2048 + h * 128)):
                    P.dma("gpsimd", lambda e, W_=W_, c0=c0: e.dma_start(out=W_[:], in_=w_in_v[:, :, c0:c0 + 128]), W_.B, writes=[W_.B])

            def accap(a, lo, hi):
                return ps[5 + a // 3][:, (a % 3) * 129 + lo:(a % 3) * 129 + hi]

            load_head(0)
            it = 0
            for h in range(NHEADS):
                if h + 1 < NHEADS:
                    load_head(h + 1)
                s = h % 2
                for tb in (2, 3):
                    proj_fm(WQ[s], 0, 128, tb, ps[tb % 2])
                    A_(lambda e, tb=tb: e.activation(out=qT[:, (tb - 2) * 512:(tb - 1) * 512], in_=ps[tb % 2][:, :], func=AF.Copy), [ps[tb % 2].B], [qT.B])
                for tb in range(4):
                    proj_fm(WK[s], 0, 128, tb, ps[2 + tb % 2])
                    V_(lambda e, tb=tb: e.tensor_copy(out=kT[:, tb * 512:(tb + 1) * 512], in_=ps[2 + tb % 2][:, :]), [ps[2 + tb % 2].B], [kT.B])
                for g4 in range(4):
                    pst = ps[g4 % 2]
                    for j in range(4):
                        i = g4 * 4 + j
                        for kc in range(16):
                            mm(pst[:, j * 128:(j + 1) * 128], hT0[:, kc, i * 128:(i + 1) * 128], WV[s][:, kc, :], kc == 0, kc == 15,
                               [WV[s].B, hT0.b[i]], pst.B)
                    V_(lambda e, g4=g4, pst=pst: e.tensor_copy(out=Va[:, g4 * 4:(g4 + 1) * 4, 0:128], in_=pst[:, :].rearrange("p (j t) -> p j t", t=128)),
                       [pst.B], [Va.B])
                if CUT == 3:
                    P.disabled = True
                for g in range(2):
                    nkb = 8 + 4 * g + 4
                    for kb in range(nkb):
                        t = kb - 8 - 4 * g
                        i0 = max(0, t)
                        N = (4 - i0) * 128
                        q0 = g * 512 + i0 * 128
                        if t >= 0:
                            wn = min(256, N); b0 = 0
                        elif t == -1:
                            wn = 128; b0 = 128
                        else:
                            wn = 0; b0 = 0
                        pref = kb < 8
                        for m in range(2):
                            pS = ps[m * 2 + (it % 2)]
                            Em = E[m][it % 2]
                            mm(pS[:, 0:N], kT[m * 64:(m + 1) * 64, kb * 128:(kb + 1) * 128], qT[m * 64:(m + 1) * 64, q0:q0 + N], True, True,
                               [kT.B, qT.B], pS.B)
                            if wn > 0:
                                V_(lambda e, pS=pS, m=m, wn=wn, b0=b0, h=h: e.scalar_tensor_tensor(out=TMP[m][:, 0:wn], in0=pS[:, 0:wn], scalar=0.125,
                                   in1=BN[:, h, b0:b0 + wn], op0=ALU.mult, op1=ALU.add), [pS.B, BN.b[h]], [TMP[m].B])
                                bcol = pbias[:, 0:1] if pref else EPSC[:, 3:4]
                                A_(lambda e, Em=Em, m=m, wn=wn, bcol=bcol: e.activation(out=Em[:, 0:wn], in_=TMP[m][:, 0:wn], func=AF.Exp, bias=bcol, scale=1.0),
                                   [TMP[m].B, pbias.B, EPSC.B], [Em.B])
                            if N > wn:
                                fcol = farb[:, 8 + h:9 + h] if pref else farb[:, h:h + 1]
                                A_(lambda e, Em=Em, pS=pS, wn=wn, N=N, fcol=fcol: e.activation(out=Em[:, wn:N], in_=pS[:, wn:N], func=AF.Exp, bias=fcol, scale=0.125),
                                   [pS.B, farb.B], [Em.B])
                            for li in range(4 - i0):
                                i = i0 + li
                                a = m * 4 + i
                                mm(accap(a, 0, 129), Em[:, li * 128:(li + 1) * 128], Va[:, kb, :], (kb == 0 and a % 3 == 0), False,
                                   [Em.B, Va.B], ps[5 + a // 3].B, sgc=True)
                        it += 1
                    if CUT == 4:
                        P.disabled = True
                    for i in range(4):
                        a1, a2 = i, 4 + i
                        B1, B2 = ps[5 + a1 // 3].B, ps[5 + a2 // 3].B
                        V_(lambda e, a1=a1: e.reciprocal(out=rc[:, 0:1], in_=accap(a1, 128, 129)), [B1], [rc.B])
                        V_(lambda e, a2=a2: e.reciprocal(out=rc[:, 1:2], in_=accap(a2, 128, 129)), [B2], [rc.B])
                        V_(lambda e: e.tensor_tensor(out=rc[:, 2:3], in0=rc[:, 1:2], in1=nlam[:, 0:1], op=ALU.mult), [rc.B, nlam.B], [rc.B])
                        V_(lambda e, a2=a2: e.tensor_scalar(out=o2[:], in0=accap(a2, 0, 128), scalar1=rc[:, 2:3], scalar2=None, op0=ALU.mult), [B2, rc.B], [o2.B])
                        V_(lambda e, a1=a1: e.scalar_tensor_tensor(out=oo[:], in0=accap(a1, 0, 128), scalar=rc[:, 0:1], in1=o2[:], op0=ALU.mult, op1=ALU.add),
                           [B1, rc.B, o2.B], [oo.B])
                        A_(lambda e: e.activation(out=junk[:], in_=oo[:], func=AF.Square, accum_out=ss[:, 0:1]), [oo.B], [junk.B, ss.B])
                        A_(lambda e: e.activation(out=ss[:, 1:2], in_=ss[:, 0:1], func=AF.Sqrt, bias=EPSC[:, 0:1], scale=1.0 / 128), [ss.B, EPSC.B], [ss.B])
                        V_(lambda e: e.reciprocal(out=ss[:, 1:2], in_=ss[:, 1:2]), [ss.B], [ss.B])
                        V_(lambda e: e.scalar_tensor_tensor(out=yb[:], in0=oo[:], scalar=ss[:, 1:2], in1=gsub[:], op0=ALU.mult, op1=ALU.mult),
                           [oo.B, ss.B, gsub.B], [yb.B])
                        pv = ps[4].t[:].bitcast(BF16)
                        P.op("tensor", lambda e, pv=pv: e.transpose(out=pv[:, 0:128], in_=yb[:], identity=identb), reads=[yb.B, cstb.B], writes=[ps[4].B])
                        c0 = g * 512 + i * 128
                        A_(lambda e, pv=pv, h=h, c0=c0: e.activation(out=yT[:, h, c0:c0 + 128], in_=pv[:, 0:128], func=AF.Copy), [ps[4].B], [yT.b[h]])
            dump(st, 2, yT[:, 0, :], [yT.b[0]], 1024)
            dump(st, 3, yT[:, NHEADS - 1, :], [yT.b[NHEADS - 1]], 1024)
            if stop == 2:
                P.wait_all("sync", dbg_bufs)
            P.flush()
            if stop == 2:
                P.disabled = True

        with ExitStack() as st:
            WR = [T(P, st, "WR0", [128, 16, 128], BF16)] * 2
            WKr = [T(P, st, "WKr0", [128, 16, 128], BF16)] * 2
            WVr = [T(P, st, "WVr0", [128, 16, 128], BF16)] * 2
            prm = T(P, st, "prm", [128, 8, 8], F32)
            for j, src in enumerate((rwkv_k_k, rwkv_k_a, rwkv_a0)):
                small_load(prm[:, :, j], prm.B, src.rearrange("(a p) -> p a", p=128))
            V_(lambda e: e.tensor_scalar(out=prm[:, :, 3], in0=prm[:, :, 1], scalar1=-1.0, scalar2=1.0, op0=ALU.mult, op1=ALU.add), [prm.B], [prm.B])
            w2b = T(P, st, "w2b", [64, 1024], BF16); a2b = T(P, st, "a2b", [64, 1024], BF16); g2b = T(P, st, "g2b", [128, 2, 1024], BF16)
            P.dma("gpsimd", lambda e: e.dma_start(out=w2b[:], in_=rwkv_w2), w2b.B, writes=[w2b.B])
            P.dma("gpsimd", lambda e: e.dma_start(out=a2b[:], in_=rwkv_a2), a2b.B, writes=[a2b.B])
            P.dma("gpsimd", lambda e: e.dma_start(out=g2b[:, 0, :], in_=rwkv_g2[0:128, :]), g2b.B, writes=[g2b.B])
            P.dma("gpsimd", lambda e: e.dma_start(out=g2b[0:32, 1, :], in_=rwkv_g2[128:160, :]), g2b.B, writes=[g2b.B])
            pp3 = T(P, st, "pp3", [128, 3, 128], F32)
            rkf = T(P, st, "rkf", [128, 8], F32)
            small_load(rkf[:], rkf.B, rwkv_r_k.rearrange("(a p) -> p a", p=128))
            RK = T(P, st, "RK", [128, 8, 2], BF16)
            V_(lambda e: e.memset(RK[:], 0.0), [], [RK.B])
            V_(lambda e: e.tensor_copy(out=RK[0:64, :, 0], in_=rkf[0:64, :]), [rkf.B], [RK.B])
            V_(lambda e: e.tensor_copy(out=RK[64:128, :, 1], in_=rkf[64:128, :]), [rkf.B], [RK.B])
            Rr = [T(P, st, "Rr%d" % i, [128, 513], F32) for i in range(3)]
            tmpr = T(P, st, "tmpr", [128, 512], F32)
            rs = T(P, st, "rs", [128, 512], F32); ks = T(P, st, "ks", [128, 512], F32); vsb = T(P, st, "vsb", [128, 512], BF16)
            af = T(P, st, "af", [128, 512], F32); kkk = T(P, st, "kkk", [128, 512], F32); sqb = T(P, st, "sqb", [128, 512], BF16)
            rinv = T(P, st, "rinv", [128, 512], F32); kk = T(P, st, "kk", [128, 512], F32); uu = tmpr
            k2 = T(P, st, "k2", [128, 512], F32); bb_ = T(P, st, "bb", [128, 512], F32)
            k2b = T(P, st, "k2b", [128, 512], BF16); bbb = T(P, st, "bbb", [128, 512], BF16); rk2 = T(P, st, "rk2", [128, 512], BF16)
            lwt = T(P, st, "lwt", [128, 128], F32)
            ex = T(P, st, "ex", [128, 4, 128], F32)
            ART = T(P, st, "ART", [128, 256], BF16)
            BhT = T(P, st, "BhT", [128, 128], BF16); KhT = T(P, st, "KhT", [128, 128], BF16)
            TMt = T(P, st, "TMt", [128, 4, 128], BF16)
            MT = T(P, st, "MT", [128, 2, 256], BF16)
            NM = [T(P, st, "NM%d" % i, [128, 256], BF16) for i in range(2)]
            Xf = T(P, st, "Xf", [128, 128], F32); Xb = T(P, st, "Xb", [128, 128], BF16)
            GTs = T(P, st, "GTs", [128, 16, 128], BF16); Hs = T(P, st, "Hs", [128, 16, 64], F32)
            QeTs = T(P, st, "QeTs", [128, 8, 128], BF16); Yls = T(P, st, "Yls", [128, 8, 128], F32)
            Vtm = T(P, st, "Vtm", [128, 8, 128], BF16); sbon = T(P, st, "sbon", [128, 8, 2], F32)
            gtm = T(P, st, "gtm", [128, 8, 128], F32)
            Sb = T(P, st, "Sb", [128, 128], BF16)
            Yt = T(P, st, "Yt", [128, 128], F32); yn = T(P, st, "yn", [128, 128], F32); ybr = T(P, st, "ybr", [128, 128], BF16)
            gst = T(P, st, "gst", [128, 2, 6], F32); gmv = T(P, st, "gmv", [128, 2, 2], F32); grs = T(P, st, "grs", [128, 2], F32)
            ident64 = cst[0:64, IDENT, 0:64]
            V_(lambda e: e.memset(GTs[:], 0.0), [], [GTs.B])

            def load_pair(c):
                s = c % 2
                for W_, c0 in ((WR[s], 3072 + c * 128), (WKr[s], 4096 + c * 128), (WVr[s], 5120 + c * 128)):
                    P.dma("gpsimd", lambda e, W_=W_, c0=c0: e.dma_start(out=W_[:], in_=w_in_v[:, :, c0:c0 + 128]), W_.B, writes=[W_.B])

            if CUT == 11:
                P.disabled = True
            load_pair(0)
            for c in range(NPAIRS):
                s = c % 2
                csl = slice(c * 128, (c + 1) * 128)
                for j3, src3 in enumerate((rwkv_w0, lnx_g, lnx_b)):
                    P.dma("sync", lambda e, j3=j3, src3=src3, c=c: e.dma_start(out=pp3[:, j3, :], in_=src3[c * 128:(c + 1) * 128].partition_broadcast(128)), pp3.B, writes=[pp3.B])
                for tb in range(4):
                    for j, (W_, dst, dstB) in enumerate(((WR[s], rs[:, :], rs.B), (WKr[s], ks[:, :], ks.B), (WVr[s], vsb[:, :], vsb.B))):
                        pst = ps[j % 2]
                        proj_fm(W_, 0, 128, tb, pst)
                        shiftmix(pst, 128, tb, Rr[j], murkv[:, j * 8 + c:j * 8 + c + 1], murkv[:, 24 + j * 8 + c:25 + j * 8 + c], murkv.B, dst, dstB, tmpr)
                    mm(ps[0][:, :], a2b[0:64, csl], xaT[0:64, tb * 512:(tb + 1) * 512], True, True, [a2b.B, xaT.B], ps[0].B)
                    A_(lambda e, c=c: e.activation(out=af[:], in_=ps[0][:, :], func=AF.Sigmoid, bias=prm[:, c, 2:3], scale=1.0), [ps[0].B, prm.B], [af.B])
                    V_(lambda e, c=c: e.tensor_scalar(out=kkk[:], in0=ks[:], scalar1=prm[:, c, 0:1], scalar2=None, op0=ALU.mult), [ks.B, prm.B], [kkk.B])
                    A_(lambda e: e.activation(out=sqb[:], in_=kkk[:], func=AF.Square), [kkk.B], [sqb.B])
                    mm(ps[1][:, :], cstb[:, BLK1, :], sqb[:], True, True, [cstb.B, sqb.B], ps[1].B)
                    A_(lambda e: e.activation(out=rinv[:], in_=ps[1][:, :], func=AF.Sqrt, bias=EPSC[:, 2:3], scale=1.0), [ps[1].B, EPSC.B], [rinv.B])
                    V_(lambda e: e.reciprocal(out=rinv[:], in_=rinv[:]), [rinv.B], [rinv.B])
                    V_(lambda e: e.tensor_tensor(out=kk[:], in0=kkk[:], in1=rinv[:], op=ALU.mult), [kkk.B, rinv.B], [kk.B])
                    V_(lambda e, c=c: e.tensor_scalar(out=uu[:], in0=af[:], scalar1=prm[:, c, 1:2], scalar2=prm[:, c, 3:4], op0=ALU.mult, op1=ALU.add), [af.B, prm.B], [uu.B])
                    V_(lambda e: e.tensor_tensor(out=k2[:], in0=ks[:], in1=uu[:], op=ALU.mult), [ks.B, uu.B], [k2.B])
                    V_(lambda e: e.tensor_tensor(out=bb_[:], in0=kk[:], in1=af[:], op=ALU.mult), [kk.B, af.B], [bb_.B])
                    A_(lambda e: e.activation(out=k2b[:], in_=k2[:], func=AF.Copy), [k2.B], [k2b.B])
                    A_(lambda e: e.activation(out=bbb[:], in_=bb_[:], func=AF.Copy), [bb_.B], [bbb.B])
                    V_(lambda e: e.tensor_tensor(out=rk2[:], in0=rs[:], in1=k2[:], op=ALU.mult), [rs.B, k2.B], [rk2.B])
                    if CUT == 12:
                        P.disabled = True
                    for q4 in range(4):
                        ch = tb * 4 + q4
                        own = ch >= 8
                        tsl = slice(q4 * 128, (q4 + 1) * 128)
                        gsl = slice(ch * 128, (ch + 1) * 128)
                        pz = ps[2]
                        mm(pz[:, 0:128], txw[0:64, gsl], w2b[0:64, csl], True, True, [txw.B, w2b.B], pz.B)
                        V_(lambda e, c=c: e.tensor_tensor(out=lwt[:], in0=pz[:, 0:128], in1=pp3[:, 0, :], op=ALU.add), [pz.B, pp3.B], [lwt.B])
                        A_(lambda e: e.activation(out=lwt[:], in_=lwt[:], func=AF.Sigmoid), [lwt.B], [lwt.B])
                        mm(pz[:, 128:384], lwt[:], cst[:, TRI_I:TRI_E + 1, :].rearrange("p a b -> p (a b)"), True, True, [lwt.B, cst.B], pz.B)
                        mm(pz[:, 384:512], cst[:, TRI_R, :], lwt[:], True, True, [lwt.B, cst.B], pz.B)
                        A_(lambda e: e.activation(out=ex[:, 0:2, :].rearrange("p a b -> p (a b)"), in_=pz[:, 128:384], func=AF.Exp), [pz.B], [ex.B])
                        A_(lambda e: e.activation(out=ex[:, 2, :], in_=pz[:, 128:256], func=AF.Exp, scale=-1.0), [pz.B], [ex.B])
                        A_(lambda e: e.activation(out=ex[:, 3, :], in_=pz[:, 384:512], func=AF.Exp), [pz.B], [ex.B])
                        V_(lambda e, tsl=tsl: e.scalar_tensor_tensor(out=ART[:, 0:128], in0=kk[:, tsl], scalar=-1.0, in1=ex[:, 1, :], op0=ALU.mult, op1=ALU.mult), [kk.B, ex.B], [ART.B])
                        V_(lambda e, tsl=tsl: e.tensor_tensor(out=ART[:, 128:256], in0=rs[:, tsl], in1=ex[:, 0, :], op=ALU.mult), [rs.B, ex.B], [ART.B])
                        V_(lambda e, tsl=tsl: e.tensor_tensor(out=BhT[:], in0=bb_[:, tsl], in1=ex[:, 2, :], op=ALU.mult), [bb_.B, ex.B], [BhT.B])
                        V_(lambda e, tsl=tsl: e.tensor_tensor(out=KhT[:], in0=k2[:, tsl], in1=ex[:, 2, :], op=ALU.mult), [k2.B, ex.B], [KhT.B])
                        pt = ps[3]
                        ptv = pt.t[:].bitcast(BF16)
                        for j, (src, sB) in enumerate(((bbb[:, tsl], bbb.B), (k2b[:, tsl], k2b.B), (vsb[:, tsl], vsb.B), (ART[:, 0:128], ART.B))):
                            P.op("tensor", lambda e, j=j, src=src, ptv=ptv: e.transpose(out=ptv[:, j * 128:(j + 1) * 128], in_=src, identity=identb), reads=[sB, cstb.B], writes=[pt.B])
                        V_(lambda e, ptv=ptv: e.tensor_tensor(out=TMt[:, 0:2, :], in0=ptv[:, 0:256].rearrange("p (a b) -> p a b", b=128),
                                                      in1=ex[:, 3:4, :].to_broadcast([128, 2, 128]), op=ALU.mult), [pt.B, ex.B], [TMt.B])
                        A_(lambda e, ptv=ptv: e.activation(out=TMt[:, 2:4, :].rearrange("p a b -> p (a b)"), in_=ptv[:, 256:512], func=AF.Copy), [pt.B], [TMt.B])
                        if own:
                            oc = ch - 8
                            A_(lambda e, oc=oc: e.activation(out=Vtm[:, oc, :], in_=TMt[:, 2, :], func=AF.Copy), [TMt.B], [Vtm.B])
                            mm(ps[7][:, 0:2], rk2[:, tsl], RK[:, c, :], True, True, [rk2.B, RK.B], ps[7].B)
                            V_(lambda e, oc=oc: e.tensor_copy(out=sbon[:, oc, :], in_=ps[7][:, 0:2]), [ps[7].B], [sbon.B])
                            mm(ps[7][:, 128:256], sxa[:, gsl], g2b[:, 0, csl], True, False, [sxa.B, g2b.B], ps[7].B)
                            mm(ps[7][:, 128:256], sxb[0:32, gsl], g2b[0:32, 1, csl], False, True, [sxb.B, g2b.B], ps[7].B)
                            V_(lambda e, oc=oc: e.tensor_copy(out=gtm[:, oc, :], in_=ps[7][:, 128:256]), [ps[7].B], [gtm.B])
                        if CUT == 13:
                            P.disabled = True
                        if CUT == 18 and ch == 8:
                            P.disabled = True
                        for hh in range(2):
                            hp = slice(hh * 64, (hh + 1) * 64)
                            tpk = (hh * 64, 0)
                            mm(ps[4][:, 0:256], BhT[hp, :], ART[hp, :], True, True, [BhT.B, ART.B], ps[4].B, tpk)
                            mm(ps[4][:, 256:512], KhT[hp, :], ART[hp, :], True, True, [KhT.B, ART.B], ps[4].B, tpk)
                            V_(lambda e: e.tensor_tensor(out=MT[:, :, :], in0=ps[4][:, :].rearrange("p (a b) -> p a b", b=256),
                                                         in1=cst[:, UT_S:UT_I + 1, :].rearrange("p a b -> p (a b)").unsqueeze(1).to_broadcast([128, 2, 256]), op=ALU.mult),
                               [ps[4].B, cst.B], [MT.B])
                            mm(ps[5][:, 0:128], ART[hp, 0:128], BhT[hp, :], True, True, [BhT.B, ART.B], ps[5].B, tpk)
                            A_(lambda e: e.activation(out=NM[0][:, 0:128], in_=MT[:, 0, 0:128], func=AF.Copy), [MT.B], [NM[0].B])
                            V_(lambda e: e.tensor_tensor(out=NM[0][:, 128:256], in0=ps[5][:, 0:128], in1=cst[:, LT_S, :], op=ALU.mult), [ps[5].B, cst.B], [NM[0].B])
                            mm(ps[5][:, 128:192], MT[:, 1, 0:128], TMt[:, 2, hp], True, True, [MT.B, TMt.B], ps[5].B)
                            A_(lambda e, hp=hp: e.activation(out=Xf[:, 0:64], in_=TMt[:, 3, hp], func=AF.Copy), [TMt.B], [Xf.B])
                            V_(lambda e: e.tensor_copy(out=Xf[:, 64:128], in_=ps[5][:, 128:192]), [ps[5].B], [Xf.B])
                            A_(lambda e: e.activation(out=Xb[:], in_=Xf[:], func=AF.Copy), [Xf.B], [Xb.B])
                            if CUT == 14:
                                P.disabled = True
                            for lv in range(7):
                                cur, nxt = NM[lv % 2], NM[(lv + 1) % 2]
                                mm(ps[6][:, 0:128], cur[:, 0:128], Xb[:], True, True, [cur.B, Xb.B], ps[6].B)
                                V_(lambda e: e.tensor_tensor(out=Xf[:], in0=Xf[:], in1=ps[6][:, 0:128], op=ALU.add), [Xf.B, ps[6].B], [Xf.B])
                                A_(lambda e: e.activation(out=Xb[:], in_=Xf[:], func=AF.Copy), [Xf.B], [Xb.B])
                                if lv < 6:
                                    mm(ps[6][:, 128:256], cur[:, 128:256], cur[:, 0:128], True, True, [cur.B], ps[6].B)
                                    mm(ps[6][:, 256:384], cur[:, 0:128], cur[:, 128:256], True, True, [cur.B], ps[6].B)
                                    V_(lambda e, nxt=nxt: e.tensor_copy(out=nxt[:], in_=ps[6][:, 128:384]), [ps[6].B], [nxt.B])
                            if CUT == 15:
                                P.disabled = True
                            tpo = (0, hh * 64)
                            mm(ps[7][hp, 256:320], Xb[:, 0:64], TMt[:, 0, hp], True, True, [Xb.B, TMt.B], ps[7].B, tpo)
                            V_(lambda e, hp=hp, ch=ch, hh=hh: e.scalar_tensor_tensor(out=GTs[hp, ch, hh * 64:(hh + 1) * 64], in0=cst[hp, IDENT, hh * 64:(hh + 1) * 64], scalar=ex[hp, 0, 127:128],
                                                                              in1=ps[7][hp, 256:320], op0=ALU.mult, op1=ALU.add), [cst.B, ex.B, ps[7].B], [GTs.B])
                            mm(ps[7][hp, 320:384], TMt[:, 0, hp], Xb[:, 64:128], True, False, [Xb.B, TMt.B], ps[7].B, tpo)
                            mm(ps[7][hp, 320:384], TMt[:, 1, hp], TMt[:, 2, hp], False, True, [TMt.B], ps[7].B, tpo)
                            V_(lambda e, hp=hp, ch=ch: e.tensor_copy(out=Hs[hp, ch, :], in_=ps[7][hp, 320:384]), [ps[7].B], [Hs.B])
                            if CUT == 16:
                                P.disabled = True
                            if own:
                                oc = ch - 8
                                mm(ps[7][hp, 384:512], Xb[:, 0:64], MT[:, 0, 128:256], True, True, [Xb.B, MT.B], ps[7].B, tpo)
                                V_(lambda e, hp=hp, oc=oc: e.tensor_tensor(out=QeTs[hp, oc, :], in0=ps[7][hp, 384:512], in1=ART[hp, 128:256], op=ALU.add),
                                   [ps[7].B, ART.B], [QeTs.B])
                                mm(ps[5][:, 192 + hh * 64:256 + hh * 64], MT[:, 0, 128:256], Xb[:, 64:128], True, False, [Xb.B, MT.B], ps[5].B)
                                mm(ps[5][:, 192 + hh * 64:256 + hh * 64], MT[:, 1, 128:256], TMt[:, 2, hp], False, True, [TMt.B, MT.B], ps[5].B)
                                V_(lambda e, hp=hp, oc=oc, hh=hh: e.tensor_copy(out=Yls[:, oc, hp], in_=ps[5][:, 192 + hh * 64:256 + hh * 64]), [ps[5].B], [Yls.B])
                if c + 1 < NPAIRS:
                    load_pair(c + 1)
                if CUT == 17:
                    P.disabled = True
                V_(lambda e: e.memset(Sb[:], 0.0), [], [Sb.B])
                for ch in range(16):
                    if CUT == 19 and ch == 8:
                        P.disabled = True
                    if ch >= 8:
                        oc = ch - 8
                        mm(ps[0][:, 0:128], QeTs[:, oc, :], Sb[:, :], True, True, [QeTs.B, Sb.B], ps[0].B)
                        V_(lambda e, oc=oc: e.tensor_tensor(out=Yt[:], in0=ps[0][:, 0:128], in1=Yls[:, oc, :], op=ALU.add), [ps[0].B, Yls.B], [Yt.B])
                    if CUT == 20 and ch == 8:
                        P.disabled = True
                    if ch < 15:
                        mm(ps[1][:, 0:128], GTs[:, ch, :], Sb[:, :], True, True, [GTs.B, Sb.B], ps[1].B)
                        for hh in range(2):
                            hp = slice(hh * 64, (hh + 1) * 64)
                            V_(lambda e, ch=ch, hp=hp: e.tensor_tensor(out=Sb[hp, hp], in0=ps[1][hp, hp], in1=Hs[hp, ch, :], op=ALU.add), [ps[1].B, Hs.B], [Sb.B])
                    if CUT == 21 and ch == 8:
                        P.disabled = True
                    if ch >= 8:
                        for hh in range(2):
                            V_(lambda e, hh=hh: e.bn_stats(out=gst[:, hh, :], in_=Yt[:, hh * 64:(hh + 1) * 64]), [Yt.B], [gst.B])
                            V_(lambda e, hh=hh: e.bn_aggr(out=gmv[:, hh, :], in_=gst[:, hh, :]), [gst.B], [gmv.B])
                        A_(lambda e: e.activation(out=grs[:], in_=gmv[:, :, 1], func=AF.Sqrt, bias=EPSC[:, 1:2], scale=1.0), [gmv.B, EPSC.B], [grs.B])
                        V_(lambda e: e.reciprocal(out=grs[:], in_=grs[:]), [grs.B], [grs.B])
                        for hh in range(2):
                            hs_ = slice(hh * 64, (hh + 1) * 64)
                            V_(lambda e, hh=hh, hs_=hs_: e.tensor_scalar(out=yn[:, hs_], in0=Yt[:, hs_], scalar1=gmv[:, hh, 0:1], scalar2=grs[:, hh:hh + 1],
                                                                       op0=ALU.subtract, op1=ALU.mult), [Yt.B, gmv.B, grs.B], [yn.B])
                        V_(lambda e, c=c: e.tensor_tensor(out=yn[:], in0=yn[:], in1=pp3[:, 1, :], op=ALU.mult), [yn.B, pp3.B], [yn.B])
                        V_(lambda e, c=c: e.tensor_tensor(out=yn[:], in0=yn[:], in1=pp3[:, 2, :], op=ALU.add), [yn.B, pp3.B], [yn.B])
                        for hh in range(2):
                            hs_ = slice(hh * 64, (hh + 1) * 64)
                            V_(lambda e, hh=hh, hs_=hs_, oc=oc: e.scalar_tensor_tensor(out=yn[:, hs_], in0=Vtm[:, oc, hs_], scalar=sbon[:, oc, hh:hh + 1], in1=yn[:, hs_],
                                                                                      op0=ALU.mult, op1=ALU.add), [Vtm.B, sbon.B, yn.B], [yn.B])
                        V_(lambda e, oc=oc: e.tensor_tensor(out=ybr[:], in0=yn[:], in1=gtm[:, oc, :], op=ALU.mult), [yn.B, gtm.B], [ybr.B])
                        if CUT == 22 and ch == 8:
                            P.disabled = True
                        pv = ps[3].t[:].bitcast(BF16)
                        P.op("tensor", lambda e, pv=pv: e.transpose(out=pv[:, 0:128], in_=ybr[:], identity=identb), reads=[ybr.B, cstb.B], writes=[ps[3].B])
                        if CUT == 23 and ch == 8:
                            P.disabled = True
                        A_(lambda e, pv=pv, c=c, oc=oc: e.activation(out=yT[:, 8 + c, oc * 128:(oc + 1) * 128], in_=pv[:, 0:128], func=AF.Copy), [ps[3].B], [yT.b[8 + c]])
            if CUT == 25:
                P.disabled = True
            if dbg and stop == 3:
                d6 = T(P, st, "d6", [128, 1024], F32)
                def dcp(c0, w, ap, bufs):
                    V_(lambda e: e.tensor_copy(out=d6[:, c0:c0 + w], in_=ap), bufs, [d6.B])
                def dsend(sl_):
                    P.dma("sync", lambda e: e.dma_start(out=dbg_d[sl_], in_=d6[:]), d6.B, reads=[d6.B])
                V_(lambda e: e.memset(d6[:], 0.0), [], [d6.B])
                dcp(0, 512, ex[:].rearrange("p a b -> p (a b)"), [ex.B]); dcp(512, 128, Xf[:], [Xf.B]); dcp(640, 128, lwt[:], [lwt.B])
                dcp(768, 128, Yt[:], [Yt.B]); dcp(896, 128, yn[:], [yn.B])
                dsend(6)
                dcp(0, 256, ART[:], [ART.B]); dcp(256, 128, BhT[:], [BhT.B]); dcp(384, 128, KhT[:], [KhT.B])
                dcp(512, 512, TMt[:].rearrange("p a b -> p (a b)"), [TMt.B])
                dsend(7)
                dcp(0, 512, MT[:].rearrange("p a b -> p (a b)"), [MT.B]); dcp(512, 64, GTs[:, 15, 0:64], [GTs.B]); dcp(576, 64, Hs[:, 15, :], [Hs.B])
                dcp(640, 128, QeTs[:, 7, :], [QeTs.B]); dcp(768, 128, Yls[:, 7, :], [Yls.B]); dcp(896, 64, Sb[:, 0:64], [Sb.B])
                dcp(960, 2, sbon[:, 7, :], [sbon.B])
                dsend(8)
                dbg_bufs.append(d6.B)
            dump(st, 4, yT[:, 8, :], [yT.b[8]], 1024)
            if not (dbg and stop == 3):
                dump(st, 5, yT[:, 7 + NPAIRS, :], [yT.b[7 + NPAIRS]], 1024)
            if stop == 3:
                P.wait_all("sync", dbg_bufs)
            P.flush()
            if stop == 3:
                P.disabled = True

        stA.close()
        with ExitStack() as st:
            acc = T(P, st, "acc", [128, 8, D], F32, nb=8)
            wt_ = T(P, st, "wt", [128, 8, 32], F32)
            accB = acc.b
            gb2 = [None, None]
            ws = [None, None, None]

            class AccT:
                def __init__(self, i):
                    self.i = i; self.B = accB[i]

                def __getitem__(self, k):
                    return acc[:, self.i, :] if k == slice(None) else acc[:, self.i, k[1]]

            def load_gb(gs, bs):
                P.dma("sync", lambda e: e.dma_start(out=gb2[0][:], in_=gs.partition_broadcast(128)), gb2[0].B, writes=[gb2[0].B])
                if bs is not None:
                    P.dma("sync", lambda e: e.dma_start(out=gb2[1][:], in_=bs.partition_broadcast(128)), gb2[1].B, writes=[gb2[1].B])

            with ExitStack() as st2:
                gb2[0] = T(P, st2, "gb2a0", [128, D], F32); gb2[1] = T(P, st2, "gb2a1", [128, D], F32)
                for i in range(3):
                    ws[i] = T(P, st2, "wsa%d" % i, [128, 16, 512], BF16)
                xt = [T(P, st2, "xt4%d" % i, [128, D], F32) for i in range(2)]
                xn = T(P, st2, "xn4", [128, D], F32)
                stt4 = T(P, st2, "stt4", [128, 4, 6], F32); mv4 = T(P, st2, "mv4", [128, 4], F32)
                load_gb(ln_in_g, ln_in_b)
                for i in range(8):
                    X = xt[i % 2]
                    P.dma("sync", lambda e, X=X, i=i: e.dma_start(out=X[:], in_=xs[(8 + i) * 128:(9 + i) * 128, :]), X.B, writes=[X.B])
                    ln_tile(P, X, xn, stt4, mv4, gb2[0], gb2[1], None, AccT(i))
                    A_(lambda e, i=i: e.activation(out=acc[:, i, :], in_=acc[:, i, :], func=AF.Copy, scale=ALPHA), [accB[i]], [accB[i]])
                w_out_v = w_out.rearrange("(kc p) n -> p kc n", p=128)
                for n in range(4):
                    W_ = ws[n % 3]
                    P.dma("gpsimd", lambda e, W_=W_, n=n: e.dma_start(out=W_[:], in_=w_out_v[:, :, n * 512:(n + 1) * 512]), W_.B, writes=[W_.B])
                    for i in range(8):
                        pst = ps[i % 2]
                        for kc in range(16):
                            mm(pst[:, :], yT[:, kc, i * 128:(i + 1) * 128], W_[:, kc, :], kc == 0, kc == 15, [yT.b[kc], W_.B], pst.B)
                        V_(lambda e, i=i, n=n, pst=pst: e.tensor_tensor(out=acc[:, i, n * 512:(n + 1) * 512], in0=acc[:, i, n * 512:(n + 1) * 512], in1=pst[:, :], op=ALU.add),
                           [accB[i], pst.B], [accB[i]])
                dump(st2, 6, acc[:, 0, 0:1024], [accB[0]], 1024)
                if stop == 4:
                    P.wait_all("sync", dbg_bufs)
                P.flush()
                if stop == 4:
                    P.disabled = True
            hT1 = T(P, st, "hT1", [128, 16, 1024], BF16, nb=8)
            with ExitStack() as st2:
                gb2[0] = T(P, st2, "gb2b0", [128, D], F32); gb2[1] = T(P, st2, "gb2b1", [128, D], F32)
                xn = T(P, st2, "xn4b", [128, D], F32)
                hb4 = T(P, st2, "hb4", [128, D], BF16)
                hlo = T(P, st2, "hlo", [128, D], BF16)
                stt4 = T(P, st2, "stt4b", [128, 4, 6], F32); mv4 = T(P, st2, "mv4b", [128, 4], F32)
                hT1l = T(P, st2, "hT1l", [128, 16, 128], BF16)
                load_gb(ln1_g, ln1_b)
                rwf = T(P, st2, "rwf", [128, 16, 36], F32); rwh = T(P, st2, "rwh", [128, 16, 36], BF16); rwl = T(P, st2, "rwl", [128, 16, 36], BF16)
                rbias = T(P, st2, "rbias", [128, 36], F32)
                P.dma("sync", lambda e: e.dma_start(out=rwf[:, :, 0:4], in_=rgw.rearrange("(kc p) n -> p kc n", p=128)), rwf.B, writes=[rwf.B])
                P.dma("sync", lambda e: e.dma_start(out=rwf[:, :, 4:36], in_=rew.rearrange("(kc p) n -> p kc n", p=128)), rwf.B, writes=[rwf.B])
                P.dma("sync", lambda e: e.dma_start(out=rbias[:, 0:4], in_=rgb.partition_broadcast(128)), rbias.B, writes=[rbias.B])
                P.dma("sync", lambda e: e.dma_start(out=rbias[:, 4:36], in_=reb.partition_broadcast(128)), rbias.B, writes=[rbias.B])
                V_(lambda e: e.tensor_copy(out=rwh[:], in_=rwf[:]), [rwf.B], [rwh.B])
                V_(lambda e: e.tensor_tensor(out=rwf[:], in0=rwf[:], in1=rwh[:], op=ALU.subtract), [rwf.B, rwh.B], [rwf.B])
                V_(lambda e: e.tensor_copy(out=rwl[:], in_=rwf[:]), [rwf.B], [rwl.B])
                L = T(P, st2, "L", [128, 36], F32); r1 = T(P, st2, "r1", [128, 8], F32)
                gm = T(P, st2, "gm", [128, 4], F32); elm = T(P, st2, "elm", [128, 32], F32); ee = T(P, st2, "ee", [128, 32], F32)
                m1 = T(P, st2, "m1", [128, 32], F32); e2 = T(P, st2, "e2", [128, 32], F32); m2 = T(P, st2, "m2", [128, 32], F32)
                for i in range(8):
                    A = AccT(i)
                    ln_tile(P, A, xn, stt4, mv4, gb2[0], gb2[1], hb4, A)
                    transpose_to_fm(P, hb4, hT1, i, ps, identb, cstb.B, pbase=2)
                    V_(lambda e, i=i: e.tensor_tensor(out=xn[:], in0=acc[:, i, :], in1=hb4[:], op=ALU.subtract), [accB[i], hb4.B], [xn.B])
                    A_(lambda e: e.activation(out=hlo[:], in_=xn[:], func=AF.Copy), [xn.B], [hlo.B])
                    transpose_to_fm(P, hlo, hT1l, 0, ps, identb, cstb.B, pbase=4)
                    tsl = slice(i * 128, (i + 1) * 128)
                    n_mm = 0
                    for (hT_, w_, sl_, bi_) in ((hT1, rwh, tsl, i), (hT1l, rwh, slice(0, 128), 0), (hT1, rwl, tsl, i)):
                        for kc in range(16):
                            mm(ps[6][:, 0:36], hT_[:, kc, sl_], w_[:, kc, :], n_mm == 0, n_mm == 47, [hT_.b[bi_], w_.B], ps[6].B)
                            n_mm += 1
                    V_(lambda e: e.tensor_tensor(out=L[:], in0=ps[6][:, 0:36], in1=rbias[:], op=ALU.add), [ps[6].B, rbias.B], [L.B])
                    V_(lambda e: e.reduce_max(out=r1[:, 0:1], in_=L[:, 0:4], axis=AX.X), [L.B], [r1.B])
                    V_(lambda e: e.tensor_scalar(out=gm[:], in0=L[:, 0:4], scalar1=r1[:, 0:1], scalar2=None, op0=ALU.is_ge), [L.B, r1.B], [gm.B])
                    V_(lambda e: e.tensor_scalar(out=r1[:, 1:2], in0=r1[:, 0:1], scalar1=-1.0, scalar2=None, op0=ALU.mult), [r1.B], [r1.B])
                    A_(lambda e: e.activation(out=ee[:, 0:4], in_=L[:, 0:4], func=AF.Exp, bias=r1[:, 1:2], scale=1.0, accum_out=r1[:, 2:3]), [L.B, r1.B], [ee.B, r1.B])
                    V_(lambda e: e.reciprocal(out=r1[:, 3:4], in_=r1[:, 2:3]), [r1.B], [r1.B])
                    V_(lambda e: e.tensor_scalar(out=gm[:], in0=gm[:], scalar1=-1.0, scalar2=-NEG, op0=ALU.add, op1=ALU.mult), [gm.B], [gm.B])
                    V_(lambda e: e.tensor_tensor(out=elm[:].rearrange("p (g e) -> p g e", e=8), in0=L[:, 4:36].rearrange("p (g e) -> p g e", e=8),
                                                 in1=gm[:].unsqueeze(2).to_broadcast([128, 4, 8]), op=ALU.add), [L.B, gm.B], [elm.B])
                    V_(lambda e: e.reduce_max(out=r1[:, 4:5], in_=elm[:], axis=AX.X), [elm.B], [r1.B])
                    V_(lambda e: e.tensor_scalar(out=r1[:, 5:6], in0=r1[:, 4:5], scalar1=-1.0, scalar2=None, op0=ALU.mult), [r1.B], [r1.B])
                    A_(lambda e: e.activation(out=ee[:], in_=elm[:], func=AF.Exp, bias=r1[:, 5:6], scale=1.0), [elm.B, r1.B], [ee.B])
                    V_(lambda e: e.tensor_scalar(out=m1[:], in0=elm[:], scalar1=r1[:, 4:5], scalar2=None, op0=ALU.is_ge), [elm.B, r1.B], [m1.B])
                    V_(lambda e: e.tensor_tensor(out=e2[:], in0=ee[:], in1=m1[:], op=ALU.mult), [ee.B, m1.B], [e2.B])
                    V_(lambda e: e.tensor_tensor(out=e2[:], in0=ee[:], in1=e2[:], op=ALU.subtract), [ee.B, e2.B], [e2.B])
                    V_(lambda e: e.reduce_max(out=r1[:, 6:7], in_=e2[:], axis=AX.X), [e2.B], [r1.B])
                    V_(lambda e: e.tensor_scalar(out=m2[:], in0=e2[:], scalar1=r1[:, 6:7], scalar2=None, op0=ALU.is_ge), [e2.B, r1.B], [m2.B])
                    V_(lambda e: e.tensor_tensor(out=m2[:], in0=m2[:], in1=e2[:], op=ALU.mult), [m2.B, e2.B], [m2.B])
                    V_(lambda e: e.tensor_tensor(out=m1[:], in0=m1[:], in1=m2[:], op=ALU.add), [m1.B, m2.B], [m1.B])
                    V_(lambda e: e.tensor_scalar(out=r1[:, 7:8], in0=r1[:, 6:7], scalar1=1.0, scalar2=None, op0=ALU.add), [r1.B], [r1.B])
                    V_(lambda e: e.reciprocal(out=r1[:, 7:8], in_=r1[:, 7:8]), [r1.B], [r1.B])
                    V_(lambda e, i=i: e.tensor_scalar(out=wt_[:, i, :], in0=m1[:], scalar1=r1[:, 7:8], scalar2=r1[:, 3:4], op0=ALU.mult, op1=ALU.mult), [m1.B, r1.B], [wt_.B])
                    A_(lambda e, i=i: e.activation(out=acc[:, i, :], in_=acc[:, i, :], func=AF.Copy, scale=ALPHA), [accB[i]], [accB[i]])
                dump(st2, 7, wt_[:, 0, :], [wt_.B], 32)
                if stop == 5:
                    P.wait_all("sync", dbg_bufs)
                P.flush()
                if stop == 5:
                    P.disabled = True
            with ExitStack() as st2:
                for i in range(3):
                    ws[i] = T(P, st2, "wsc%d" % i, [128, 16, 512], BF16)
                hid = [T(P, st2, "hid0", [128, 4, 1024], BF16)] * 2
                sg = [T(P, st2, "sg%d" % i, [128, 512], BF16) for i in range(2)]
                nld = 0
                for ex_ in range(NEXP):
                    Wg, Wu, Wd = ws[0], ws[1], ws[2]
                    P.dma("gpsimd", lambda e, ex_=ex_: e.dma_start(out=ws[0][:], in_=wg_d[ex_].rearrange("(kc p) n -> p kc n", p=128)), ws[0].B, writes=[ws[0].B])
                    P.dma("gpsimd", lambda e, ex_=ex_: e.dma_start(out=ws[1][:], in_=wu_d[ex_].rearrange("(kc p) n -> p kc n", p=128)), ws[1].B, writes=[ws[1].B])
                    P.dma("gpsimd", lambda e, ex_=ex_: e.dma_start(out=ws[2][:].rearrange("p (a b) n -> p a b n", b=4), in_=wd_d[ex_].rearrange("(fc p) (b n) -> p fc b n", p=128, n=512)),
                          ws[2].B, writes=[ws[2].B])
                    H_ = hid[ex_ % 2]
                    k_ = 0
                    for tb in range(2):
                        for fc in range(4):
                            pg, pu = ps[(k_ % 2) * 2], ps[(k_ % 2) * 2 + 1]
                            S_ = sg[k_ % 2]
                            for kc in range(16):
                                mm(pg[:, :], Wg[:, kc, fc * 128:(fc + 1) * 128], hT1[:, kc, tb * 512:(tb + 1) * 512], kc == 0, kc == 15, [Wg.B] + hT1.b[tb * 4:(tb + 1) * 4], pg.B)
                            for kc in range(16):
                                mm(pu[:, :], Wu[:, kc, fc * 128:(fc + 1) * 128], hT1[:, kc, tb * 512:(tb + 1) * 512], kc == 0, kc == 15, [Wu.B] + hT1.b[tb * 4:(tb + 1) * 4], pu.B)
                            A_(lambda e, S_=S_, pg=pg: e.activation(out=S_[:], in_=pg[:, :], func=AF.Silu), [pg.B], [S_.B])
                            V_(lambda e, S_=S_, pu=pu, H_=H_, fc=fc, tb=tb: e.tensor_tensor(out=H_[:, fc, tb * 512:(tb + 1) * 512], in0=S_[:], in1=pu[:, :], op=ALU.mult),
                               [S_.B, pu.B], [H_.B])
                            k_ += 1
                    for i in range(8):
                        for n in range(4):
                            po = ps[4 + (i * 4 + n) % 4]
                            for fc in range(4):
                                mm(po[:, :], H_[:, fc, i * 128:(i + 1) * 128], Wd[:, fc * 4 + n, :], fc == 0, fc == 3, [H_.B, Wd.B], po.B)
                            V_(lambda e, i=i, n=n, po=po, ex_=ex_: e.scalar_tensor_tensor(out=acc[:, i, n * 512:(n + 1) * 512], in0=po[:, :], scalar=wt_[:, i, ex_:ex_ + 1],
                                                                                  in1=acc[:, i, n * 512:(n + 1) * 512], op0=ALU.mult, op1=ALU.add), [po.B, wt_.B, accB[i]], [accB[i]])
                dump(st2, 8, acc[:, 0, 0:1024], [accB[0]], 1024)
                if stop == 6:
                    P.wait_all("sync", dbg_bufs)
                P.flush()
                if stop == 6:
                    P.disabled = True
            with ExitStack() as st2:
                gb2[0] = T(P, st2, "gb2d0", [128, D], F32); gb2[1] = T(P, st2, "gb2d1", [128, D], F32)
                ws[0] = T(P, st2, "wsd0", [128, 16, 512], BF16)
                xn = T(P, st2, "xn5", [128, D], F32); hb4 = T(P, st2, "hb5", [128, D], BF16)
                stt4 = T(P, st2, "stt5", [128, 4, 6], F32); mv4 = T(P, st2, "mv5", [128, 4], F32)
                pf = T(P, st2, "pf", [128, 256], F32); pb = T(P, st2, "pb", [128, 256], BF16)
                pT = T(P, st2, "pT", [128, 2, 1024], BF16, nb=8)
                plw = T(P, st2, "plw", [128, 2, D], BF16)
                ssq = T(P, st2, "ssq", [128, 8, 4], F32); rsp = T(P, st2, "rsp", [128, 8], F32)
                sgt = T(P, st2, "sgt", [128, 512], F32); t1_ = T(P, st2, "t1", [128, 512], F32)
                load_gb(ln2_g, ln2_b)
                P.dma("gpsimd", lambda e: e.dma_start(out=plw[:], in_=ple_w.rearrange("(kc p) n -> p kc n", p=128)), plw.B, writes=[plw.B])
                if CUT == 31:
                    P.disabled = True
                for i in range(8):
                    A = AccT(i)
                    ln_tile(P, A, xn, stt4, mv4, gb2[0], gb2[1], hb4, A)
                    transpose_to_fm(P, hb4, hT1, i, ps, identb, cstb.B, pbase=2)
                    P.dma("sync", lambda e, i=i: e.dma_start(out=pf[:], in_=p_in[i * 128:(i + 1) * 128, :]), pf.B, writes=[pf.B])
                    V_(lambda e: e.tensor_copy(out=pb[:], in_=pf[:]), [pf.B], [pb.B])
                    pv = ps[4].t[:].bitcast(BF16)
                    for k in range(2):
                        P.op("tensor", lambda e, k=k, pv=pv: e.transpose(out=pv[:, k * 128:(k + 1) * 128], in_=pb[:, k * 128:(k + 1) * 128], identity=identb), reads=[pb.B, cstb.B], writes=[ps[4].B])
                    V_(lambda e, i=i, pv=pv: e.tensor_copy(out=pT[:, :, i * 128:(i + 1) * 128], in_=pv[:, 0:256].rearrange("p (k t) -> p k t", t=128)), [ps[4].B], [pT.b[i]])
                    for n in range(4):
                        pp = ps[5 + n % 2]
                        for k in range(2):
                            mm(pp[:, :], pT[:, k, i * 128:(i + 1) * 128], plw[:, k, n * 512:(n + 1) * 512], k == 0, k == 1, [pT.b[i], plw.B], pp.B)
                        A_(lambda e, i=i, n=n, pp=pp: e.activation(out=t1_[:], in_=pp[:, :], func=AF.Square, accum_out=ssq[:, i, n:n + 1]), [pp.B], [t1_.B, ssq.B])
                if CUT == 32:
                    P.disabled = True
                V_(lambda e: e.reduce_sum(out=rsp[:], in_=ssq[:], axis=AX.X), [ssq.B], [rsp.B])
                A_(lambda e: e.activation(out=rsp[:], in_=rsp[:], func=AF.Sqrt, bias=EPSC[:, 0:1], scale=1.0 / D), [rsp.B, EPSC.B], [rsp.B])
                V_(lambda e: e.reciprocal(out=rsp[:], in_=rsp[:]), [rsp.B], [rsp.B])
                P.dma("sync", lambda e: e.dma_start(out=gb2[0][:], in_=ple_ng.partition_broadcast(128)), gb2[0].B, writes=[gb2[0].B])
                if CUT == 33:
                    P.disabled = True
                pgw_v = ple_gw.rearrange("(kc p) n -> p kc n", p=128)
                for n in range(4):
                    W_ = ws[0]
                    P.dma("gpsimd", lambda e, W_=W_, n=n: e.dma_start(out=W_[:], in_=pgw_v[:, :, n * 512:(n + 1) * 512]), W_.B, writes=[W_.B])
                    nsl = slice(n * 512, (n + 1) * 512)
                    for i in range(8):
                        pgt, pp = ps[(i % 2) * 2], ps[(i % 2) * 2 + 1]
                        for kc in range(16):
                            mm(pgt[:, :], hT1[:, kc, i * 128:(i + 1) * 128], W_[:, kc, :], kc == 0, kc == 15, [hT1.b[i], W_.B], pgt.B)
                        for k in range(2):
                            mm(pp[:, :], pT[:, k, i * 128:(i + 1) * 128], plw[:, k, nsl], k == 0, k == 1, [pT.b[i], plw.B], pp.B)
                        A_(lambda e, pgt=pgt: e.activation(out=sgt[:], in_=pgt[:, :], func=AF.Sigmoid), [pgt.B], [sgt.B])
                        V_(lambda e, i=i, pp=pp, nsl=nsl: e.scalar_tensor_tensor(out=t1_[:], in0=pp[:, :], scalar=rsp[:, i:i + 1], in1=gb2[0][:, nsl], op0=ALU.mult, op1=ALU.mult),
                           [pp.B, rsp.B, gb2[0].B], [t1_.B])
                        V_(lambda e: e.tensor_tensor(out=t1_[:], in0=t1_[:], in1=sgt[:], op=ALU.mult), [t1_.B, sgt.B], [t1_.B])
                        V_(lambda e, i=i, nsl=nsl: e.tensor_tensor(out=acc[:, i, nsl], in0=acc[:, i, nsl], in1=t1_[:], op=ALU.add), [accB[i], t1_.B], [accB[i]])
                        P.dma("sync", lambda e, i=i, nsl=nsl: e.dma_start(out=out_d[i * 128:(i + 1) * 128, nsl], in_=acc[:, i, nsl]), accB[i], reads=[accB[i]])
                if CUT == 34:
                    P.disabled = True
                P.wait_all("sync", accB + dbg_bufs)
                if stop == 7:
                    P.wait_all("sync", dbg_bufs)
                P.flush()
      except _Stop:
        pass
    return nc


EPSC = None


def rsqrt(P, out, in_, outB, inB, eps_ap):
    P.op("scalar", lambda e: e.activation(out=out, in_=in_, func=AF.Sqrt, bias=eps_ap, scale=1.0), reads=[inB, EPSC.B], writes=[outB])
    P.op("vector", lambda e: e.reciprocal(out=out, in_=out), reads=[outB], writes=[outB])


def ln_tile(P, X, XN, ST, MV, gbc, bbc, out_bf, out_f32, eps=1e-5):
    for j in range(4):
        P.op("vector", lambda e, j=j: e.bn_stats(out=ST[:, j, :], in_=X[:, j * 512:(j + 1) * 512]), reads=[X.B], writes=[ST.B])
    P.op("vector", lambda e: e.bn_aggr(out=MV[:, 0:2], in_=ST[:]), reads=[ST.B], writes=[MV.B])
    rsqrt(P, MV[:, 2:3], MV[:, 1:2], MV.B, MV.B, EPSC[:, 0:1])
    P.op("vector", lambda e: e.scalar_tensor_tensor(out=MV[:, 3:4], in0=MV[:, 0:1], scalar=-1.0, in1=MV[:, 2:3], op0=ALU.mult, op1=ALU.mult),
         reads=[MV.B], writes=[MV.B])
    P.op("scalar", lambda e: e.activation(out=XN[:], in_=X[:], func=AF.Identity, bias=MV[:, 3:4], scale=MV[:, 2:3]),
         reads=[X.B, MV.B], writes=[XN.B])
    P.op("vector", lambda e: e.tensor_tensor(out=XN[:], in0=XN[:], in1=gbc[:], op=ALU.mult), reads=[XN.B, gbc.B], writes=[XN.B])
    if out_f32 is not None:
        P.op("vector", lambda e: e.tensor_tensor(out=out_f32[:], in0=XN[:], in1=bbc[:], op=ALU.add), reads=[XN.B, bbc.B], writes=[out_f32.B])
        if out_bf is not None:
            P.op("scalar", lambda e: e.activation(out=out_bf[:], in_=out_f32[:], func=AF.Copy), reads=[out_f32.B], writes=[out_bf.B])
    else:
        P.op("vector", lambda e: e.tensor_tensor(out=out_bf[:], in0=XN[:], in1=bbc[:], op=ALU.add), reads=[XN.B, bbc.B], writes=[out_bf.B])


def transpose_to_fm(P, HB, dstT, ti, ps, identb, identB, pbase=0):
    for half in range(2):
        pst = ps[pbase + half]
        pv = pst.t[:].bitcast(BF16)
        for j in range(8):
            kc = half * 8 + j
            P.op("tensor", lambda e, kc=kc, j=j, pv=pv: e.transpose(out=pv[:, j * 128:(j + 1) * 128], in_=HB[:, kc * 128:(kc + 1) * 128], identity=identb),
                 reads=[HB.B, identB], writes=[pst.B])
        eng = "scalar" if half == 0 else "vector"
        dst = dstT[:, half * 8:(half + 1) * 8, ti * 128:(ti + 1) * 128]
        src = pv.rearrange("p (j t) -> p j t", t=128)
        if eng == "scalar":
            P.op("scalar", lambda e, dst=dst, src=src: e.activation(out=dst, in_=src, func=AF.Copy), reads=[pst.B], writes=[dstT.b[ti]])
        else:
            P.op("vector", lambda e, dst=dst, src=src: e.tensor_copy(out=dst, in_=src), reads=[pst.B], writes=[dstT.b[ti]])


def make_consts():
    c = np.zeros((8, 128, 128), np.float32)
    i = np.arange(128)
    c[0] = np.eye(128)
    c[1] = (i[:, None] <= i[None, :]) * -EM05
    c[2] = (i[:, None] < i[None, :]) * -EM05
    c[3] = (i[:, None] > i[None, :]) * -EM05
    c[4] = (i[None, :] > i[:, None])
    c[5] = (i[None, :] >= i[:, None])
    c[6] = (i[:, None] > i[None, :])
    c[7] = (i[:, None] // 64 == i[None, :] // 64)
    n = np.arange(-127, 256)
    nn = np.maximum(n, 0)
    nf = np.maximum(nn, 1).astype(np.float32)
    large = 16 + (np.log(nf / 16) / math.log(128 / 16) * 16).astype(np.int32)
    large = np.minimum(large, 31)
    bucket = np.where(nn < 16, nn, large)
    oh = np.zeros((33, 383), np.float32)
    oh[bucket, np.arange(383)] = 1.0
    oh[:32, n < 0] = 0.0
    oh[32, n < 0] = 1.0
    return c, oh


_CACHE = {}


def kernel(**inputs):
    dbg = inputs.pop("_dbg", None)
    stop = inputs.pop("_stop", 99)
    key = ("nc", dbg, stop)
    if key not in _CACHE:
        _CACHE[key] = build(dbg, stop)
    nc = _CACHE[key]
    x = np.asarray(inputs["x"], np.float32)
    p = np.asarray(inputs["p"], np.float32)
    cm, oh = make_consts()
    shared = {"cmat": cm, "onehot": oh}
    for k, v in inputs.items():
        if k in ("x", "p"):
            continue
        a = np.asarray(v, np.float32)
        if a.shape[0] == 1 and k not in ("ln_in_g", "ln_in_b"):
            a = a[0]
        if k in ("moe_w_gate", "moe_w_up", "moe_w_down"):
            a = a.reshape(32, a.shape[-2], a.shape[-1])
            if stop < 6:
                a = a[:1]
        if k in ("rwkv_r_k",):
            a = a.reshape(-1)
        shared[k] = np.ascontiguousarray(a)
    in_maps = []
    for c in range(8):
        b, s = c // 2, c % 2
        seq = np.zeros((2048, 2048), np.float32)
        if s == 1:
            seq[:] = x[b]
        else:
            seq[1024:] = x[b, :1024]
        m = dict(shared)
        m["xs"] = seq
        m["p"] = np.ascontiguousarray(p[0, b, s * 1024:(s + 1) * 1024])
        m["flag"] = np.full((128, 1), float(s), np.float32)
        in_maps.append(m)
    res = run_bass_kernel_spmd(nc, in_maps, core_ids=list(range(8)))
    if dbg:
        return [(r["dbg"], r["out"]) for r in res.results]
    out = np.zeros((4, 2048, 2048), np.float32)
    for c in range(8):
        b, s = c // 2, c % 2
        out[b, s * 1024:(s + 1) * 1024] = res.results[c]["out"]
    return out
```

```python
import math
from contextlib import ExitStack
import numpy as np
import concourse.bass as bass
import concourse.mybir as mybir
from concourse.bass_utils import run_bass_kernel_spmd

F32 = mybir.dt.float32
BF16 = mybir.dt.bfloat16
AF = mybir.ActivationFunctionType
ALU = mybir.AluOpType
AX = mybir.AxisListType
ENGS = ("sync", "scalar", "vector", "gpsimd", "tensor")
ALPHA = 2.0 ** 0.25
LAMBDA_INIT = 0.8 - 0.6
NEG = -1.0e30
EM05 = math.exp(-0.5)
NHEADS = 8
NPAIRS = 8
CUT = 0
NEXP = 32
OUTSEL = tuple(range(8))


class Buf:
    __slots__ = ("name", "w", "r", "dsem", "dcnt")

    def __init__(self, name):
        self.name = name
        self.w = None
        self.r = []
        self.dsem = None
        self.dcnt = 0


class Prog:
    def __init__(self, nc, stack):
        self.nc = nc
        self.stack = stack
        self.ops = {e: [] for e in ENGS}
        self.sem = {e: stack.enter_context(nc.semaphore("c_" + e)) for e in ("scalar", "vector", "gpsimd", "tensor")}
        self.cnt = {e: 0 for e in ENGS}
        self.seen = {e: {} for e in ENGS}
        self.semobj = {}
        self.nbuf = 0
        self.disabled = False
        self.dbufs = []

    def buf(self, name=None):
        self.nbuf += 1
        return Buf(name or ("b%d" % self.nbuf))

    def _deps(self, eng, reads, writes):
        deps = []
        for b in reads:
            if b.w is not None:
                deps.append(b.w)
        for b in writes:
            if b.w is not None:
                deps.append(b.w)
            deps.extend(b.r)
        waits = {}
        for (sid, val, src) in deps:
            if src == "tensor" and eng == "tensor":
                continue
            if self.seen[eng].get(sid, 0) >= val:
                continue
            if waits.get(sid, 0) < val:
                waits[sid] = val
        for sid, val in waits.items():
            self.seen[eng][sid] = val
        return [(self.semobj[sid], val) for sid, val in waits.items()]

    def op(self, eng, fn, reads=(), writes=()):
        if self.disabled:
            return None
        waits = self._deps(eng, reads, writes)
        self.cnt[eng] += 1
        sem = self.sem[eng]
        self.semobj[id(sem)] = sem
        tok = (id(sem), self.cnt[eng], eng)
        self.ops[eng].append((waits, fn, (sem, 1)))
        for b in reads:
            b.r.append(tok)
        for b in writes:
            b.w = tok
            b.r = []
        return tok

    def dma(self, eng, fn, sbuf_buf, reads=(), writes=()):
        if self.disabled:
            return None
        waits = self._deps(eng, reads, writes)
        if sbuf_buf.dsem is None:
            sbuf_buf.dsem = self.stack.enter_context(self.nc.semaphore("d_" + sbuf_buf.name))
            self.semobj[id(sbuf_buf.dsem)] = sbuf_buf.dsem
            self.dbufs.append(sbuf_buf)
        sbuf_buf.dcnt += 16
        tok = (id(sbuf_buf.dsem), sbuf_buf.dcnt, "dma")
        self.ops[eng].append((waits, fn, (sbuf_buf.dsem, 16)))
        for b in reads:
            b.r.append(tok)
        for b in writes:
            b.w = tok
            b.r = []
        return tok

    def wait_all(self, eng, bufs):
        if self.disabled:
            return
        waits = self._deps(eng, bufs, bufs)
        self.ops[eng].append((waits, None, None))

    def barrier(self):
        toks = []
        for x in ("scalar", "vector", "gpsimd", "tensor"):
            if self.cnt[x] > 0:
                self.semobj[id(self.sem[x])] = self.sem[x]
                toks.append((id(self.sem[x]), self.cnt[x]))
        for b in self.dbufs:
            toks.append((id(b.dsem), b.dcnt))
        for eng in ENGS:
            waits = []
            for sid, val in toks:
                if self.seen[eng].get(sid, 0) >= val:
                    continue
                self.seen[eng][sid] = val
                waits.append((self.semobj[sid], val))
            if waits:
                self.ops[eng].append((waits, None, None))

    def flush(self):
        self.barrier()
        nc = self.nc
        ops = self.ops
        self.ops = {e: [] for e in ENGS}

        def replay(lst, e):
            for waits, fn, inc in lst:
                for sem, val in waits:
                    e.wait_ge(sem, val)
                if fn is not None:
                    ins = fn(e)
                    ins.then_inc(inc[0], inc[1])

        with nc.Block() as block:
            @block.sync
            def _(e):
                replay(ops["sync"], e)

            @block.scalar
            def _(e):
                replay(ops["scalar"], e)

            @block.vector
            def _(e):
                replay(ops["vector"], e)

            @block.gpsimd
            def _(e):
                replay(ops["gpsimd"], e)

            @block.tensor
            def _(e):
                replay(ops["tensor"], e)


class T:
    def __init__(self, P, stack, name, shape, dtype, psum=False, nb=1):
        nc = P.nc
        if psum:
            self.t = stack.enter_context(nc.psum_tensor("t_" + name, shape, dtype))
        else:
            self.t = stack.enter_context(nc.sbuf_tensor("t_" + name, shape, dtype))
        self.b = [P.buf(name + "_%d" % i) for i in range(nb)]
        self.B = self.b[0]

    def __getitem__(self, k):
        return self.t[k]


class _Stop(Exception):
    pass


def build(dbg=None, stop=99):
    nc = bass.Bass("TRN2", target_bir_lowering=False)
    D = 2048
    dram = {}

    def din(name, shape):
        dram[name] = nc.dram_tensor(name, list(shape), F32, kind="ExternalInput").ap()
        return dram[name]

    xs = din("xs", [2048, D])
    p_in = din("p", [1024, 256])
    flag_d = din("flag", [128, 1])
    ln_in_g = din("ln_in_g", [D]); ln_in_b = din("ln_in_b", [D])
    rel_bias = din("rel_bias", [32, 8])
    w_in = din("w_in", [D, 6432])
    lamq1 = din("diff_lam_q1", [64]); lamk1 = din("diff_lam_k1", [64])
    lamq2 = din("diff_lam_q2", [64]); lamk2 = din("diff_lam_k2", [64])
    subln_g = din("diff_subln_g", [128])
    rwkv_mu = din("rwkv_mu", [3360])
    rwkv_w0 = din("rwkv_w0", [1024]); rwkv_w2 = din("rwkv_w2", [64, 1024])
    rwkv_a0 = din("rwkv_a0", [1024]); rwkv_a2 = din("rwkv_a2", [64, 1024])
    rwkv_g2 = din("rwkv_g2", [160, 1024])
    rwkv_k_k = din("rwkv_k_k", [1024]); rwkv_k_a = din("rwkv_k_a", [1024])
    rwkv_r_k = din("rwkv_r_k", [1024])
    lnx_g = din("rwkv_lnx_g", [1024]); lnx_b = din("rwkv_lnx_b", [1024])
    w_out = din("w_out", [D, D])
    ln1_g = din("ln1_g", [D]); ln1_b = din("ln1_b", [D])
    rgw = din("router_group_w", [D, 4]); rgb = din("router_group_b", [4])
    rew = din("router_expert_w", [D, 32]); reb = din("router_expert_b", [32])
    nE = 32 if stop >= 6 else 1
    wg_d = din("moe_w_gate", [nE, D, 512]); wu_d = din("moe_w_up", [nE, D, 512]); wd_d = din("moe_w_down", [nE, 512, D])
    ln2_g = din("ln2_g", [D]); ln2_b = din("ln2_b", [D])
    ple_w = din("ple_w", [256, D]); ple_ng = din("ple_norm_g", [D]); ple_gw = din("ple_gate_w", [D, D])
    cmat = din("cmat", [8, 128, 128])
    oh_d = din("onehot", [33, 383])
    out_d = nc.dram_tensor("out", [1024, D], F32, kind="ExternalOutput").ap()
    dbg_d = None
    if dbg:
        dbg_d = nc.dram_tensor("dbg", list(dbg), F32, kind="ExternalOutput").ap()
    trep_d = nc.dram_tensor("trep", [8, 128, 383], F32, kind="Internal").ap()

    stA = ExitStack()
    with ExitStack() as st0:
      try:
        P = Prog(nc, st0)
        stB = st0
        yT = T(P, stB, "yT", [128, 16, 1024], BF16, nb=16)
        cst = T(P, st0, "cst", [128, 8, 128], F32)
        cstb = T(P, st0, "cstb", [128, 8, 128], BF16)
        flag = T(P, st0, "flag", [128, 1], F32)
        pbias = T(P, st0, "pbias", [128, 1], F32)
        ps = [T(P, st0, "ps%d" % i, [128, 512], F32, psum=True) for i in range(8)]
        IDENT, TRI_I, TRI_E, TRI_R, UT_S, UT_I, LT_S, BLK1 = range(8)
        identb = cstb[:, IDENT, :]

        global EPSC
        EPSC = T(P, st0, "epsc", [128, 4], F32)
        P.op("vector", lambda e: e.memset(EPSC[:, 0:1], 1e-5), writes=[EPSC.B])
        P.op("vector", lambda e: e.memset(EPSC[:, 1:2], 64e-5), writes=[EPSC.B])
        P.op("vector", lambda e: e.memset(EPSC[:, 2:3], 1e-24), writes=[EPSC.B])
        P.op("vector", lambda e: e.memset(EPSC[:, 3:4], 0.0), writes=[EPSC.B])
        P.dma("sync", lambda e: e.dma_start(out=cst[:], in_=cmat.rearrange("m p n -> p m n")), cst.B, writes=[cst.B])
        P.dma("sync", lambda e: e.dma_start(out=flag[:], in_=flag_d), flag.B, writes=[flag.B])
        P.op("vector", lambda e: e.tensor_copy(out=cstb[:], in_=cst[:]), reads=[cst.B], writes=[cstb.B])
        P.op("vector", lambda e: e.tensor_scalar(out=pbias[:], in0=flag[:], scalar1=-1.0, scalar2=-NEG, op0=ALU.add, op1=ALU.mult),
             reads=[flag.B], writes=[pbias.B])

        dbg_bufs = []

        def dump(stk, slot, ap, bufs, width, parts=128):
            if not dbg:
                return
            tmp = T(P, stk, "dbg%d" % slot, [128, width], F32)
            P.op("vector", lambda e: e.tensor_copy(out=tmp[0:parts, :], in_=ap), reads=bufs, writes=[tmp.B])
            P.dma("sync", lambda e: e.dma_start(out=dbg_d[slot, 0:parts, 0:width], in_=tmp[0:parts, :]), tmp.B, reads=[tmp.B])
            dbg_bufs.append(tmp.B)

        def mm(out, lhsT, rhs, start, stop, reads, wB, tp=None, sgc=False):
            kw = {}
            if tp is not None:
                kw["tile_position"] = tp
            if sgc:
                kw["skip_group_check"] = True
            P.op("tensor", lambda e: e.matmul(out=out, lhsT=lhsT, rhs=rhs, start=start, stop=stop, **kw), reads=reads, writes=[wB])

        def V_(fn, reads, writes):
            P.op("vector", fn, reads=reads, writes=writes)

        def A_(fn, reads, writes):
            P.op("scalar", fn, reads=reads, writes=writes)

        def small_load(dst_ap, dstB, src_ap):
            P.dma("sync", lambda e: e.dma_start(out=dst_ap, in_=src_ap, allow_slow_non_contiguous=True), dstB, writes=[dstB])

        w_in_v = w_in.rearrange("(kc p) n -> p kc n", p=128)

        def proj_fm(wt, c0, M, tb, pst):
            for kc in range(16):
                mm(pst[0:M, 0:512], wt[:, kc, c0:c0 + M], hT0[:, kc, tb * 512:(tb + 1) * 512], kc == 0, kc == 15,
                   [wt.B] + hT0.b[tb * 4:(tb + 1) * 4], pst.B)

        def shiftmix(pst, M, tb, R, mucol, omucol, muB, out_ap, outB, tmp):
            if tb == 0:
                V_(lambda e: e.memset(R[:], 0.0), [], [R.B])
            else:
                V_(lambda e: e.tensor_copy(out=R[:, 0:1], in_=R[:, 512:513]), [R.B], [R.B])
            if tb < 2:
                A_(lambda e: e.activation(out=R[0:M, 1:513], in_=pst[0:M, 0:512], func=AF.Copy, scale=flag[0:M, 0:1]), [pst.B, flag.B, R.B], [R.B])
            else:
                A_(lambda e: e.activation(out=R[0:M, 1:513], in_=pst[0:M, 0:512], func=AF.Copy), [pst.B, R.B], [R.B])
            V_(lambda e: e.tensor_scalar(out=tmp[0:M, :], in0=R[0:M, 1:513], scalar1=omucol, scalar2=None, op0=ALU.mult), [R.B, muB], [tmp.B])
            V_(lambda e: e.scalar_tensor_tensor(out=out_ap, in0=R[0:M, 0:512], scalar=mucol, in1=tmp[0:M, :], op0=ALU.mult, op1=ALU.add),
               [R.B, muB, tmp.B], [outB])

        mul_ = T(P, st0, "mul", [128, 8], F32)
        murkv = T(P, st0, "murkv", [128, 48], F32)
        st0.enter_context(stA)
        hT0 = T(P, stA, "hT0", [128, 16, 2048], BF16, nb=16)
        txw = T(P, stA, "txw", [64, 2048], BF16)
        xaT = T(P, stA, "xaT", [64, 2048], BF16)
        sxa = T(P, stA, "sxa", [128, 2048], BF16)
        sxb = T(P, stA, "sxb", [32, 2048], BF16)
        V_(lambda e: e.memset(mul_[:], 0.0), [], [mul_.B])
        small_load(mul_[0:64, 0:1], mul_.B, rwkv_mu[3072:3136].rearrange("(p a) -> p a", a=1))
        small_load(mul_[0:64, 1:2], mul_.B, rwkv_mu[3136:3200].rearrange("(p a) -> p a", a=1))
        small_load(mul_[0:128, 2:3], mul_.B, rwkv_mu[3200:3328].rearrange("(p a) -> p a", a=1))
        small_load(mul_[0:32, 3:4], mul_.B, rwkv_mu[3328:3360].rearrange("(p a) -> p a", a=1))
        V_(lambda e: e.tensor_scalar(out=mul_[:, 4:8], in0=mul_[:, 0:4], scalar1=-1.0, scalar2=1.0, op0=ALU.mult, op1=ALU.add), [mul_.B], [mul_.B])
        small_load(murkv[:, 0:24], murkv.B, rwkv_mu[0:3072].rearrange("(a p) -> p a", p=128))
        V_(lambda e: e.tensor_scalar(out=murkv[:, 24:48], in0=murkv[:, 0:24], scalar1=-1.0, scalar2=1.0, op0=ALU.mult, op1=ALU.add), [murkv.B], [murkv.B])

        with ExitStack() as st:
            gbc = T(P, st, "gbc", [128, D], F32); bbc = T(P, st, "bbc", [128, D], F32)
            P.dma("sync", lambda e: e.dma_start(out=gbc[:], in_=ln_in_g.partition_broadcast(128)), gbc.B, writes=[gbc.B])
            P.dma("sync", lambda e: e.dma_start(out=bbc[:], in_=ln_in_b.partition_broadcast(128)), bbc.B, writes=[bbc.B])
            wl = T(P, st, "wl", [128, 16, 288], BF16)
            P.dma("gpsimd", lambda e: e.dma_start(out=wl[:], in_=w_in_v[:, :, 6144:6432]), wl.B, writes=[wl.B])
            xt = [T(P, st, "xt%d" % i, [128, D], F32) for i in range(2)]
            xn = [T(P, st, "xn%d" % i, [128, D], F32) for i in range(2)]
            hb = [T(P, st, "hb%d" % i, [128, D], BF16) for i in range(2)]
            stt = [T(P, st, "stt%d" % i, [128, 4, 6], F32) for i in range(2)]
            mv = [T(P, st, "mv%d" % i, [128, 4], F32) for i in range(2)]
            for i in range(16):
                s = i % 2
                X = xt[s]
                P.dma("sync", lambda e, X=X, i=i: e.dma_start(out=X[:], in_=xs[i * 128:(i + 1) * 128, :]), X.B, writes=[X.B])
                ln_tile(P, X, xn[s], stt[s], mv[s], gbc, bbc, hb[s], None)
                transpose_to_fm(P, hb[s], hT0, i, ps, identb, cstb.B)
            Rl = [T(P, st, "Rl%d" % i, [128, 513], F32) for i in range(4)]
            tmpl = T(P, st, "tmpl", [128, 512], F32)
            xsl = T(P, st, "xsl", [128, 512], F32)
            groups = [(0, 64, txw, AF.Tanh), (64, 64, xaT, AF.Copy), (128, 128, sxa, AF.Sigmoid), (256, 32, sxb, AF.Sigmoid)]
            for tb in range(4):
                for gi, (c0, M, dst, fn_) in enumerate(groups):
                    pst = ps[2 + (gi % 2)]
                    proj_fm(wl, c0, M, tb, pst)
                    shiftmix(pst, M, tb, Rl[gi], mul_[0:M, gi:gi + 1], mul_[0:M, 4 + gi:5 + gi], mul_.B, xsl[0:M, :], xsl.B, tmpl)
                    A_(lambda e, dst=dst, M=M, fn_=fn_, tb=tb: e.activation(out=dst[0:M, tb * 512:(tb + 1) * 512], in_=xsl[0:M, :], func=fn_),
                       [xsl.B], [dst.B])
            dump(st, 0, hT0[:, 3, 0:1024], hT0.b, 1024)
            dump(st, 1, txw[0:64, 1024:2048], [txw.B], 1024, 64)
            if stop == 1:
                P.wait_all("sync", dbg_bufs)
            P.flush()
            if stop == 1:
                P.disabled = True

        with ExitStack() as st:
            rb = T(P, st, "rb", [33, 8], F32)
            oh = T(P, st, "oh", [33, 383], F32)
            P.dma("sync", lambda e: e.dma_start(out=rb[0:32, :], in_=rel_bias), rb.B, writes=[rb.B])
            V_(lambda e: e.memset(rb[32:33, :], NEG), [], [rb.B])
            P.dma("sync", lambda e: e.dma_start(out=oh[:], in_=oh_d), oh.B, writes=[oh.B])
            rbh = T(P, st, "rbh", [33, 128], F32)
            treps = T(P, st, "treps", [128, 383], F32)
            BN = T(P, st, "BN", [128, 8, 256], F32, nb=8)
            farb = T(P, st, "farb", [128, 16], F32)
            drb = [P.buf("drb%d" % h) for h in range(8)]
            for h in range(8):
                V_(lambda e, h=h: e.tensor_copy(out=rbh[:], in_=rb[:, h:h + 1].to_broadcast([33, 128])), [rb.B], [rbh.B])
                mm(ps[0][:, 0:383], rbh[:], oh[:], True, True, [rbh.B, oh.B], ps[0].B)
                V_(lambda e: e.tensor_copy(out=treps[:], in_=ps[0][:, 0:383]), [ps[0].B], [treps.B])
                V_(lambda e, h=h: e.tensor_copy(out=farb[:, h:h + 1], in_=treps[:, 382:383]), [treps.B], [farb.B])
                P.dma("sync", lambda e, h=h: e.dma_start(out=trep_d[h], in_=treps[:]), treps.B, reads=[treps.B], writes=[drb[h]])
                skew = bass.AP(tensor=trep_d.tensor, offset=h * 128 * 383 + 127, ap=[[382, 128], [1, 256]])
                P.dma("sync", lambda e, h=h, skew=skew: e.dma_start(out=BN[:, h, :], in_=skew), BN.b[h], reads=[drb[h]], writes=[BN.b[h]])
            V_(lambda e: e.tensor_scalar(out=farb[:, 8:16], in0=farb[:, 0:8], scalar1=pbias[:, 0:1], scalar2=None, op0=ALU.add), [farb.B, pbias.B], [farb.B])
            if CUT == 1:
                P.disabled = True
            lq = T(P, st, "lq", [1, 4, 64], F32)
            for j, src in enumerate((lamq1, lamk1, lamq2, lamk2)):
                P.dma("sync", lambda e, j=j, src=src: e.dma_start(out=lq[0:1, j, :], in_=src.rearrange("(a n) -> a n", a=1)), lq.B, writes=[lq.B])
            lt = T(P, st, "lt", [1, 8], F32)
            lpr = T(P, st, "lpr", [1, 2, 64], F32)
            V_(lambda e: e.tensor_tensor(out=lpr[0:1, 0, :], in0=lq[0:1, 0, :], in1=lq[0:1, 1, :], op=ALU.mult), [lq.B], [lpr.B])
            V_(lambda e: e.tensor_tensor(out=lpr[0:1, 1, :], in0=lq[0:1, 2, :], in1=lq[0:1, 3, :], op=ALU.mult), [lq.B], [lpr.B])
            V_(lambda e: e.reduce_sum(out=lt[0:1, 0:2], in_=lpr[0:1, :, :], axis=AX.X), [lpr.B], [lt.B])
            A_(lambda e: e.activation(out=lt[0:1, 2:4], in_=lt[0:1, 0:2], func=AF.Exp), [lt.B], [lt.B])
            V_(lambda e: e.tensor_tensor(out=lt[0:1, 4:5], in0=lt[0:1, 3:4], in1=lt[0:1, 2:3], op=ALU.subtract), [lt.B], [lt.B])
            V_(lambda e: e.tensor_scalar(out=lt[0:1, 5:6], in0=lt[0:1, 4:5], scalar1=-LAMBDA_INIT, scalar2=None, op0=ALU.add), [lt.B], [lt.B])
            ones1 = T(P, st, "ones1", [1, 128], F32)
            V_(lambda e: e.memset(ones1[:], 1.0), [], [ones1.B])
            nlam = T(P, st, "nlam", [128, 1], F32)
            mm(ps[1][:, 0:1], ones1[0:1, :], lt[0:1, 5:6], True, True, [ones1.B, lt.B], ps[1].B)
            V_(lambda e: e.tensor_copy(out=nlam[:], in_=ps[1][:, 0:1]), [ps[1].B], [nlam.B])
            gsub = T(P, st, "gsub", [128, 128], F32)
            P.dma("sync", lambda e: e.dma_start(out=gsub[:], in_=subln_g.partition_broadcast(128)), gsub.B, writes=[gsub.B])
            V_(lambda e: e.tensor_scalar(out=gsub[:], in0=gsub[:], scalar1=1.0 - LAMBDA_INIT, scalar2=None, op0=ALU.mult), [gsub.B], [gsub.B])
            if CUT == 2:
                P.disabled = True

            WQ = [T(P, st, "WQ%d" % i, [128, 16, 128], BF16) for i in range(2)]
            WK = [T(P, st, "WK%d" % i, [128, 16, 128], BF16) for i in range(2)]
            WV = [T(P, st, "WV%d" % i, [128, 16, 128], BF16) for i in range(2)]
            qT = T(P, st, "qT", [128, 1024], BF16)
            kT = T(P, st, "kT", [128, 2048], BF16)
            Va = T(P, st, "Va", [128, 16, 129], BF16)
            V_(lambda e: e.memset(Va[:, :, 128:129], 1.0), [], [Va.B])
            E = [[T(P, st, "E%d%d" % (m, j), [128, 512], BF16) for j in range(2)] for m in range(2)]
            TMP = [T(P, st, "TMPa%d" % m, [128, 256], F32) for m in range(2)]
            rc = T(P, st, "rc", [128, 4], F32)
            o2 = T(P, st, "o2", [128, 128], F32)
            oo = T(P, st, "oo", [128, 128], F32)
            junk = T(P, st, "junk", [128, 128], F32)
            ss = T(P, st, "ss", [128, 2], F32)
            yb = T(P, st, "yb", [128, 128], BF16)

            def load_head(h):
                s = h % 2
                for W_, c0 in ((WQ[s], h * 128), (WK[s], 1024 + h * 128), (WV[s], 2048 + h * 128)):
                    P.dma("gpsimd", lambda e, W_=W_, c0=c0: e.dma_start(out=W_[:], in_=w_in_v[:, :, c0:c0 + 128]), W_.B, writes=[W_.B])

            def accap(a, lo, hi):
                return ps[5 + a // 3][:, (a % 3) * 129 + lo:(a % 3) * 129 + hi]

            load_head(0)
            it = 0
            for h in range(NHEADS):
                if h + 1 < NHEADS:
                    load_head(h + 1)
                s = h % 2
                for tb in (2, 3):
                    proj_fm(WQ[s], 0, 128, tb, ps[tb % 2])
                    A_(lambda e, tb=tb: e.activation(out=qT[:, (tb - 2) * 512:(tb - 1) * 512], in_=ps[tb % 2][:, :], func=AF.Copy), [ps[tb % 2].B], [qT.B])
                for tb in range(4):
                    proj_fm(WK[s], 0, 128, tb, ps[2 + tb % 2])
                    V_(lambda e, tb=tb: e.tensor_copy(out=kT[:, tb * 512:(tb + 1) * 512], in_=ps[2 + tb % 2][:, :]), [ps[2 + tb % 2].B], [kT.B])
                for g4 in range(4):
                    pst = ps[g4 % 2]
                    for j in range(4):
                        i = g4 * 4 + j
                        for kc in range(16):
                            mm(pst[:, j * 128:(j + 1) * 128], hT0[:, kc, i * 128:(i + 1) * 128], WV[s][:, kc, :], kc == 0, kc == 15,
                               [WV[s].B, hT0.b[i]], pst.B)
                    V_(lambda e, g4=g4, pst=pst: e.tensor_copy(out=Va[:, g4 * 4:(g4 + 1) * 4, 0:128], in_=pst[:, :].rearrange("p (j t) -> p j t", t=128)),
                       [pst.B], [Va.B])
                if CUT == 3:
                    P.disabled = True
                for g in range(2):
                    nkb = 8 + 4 * g + 4
                    for kb in range(nkb):
                        t = kb - 8 - 4 * g
                        i0 = max(0, t)
                        N = (4 - i0) * 128
                        q0 = g * 512 + i0 * 128
                        if t >= 0:
                            wn = min(256, N); b0 = 0
                        elif t == -1:
                            wn = 128; b0 = 128
                        else:
                            wn = 0; b0 = 0
                        pref = kb < 8
                        for m in range(2):
                            pS = ps[m * 2 + (it % 2)]
                            Em = E[m][it % 2]
                            mm(pS[:, 0:N], kT[m * 64:(m + 1) * 64, kb * 128:(kb + 1) * 128], qT[m * 64:(m + 1) * 64, q0:q0 + N], True, True,
                               [kT.B, qT.B], pS.B)
                            if wn > 0:
                                V_(lambda e, pS=pS, m=m, wn=wn, b0=b0, h=h: e.scalar_tensor_tensor(out=TMP[m][:, 0:wn], in0=pS[:, 0:wn], scalar=0.125,
                                   in1=BN[:, h, b0:b0 + wn], op0=ALU.mult, op1=ALU.add), [pS.B, BN.b[h]], [TMP[m].B])
                                bcol = pbias[:, 0:1] if pref else EPSC[:, 3:4]
                                A_(lambda e, Em=Em, m=m, wn=wn, bcol=bcol: e.activation(out=Em[:, 0:wn], in_=TMP[m][:, 0:wn], func=AF.Exp, bias=bcol, scale=1.0),
                                   [TMP[m].B, pbias.B, EPSC.B], [Em.B])
                            if N > wn:
                                fcol = farb[:, 8 + h:9 + h] if pref else farb[:, h:h + 1]
                                A_(lambda e, Em=Em, pS=pS, wn=wn, N=N, fcol=fcol: e.activation(out=Em[:, wn:N], in_=pS[:, wn:N], func=AF.Exp, bias=fcol, scale=0.125),
                                   [pS.B, farb.B], [Em.B])
                            for li in range(4 - i0):
                                i = i0 + li
                                a = m * 4 + i
                                mm(accap(a, 0, 129), Em[:, li * 128:(li + 1) * 128], Va[:, kb, :], (kb == 0 and a % 3 == 0), False,
                                   [Em.B, Va.B], ps[5 + a // 3].B, sgc=True)
                        it += 1
                    if CUT == 4:
                        P.disabled = True
                    for i in range(4):
                        a1, a2 = i, 4 + i
                        B1, B2 = ps[5 + a1 // 3].B, ps[5 + a2 // 3].B
                        V_(lambda e, a1=a1: e.reciprocal(out=rc[:, 0:1], in_=accap(a1, 128, 129)), [B1], [rc.B])
                        V_(lambda e, a2=a2: e.reciprocal(out=rc[:, 1:2], in_=accap(a2, 128, 129)), [B2], [rc.B])
                        V_(lambda e: e.tensor_tensor(out=rc[:, 2:3], in0=rc[:, 1:2], in1=nlam[:, 0:1], op=ALU.mult), [rc.B, nlam.B], [rc.B])
                        V_(lambda e, a2=a2: e.tensor_scalar(out=o2[:], in0=accap(a2, 0, 128), scalar1=rc[:, 2:3], scalar2=None, op0=ALU.mult), [B2, rc.B], [o2.B])
                        V_(lambda e, a1=a1: e.scalar_tensor_tensor(out=oo[:], in0=accap(a1, 0, 128), scalar=rc[:, 0:1], in1=o2[:], op0=ALU.mult, op1=ALU.add),
                           [B1, rc.B, o2.B], [oo.B])
                        A_(lambda e: e.activation(out=junk[:], in_=oo[:], func=AF.Square, accum_out=ss[:, 0:1]), [oo.B], [junk.B, ss.B])
                        A_(lambda e: e.activation(out=ss[:, 1:2], in_=ss[:, 0:1], func=AF.Sqrt, bias=EPSC[:, 0:1], scale=1.0 / 128), [ss.B, EPSC.B], [ss.B])
                        V_(lambda e: e.reciprocal(out=ss[:, 1:2], in_=ss[:, 1:2]), [ss.B], [ss.B])
                        V_(lambda e: e.scalar_tensor_tensor(out=yb[:], in0=oo[:], scalar=ss[:, 1:2], in1=gsub[:], op0=ALU.mult, op1=ALU.mult),
                           [oo.B, ss.B, gsub.B], [yb.B])
                        pv = ps[4].t[:].bitcast(BF16)
                        P.op("tensor", lambda e, pv=pv: e.transpose(out=pv[:, 0:128], in_=yb[:], identity=identb), reads=[yb.B, cstb.B], writes=[ps[4].B])
                        c0 = g * 512 + i * 128
                        A_(lambda e, pv=pv, h=h, c0=c0: e.activation(out=yT[:, h, c0:c0 + 128], in_=pv[:, 0:128], func=AF.Copy), [ps[4].B], [yT.b[h]])
            dump(st, 2, yT[:, 0, :], [yT.b[0]], 1024)
            dump(st, 3, yT[:, NHEADS - 1, :], [yT.b[NHEADS - 1]], 1024)
            if stop == 2:
                P.wait_all("sync", dbg_bufs)
            P.flush()
            if stop == 2:
                P.disabled = True

        with ExitStack() as st:
            WR = [T(P, st, "WR0", [128, 16, 128], BF16)] * 2
            WKr = [T(P, st, "WKr0", [128, 16, 128], BF16)] * 2
            WVr = [T(P, st, "WVr0", [128, 16, 128], BF16)] * 2
            prm = T(P, st, "prm", [128, 8, 8], F32)
            for j, src in enumerate((rwkv_k_k, rwkv_k_a, rwkv_a0)):
                small_load(prm[:, :, j], prm.B, src.rearrange("(a p) -> p a", p=128))
            V_(lambda e: e.tensor_scalar(out=prm[:, :, 3], in0=prm[:, :, 1], scalar1=-1.0, scalar2=1.0, op0=ALU.mult, op1=ALU.add), [prm.B], [prm.B])
            w2b = T(P, st, "w2b", [64, 1024], BF16); a2b = T(P, st, "a2b", [64, 1024], BF16); g2b = T(P, st, "g2b", [128, 2, 1024], BF16)
            P.dma("gpsimd", lambda e: e.dma_start(out=w2b[:], in_=rwkv_w2), w2b.B, writes=[w2b.B])
            P.dma("gpsimd", lambda e: e.dma_start(out=a2b[:], in_=rwkv_a2), a2b.B, writes=[a2b.B])
            P.dma("gpsimd", lambda e: e.dma_start(out=g2b[:, 0, :], in_=rwkv_g2[0:128, :]), g2b.B, writes=[g2b.B])
            P.dma("gpsimd", lambda e: e.dma_start(out=g2b[0:32, 1, :], in_=rwkv_g2[128:160, :]), g2b.B, writes=[g2b.B])
            pp3 = T(P, st, "pp3", [128, 3, 128], F32)
            rkf = T(P, st, "rkf", [128, 8], F32)
            small_load(rkf[:], rkf.B, rwkv_r_k.rearrange("(a p) -> p a", p=128))
            RK = T(P, st, "RK", [128, 8, 2], BF16)
            V_(lambda e: e.memset(RK[:], 0.0), [], [RK.B])
            V_(lambda e: e.tensor_copy(out=RK[0:64, :, 0], in_=rkf[0:64, :]), [rkf.B], [RK.B])
            V_(lambda e: e.tensor_copy(out=RK[64:128, :, 1], in_=rkf[64:128, :]), [rkf.B], [RK.B])
            Rr = [T(P, st, "Rr%d" % i, [128, 513], F32) for i in range(3)]
            tmpr = T(P, st, "tmpr", [128, 512], F32)
            rs = T(P, st, "rs", [128, 512], F32); ks = T(P, st, "ks", [128, 512], F32); vsb = T(P, st, "vsb", [128, 512], BF16)
            af = T(P, st, "af", [128, 512], F32); kkk = T(P, st, "kkk", [128, 512], F32); sqb = T(P, st, "sqb", [128, 512], BF16)
            rinv = T(P, st, "rinv", [128, 512], F32); kk = T(P, st, "kk", [128, 512], F32); uu = tmpr
            k2 = T(P, st, "k2", [128, 512], F32); bb_ = T(P, st, "bb", [128, 512], F32)
            k2b = T(P, st, "k2b", [128, 512], BF16); bbb = T(P, st, "bbb", [128, 512], BF16); rk2 = T(P, st, "rk2", [128, 512], BF16)
            lwt = T(P, st, "lwt", [128, 128], F32)
            ex = T(P, st, "ex", [128, 4, 128], F32)
            ART = T(P, st, "ART", [128, 256], BF16)
            BhT = T(P, st, "BhT", [128, 128], BF16); KhT = T(P, st, "KhT", [128, 128], BF16)
            TMt = T(P, st, "TMt", [128, 4, 128], BF16)
            MTh = [T(P, st, "MT%d" % h_, [128, 2, 256], BF16) for h_ in range(2)]
            NMh = [[T(P, st, "NM%d%d" % (h_, i), [128, 256], BF16) for i in range(2)] for h_ in range(2)]
            Xfh = [T(P, st, "Xf%d" % h_, [128, 128], F32) for h_ in range(2)]; Xbh = [T(P, st, "Xb%d" % h_, [128, 128], BF16) for h_ in range(2)]
            MT, Xf = MTh[1], Xfh[1]
            pBx = [P.buf("pBx%d" % h_) for h_ in range(2)]; pBn = [P.buf("pBn%d" % h_) for h_ in range(2)]; pBq = [P.buf("pBq%d" % h_) for h_ in range(2)]
            GTs = T(P, st, "GTs", [128, 16, 128], BF16); Hs = T(P, st, "Hs", [128, 16, 64], F32)
            QeTs = T(P, st, "QeTs", [128, 8, 128], BF16); Yls = T(P, st, "Yls", [128, 8, 128], F32)
            Vtm = T(P, st, "Vtm", [128, 8, 128], BF16); sbon = T(P, st, "sbon", [128, 8, 2], F32)
            gtm = T(P, st, "gtm", [128, 8, 128], F32)
            Sb = T(P, st, "Sb", [128, 128], BF16)
            Yt = T(P, st, "Yt", [128, 128], F32); yn = T(P, st, "yn", [128, 128], F32); ybr = T(P, st, "ybr", [128, 128], BF16)
            gst = T(P, st, "gst", [128, 2, 6], F32); gmv = T(P, st, "gmv", [128, 2, 2], F32); grs = T(P, st, "grs", [128, 2], F32)
            ident64 = cst[0:64, IDENT, 0:64]
            V_(lambda e: e.memset(GTs[:], 0.0), [], [GTs.B])

            def load_pair(c):
                s = c % 2
                for W_, c0 in ((WR[s], 3072 + c * 128), (WKr[s], 4096 + c * 128), (WVr[s], 5120 + c * 128)):
                    P.dma("gpsimd", lambda e, W_=W_, c0=c0: e.dma_start(out=W_[:], in_=w_in_v[:, :, c0:c0 + 128]), W_.B, writes=[W_.B])

            if CUT == 11:
                P.disabled = True
            load_pair(0)
            for c in range(NPAIRS):
                s = c % 2
                csl = slice(c * 128, (c + 1) * 128)
                for j3, src3 in enumerate((rwkv_w0, lnx_g, lnx_b)):
                    P.dma("sync", lambda e, j3=j3, src3=src3, c=c: e.dma_start(out=pp3[:, j3, :], in_=src3[c * 128:(c + 1) * 128].partition_broadcast(128)), pp3.B, writes=[pp3.B])
                for tb in range(4):
                    for j, (W_, dst, dstB) in enumerate(((WR[s], rs[:, :], rs.B), (WKr[s], ks[:, :], ks.B), (WVr[s], vsb[:, :], vsb.B))):
                        pst = ps[j % 2]
                        proj_fm(W_, 0, 128, tb, pst)
                        shiftmix(pst, 128, tb, Rr[j], murkv[:, j * 8 + c:j * 8 + c + 1], murkv[:, 24 + j * 8 + c:25 + j * 8 + c], murkv.B, dst, dstB, tmpr)
                    mm(ps[0][:, :], a2b[0:64, csl], xaT[0:64, tb * 512:(tb + 1) * 512], True, True, [a2b.B, xaT.B], ps[0].B)
                    A_(lambda e, c=c: e.activation(out=af[:], in_=ps[0][:, :], func=AF.Sigmoid, bias=prm[:, c, 2:3], scale=1.0), [ps[0].B, prm.B], [af.B])
                    V_(lambda e, c=c: e.tensor_scalar(out=kkk[:], in0=ks[:], scalar1=prm[:, c, 0:1], scalar2=None, op0=ALU.mult), [ks.B, prm.B], [kkk.B])
                    A_(lambda e: e.activation(out=sqb[:], in_=kkk[:], func=AF.Square), [kkk.B], [sqb.B])
                    mm(ps[1][:, :], cstb[:, BLK1, :], sqb[:], True, True, [cstb.B, sqb.B], ps[1].B)
                    A_(lambda e: e.activation(out=rinv[:], in_=ps[1][:, :], func=AF.Sqrt, bias=EPSC[:, 2:3], scale=1.0), [ps[1].B, EPSC.B], [rinv.B])
                    V_(lambda e: e.reciprocal(out=rinv[:], in_=rinv[:]), [rinv.B], [rinv.B])
                    V_(lambda e: e.tensor_tensor(out=kk[:], in0=kkk[:], in1=rinv[:], op=ALU.mult), [kkk.B, rinv.B], [kk.B])
                    V_(lambda e, c=c: e.tensor_scalar(out=uu[:], in0=af[:], scalar1=prm[:, c, 1:2], scalar2=prm[:, c, 3:4], op0=ALU.mult, op1=ALU.add), [af.B, prm.B], [uu.B])
                    V_(lambda e: e.tensor_tensor(out=k2[:], in0=ks[:], in1=uu[:], op=ALU.mult), [ks.B, uu.B], [k2.B])
                    V_(lambda e: e.tensor_tensor(out=bb_[:], in0=kk[:], in1=af[:], op=ALU.mult), [kk.B, af.B], [bb_.B])
                    A_(lambda e: e.activation(out=k2b[:], in_=k2[:], func=AF.Copy), [k2.B], [k2b.B])
                    A_(lambda e: e.activation(out=bbb[:], in_=bb_[:], func=AF.Copy), [bb_.B], [bbb.B])
                    V_(lambda e: e.tensor_tensor(out=rk2[:], in0=rs[:], in1=k2[:], op=ALU.mult), [rs.B, k2.B], [rk2.B])
                    if CUT == 12:
                        P.disabled = True
                    for q4 in range(4):
                        ch = tb * 4 + q4
                        own = ch >= 8
                        tsl = slice(q4 * 128, (q4 + 1) * 128)
                        gsl = slice(ch * 128, (ch + 1) * 128)
                        pz = ps[2]
                        mm(pz[:, 0:128], txw[0:64, gsl], w2b[0:64, csl], True, True, [txw.B, w2b.B], pz.B)
                        V_(lambda e, c=c: e.tensor_tensor(out=lwt[:], in0=pz[:, 0:128], in1=pp3[:, 0, :], op=ALU.add), [pz.B, pp3.B], [lwt.B])
                        A_(lambda e: e.activation(out=lwt[:], in_=lwt[:], func=AF.Sigmoid), [lwt.B], [lwt.B])
                        mm(pz[:, 128:384], lwt[:], cst[:, TRI_I:TRI_E + 1, :].rearrange("p a b -> p (a b)"), True, True, [lwt.B, cst.B], pz.B)
                        mm(pz[:, 384:512], cst[:, TRI_R, :], lwt[:], True, True, [lwt.B, cst.B], pz.B)
                        A_(lambda e: e.activation(out=ex[:, 0:2, :].rearrange("p a b -> p (a b)"), in_=pz[:, 128:384], func=AF.Exp), [pz.B], [ex.B])
                        A_(lambda e: e.activation(out=ex[:, 2, :], in_=pz[:, 128:256], func=AF.Exp, scale=-1.0), [pz.B], [ex.B])
                        A_(lambda e: e.activation(out=ex[:, 3, :], in_=pz[:, 384:512], func=AF.Exp), [pz.B], [ex.B])
                        V_(lambda e, tsl=tsl: e.scalar_tensor_tensor(out=ART[:, 0:128], in0=kk[:, tsl], scalar=-1.0, in1=ex[:, 1, :], op0=ALU.mult, op1=ALU.mult), [kk.B, ex.B], [ART.B])
                        V_(lambda e, tsl=tsl: e.tensor_tensor(out=ART[:, 128:256], in0=rs[:, tsl], in1=ex[:, 0, :], op=ALU.mult), [rs.B, ex.B], [ART.B])
                        V_(lambda e, tsl=tsl: e.tensor_tensor(out=BhT[:], in0=bb_[:, tsl], in1=ex[:, 2, :], op=ALU.mult), [bb_.B, ex.B], [BhT.B])
                        V_(lambda e, tsl=tsl: e.tensor_tensor(out=KhT[:], in0=k2[:, tsl], in1=ex[:, 2, :], op=ALU.mult), [k2.B, ex.B], [KhT.B])
                        pt = ps[3]
                        ptv = pt.t[:].bitcast(BF16)
                        for j, (src, sB) in enumerate(((bbb[:, tsl], bbb.B), (k2b[:, tsl], k2b.B), (vsb[:, tsl], vsb.B), (ART[:, 0:128], ART.B))):
                            P.op("tensor", lambda e, j=j, src=src, ptv=ptv: e.transpose(out=ptv[:, j * 128:(j + 1) * 128], in_=src, identity=identb), reads=[sB, cstb.B], writes=[pt.B])
                        V_(lambda e, ptv=ptv: e.tensor_tensor(out=TMt[:, 0:2, :], in0=ptv[:, 0:256].rearrange("p (a b) -> p a b", b=128),
                                                      in1=ex[:, 3:4, :].to_broadcast([128, 2, 128]), op=ALU.mult), [pt.B, ex.B], [TMt.B])
                        A_(lambda e, ptv=ptv: e.activation(out=TMt[:, 2:4, :].rearrange("p a b -> p (a b)"), in_=ptv[:, 256:512], func=AF.Copy), [pt.B], [TMt.B])
                        if own:
                            oc = ch - 8
                            A_(lambda e, oc=oc: e.activation(out=Vtm[:, oc, :], in_=TMt[:, 2, :], func=AF.Copy), [TMt.B], [Vtm.B])
                            mm(ps[3][:, 256:258], rk2[:, tsl], RK[:, c, :], True, True, [rk2.B, RK.B], ps[3].B)
                            V_(lambda e, oc=oc: e.tensor_copy(out=sbon[:, oc, :], in_=ps[3][:, 256:258]), [ps[3].B], [sbon.B])
                            mm(ps[3][:, 384:512], sxa[:, gsl], g2b[:, 0, csl], True, False, [sxa.B, g2b.B], ps[3].B)
                            mm(ps[3][:, 384:512], sxb[0:32, gsl], g2b[0:32, 1, csl], False, True, [sxb.B, g2b.B], ps[3].B)
                            V_(lambda e, oc=oc: e.tensor_copy(out=gtm[:, oc, :], in_=ps[3][:, 384:512]), [ps[3].B], [gtm.B])
                        if CUT == 13:
                            P.disabled = True
                        if CUT == 18 and ch == 8:
                            P.disabled = True
                        def chain(ch, hh, own, tsl):
                            hp = slice(hh * 64, (hh + 1) * 64)
                            tpk = (hh * 64, 0)
                            tpo = (0, hh * 64)
                            pA, pB = ps[4 + hh], ps[6 + hh]
                            bX = bN = bQ = pB.B
                            NM, Xf, Xb, MT = NMh[hh], Xfh[hh], Xbh[hh], MTh[hh]
                            mm(pA[:, 0:256], BhT[hp, :], ART[hp, :], True, True, [BhT.B, ART.B], pA.B, tpk)
                            mm(pA[:, 256:512], KhT[hp, :], ART[hp, :], True, True, [KhT.B, ART.B], pA.B, tpk)
                            mm(pB[:, 384:512], ART[hp, 0:128], BhT[hp, :], True, True, [BhT.B, ART.B], bQ, tpk)
                            yield
                            V_(lambda e: e.tensor_tensor(out=MT[:, :, :], in0=pA[:, :].rearrange("p (a b) -> p a b", b=256),
                                                         in1=cst[:, UT_S:UT_I + 1, :].rearrange("p a b -> p (a b)").unsqueeze(1).to_broadcast([128, 2, 256]), op=ALU.mult),
                               [pA.B, cst.B], [MT.B])
                            V_(lambda e: e.tensor_tensor(out=NM[0][:, 128:256], in0=pB[:, 384:512], in1=cst[:, LT_S, :], op=ALU.mult), [bQ, cst.B], [NM[0].B])
                            A_(lambda e: e.activation(out=Xf[:, 0:64], in_=TMt[:, 3, hp], func=AF.Copy), [TMt.B], [Xf.B])
                            A_(lambda e: e.activation(out=NM[0][:, 0:128], in_=MT[:, 0, 0:128], func=AF.Copy), [MT.B], [NM[0].B])
                            yield
                            mm(pA[:, 0:64], MT[:, 1, 0:128], TMt[:, 2, hp], True, True, [MT.B, TMt.B], pA.B)
                            yield
                            V_(lambda e: e.tensor_copy(out=Xf[:, 64:128], in_=pA[:, 0:64]), [pA.B], [Xf.B])
                            A_(lambda e: e.activation(out=Xb[:], in_=Xf[:], func=AF.Copy), [Xf.B], [Xb.B])
                            yield
                            for lv in range(7):
                                cur, nxt = NM[lv % 2], NM[(lv + 1) % 2]
                                mm(pB[:, 0:128], cur[:, 0:128], Xb[:], True, True, [cur.B, Xb.B], bX)
                                if lv < 6:
                                    mm(pB[:, 128:256], cur[:, 128:256], cur[:, 0:128], True, True, [cur.B], bN)
                                    mm(pB[:, 256:384], cur[:, 0:128], cur[:, 128:256], True, True, [cur.B], bN)
                                yield
                                V_(lambda e: e.tensor_tensor(out=Xf[:], in0=Xf[:], in1=pB[:, 0:128], op=ALU.add), [Xf.B, bX], [Xf.B])
                                A_(lambda e: e.activation(out=Xb[:], in_=Xf[:], func=AF.Copy), [Xf.B], [Xb.B])
                                if lv < 6:
                                    V_(lambda e, nxt=nxt: e.tensor_copy(out=nxt[:], in_=pB[:, 128:384]), [bN], [nxt.B])
                                yield
                            mm(pA[hp, 64:128], Xb[:, 0:64], TMt[:, 0, hp], True, True, [Xb.B, TMt.B], pA.B, tpo)
                            mm(pA[hp, 128:192], TMt[:, 0, hp], Xb[:, 64:128], True, False, [Xb.B, TMt.B], pA.B, tpo)
                            mm(pA[hp, 128:192], TMt[:, 1, hp], TMt[:, 2, hp], False, True, [TMt.B], pA.B, tpo)
                            if own:
                                mm(pB[hp, 384:512], Xb[:, 0:64], MT[:, 0, 128:256], True, True, [Xb.B, MT.B], bQ, tpo)
                                mm(pA[:, 192:256], MT[:, 0, 128:256], Xb[:, 64:128], True, False, [Xb.B, MT.B], pA.B)
                                mm(pA[:, 192:256], MT[:, 1, 128:256], TMt[:, 2, hp], False, True, [TMt.B, MT.B], pA.B)
                            yield
                            V_(lambda e: e.scalar_tensor_tensor(out=GTs[hp, ch, hh * 64:(hh + 1) * 64], in0=cst[hp, IDENT, hh * 64:(hh + 1) * 64], scalar=ex[hp, 0, 127:128],
                                                                in1=pA[hp, 64:128], op0=ALU.mult, op1=ALU.add), [cst.B, ex.B, pA.B], [GTs.B])
                            V_(lambda e: e.tensor_copy(out=Hs[hp, ch, :], in_=pA[hp, 128:192]), [pA.B], [Hs.B])
                            if own:
                                oc = ch - 8
                                V_(lambda e: e.tensor_tensor(out=QeTs[hp, oc, :], in0=pB[hp, 384:512], in1=ART[hp, 128:256], op=ALU.add),
                                   [bQ, ART.B], [QeTs.B])
                                V_(lambda e: e.tensor_copy(out=Yls[:, oc, hp], in_=pA[:, 192:256]), [pA.B], [Yls.B])

                        gens = [chain(ch, 0, own, tsl), chain(ch, 1, own, tsl)]
                        while gens:
                            for g_ in list(gens):
                                try:
                                    next(g_)
                                except StopIteration:
                                    gens.remove(g_)
                if c + 1 < NPAIRS:
                    load_pair(c + 1)
                if CUT == 17:
                    P.disabled = True
                V_(lambda e: e.memset(Sb[:], 0.0), [], [Sb.B])
                for ch in range(16):
                    if CUT == 19 and ch == 8:
                        P.disabled = True
                    if ch >= 8:
                        oc = ch - 8
                        mm(ps[0][:, 0:128], QeTs[:, oc, :], Sb[:, :], True, True, [QeTs.B, Sb.B], ps[0].B)
                        V_(lambda e, oc=oc: e.tensor_tensor(out=Yt[:], in0=ps[0][:, 0:128], in1=Yls[:, oc, :], op=ALU.add), [ps[0].B, Yls.B], [Yt.B])
                    if CUT == 20 and ch == 8:
                        P.disabled = True
                    if ch < 15:
                        mm(ps[1][:, 0:128], GTs[:, ch, :], Sb[:, :], True, True, [GTs.B, Sb.B], ps[1].B)
                        for hh in range(2):
                            hp = slice(hh * 64, (hh + 1) * 64)
                            V_(lambda e, ch=ch, hp=hp: e.tensor_tensor(out=Sb[hp, hp], in0=ps[1][hp, hp], in1=Hs[hp, ch, :], op=ALU.add), [ps[1].B, Hs.B], [Sb.B])
                    if CUT == 21 and ch == 8:
                        P.disabled = True
                    if ch >= 8:
                        for hh in range(2):
                            V_(lambda e, hh=hh: e.bn_stats(out=gst[:, hh, :], in_=Yt[:, hh * 64:(hh + 1) * 64]), [Yt.B], [gst.B])
                            V_(lambda e, hh=hh: e.bn_aggr(out=gmv[:, hh, :], in_=gst[:, hh, :]), [gst.B], [gmv.B])
                        A_(lambda e: e.activation(out=grs[:], in_=gmv[:, :, 1], func=AF.Sqrt, bias=EPSC[:, 1:2], scale=1.0), [gmv.B, EPSC.B], [grs.B])
                        V_(lambda e: e.reciprocal(out=grs[:], in_=grs[:]), [grs.B], [grs.B])
                        for hh in range(2):
                            hs_ = slice(hh * 64, (hh + 1) * 64)
                            V_(lambda e, hh=hh, hs_=hs_: e.tensor_scalar(out=yn[:, hs_], in0=Yt[:, hs_], scalar1=gmv[:, hh, 0:1], scalar2=grs[:, hh:hh + 1],
                                                                       op0=ALU.subtract, op1=ALU.mult), [Yt.B, gmv.B, grs.B], [yn.B])
                        V_(lambda e, c=c: e.tensor_tensor(out=yn[:], in0=yn[:], in1=pp3[:, 1, :], op=ALU.mult), [yn.B, pp3.B], [yn.B])
                        V_(lambda e, c=c: e.tensor_tensor(out=yn[:], in0=yn[:], in1=pp3[:, 2, :], op=ALU.add), [yn.B, pp3.B], [yn.B])
                        for hh in range(2):
                            hs_ = slice(hh * 64, (hh + 1) * 64)
                            V_(lambda e, hh=hh, hs_=hs_, oc=oc: e.scalar_tensor_tensor(out=yn[:, hs_], in0=Vtm[:, oc, hs_], scalar=sbon[:, oc, hh:hh + 1], in1=yn[:, hs_],
                                                                                      op0=ALU.mult, op1=ALU.add), [Vtm.B, sbon.B, yn.B], [yn.B])
                        V_(lambda e, oc=oc: e.tensor_tensor(out=ybr[:], in0=yn[:], in1=gtm[:, oc, :], op=ALU.mult), [yn.B, gtm.B], [ybr.B])
                        if CUT == 22 and ch == 8:
                            P.disabled = True
                        pv = ps[3].t[:].bitcast(BF16)
                        P.op("tensor", lambda e, pv=pv: e.transpose(out=pv[:, 0:128], in_=ybr[:], identity=identb), reads=[ybr.B, cstb.B], writes=[ps[3].B])
                        if CUT == 23 and ch == 8:
                            P.disabled = True
                        A_(lambda e, pv=pv, c=c, oc=oc: e.activation(out=yT[:, 8 + c, oc * 128:(oc + 1) * 128], in_=pv[:, 0:128], func=AF.Copy), [ps[3].B], [yT.b[8 + c]])
            if CUT == 25:
                P.disabled = True
            if dbg and stop == 3 and CUT == 77:
                d6 = T(P, st, "d6", [128, 1024], F32)
                def dcp(c0, w, ap, bufs):
                    V_(lambda e: e.tensor_copy(out=d6[:, c0:c0 + w], in_=ap), bufs, [d6.B])
                def dsend(sl_):
                    P.dma("sync", lambda e: e.dma_start(out=dbg_d[sl_], in_=d6[:]), d6.B, reads=[d6.B])
                V_(lambda e: e.memset(d6[:], 0.0), [], [d6.B])
                dcp(0, 512, ex[:].rearrange("p a b -> p (a b)"), [ex.B]); dcp(512, 128, Xf[:], [Xf.B]); dcp(640, 128, lwt[:], [lwt.B])
                dcp(768, 128, Yt[:], [Yt.B]); dcp(896, 128, yn[:], [yn.B])
                dsend(6)
                dcp(0, 256, ART[:], [ART.B]); dcp(256, 128, BhT[:], [BhT.B]); dcp(384, 128, KhT[:], [KhT.B])
                dcp(512, 512, TMt[:].rearrange("p a b -> p (a b)"), [TMt.B])
                dsend(7)
                dcp(0, 512, MT[:].rearrange("p a b -> p (a b)"), [MT.B]); dcp(512, 64, GTs[:, 15, 0:64], [GTs.B]); dcp(576, 64, Hs[:, 15, :], [Hs.B])
                dcp(640, 128, QeTs[:, 7, :], [QeTs.B]); dcp(768, 128, Yls[:, 7, :], [Yls.B]); dcp(896, 64, Sb[:, 0:64], [Sb.B])
                dcp(960, 2, sbon[:, 7, :], [sbon.B])
                dsend(8)
                dbg_bufs.append(d6.B)
            dump(st, 4, yT[:, 8, :], [yT.b[8]], 1024)
            if not (dbg and stop == 3):
                dump(st, 5, yT[:, 7 + NPAIRS, :], [yT.b[7 + NPAIRS]], 1024)
            if stop == 3:
                P.wait_all("sync", dbg_bufs)
            P.flush()
            if stop == 3:
                P.disabled = True

        stA.close()
        with ExitStack() as st:
            acc = T(P, st, "acc", [128, 8, D], F32, nb=8)
            wt_ = T(P, st, "wt", [128, 8, 32], F32)
            accB = acc.b
            gb2 = [None, None]
            ws = [None, None, None]

            class AccT:
                def __init__(self, i):
                    self.i = i; self.B = accB[i]

                def __getitem__(self, k):
                    return acc[:, self.i, :] if k == slice(None) else acc[:, self.i, k[1]]

            def load_gb(gs, bs):
                P.dma("sync", lambda e: e.dma_start(out=gb2[0][:], in_=gs.partition_broadcast(128)), gb2[0].B, writes=[gb2[0].B])
                if bs is not None:
                    P.dma("sync", lambda e: e.dma_start(out=gb2[1][:], in_=bs.partition_broadcast(128)), gb2[1].B, writes=[gb2[1].B])

            with ExitStack() as st2:
                gb2[0] = T(P, st2, "gb2a0", [128, D], F32); gb2[1] = T(P, st2, "gb2a1", [128, D], F32)
                for i in range(3):
                    ws[i] = T(P, st2, "wsa%d" % i, [128, 16, 512], BF16)
                xt = [T(P, st2, "xt4%d" % i, [128, D], F32) for i in range(2)]
                xn = T(P, st2, "xn4", [128, D], F32)
                stt4 = T(P, st2, "stt4", [128, 4, 6], F32); mv4 = T(P, st2, "mv4", [128, 4], F32)
                load_gb(ln_in_g, ln_in_b)
                for i in range(8):
                    X = xt[i % 2]
                    P.dma("sync", lambda e, X=X, i=i: e.dma_start(out=X[:], in_=xs[(8 + i) * 128:(9 + i) * 128, :]), X.B, writes=[X.B])
                    ln_tile(P, X, xn, stt4, mv4, gb2[0], gb2[1], None, AccT(i))
                    A_(lambda e, i=i: e.activation(out=acc[:, i, :], in_=acc[:, i, :], func=AF.Copy, scale=ALPHA), [accB[i]], [accB[i]])
                w_out_v = w_out.rearrange("(kc p) n -> p kc n", p=128)
                for n in range(4):
                    W_ = ws[n % 3]
                    P.dma("gpsimd", lambda e, W_=W_, n=n: e.dma_start(out=W_[:], in_=w_out_v[:, :, n * 512:(n + 1) * 512]), W_.B, writes=[W_.B])
                    for i in range(8):
                        pst = ps[i % 2]
                        for kc in range(16):
                            mm(pst[:, :], yT[:, kc, i * 128:(i + 1) * 128], W_[:, kc, :], kc == 0, kc == 15, [yT.b[kc], W_.B], pst.B)
                        V_(lambda e, i=i, n=n, pst=pst: e.tensor_tensor(out=acc[:, i, n * 512:(n + 1) * 512], in0=acc[:, i, n * 512:(n + 1) * 512], in1=pst[:, :], op=ALU.add),
                           [accB[i], pst.B], [accB[i]])
                dump(st2, 6, acc[:, 0, 0:1024], [accB[0]], 1024)
                if stop == 4:
                    P.wait_all("sync", dbg_bufs)
                P.flush()
                if stop == 4:
                    P.disabled = True
            hT1 = T(P, st, "hT1", [128, 16, 1024], BF16, nb=8)
            with ExitStack() as st2:
                gb2[0] = T(P, st2, "gb2b0", [128, D], F32); gb2[1] = T(P, st2, "gb2b1", [128, D], F32)
                xn = T(P, st2, "xn4b", [128, D], F32)
                hb4 = T(P, st2, "hb4", [128, D], BF16)
                hlo = T(P, st2, "hlo", [128, D], BF16)
                stt4 = T(P, st2, "stt4b", [128, 4, 6], F32); mv4 = T(P, st2, "mv4b", [128, 4], F32)
                hT1l = T(P, st2, "hT1l", [128, 16, 128], BF16)
                load_gb(ln1_g, ln1_b)
                rwf = T(P, st2, "rwf", [128, 16, 36], F32); rwh = T(P, st2, "rwh", [128, 16, 36], BF16); rwl = T(P, st2, "rwl", [128, 16, 36], BF16)
                rbias = T(P, st2, "rbias", [128, 36], F32)
                P.dma("sync", lambda e: e.dma_start(out=rwf[:, :, 0:4], in_=rgw.rearrange("(kc p) n -> p kc n", p=128)), rwf.B, writes=[rwf.B])
                P.dma("sync", lambda e: e.dma_start(out=rwf[:, :, 4:36], in_=rew.rearrange("(kc p) n -> p kc n", p=128)), rwf.B, writes=[rwf.B])
                P.dma("sync", lambda e: e.dma_start(out=rbias[:, 0:4], in_=rgb.partition_broadcast(128)), rbias.B, writes=[rbias.B])
                P.dma("sync", lambda e: e.dma_start(out=rbias[:, 4:36], in_=reb.partition_broadcast(128)), rbias.B, writes=[rbias.B])
                V_(lambda e: e.tensor_copy(out=rwh[:], in_=rwf[:]), [rwf.B], [rwh.B])
                V_(lambda e: e.tensor_tensor(out=rwf[:], in0=rwf[:], in1=rwh[:], op=ALU.subtract), [rwf.B, rwh.B], [rwf.B])
                V_(lambda e: e.tensor_copy(out=rwl[:], in_=rwf[:]), [rwf.B], [rwl.B])
                L = T(P, st2, "L", [128, 36], F32); r1 = T(P, st2, "r1", [128, 8], F32)
                gm = T(P, st2, "gm", [128, 4], F32); elm = T(P, st2, "elm", [128, 32], F32); ee = T(P, st2, "ee", [128, 32], F32)
                m1 = T(P, st2, "m1", [128, 32], F32); e2 = T(P, st2, "e2", [128, 32], F32); m2 = T(P, st2, "m2", [128, 32], F32)
                for i in range(8):
                    A = AccT(i)
                    ln_tile(P, A, xn, stt4, mv4, gb2[0], gb2[1], hb4, A)
                    transpose_to_fm(P, hb4, hT1, i, ps, identb, cstb.B, pbase=2)
                    V_(lambda e, i=i: e.tensor_tensor(out=xn[:], in0=acc[:, i, :], in1=hb4[:], op=ALU.subtract), [accB[i], hb4.B], [xn.B])
                    A_(lambda e: e.activation(out=hlo[:], in_=xn[:], func=AF.Copy), [xn.B], [hlo.B])
                    transpose_to_fm(P, hlo, hT1l, 0, ps, identb, cstb.B, pbase=4)
                    tsl = slice(i * 128, (i + 1) * 128)
                    n_mm = 0
                    for (hT_, w_, sl_, bi_) in ((hT1, rwh, tsl, i), (hT1l, rwh, slice(0, 128), 0), (hT1, rwl, tsl, i)):
                        for kc in range(16):
                            mm(ps[6][:, 0:36], hT_[:, kc, sl_], w_[:, kc, :], n_mm == 0, n_mm == 47, [hT_.b[bi_], w_.B], ps[6].B)
                            n_mm += 1
                    V_(lambda e: e.tensor_tensor(out=L[:], in0=ps[6][:, 0:36], in1=rbias[:], op=ALU.add), [ps[6].B, rbias.B], [L.B])
                    V_(lambda e: e.reduce_max(out=r1[:, 0:1], in_=L[:, 0:4], axis=AX.X), [L.B], [r1.B])
                    V_(lambda e: e.tensor_scalar(out=gm[:], in0=L[:, 0:4], scalar1=r1[:, 0:1], scalar2=None, op0=ALU.is_ge), [L.B, r1.B], [gm.B])
                    V_(lambda e: e.tensor_scalar(out=r1[:, 1:2], in0=r1[:, 0:1], scalar1=-1.0, scalar2=None, op0=ALU.mult), [r1.B], [r1.B])
                    A_(lambda e: e.activation(out=ee[:, 0:4], in_=L[:, 0:4], func=AF.Exp, bias=r1[:, 1:2], scale=1.0, accum_out=r1[:, 2:3]), [L.B, r1.B], [ee.B, r1.B])
                    V_(lambda e: e.reciprocal(out=r1[:, 3:4], in_=r1[:, 2:3]), [r1.B], [r1.B])
                    V_(lambda e: e.tensor_scalar(out=gm[:], in0=gm[:], scalar1=-1.0, scalar2=-NEG, op0=ALU.add, op1=ALU.mult), [gm.B], [gm.B])
                    V_(lambda e: e.tensor_tensor(out=elm[:].rearrange("p (g e) -> p g e", e=8), in0=L[:, 4:36].rearrange("p (g e) -> p g e", e=8),
                                                 in1=gm[:].unsqueeze(2).to_broadcast([128, 4, 8]), op=ALU.add), [L.B, gm.B], [elm.B])
                    V_(lambda e: e.reduce_max(out=r1[:, 4:5], in_=elm[:], axis=AX.X), [elm.B], [r1.B])
                    V_(lambda e: e.tensor_scalar(out=r1[:, 5:6], in0=r1[:, 4:5], scalar1=-1.0, scalar2=None, op0=ALU.mult), [r1.B], [r1.B])
                    A_(lambda e: e.activation(out=ee[:], in_=elm[:], func=AF.Exp, bias=r1[:, 5:6], scale=1.0), [elm.B, r1.B], [ee.B])
                    V_(lambda e: e.tensor_scalar(out=m1[:], in0=elm[:], scalar1=r1[:, 4:5], scalar2=None, op0=ALU.is_ge), [elm.B, r1.B], [m1.B])
                    V_(lambda e: e.tensor_tensor(out=e2[:], in0=ee[:], in1=m1[:], op=ALU.mult), [ee.B, m1.B], [e2.B])
                    V_(lambda e: e.tensor_tensor(out=e2[:], in0=ee[:], in1=e2[:], op=ALU.subtract), [ee.B, e2.B], [e2.B])
                    V_(lambda e: e.reduce_max(out=r1[:, 6:7], in_=e2[:], axis=AX.X), [e2.B], [r1.B])
                    V_(lambda e: e.tensor_scalar(out=m2[:], in0=e2[:], scalar1=r1[:, 6:7], scalar2=None, op0=ALU.is_ge), [e2.B, r1.B], [m2.B])
                    V_(lambda e: e.tensor_tensor(out=m2[:], in0=m2[:], in1=e2[:], op=ALU.mult), [m2.B, e2.B], [m2.B])
                    V_(lambda e: e.tensor_tensor(out=m1[:], in0=m1[:], in1=m2[:], op=ALU.add), [m1.B, m2.B], [m1.B])
                    V_(lambda e: e.tensor_scalar(out=r1[:, 7:8], in0=r1[:, 6:7], scalar1=1.0, scalar2=None, op0=ALU.add), [r1.B], [r1.B])
                    V_(lambda e: e.reciprocal(out=r1[:, 7:8], in_=r1[:, 7:8]), [r1.B], [r1.B])
                    V_(lambda e, i=i: e.tensor_scalar(out=wt_[:, i, :], in0=m1[:], scalar1=r1[:, 7:8], scalar2=r1[:, 3:4], op0=ALU.mult, op1=ALU.mult), [m1.B, r1.B], [wt_.B])
                    A_(lambda e, i=i: e.activation(out=acc[:, i, :], in_=acc[:, i, :], func=AF.Copy, scale=ALPHA), [accB[i]], [accB[i]])
                dump(st2, 7, wt_[:, 0, :], [wt_.B], 32)
                if stop == 5:
                    P.wait_all("sync", dbg_bufs)
                P.flush()
                if stop == 5:
                    P.disabled = True
            with ExitStack() as st2:
                for i in range(3):
                    ws[i] = T(P, st2, "wsc%d" % i, [128, 16, 512], BF16)
                hid = [T(P, st2, "hid0", [128, 4, 1024], BF16)] * 2
                sg = [T(P, st2, "sg%d" % i, [128, 512], BF16) for i in range(2)]
                nld = 0
                for ex_ in range(NEXP):
                    Wg, Wu, Wd = ws[0], ws[1], ws[2]
                    P.dma("gpsimd", lambda e, ex_=ex_: e.dma_start(out=ws[0][:], in_=wg_d[ex_].rearrange("(kc p) n -> p kc n", p=128)), ws[0].B, writes=[ws[0].B])
                    P.dma("gpsimd", lambda e, ex_=ex_: e.dma_start(out=ws[1][:], in_=wu_d[ex_].rearrange("(kc p) n -> p kc n", p=128)), ws[1].B, writes=[ws[1].B])
                    P.dma("gpsimd", lambda e, ex_=ex_: e.dma_start(out=ws[2][:].rearrange("p (a b) n -> p a b n", b=4), in_=wd_d[ex_].rearrange("(fc p) (b n) -> p fc b n", p=128, n=512)),
                          ws[2].B, writes=[ws[2].B])
                    H_ = hid[ex_ % 2]
                    k_ = 0
                    for tb in range(2):
                        for fc in range(4):
                            pg, pu = ps[(k_ % 2) * 2], ps[(k_ % 2) * 2 + 1]
                            S_ = sg[k_ % 2]
                            for kc in range(16):
                                mm(pg[:, :], Wg[:, kc, fc * 128:(fc + 1) * 128], hT1[:, kc, tb * 512:(tb + 1) * 512], kc == 0, kc == 15, [Wg.B] + hT1.b[tb * 4:(tb + 1) * 4], pg.B)
                            for kc in range(16):
                                mm(pu[:, :], Wu[:, kc, fc * 128:(fc + 1) * 128], hT1[:, kc, tb * 512:(tb + 1) * 512], kc == 0, kc == 15, [Wu.B] + hT1.b[tb * 4:(tb + 1) * 4], pu.B)
                            A_(lambda e, S_=S_, pg=pg: e.activation(out=S_[:], in_=pg[:, :], func=AF.Silu), [pg.B], [S_.B])
                            V_(lambda e, S_=S_, pu=pu, H_=H_, fc=fc, tb=tb: e.tensor_tensor(out=H_[:, fc, tb * 512:(tb + 1) * 512], in0=S_[:], in1=pu[:, :], op=ALU.mult),
                               [S_.B, pu.B], [H_.B])
                            k_ += 1
                    for i in range(8):
                        for n in range(4):
                            po = ps[4 + (i * 4 + n) % 4]
                            for fc in range(4):
                                mm(po[:, :], H_[:, fc, i * 128:(i + 1) * 128], Wd[:, fc * 4 + n, :], fc == 0, fc == 3, [H_.B, Wd.B], po.B)
                            V_(lambda e, i=i, n=n, po=po, ex_=ex_: e.scalar_tensor_tensor(out=acc[:, i, n * 512:(n + 1) * 512], in0=po[:, :], scalar=wt_[:, i, ex_:ex_ + 1],
                                                                                  in1=acc[:, i, n * 512:(n + 1) * 512], op0=ALU.mult, op1=ALU.add), [po.B, wt_.B, accB[i]], [accB[i]])
                dump(st2, 8, acc[:, 0, 0:1024], [accB[0]], 1024)
                if stop == 6:
                    P.wait_all("sync", dbg_bufs)
                P.flush()
                if stop == 6:
                    P.disabled = True
            with ExitStack() as st2:
                gb2[0] = T(P, st2, "gb2d0", [128, D], F32); gb2[1] = T(P, st2, "gb2d1", [128, D], F32)
                ws[0] = T(P, st2, "wsd0", [128, 16, 512], BF16)
                xn = T(P, st2, "xn5", [128, D], F32); hb4 = T(P, st2, "hb5", [128, D], BF16)
                stt4 = T(P, st2, "stt5", [128, 4, 6], F32); mv4 = T(P, st2, "mv5", [128, 4], F32)
                pf = T(P, st2, "pf", [128, 256], F32); pb = T(P, st2, "pb", [128, 256], BF16)
                pT = T(P, st2, "pT", [128, 2, 1024], BF16, nb=8)
                plw = T(P, st2, "plw", [128, 2, D], BF16)
                ssq = T(P, st2, "ssq", [128, 8, 4], F32); rsp = T(P, st2, "rsp", [128, 8], F32)
                sgt = T(P, st2, "sgt", [128, 512], F32); t1_ = T(P, st2, "t1", [128, 512], F32)
                load_gb(ln2_g, ln2_b)
                P.dma("gpsimd", lambda e: e.dma_start(out=plw[:], in_=ple_w.rearrange("(kc p) n -> p kc n", p=128)), plw.B, writes=[plw.B])
                if CUT == 31:
                    P.disabled = True
                for i in range(8):
                    A = AccT(i)
                    ln_tile(P, A, xn, stt4, mv4, gb2[0], gb2[1], hb4, A)
                    transpose_to_fm(P, hb4, hT1, i, ps, identb, cstb.B, pbase=2)
                    P.dma("sync", lambda e, i=i: e.dma_start(out=pf[:], in_=p_in[i * 128:(i + 1) * 128, :]), pf.B, writes=[pf.B])
                    V_(lambda e: e.tensor_copy(out=pb[:], in_=pf[:]), [pf.B], [pb.B])
                    pv = ps[4].t[:].bitcast(BF16)
                    for k in range(2):
                        P.op("tensor", lambda e, k=k, pv=pv: e.transpose(out=pv[:, k * 128:(k + 1) * 128], in_=pb[:, k * 128:(k + 1) * 128], identity=identb), reads=[pb.B, cstb.B], writes=[ps[4].B])
                    V_(lambda e, i=i, pv=pv: e.tensor_copy(out=pT[:, :, i * 128:(i + 1) * 128], in_=pv[:, 0:256].rearrange("p (k t) -> p k t", t=128)), [ps[4].B], [pT.b[i]])
                    for n in range(4):
                        pp = ps[5 + n % 2]
                        for k in range(2):
                            mm(pp[:, :], pT[:, k, i * 128:(i + 1) * 128], plw[:, k, n * 512:(n + 1) * 512], k == 0, k == 1, [pT.b[i], plw.B], pp.B)
                        A_(lambda e, i=i, n=n, pp=pp: e.activation(out=t1_[:], in_=pp[:, :], func=AF.Square, accum_out=ssq[:, i, n:n + 1]), [pp.B], [t1_.B, ssq.B])
                if CUT == 32:
                    P.disabled = True
                V_(lambda e: e.reduce_sum(out=rsp[:], in_=ssq[:], axis=AX.X), [ssq.B], [rsp.B])
                A_(lambda e: e.activation(out=rsp[:], in_=rsp[:], func=AF.Sqrt, bias=EPSC[:, 0:1], scale=1.0 / D), [rsp.B, EPSC.B], [rsp.B])
                V_(lambda e: e.reciprocal(out=rsp[:], in_=rsp[:]), [rsp.B], [rsp.B])
                P.dma("sync", lambda e: e.dma_start(out=gb2[0][:], in_=ple_ng.partition_broadcast(128)), gb2[0].B, writes=[gb2[0].B])
                if CUT == 33:
                    P.disabled = True
                pgw_v = ple_gw.rearrange("(kc p) n -> p kc n", p=128)
                for n in range(4):
                    W_ = ws[0]
                    P.dma("gpsimd", lambda e, W_=W_, n=n: e.dma_start(out=W_[:], in_=pgw_v[:, :, n * 512:(n + 1) * 512]), W_.B, writes=[W_.B])
                    nsl = slice(n * 512, (n + 1) * 512)
                    for i in range(8):
                        pgt, pp = ps[(i % 2) * 2], ps[(i % 2) * 2 + 1]
                        for kc in range(16):
                            mm(pgt[:, :], hT1[:, kc, i * 128:(i + 1) * 128], W_[:, kc, :], kc == 0, kc == 15, [hT1.b[i], W_.B], pgt.B)
                        for k in range(2):
                            mm(pp[:, :], pT[:, k, i * 128:(i + 1) * 128], plw[:, k, nsl], k == 0, k == 1, [pT.b[i], plw.B], pp.B)
                        A_(lambda e, pgt=pgt: e.activation(out=sgt[:], in_=pgt[:, :], func=AF.Sigmoid), [pgt.B], [sgt.B])
                        V_(lambda e, i=i, pp=pp, nsl=nsl: e.scalar_tensor_tensor(out=t1_[:], in0=pp[:, :], scalar=rsp[:, i:i + 1], in1=gb2[0][:, nsl], op0=ALU.mult, op1=ALU.mult),
                           [pp.B, rsp.B, gb2[0].B], [t1_.B])
                        V_(lambda e: e.tensor_tensor(out=t1_[:], in0=t1_[:], in1=sgt[:], op=ALU.mult), [t1_.B, sgt.B], [t1_.B])
                        V_(lambda e, i=i, nsl=nsl: e.tensor_tensor(out=acc[:, i, nsl], in0=acc[:, i, nsl], in1=t1_[:], op=ALU.add), [accB[i], t1_.B], [accB[i]])
                        P.dma("sync", lambda e, i=i, nsl=nsl: e.dma_start(out=out_d[i * 128:(i + 1) * 128, nsl], in_=acc[:, i, nsl]), accB[i], reads=[accB[i]])
                if CUT == 34:
                    P.disabled = True
                P.wait_all("sync", accB + dbg_bufs)
                if stop == 7:
                    P.wait_all("sync", dbg_bufs)
                P.flush()
      except _Stop:
        pass
    return nc


EPSC = None


def rsqrt(P, out, in_, outB, inB, eps_ap):
    P.op("scalar", lambda e: e.activation(out=out, in_=in_, func=AF.Sqrt, bias=eps_ap, scale=1.0), reads=[inB, EPSC.B], writes=[outB])
    P.op("vector", lambda e: e.reciprocal(out=out, in_=out), reads=[outB], writes=[outB])


def ln_tile(P, X, XN, ST, MV, gbc, bbc, out_bf, out_f32, eps=1e-5):
    for j in range(4):
        P.op("vector", lambda e, j=j: e.bn_stats(out=ST[:, j, :], in_=X[:, j * 512:(j + 1) * 512]), reads=[X.B], writes=[ST.B])
    P.op("vector", lambda e: e.bn_aggr(out=MV[:, 0:2], in_=ST[:]), reads=[ST.B], writes=[MV.B])
    rsqrt(P, MV[:, 2:3], MV[:, 1:2], MV.B, MV.B, EPSC[:, 0:1])
    P.op("vector", lambda e: e.scalar_tensor_tensor(out=MV[:, 3:4], in0=MV[:, 0:1], scalar=-1.0, in1=MV[:, 2:3], op0=ALU.mult, op1=ALU.mult),
         reads=[MV.B], writes=[MV.B])
    P.op("scalar", lambda e: e.activation(out=XN[:], in_=X[:], func=AF.Identity, bias=MV[:, 3:4], scale=MV[:, 2:3]),
         reads=[X.B, MV.B], writes=[XN.B])
    P.op("vector", lambda e: e.tensor_tensor(out=XN[:], in0=XN[:], in1=gbc[:], op=ALU.mult), reads=[XN.B, gbc.B], writes=[XN.B])
    if out_f32 is not None:
        P.op("vector", lambda e: e.tensor_tensor(out=out_f32[:], in0=XN[:], in1=bbc[:], op=ALU.add), reads=[XN.B, bbc.B], writes=[out_f32.B])
        if out_bf is not None:
            P.op("scalar", lambda e: e.activation(out=out_bf[:], in_=out_f32[:], func=AF.Copy), reads=[out_f32.B], writes=[out_bf.B])
    else:
        P.op("vector", lambda e: e.tensor_tensor(out=out_bf[:], in0=XN[:], in1=bbc[:], op=ALU.add), reads=[XN.B, bbc.B], writes=[out_bf.B])


def transpose_to_fm(P, HB, dstT, ti, ps, identb, identB, pbase=0):
    for half in range(2):
        pst = ps[pbase + half]
        pv = pst.t[:].bitcast(BF16)
        for j in range(8):
            kc = half * 8 + j
            P.op("tensor", lambda e, kc=kc, j=j, pv=pv: e.transpose(out=pv[:, j * 128:(j + 1) * 128], in_=HB[:, kc * 128:(kc + 1) * 128], identity=identb),
                 reads=[HB.B, identB], writes=[pst.B])
        eng = "scalar" if half == 0 else "vector"
        dst = dstT[:, half * 8:(half + 1) * 8, ti * 128:(ti + 1) * 128]
        src = pv.rearrange("p (j t) -> p j t", t=128)
        if eng == "scalar":
            P.op("scalar", lambda e, dst=dst, src=src: e.activation(out=dst, in_=src, func=AF.Copy), reads=[pst.B], writes=[dstT.b[ti]])
        else:
            P.op("vector", lambda e, dst=dst, src=src: e.tensor_copy(out=dst, in_=src), reads=[pst.B], writes=[dstT.b[ti]])


def make_consts():
    c = np.zeros((8, 128, 128), np.float32)
    i = np.arange(128)
    c[0] = np.eye(128)
    c[1] = (i[:, None] <= i[None, :]) * -EM05
    c[2] = (i[:, None] < i[None, :]) * -EM05
    c[3] = (i[:, None] > i[None, :]) * -EM05
    c[4] = (i[None, :] > i[:, None])
    c[5] = (i[None, :] >= i[:, None])
    c[6] = (i[:, None] > i[None, :])
    c[7] = (i[:, None] // 64 == i[None, :] // 64)
    n = np.arange(-127, 256)
    nn = np.maximum(n, 0)
    nf = np.maximum(nn, 1).astype(np.float32)
    large = 16 + (np.log(nf / 16) / math.log(128 / 16) * 16).astype(np.int32)
    large = np.minimum(large, 31)
    bucket = np.where(nn < 16, nn, large)
    oh = np.zeros((33, 383), np.float32)
    oh[bucket, np.arange(383)] = 1.0
    oh[:32, n < 0] = 0.0
    oh[32, n < 0] = 1.0
    return c, oh


_CACHE = {}


def kernel(**inputs):
    dbg = inputs.pop("_dbg", None)
    stop = inputs.pop("_stop", 99)
    key = ("nc", dbg, stop)
    if key not in _CACHE:
        _CACHE[key] = build(dbg, stop)
    nc = _CACHE[key]
    x = np.asarray(inputs["x"], np.float32)
    p = np.asarray(inputs["p"], np.float32)
    cm, oh = make_consts()
    shared = {"cmat": cm, "onehot": oh}
    for k, v in inputs.items():
        if k in ("x", "p"):
            continue
        a = np.asarray(v, np.float32)
        if a.shape[0] == 1 and k not in ("ln_in_g", "ln_in_b"):
            a = a[0]
        if k in ("moe_w_gate", "moe_w_up", "moe_w_down"):
            a = a.reshape(32, a.shape[-2], a.shape[-1])
            if stop < 6:
                a = a[:1]
        if k in ("rwkv_r_k",):
            a = a.reshape(-1)
        shared[k] = np.ascontiguousarray(a)
    in_maps = []
    for c in range(8):
        b, s = c // 2, c % 2
        seq = np.zeros((2048, 2048), np.float32)
        if s == 1:
            seq[:] = x[b]
        else:
            seq[1024:] = x[b, :1024]
        m = dict(shared)
        m["xs"] = seq
        m["p"] = np.ascontiguousarray(p[0, b, s * 1024:(s + 1) * 1024])
        m["flag"] = np.full((128, 1), float(s), np.float32)
        in_maps.append(m)
    res = run_bass_kernel_spmd(nc, in_maps, core_ids=list(range(8)))
    if dbg:
        return [(r["dbg"], r["out"]) for r in res.results]
    out = np.zeros((4, 2048, 2048), np.float32)
    for c in range(8):
        b, s = c // 2, c % 2
        out[b, s * 1024:(s + 1) * 1024] = res.results[c]["out"]
    return out
```

```python
import math
from contextlib import ExitStack
import numpy as np
import concourse.bass as bass
import concourse.mybir as mybir
from concourse.bass_utils import run_bass_kernel_spmd

F32 = mybir.dt.float32
BF16 = mybir.dt.bfloat16
AF = mybir.ActivationFunctionType
ALU = mybir.AluOpType
AX = mybir.AxisListType
ENGS = ("sync", "scalar", "vector", "gpsimd", "tensor")
ALPHA = 2.0 ** 0.25
LAMBDA_INIT = 0.8 - 0.6
NEG = -1.0e30
EM05 = math.exp(-0.5)
NHEADS = 8
NPAIRS = 8
CUT = 0
NEXP = 32
OUTSEL = tuple(range(8))


class Buf:
    __slots__ = ("name", "w", "r", "dsem", "dcnt")

    def __init__(self, name):
        self.name = name
        self.w = None
        self.r = []
        self.dsem = None
        self.dcnt = 0


class Prog:
    def __init__(self, nc, stack):
        self.nc = nc
        self.stack = stack
        self.ops = {e: [] for e in ENGS}
        self.sem = {e: stack.enter_context(nc.semaphore("c_" + e)) for e in ("scalar", "vector", "gpsimd", "tensor")}
        self.cnt = {e: 0 for e in ENGS}
        self.seen = {e: {} for e in ENGS}
        self.semobj = {}
        self.nbuf = 0
        self.disabled = False
        self.dbufs = []

    def buf(self, name=None):
        self.nbuf += 1
        return Buf(name or ("b%d" % self.nbuf))

    def _deps(self, eng, reads, writes):
        deps = []
        for b in reads:
            if b.w is not None:
                deps.append(b.w)
        for b in writes:
            if b.w is not None:
                deps.append(b.w)
            deps.extend(b.r)
        waits = {}
        for (sid, val, src) in deps:
            if src == "tensor" and eng == "tensor":
                continue
            if self.seen[eng].get(sid, 0) >= val:
                continue
            if waits.get(sid, 0) < val:
                waits[sid] = val
        for sid, val in waits.items():
            self.seen[eng][sid] = val
        return [(self.semobj[sid], val) for sid, val in waits.items()]

    def op(self, eng, fn, reads=(), writes=()):
        if self.disabled:
            return None
        waits = self._deps(eng, reads, writes)
        self.cnt[eng] += 1
        sem = self.sem[eng]
        self.semobj[id(sem)] = sem
        tok = (id(sem), self.cnt[eng], eng)
        self.ops[eng].append((waits, fn, (sem, 1)))
        for b in reads:
            b.r.append(tok)
        for b in writes:
            b.w = tok
            b.r = []
        return tok

    def dma(self, eng, fn, sbuf_buf, reads=(), writes=()):
        if self.disabled:
            return None
        waits = self._deps(eng, reads, writes)
        if sbuf_buf.dsem is None:
            sbuf_buf.dsem = self.stack.enter_context(self.nc.semaphore("d_" + sbuf_buf.name))
            self.semobj[id(sbuf_buf.dsem)] = sbuf_buf.dsem
            self.dbufs.append(sbuf_buf)
        sbuf_buf.dcnt += 16
        tok = (id(sbuf_buf.dsem), sbuf_buf.dcnt, "dma")
        self.ops[eng].append((waits, fn, (sbuf_buf.dsem, 16)))
        for b in reads:
            b.r.append(tok)
        for b in writes:
            b.w = tok
            b.r = []
        return tok

    def wait_all(self, eng, bufs):
        if self.disabled:
            return
        waits = self._deps(eng, bufs, bufs)
        self.ops[eng].append((waits, None, None))

    def barrier(self):
        toks = []
        for x in ("scalar", "vector", "gpsimd", "tensor"):
            if self.cnt[x] > 0:
                self.semobj[id(self.sem[x])] = self.sem[x]
                toks.append((id(self.sem[x]), self.cnt[x]))
        for b in self.dbufs:
            toks.append((id(b.dsem), b.dcnt))
        for eng in ENGS:
            waits = []
            for sid, val in toks:
                if self.seen[eng].get(sid, 0) >= val:
                    continue
                self.seen[eng][sid] = val
                waits.append((self.semobj[sid], val))
            if waits:
                self.ops[eng].append((waits, None, None))

    def flush(self):
        self.barrier()
        nc = self.nc
        ops = self.ops
        self.ops = {e: [] for e in ENGS}

        def replay(lst, e):
            for waits, fn, inc in lst:
                for sem, val in waits:
                    e.wait_ge(sem, val)
                if fn is not None:
                    ins = fn(e)
                    ins.then_inc(inc[0], inc[1])

        with nc.Block() as block:
            @block.sync
            def _(e):
                replay(ops["sync"], e)

            @block.scalar
            def _(e):
                replay(ops["scalar"], e)

            @block.vector
            def _(e):
                replay(ops["vector"], e)

            @block.gpsimd
            def _(e):
                replay(ops["gpsimd"], e)

            @block.tensor
            def _(e):
                replay(ops["tensor"], e)


class T:
    def __init__(self, P, stack, name, shape, dtype, psum=False, nb=1):
        nc = P.nc
        if psum:
            self.t = stack.enter_context(nc.psum_tensor("t_" + name, shape, dtype))
        else:
            self.t = stack.enter_context(nc.sbuf_tensor("t_" + name, shape, dtype))
        self.b = [P.buf(name + "_%d" % i) for i in range(nb)]
        self.B = self.b[0]

    def __getitem__(self, k):
        return self.t[k]


class _Stop(Exception):
    pass


def build(dbg=None, stop=99):
    nc = bass.Bass("TRN2", target_bir_lowering=False)
    D = 2048
    dram = {}

    def din(name, shape):
        dram[name] = nc.dram_tensor(name, list(shape), F32, kind="ExternalInput").ap()
        return dram[name]

    xs = din("xs", [2048, D])
    p_in = din("p", [1024, 256])
    flag_d = din("flag", [128, 1])
    ln_in_g = din("ln_in_g", [D]); ln_in_b = din("ln_in_b", [D])
    rel_bias = din("rel_bias", [32, 8])
    w_in = din("w_in", [D, 6432])
    lamq1 = din("diff_lam_q1", [64]); lamk1 = din("diff_lam_k1", [64])
    lamq2 = din("diff_lam_q2", [64]); lamk2 = din("diff_lam_k2", [64])
    subln_g = din("diff_subln_g", [128])
    rwkv_mu = din("rwkv_mu", [3360])
    rwkv_w0 = din("rwkv_w0", [1024]); rwkv_w2 = din("rwkv_w2", [64, 1024])
    rwkv_a0 = din("rwkv_a0", [1024]); rwkv_a2 = din("rwkv_a2", [64, 1024])
    rwkv_g2 = din("rwkv_g2", [160, 1024])
    rwkv_k_k = din("rwkv_k_k", [1024]); rwkv_k_a = din("rwkv_k_a", [1024])
    rwkv_r_k = din("rwkv_r_k", [1024])
    lnx_g = din("rwkv_lnx_g", [1024]); lnx_b = din("rwkv_lnx_b", [1024])
    w_out = din("w_out", [D, D])
    ln1_g = din("ln1_g", [D]); ln1_b = din("ln1_b", [D])
    rgw = din("router_group_w", [D, 4]); rgb = din("router_group_b", [4])
    rew = din("router_expert_w", [D, 32]); reb = din("router_expert_b", [32])
    nE = 32 if stop >= 6 else 1
    wg_d = din("moe_w_gate", [nE, D, 512]); wu_d = din("moe_w_up", [nE, D, 512]); wd_d = din("moe_w_down", [nE, 512, D])
    ln2_g = din("ln2_g", [D]); ln2_b = din("ln2_b", [D])
    ple_w = din("ple_w", [256, D]); ple_ng = din("ple_norm_g", [D]); ple_gw = din("ple_gate_w", [D, D])
    cmat = din("cmat", [8, 128, 128])
    oh_d = din("onehot", [33, 383])
    out_d = nc.dram_tensor("out", [1024, D], F32, kind="ExternalOutput").ap()
    dbg_d = None
    if dbg:
        dbg_d = nc.dram_tensor("dbg", list(dbg), F32, kind="ExternalOutput").ap()
    trep_d = nc.dram_tensor("trep", [8, 128, 383], F32, kind="Internal").ap()

    stA = ExitStack()
    with ExitStack() as st0:
      try:
        P = Prog(nc, st0)
        stB = st0
        yT = T(P, stB, "yT", [128, 16, 1024], BF16, nb=16)
        cst = T(P, st0, "cst", [128, 8, 128], F32)
        cstb = T(P, st0, "cstb", [128, 8, 128], BF16)
        flag = T(P, st0, "flag", [128, 1], F32)
        pbias = T(P, st0, "pbias", [128, 1], F32)
        ps = [T(P, st0, "ps%d" % i, [128, 512], F32, psum=True) for i in range(8)]
        IDENT, TRI_I, TRI_E, TRI_R, UT_S, UT_I, LT_S, BLK1 = range(8)
        identb = cstb[:, IDENT, :]

        global EPSC
        EPSC = T(P, st0, "epsc", [128, 4], F32)
        P.op("vector", lambda e: e.memset(EPSC[:, 0:1], 1e-5), writes=[EPSC.B])
        P.op("vector", lambda e: e.memset(EPSC[:, 1:2], 64e-5), writes=[EPSC.B])
        P.op("vector", lambda e: e.memset(EPSC[:, 2:3], 1e-24), writes=[EPSC.B])
        P.op("vector", lambda e: e.memset(EPSC[:, 3:4], 0.0), writes=[EPSC.B])
        P.dma("sync", lambda e: e.dma_start(out=cst[:], in_=cmat.rearrange("m p n -> p m n")), cst.B, writes=[cst.B])
        P.dma("sync", lambda e: e.dma_start(out=flag[:], in_=flag_d), flag.B, writes=[flag.B])
        P.op("vector", lambda e: e.tensor_copy(out=cstb[:], in_=cst[:]), reads=[cst.B], writes=[cstb.B])
        P.op("vector", lambda e: e.tensor_scalar(out=pbias[:], in0=flag[:], scalar1=-1.0, scalar2=-NEG, op0=ALU.add, op1=ALU.mult),
             reads=[flag.B], writes=[pbias.B])

        dbg_bufs = []

        def dump(stk, slot, ap, bufs, width, parts=128):
            if not dbg:
                return
            tmp = T(P, stk, "dbg%d" % slot, [128, width], F32)
            P.op("vector", lambda e: e.tensor_copy(out=tmp[0:parts, :], in_=ap), reads=bufs, writes=[tmp.B])
            P.dma("sync", lambda e: e.dma_start(out=dbg_d[slot, 0:parts, 0:width], in_=tmp[0:parts, :]), tmp.B, reads=[tmp.B])
            dbg_bufs.append(tmp.B)

        def mm(out, lhsT, rhs, start, stop, reads, wB, tp=None, sgc=False):
            kw = {}
            if tp is not None:
                kw["tile_position"] = tp
            if sgc:
                kw["skip_group_check"] = True
            P.op("tensor", lambda e: e.matmul(out=out, lhsT=lhsT, rhs=rhs, start=start, stop=stop, **kw), reads=reads, writes=[wB])

        def V_(fn, reads, writes):
            P.op("vector", fn, reads=reads, writes=writes)

        def A_(fn, reads, writes):
            P.op("scalar", fn, reads=reads, writes=writes)

        def small_load(dst_ap, dstB, src_ap):
            P.dma("sync", lambda e: e.dma_start(out=dst_ap, in_=src_ap, allow_slow_non_contiguous=True), dstB, writes=[dstB])

        w_in_v = w_in.rearrange("(kc p) n -> p kc n", p=128)

        def proj_fm(wt, c0, M, tb, pst):
            for kc in range(16):
                mm(pst[0:M, 0:512], wt[:, kc, c0:c0 + M], hT0[:, kc, tb * 512:(tb + 1) * 512], kc == 0, kc == 15,
                   [wt.B] + hT0.b[tb * 4:(tb + 1) * 4], pst.B)

        def shiftmix(pst, M, tb, R, mucol, omucol, muB, out_ap, outB, tmp):
            if tb == 0:
                V_(lambda e: e.memset(R[:], 0.0), [], [R.B])
            else:
                V_(lambda e: e.tensor_copy(out=R[:, 0:1], in_=R[:, 512:513]), [R.B], [R.B])
            if tb < 2:
                A_(lambda e: e.activation(out=R[0:M, 1:513], in_=pst[0:M, 0:512], func=AF.Copy, scale=flag[0:M, 0:1]), [pst.B, flag.B, R.B], [R.B])
            else:
                A_(lambda e: e.activation(out=R[0:M, 1:513], in_=pst[0:M, 0:512], func=AF.Copy), [pst.B, R.B], [R.B])
            V_(lambda e: e.tensor_scalar(out=tmp[0:M, :], in0=R[0:M, 1:513], scalar1=omucol, scalar2=None, op0=ALU.mult), [R.B, muB], [tmp.B])
            V_(lambda e: e.scalar_tensor_tensor(out=out_ap, in0=R[0:M, 0:512], scalar=mucol, in1=tmp[0:M, :], op0=ALU.mult, op1=ALU.add),
               [R.B, muB, tmp.B], [outB])

        mul_ = T(P, st0, "mul", [128, 8], F32)
        murkv = T(P, st0, "murkv", [128, 48], F32)
        st0.enter_context(stA)
        hT0 = T(P, stA, "hT0", [128, 16, 2048], BF16, nb=16)
        txw = T(P, stA, "txw", [64, 2048], BF16)
        xaT = T(P, stA, "xaT", [64, 2048], BF16)
        sxa = T(P, stA, "sxa", [128, 2048], BF16)
        sxb = T(P, stA, "sxb", [32, 2048], BF16)
        V_(lambda e: e.memset(mul_[:], 0.0), [], [mul_.B])
        small_load(mul_[0:64, 0:1], mul_.B, rwkv_mu[3072:3136].rearrange("(p a) -> p a", a=1))
        small_load(mul_[0:64, 1:2], mul_.B, rwkv_mu[3136:3200].rearrange("(p a) -> p a", a=1))
        small_load(mul_[0:128, 2:3], mul_.B, rwkv_mu[3200:3328].rearrange("(p a) -> p a", a=1))
        small_load(mul_[0:32, 3:4], mul_.B, rwkv_mu[3328:3360].rearrange("(p a) -> p a", a=1))
        V_(lambda e: e.tensor_scalar(out=mul_[:, 4:8], in0=mul_[:, 0:4], scalar1=-1.0, scalar2=1.0, op0=ALU.mult, op1=ALU.add), [mul_.B], [mul_.B])
        small_load(murkv[:, 0:24], murkv.B, rwkv_mu[0:3072].rearrange("(a p) -> p a", p=128))
        V_(lambda e: e.tensor_scalar(out=murkv[:, 24:48], in0=murkv[:, 0:24], scalar1=-1.0, scalar2=1.0, op0=ALU.mult, op1=ALU.add), [murkv.B], [murkv.B])

        with ExitStack() as st:
            gbc = T(P, st, "gbc", [128, D], F32); bbc = T(P, st, "bbc", [128, D], F32)
            P.dma("sync", lambda e: e.dma_start(out=gbc[:], in_=ln_in_g.partition_broadcast(128)), gbc.B, writes=[gbc.B])
            P.dma("sync", lambda e: e.dma_start(out=bbc[:], in_=ln_in_b.partition_broadcast(128)), bbc.B, writes=[bbc.B])
            wl = T(P, st, "wl", [128, 16, 288], BF16)
            P.dma("gpsimd", lambda e: e.dma_start(out=wl[:], in_=w_in_v[:, :, 6144:6432]), wl.B, writes=[wl.B])
            xt = [T(P, st, "xt%d" % i, [128, D], F32) for i in range(2)]
            xn = [T(P, st, "xn%d" % i, [128, D], F32) for i in range(2)]
            hb = [T(P, st, "hb%d" % i, [128, D], BF16) for i in range(2)]
            stt = [T(P, st, "stt%d" % i, [128, 4, 6], F32) for i in range(2)]
            mv = [T(P, st, "mv%d" % i, [128, 4], F32) for i in range(2)]
            for i in range(16):
                s = i % 2
                X = xt[s]
                P.dma("sync", lambda e, X=X, i=i: e.dma_start(out=X[:], in_=xs[i * 128:(i + 1) * 128, :]), X.B, writes=[X.B])
                ln_tile(P, X, xn[s], stt[s], mv[s], gbc, bbc, hb[s], None)
                transpose_to_fm(P, hb[s], hT0, i, ps, identb, cstb.B)
            Rl = [T(P, st, "Rl%d" % i, [128, 513], F32) for i in range(4)]
            tmpl = T(P, st, "tmpl", [128, 512], F32)
            xsl = T(P, st, "xsl", [128, 512], F32)
            groups = [(0, 64, txw, AF.Tanh), (64, 64, xaT, AF.Copy), (128, 128, sxa, AF.Sigmoid), (256, 32, sxb, AF.Sigmoid)]
            for tb in range(4):
                for gi, (c0, M, dst, fn_) in enumerate(groups):
                    pst = ps[2 + (gi % 2)]
                    proj_fm(wl, c0, M, tb, pst)
                    shiftmix(pst, M, tb, Rl[gi], mul_[0:M, gi:gi + 1], mul_[0:M, 4 + gi:5 + gi], mul_.B, xsl[0:M, :], xsl.B, tmpl)
                    A_(lambda e, dst=dst, M=M, fn_=fn_, tb=tb: e.activation(out=dst[0:M, tb * 512:(tb + 1) * 512], in_=xsl[0:M, :], func=fn_),
                       [xsl.B], [dst.B])
            dump(st, 0, hT0[:, 3, 0:1024], hT0.b, 1024)
            dump(st, 1, txw[0:64, 1024:2048], [txw.B], 1024, 64)
            if stop == 1:
                P.wait_all("sync", dbg_bufs)
            P.flush()
            if stop == 1:
                P.disabled = True

        with ExitStack() as st:
            rb = T(P, st, "rb", [33, 8], F32)
            oh = T(P, st, "oh", [33, 383], F32)
            P.dma("sync", lambda e: e.dma_start(out=rb[0:32, :], in_=rel_bias), rb.B, writes=[rb.B])
            V_(lambda e: e.memset(rb[32:33, :], NEG), [], [rb.B])
            P.dma("sync", lambda e: e.dma_start(out=oh[:], in_=oh_d), oh.B, writes=[oh.B])
            rbh = T(P, st, "rbh", [33, 128], F32)
            treps = T(P, st, "treps", [128, 383], F32)
            BN = T(P, st, "BN", [128, 8, 256], F32, nb=8)
            farb = T(P, st, "farb", [128, 16], F32)
            drb = [P.buf("drb%d" % h) for h in range(8)]
            for h in range(8):
                V_(lambda e, h=h: e.tensor_copy(out=rbh[:], in_=rb[:, h:h + 1].to_broadcast([33, 128])), [rb.B], [rbh.B])
                mm(ps[0][:, 0:383], rbh[:], oh[:], True, True, [rbh.B, oh.B], ps[0].B)
                V_(lambda e: e.tensor_copy(out=treps[:], in_=ps[0][:, 0:383]), [ps[0].B], [treps.B])
                V_(lambda e, h=h: e.tensor_copy(out=farb[:, h:h + 1], in_=treps[:, 382:383]), [treps.B], [farb.B])
                P.dma("sync", lambda e, h=h: e.dma_start(out=trep_d[h], in_=treps[:]), treps.B, reads=[treps.B], writes=[drb[h]])
                skew = bass.AP(tensor=trep_d.tensor, offset=h * 128 * 383 + 127, ap=[[382, 128], [1, 256]])
                P.dma("sync", lambda e, h=h, skew=skew: e.dma_start(out=BN[:, h, :], in_=skew), BN.b[h], reads=[drb[h]], writes=[BN.b[h]])
            V_(lambda e: e.tensor_scalar(out=farb[:, 8:16], in0=farb[:, 0:8], scalar1=pbias[:, 0:1], scalar2=None, op0=ALU.add), [farb.B, pbias.B], [farb.B])
            if CUT == 1:
                P.disabled = True
            lq = T(P, st, "lq", [1, 4, 64], F32)
            for j, src in enumerate((lamq1, lamk1, lamq2, lamk2)):
                P.dma("sync", lambda e, j=j, src=src: e.dma_start(out=lq[0:1, j, :], in_=src.rearrange("(a n) -> a n", a=1)), lq.B, writes=[lq.B])
            lt = T(P, st, "lt", [1, 8], F32)
            lpr = T(P, st, "lpr", [1, 2, 64], F32)
            V_(lambda e: e.tensor_tensor(out=lpr[0:1, 0, :], in0=lq[0:1, 0, :], in1=lq[0:1, 1, :], op=ALU.mult), [lq.B], [lpr.B])
            V_(lambda e: e.tensor_tensor(out=lpr[0:1, 1, :], in0=lq[0:1, 2, :], in1=lq[0:1, 3, :], op=ALU.mult), [lq.B], [lpr.B])
            V_(lambda e: e.reduce_sum(out=lt[0:1, 0:2], in_=lpr[0:1, :, :], axis=AX.X), [lpr.B], [lt.B])
            A_(lambda e: e.activation(out=lt[0:1, 2:4], in_=lt[0:1, 0:2], func=AF.Exp), [lt.B], [lt.B])
            V_(lambda e: e.tensor_tensor(out=lt[0:1, 4:5], in0=lt[0:1, 3:4], in1=lt[0:1, 2:3], op=ALU.subtract), [lt.B], [lt.B])
            V_(lambda e: e.tensor_scalar(out=lt[0:1, 5:6], in0=lt[0:1, 4:5], scalar1=-LAMBDA_INIT, scalar2=None, op0=ALU.add), [lt.B], [lt.B])
            ones1 = T(P, st, "ones1", [1, 128], F32)
            V_(lambda e: e.memset(ones1[:], 1.0), [], [ones1.B])
            nlam = T(P, st, "nlam", [128, 1], F32)
            mm(ps[1][:, 0:1], ones1[0:1, :], lt[0:1, 5:6], True, True, [ones1.B, lt.B], ps[1].B)
            V_(lambda e: e.tensor_copy(out=nlam[:], in_=ps[1][:, 0:1]), [ps[1].B], [nlam.B])
            gsub = T(P, st, "gsub", [128, 128], F32)
            P.dma("sync", lambda e: e.dma_start(out=gsub[:], in_=subln_g.partition_broadcast(128)), gsub.B, writes=[gsub.B])
            V_(lambda e: e.tensor_scalar(out=gsub[:], in0=gsub[:], scalar1=1.0 - LAMBDA_INIT, scalar2=None, op0=ALU.mult), [gsub.B], [gsub.B])
            if CUT == 2:
                P.disabled = True

            WQ = [T(P, st, "WQ%d" % i, [128, 16, 128], BF16) for i in range(2)]
            WK = [T(P, st, "WK%d" % i, [128, 16, 128], BF16) for i in range(2)]
            WV = [T(P, st, "WV%d" % i, [128, 16, 128], BF16) for i in range(2)]
            qT = T(P, st, "qT", [128, 1024], BF16)
            kT = T(P, st, "kT", [128, 2048], BF16)
            Va = T(P, st, "Va", [128, 16, 129], BF16)
            V_(lambda e: e.memset(Va[:, :, 128:129], 1.0), [], [Va.B])
            E = [[T(P, st, "E%d%d" % (m, j), [128, 512], BF16) for j in range(2)] for m in range(2)]
            TMP = [T(P, st, "TMPa%d" % m, [128, 256], F32) for m in range(2)]
            rc = T(P, st, "rc", [128, 4], F32)
            o2 = T(P, st, "o2", [128, 128], F32)
            oo = T(P, st, "oo", [128, 128], F32)
            junk = T(P, st, "junk", [128, 128], F32)
            ss = T(P, st, "ss", [128, 2], F32)
            yb = T(P, st, "yb", [128, 128], BF16)

            def load_head(h):
                s = h % 2
                for W_, c0 in ((WQ[s], h * 128), (WK[s], 1024 + h * 128), (WV[s], 2048 + h * 128)):
                    P.dma("gpsimd", lambda e, W_=W_, c0=c0: e.dma_start(out=W_[:], in_=w_in_v[:, :, c0:c0 + 128]), W_.B, writes=[W_.B])

            def accap(a, lo, hi):
                return ps[5 + a // 3][:, (a % 3) * 129 + lo:(a % 3) * 129 + hi]

            load_head(0)
            it = 0
            for h in range(NHEADS):
                if h + 1 < NHEADS:
                    load_head(h + 1)
                s = h % 2
                for tb in (2, 3):
                    proj_fm(WQ[s], 0, 128, tb, ps[tb % 2])
                    A_(lambda e, tb=tb: e.activation(out=qT[:, (tb - 2) * 512:(tb - 1) * 512], in_=ps[tb % 2][:, :], func=AF.Copy), [ps[tb % 2].B], [qT.B])
                for tb in range(4):
                    proj_fm(WK[s], 0, 128, tb, ps[2 + tb % 2])
                    V_(lambda e, tb=tb: e.tensor_copy(out=kT[:, tb * 512:(tb + 1) * 512], in_=ps[2 + tb % 2][:, :]), [ps[2 + tb % 2].B], [kT.B])
                for g4 in range(4):
                    pst = ps[g4 % 2]
                    for j in range(4):
                        i = g4 * 4 + j
                        for kc in range(16):
                            mm(pst[:, j * 128:(j + 1) * 128], hT0[:, kc, i * 128:(i + 1) * 128], WV[s][:, kc, :], kc == 0, kc == 15,
                               [WV[s].B, hT0.b[i]], pst.B)
                    V_(lambda e, g4=g4, pst=pst: e.tensor_copy(out=Va[:, g4 * 4:(g4 + 1) * 4, 0:128], in_=pst[:, :].rearrange("p (j t) -> p j t", t=128)),
                       [pst.B], [Va.B])
                if CUT == 3:
                    P.disabled = True
                for g in range(2):
                    nkb = 8 + 4 * g + 4
                    for kb in range(nkb):
                        t = kb - 8 - 4 * g
                        i0 = max(0, t)
                        N = (4 - i0) * 128
                        q0 = g * 512 + i0 * 128
                        if t >= 0:
                            wn = min(256, N); b0 = 0
                        elif t == -1:
                            wn = 128; b0 = 128
                        else:
                            wn = 0; b0 = 0
                        pref = kb < 8
                        for m in range(2):
                            pS = ps[m * 2 + (it % 2)]
                            Em = E[m][it % 2]
                            mm(pS[:, 0:N], kT[m * 64:(m + 1) * 64, kb * 128:(kb + 1) * 128], qT[m * 64:(m + 1) * 64, q0:q0 + N], True, True,
                               [kT.B, qT.B], pS.B)
                            if wn > 0:
                                V_(lambda e, pS=pS, m=m, wn=wn, b0=b0, h=h: e.scalar_tensor_tensor(out=TMP[m][:, 0:wn], in0=pS[:, 0:wn], scalar=0.125,
                                   in1=BN[:, h, b0:b0 + wn], op0=ALU.mult, op1=ALU.add), [pS.B, BN.b[h]], [TMP[m].B])
                                bcol = pbias[:, 0:1] if pref else EPSC[:, 3:4]
                                A_(lambda e, Em=Em, m=m, wn=wn, bcol=bcol: e.activation(out=Em[:, 0:wn], in_=TMP[m][:, 0:wn], func=AF.Exp, bias=bcol, scale=1.0),
                                   [TMP[m].B, pbias.B, EPSC.B], [Em.B])
                            if N > wn:
                                fcol = farb[:, 8 + h:9 + h] if pref else farb[:, h:h + 1]
                                A_(lambda e, Em=Em, pS=pS, wn=wn, N=N, fcol=fcol: e.activation(out=Em[:, wn:N], in_=pS[:, wn:N], func=AF.Exp, bias=fcol, scale=0.125),
                                   [pS.B, farb.B], [Em.B])
                            for li in range(4 - i0):
                                i = i0 + li
                                a = m * 4 + i
                                mm(accap(a, 0, 129), Em[:, li * 128:(li + 1) * 128], Va[:, kb, :], (kb == 0 and a % 3 == 0), False,
                                   [Em.B, Va.B], ps[5 + a // 3].B, sgc=True)
                        it += 1
                    if CUT == 4:
                        P.disabled = True
                    for i in range(4):
                        a1, a2 = i, 4 + i
                        B1, B2 = ps[5 + a1 // 3].B, ps[5 + a2 // 3].B
                        V_(lambda e, a1=a1: e.reciprocal(out=rc[:, 0:1], in_=accap(a1, 128, 129)), [B1], [rc.B])
                        V_(lambda e, a2=a2: e.reciprocal(out=rc[:, 1:2], in_=accap(a2, 128, 129)), [B2], [rc.B])
                        V_(lambda e: e.tensor_tensor(out=rc[:, 2:3], in0=rc[:, 1:2], in1=nlam[:, 0:1], op=ALU.mult), [rc.B, nlam.B], [rc.B])
                        V_(lambda e, a2=a2: e.tensor_scalar(out=o2[:], in0=accap(a2, 0, 128), scalar1=rc[:, 2:3], scalar2=None, op0=ALU.mult), [B2, rc.B], [o2.B])
                        V_(lambda e, a1=a1: e.scalar_tensor_tensor(out=oo[:], in0=accap(a1, 0, 128), scalar=rc[:, 0:1], in1=o2[:], op0=ALU.mult, op1=ALU.add),
                           [B1, rc.B, o2.B], [oo.B])
                        A_(lambda e: e.activation(out=junk[:], in_=oo[:], func=AF.Square, accum_out=ss[:, 0:1]), [oo.B], [junk.B, ss.B])
                        A_(lambda e: e.activation(out=ss[:, 1:2], in_=ss[:, 0:1], func=AF.Sqrt, bias=EPSC[:, 0:1], scale=1.0 / 128), [ss.B, EPSC.B], [ss.B])
                        V_(lambda e: e.reciprocal(out=ss[:, 1:2], in_=ss[:, 1:2]), [ss.B], [ss.B])
                        V_(lambda e: e.scalar_tensor_tensor(out=yb[:], in0=oo[:], scalar=ss[:, 1:2], in1=gsub[:], op0=ALU.mult, op1=ALU.mult),
                           [oo.B, ss.B, gsub.B], [yb.B])
                        pv = ps[4].t[:].bitcast(BF16)
                        P.op("tensor", lambda e, pv=pv: e.transpose(out=pv[:, 0:128], in_=yb[:], identity=identb), reads=[yb.B, cstb.B], writes=[ps[4].B])
                        c0 = g * 512 + i * 128
                        A_(lambda e, pv=pv, h=h, c0=c0: e.activation(out=yT[:, h, c0:c0 + 128], in_=pv[:, 0:128], func=AF.Copy), [ps[4].B], [yT.b[h]])
            dump(st, 2, yT[:, 0, :], [yT.b[0]], 1024)
            dump(st, 3, yT[:, NHEADS - 1, :], [yT.b[NHEADS - 1]], 1024)
            if stop == 2:
                P.wait_all("sync", dbg_bufs)
            P.flush()
            if stop == 2:
                P.disabled = True

        with ExitStack() as st:
            WR = [T(P, st, "WR0", [128, 16, 128], BF16)] * 2
            WKr = [T(P, st, "WKr0", [128, 16, 128], BF16)] * 2
            WVr = [T(P, st, "WVr0", [128, 16, 128], BF16)] * 2
            prm = T(P, st, "prm", [128, 8, 8], F32)
            for j, src in enumerate((rwkv_k_k, rwkv_k_a, rwkv_a0)):
                small_load(prm[:, :, j], prm.B, src.rearrange("(a p) -> p a", p=128))
            V_(lambda e: e.tensor_scalar(out=prm[:, :, 3], in0=prm[:, :, 1], scalar1=-1.0, scalar2=1.0, op0=ALU.mult, op1=ALU.add), [prm.B], [prm.B])
            w2b = T(P, st, "w2b", [64, 1024], BF16); a2b = T(P, st, "a2b", [64, 1024], BF16); g2b = T(P, st, "g2b", [128, 2, 1024], BF16)
            P.dma("gpsimd", lambda e: e.dma_start(out=w2b[:], in_=rwkv_w2), w2b.B, writes=[w2b.B])
            P.dma("gpsimd", lambda e: e.dma_start(out=a2b[:], in_=rwkv_a2), a2b.B, writes=[a2b.B])
            P.dma("gpsimd", lambda e: e.dma_start(out=g2b[:, 0, :], in_=rwkv_g2[0:128, :]), g2b.B, writes=[g2b.B])
            P.dma("gpsimd", lambda e: e.dma_start(out=g2b[0:32, 1, :], in_=rwkv_g2[128:160, :]), g2b.B, writes=[g2b.B])
            pp3 = T(P, st, "pp3", [128, 3, 128], F32)
            rkf = T(P, st, "rkf", [128, 8], F32)
            small_load(rkf[:], rkf.B, rwkv_r_k.rearrange("(a p) -> p a", p=128))
            RK = T(P, st, "RK", [128, 8, 2], BF16)
            V_(lambda e: e.memset(RK[:], 0.0), [], [RK.B])
            V_(lambda e: e.tensor_copy(out=RK[0:64, :, 0], in_=rkf[0:64, :]), [rkf.B], [RK.B])
            V_(lambda e: e.tensor_copy(out=RK[64:128, :, 1], in_=rkf[64:128, :]), [rkf.B], [RK.B])
            Rr = [T(P, st, "Rr%d" % i, [128, 513], F32) for i in range(3)]
            tmpr = T(P, st, "tmpr", [128, 512], F32)
            rs = T(P, st, "rs", [128, 512], F32); ks = T(P, st, "ks", [128, 512], F32); vsb = T(P, st, "vsb", [128, 512], BF16)
            af = T(P, st, "af", [128, 512], F32); kkk = T(P, st, "kkk", [128, 512], F32); sqb = T(P, st, "sqb", [128, 512], BF16)
            rinv = T(P, st, "rinv", [128, 512], F32); kk = T(P, st, "kk", [128, 512], F32); uu = tmpr
            k2 = T(P, st, "k2", [128, 512], F32); bb_ = T(P, st, "bb", [128, 512], F32)
            k2b = T(P, st, "k2b", [128, 512], BF16); bbb = T(P, st, "bbb", [128, 512], BF16); rk2 = T(P, st, "rk2", [128, 512], BF16)
            lwt2 = [T(P, st, "lwt%d" % i, [128, 128], F32) for i in range(2)]
            ex2 = [T(P, st, "ex%d" % i, [128, 4, 128], F32) for i in range(2)]
            ART2 = [T(P, st, "ART%d" % i, [128, 256], BF16) for i in range(2)]
            BhT2 = [T(P, st, "BhT%d" % i, [128, 128], BF16) for i in range(2)]; KhT2 = [T(P, st, "KhT%d" % i, [128, 128], BF16) for i in range(2)]
            TMt2 = [T(P, st, "TMt%d" % i, [128, 4, 128], BF16) for i in range(2)]
            lwt, ex, ART, BhT, KhT, TMt = lwt2[1], ex2[1], ART2[1], BhT2[1], KhT2[1], TMt2[1]
            MTh = [T(P, st, "MT%d" % h_, [128, 2, 256], BF16) for h_ in range(2)]
            NMh = [[T(P, st, "NM%d%d" % (h_, i), [128, 256], BF16) for i in range(2)] for h_ in range(2)]
            Xfh = [T(P, st, "Xf%d" % h_, [128, 128], F32) for h_ in range(2)]; Xbh = [T(P, st, "Xb%d" % h_, [128, 128], BF16) for h_ in range(2)]
            MT, Xf = MTh[1], Xfh[1]
            pBx = [P.buf("pBx%d" % h_) for h_ in range(2)]; pBn = [P.buf("pBn%d" % h_) for h_ in range(2)]; pBq = [P.buf("pBq%d" % h_) for h_ in range(2)]
            GTs = T(P, st, "GTs", [128, 16, 128], BF16); Hs = T(P, st, "Hs", [128, 16, 64], F32)
            QeTs = T(P, st, "QeTs", [128, 8, 128], BF16); Yls = T(P, st, "Yls", [128, 8, 128], F32)
            Vtm = T(P, st, "Vtm", [128, 8, 128], BF16); sbon = T(P, st, "sbon", [128, 8, 2], F32)
            gtm = T(P, st, "gtm", [128, 8, 128], F32)
            Sb = T(P, st, "Sb", [128, 128], BF16)
            Yt = T(P, st, "Yt", [128, 128], F32); yn = T(P, st, "yn", [128, 128], F32); ybr = T(P, st, "ybr", [128, 128], BF16)
            gst = T(P, st, "gst", [128, 2, 6], F32); gmv = T(P, st, "gmv", [128, 2, 2], F32); grs = T(P, st, "grs", [128, 2], F32)
            ident64 = cst[0:64, IDENT, 0:64]
            V_(lambda e: e.memset(GTs[:], 0.0), [], [GTs.B])

            def load_pair(c):
                s = c % 2
                for W_, c0 in ((WR[s], 3072 + c * 128), (WKr[s], 4096 + c * 128), (WVr[s], 5120 + c * 128)):
                    P.dma("gpsimd", lambda e, W_=W_, c0=c0: e.dma_start(out=W_[:], in_=w_in_v[:, :, c0:c0 + 128]), W_.B, writes=[W_.B])

            if CUT == 11:
                P.disabled = True
            load_pair(0)
            for c in range(NPAIRS):
                s = c % 2
                csl = slice(c * 128, (c + 1) * 128)
                for j3, src3 in enumerate((rwkv_w0, lnx_g, lnx_b)):
                    P.dma("sync", lambda e, j3=j3, src3=src3, c=c: e.dma_start(out=pp3[:, j3, :], in_=src3[c * 128:(c + 1) * 128].partition_broadcast(128)), pp3.B, writes=[pp3.B])
                for tb in range(4):
                    for j, (W_, dst, dstB) in enumerate(((WR[s], rs[:, :], rs.B), (WKr[s], ks[:, :], ks.B), (WVr[s], vsb[:, :], vsb.B))):
                        pst = ps[j % 2]
                        proj_fm(W_, 0, 128, tb, pst)
                        shiftmix(pst, 128, tb, Rr[j], murkv[:, j * 8 + c:j * 8 + c + 1], murkv[:, 24 + j * 8 + c:25 + j * 8 + c], murkv.B, dst, dstB, tmpr)
                    mm(ps[0][:, :], a2b[0:64, csl], xaT[0:64, tb * 512:(tb + 1) * 512], True, True, [a2b.B, xaT.B], ps[0].B)
                    A_(lambda e, c=c: e.activation(out=af[:], in_=ps[0][:, :], func=AF.Sigmoid, bias=prm[:, c, 2:3], scale=1.0), [ps[0].B, prm.B], [af.B])
                    V_(lambda e, c=c: e.tensor_scalar(out=kkk[:], in0=ks[:], scalar1=prm[:, c, 0:1], scalar2=None, op0=ALU.mult), [ks.B, prm.B], [kkk.B])
                    A_(lambda e: e.activation(out=sqb[:], in_=kkk[:], func=AF.Square), [kkk.B], [sqb.B])
                    mm(ps[1][:, :], cstb[:, BLK1, :], sqb[:], True, True, [cstb.B, sqb.B], ps[1].B)
                    A_(lambda e: e.activation(out=rinv[:], in_=ps[1][:, :], func=AF.Sqrt, bias=EPSC[:, 2:3], scale=1.0), [ps[1].B, EPSC.B], [rinv.B])
                    V_(lambda e: e.reciprocal(out=rinv[:], in_=rinv[:]), [rinv.B], [rinv.B])
                    V_(lambda e: e.tensor_tensor(out=kk[:], in0=kkk[:], in1=rinv[:], op=ALU.mult), [kkk.B, rinv.B], [kk.B])
                    V_(lambda e, c=c: e.tensor_scalar(out=uu[:], in0=af[:], scalar1=prm[:, c, 1:2], scalar2=prm[:, c, 3:4], op0=ALU.mult, op1=ALU.add), [af.B, prm.B], [uu.B])
                    V_(lambda e: e.tensor_tensor(out=k2[:], in0=ks[:], in1=uu[:], op=ALU.mult), [ks.B, uu.B], [k2.B])
                    V_(lambda e: e.tensor_tensor(out=bb_[:], in0=kk[:], in1=af[:], op=ALU.mult), [kk.B, af.B], [bb_.B])
                    A_(lambda e: e.activation(out=k2b[:], in_=k2[:], func=AF.Copy), [k2.B], [k2b.B])
                    A_(lambda e: e.activation(out=bbb[:], in_=bb_[:], func=AF.Copy), [bb_.B], [bbb.B])
                    V_(lambda e: e.tensor_tensor(out=rk2[:], in0=rs[:], in1=k2[:], op=ALU.mult), [rs.B, k2.B], [rk2.B])
                    if CUT == 12:
                        P.disabled = True
                    def prologue(ch, q4, bs):
                        own = ch >= 8
                        tsl = slice(q4 * 128, (q4 + 1) * 128)
                        gsl = slice(ch * 128, (ch + 1) * 128)
                        lwt, ex, ART, BhT, KhT, TMt = lwt2[bs], ex2[bs], ART2[bs], BhT2[bs], KhT2[bs], TMt2[bs]
                        pz = ps[2]
                        mm(pz[:, 0:128], txw[0:64, gsl], w2b[0:64, csl], True, True, [txw.B, w2b.B], pz.B)
                        yield
                        V_(lambda e: e.tensor_tensor(out=lwt[:], in0=pz[:, 0:128], in1=pp3[:, 0, :], op=ALU.add), [pz.B, pp3.B], [lwt.B])
                        A_(lambda e: e.activation(out=lwt[:], in_=lwt[:], func=AF.Sigmoid), [lwt.B], [lwt.B])
                        yield
                        mm(pz[:, 128:384], lwt[:], cst[:, TRI_I:TRI_E + 1, :].rearrange("p a b -> p (a b)"), True, True, [lwt.B, cst.B], pz.B)
                        mm(pz[:, 384:512], cst[:, TRI_R, :], lwt[:], True, True, [lwt.B, cst.B], pz.B)
                        yield
                        A_(lambda e: e.activation(out=ex[:, 0:2, :].rearrange("p a b -> p (a b)"), in_=pz[:, 128:384], func=AF.Exp), [pz.B], [ex.B])
                        A_(lambda e: e.activation(out=ex[:, 2, :], in_=pz[:, 128:256], func=AF.Exp, scale=-1.0), [pz.B], [ex.B])
                        A_(lambda e: e.activation(out=ex[:, 3, :], in_=pz[:, 384:512], func=AF.Exp), [pz.B], [ex.B])
                        yield
                        V_(lambda e: e.scalar_tensor_tensor(out=ART[:, 0:128], in0=kk[:, tsl], scalar=-1.0, in1=ex[:, 1, :], op0=ALU.mult, op1=ALU.mult), [kk.B, ex.B], [ART.B])
                        V_(lambda e: e.tensor_tensor(out=ART[:, 128:256], in0=rs[:, tsl], in1=ex[:, 0, :], op=ALU.mult), [rs.B, ex.B], [ART.B])
                        V_(lambda e: e.tensor_tensor(out=BhT[:], in0=bb_[:, tsl], in1=ex[:, 2, :], op=ALU.mult), [bb_.B, ex.B], [BhT.B])
                        V_(lambda e: e.tensor_tensor(out=KhT[:], in0=k2[:, tsl], in1=ex[:, 2, :], op=ALU.mult), [k2.B, ex.B], [KhT.B])
                        yield
                        pt = ps[3]
                        ptv = pt.t[:].bitcast(BF16)
                        for j, (src, sB) in enumerate(((bbb[:, tsl], bbb.B), (k2b[:, tsl], k2b.B), (vsb[:, tsl], vsb.B), (ART[:, 0:128], ART.B))):
                            P.op("tensor", lambda e, j=j, src=src, ptv=ptv: e.transpose(out=ptv[:, j * 128:(j + 1) * 128], in_=src, identity=identb), reads=[sB, cstb.B], writes=[pt.B])
                        yield
                        V_(lambda e: e.tensor_tensor(out=TMt[:, 0:2, :], in0=ptv[:, 0:256].rearrange("p (a b) -> p a b", b=128),
                                                     in1=ex[:, 3:4, :].to_broadcast([128, 2, 128]), op=ALU.mult), [pt.B, ex.B], [TMt.B])
                        A_(lambda e: e.activation(out=TMt[:, 2:4, :].rearrange("p a b -> p (a b)"), in_=ptv[:, 256:512], func=AF.Copy), [pt.B], [TMt.B])
                        if own:
                            oc = ch - 8
                            yield
                            A_(lambda e: e.activation(out=Vtm[:, oc, :], in_=TMt[:, 2, :], func=AF.Copy), [TMt.B], [Vtm.B])
                            mm(ps[3][:, 256:258], rk2[:, tsl], RK[:, c, :], True, True, [rk2.B, RK.B], ps[3].B)
                            mm(ps[3][:, 384:512], sxa[:, gsl], g2b[:, 0, csl], True, False, [sxa.B, g2b.B], ps[3].B)
                            mm(ps[3][:, 384:512], sxb[0:32, gsl], g2b[0:32, 1, csl], False, True, [sxb.B, g2b.B], ps[3].B)
                            yield
                            V_(lambda e: e.tensor_copy(out=sbon[:, oc, :], in_=ps[3][:, 256:258]), [ps[3].B], [sbon.B])
                            V_(lambda e: e.tensor_copy(out=gtm[:, oc, :], in_=ps[3][:, 384:512]), [ps[3].B], [gtm.B])

                    def chain(ch, hh, own, tsl, bs):
                        hp = slice(hh * 64, (hh + 1) * 64)
                        tpk = (hh * 64, 0)
                        tpo = (0, hh * 64)
                        ex, ART, BhT, KhT, TMt = ex2[bs], ART2[bs], BhT2[bs], KhT2[bs], TMt2[bs]
                        pA, pB = ps[4 + hh], ps[6 + hh]
                        bX = bN = bQ = pB.B
                        NM, Xf, Xb, MT = NMh[hh], Xfh[hh], Xbh[hh], MTh[hh]
                        mm(pA[:, 0:256], BhT[hp, :], ART[hp, :], True, True, [BhT.B, ART.B], pA.B, tpk)
                        mm(pA[:, 256:512], KhT[hp, :], ART[hp, :], True, True, [KhT.B, ART.B], pA.B, tpk)
                        mm(pB[:, 384:512], ART[hp, 0:128], BhT[hp, :], True, True, [BhT.B, ART.B], bQ, tpk)
                        yield
                        V_(lambda e: e.tensor_tensor(out=MT[:, :, :], in0=pA[:, :].rearrange("p (a b) -> p a b", b=256),
                                                     in1=cst[:, UT_S:UT_I + 1, :].rearrange("p a b -> p (a b)").unsqueeze(1).to_broadcast([128, 2, 256]), op=ALU.mult),
                           [pA.B, cst.B], [MT.B])
                        V_(lambda e: e.tensor_tensor(out=NM[0][:, 128:256], in0=pB[:, 384:512], in1=cst[:, LT_S, :], op=ALU.mult), [bQ, cst.B], [NM[0].B])
                        A_(lambda e: e.activation(out=Xf[:, 0:64], in_=TMt[:, 3, hp], func=AF.Copy), [TMt.B], [Xf.B])
                        A_(lambda e: e.activation(out=NM[0][:, 0:128], in_=MT[:, 0, 0:128], func=AF.Copy), [MT.B], [NM[0].B])
                        yield
                        mm(pA[:, 0:64], MT[:, 1, 0:128], TMt[:, 2, hp], True, True, [MT.B, TMt.B], pA.B)
                        yield
                        V_(lambda e: e.tensor_copy(out=Xf[:, 64:128], in_=pA[:, 0:64]), [pA.B], [Xf.B])
                        A_(lambda e: e.activation(out=Xb[:], in_=Xf[:], func=AF.Copy), [Xf.B], [Xb.B])
                        yield
                        for lv in range(7):
                            cur, nxt = NM[lv % 2], NM[(lv + 1) % 2]
                            mm(pB[:, 0:128], cur[:, 0:128], Xb[:], True, True, [cur.B, Xb.B], bX)
                            if lv < 6:
                                mm(pB[:, 128:256], cur[:, 128:256], cur[:, 0:128], True, True, [cur.B], bN)
                                mm(pB[:, 256:384], cur[:, 0:128], cur[:, 128:256], True, True, [cur.B], bN)
                            yield
                            V_(lambda e: e.tensor_tensor(out=Xf[:], in0=Xf[:], in1=pB[:, 0:128], op=ALU.add), [Xf.B, bX], [Xf.B])
                            A_(lambda e: e.activation(out=Xb[:], in_=Xf[:], func=AF.Copy), [Xf.B], [Xb.B])
                            if lv < 6:
                                V_(lambda e, nxt=nxt: e.tensor_copy(out=nxt[:], in_=pB[:, 128:384]), [bN], [nxt.B])
                            yield
                        mm(pA[hp, 64:128], Xb[:, 0:64], TMt[:, 0, hp], True, True, [Xb.B, TMt.B], pA.B, tpo)
                        mm(pA[hp, 128:192], TMt[:, 0, hp], Xb[:, 64:128], True, False, [Xb.B, TMt.B], pA.B, tpo)
                        mm(pA[hp, 128:192], TMt[:, 1, hp], TMt[:, 2, hp], False, True, [TMt.B], pA.B, tpo)
                        if own:
                            mm(pB[hp, 384:512], Xb[:, 0:64], MT[:, 0, 128:256], True, True, [Xb.B, MT.B], bQ, tpo)
                            mm(pA[:, 192:256], MT[:, 0, 128:256], Xb[:, 64:128], True, False, [Xb.B, MT.B], pA.B)
                            mm(pA[:, 192:256], MT[:, 1, 128:256], TMt[:, 2, hp], False, True, [TMt.B, MT.B], pA.B)
                        yield
                        V_(lambda e: e.scalar_tensor_tensor(out=GTs[hp, ch, hh * 64:(hh + 1) * 64], in0=cst[hp, IDENT, hh * 64:(hh + 1) * 64], scalar=ex[hp, 0, 127:128],
                                                            in1=pA[hp, 64:128], op0=ALU.mult, op1=ALU.add), [cst.B, ex.B, pA.B], [GTs.B])
                        V_(lambda e: e.tensor_copy(out=Hs[hp, ch, :], in_=pA[hp, 128:192]), [pA.B], [Hs.B])
                        if own:
                            oc = ch - 8
                            V_(lambda e: e.tensor_tensor(out=QeTs[hp, oc, :], in0=pB[hp, 384:512], in1=ART[hp, 128:256], op=ALU.add),
                               [bQ, ART.B], [QeTs.B])
                            V_(lambda e: e.tensor_copy(out=Yls[:, oc, hp], in_=pA[:, 192:256]), [pA.B], [Yls.B])

                    def run_rr(gens):
                        while gens:
                            for g_ in list(gens):
                                try:
                                    next(g_)
                                except StopIteration:
                                    gens.remove(g_)

                    run_rr([prologue(tb * 4, 0, 0)])
                    for q4 in range(4):
                        ch = tb * 4 + q4
                        own = ch >= 8
                        tsl = slice(q4 * 128, (q4 + 1) * 128)
                        gens = [chain(ch, 0, own, tsl, q4 % 2), chain(ch, 1, own, tsl, q4 % 2)]
                        if q4 < 3:
                            gens.append(prologue(ch + 1, q4 + 1, (q4 + 1) % 2))
                        run_rr(gens)
                if c + 1 < NPAIRS:
                    load_pair(c + 1)
                if CUT == 17:
                    P.disabled = True
                V_(lambda e: e.memset(Sb[:], 0.0), [], [Sb.B])
                for ch in range(16):
                    if CUT == 19 and ch == 8:
                        P.disabled = True
                    if ch >= 8:
                        oc = ch - 8
                        mm(ps[0][:, 0:128], QeTs[:, oc, :], Sb[:, :], True, True, [QeTs.B, Sb.B], ps[0].B)
                        V_(lambda e, oc=oc: e.tensor_tensor(out=Yt[:], in0=ps[0][:, 0:128], in1=Yls[:, oc, :], op=ALU.add), [ps[0].B, Yls.B], [Yt.B])
                    if CUT == 20 and ch == 8:
                        P.disabled = True
                    if ch < 15:
                        mm(ps[1][:, 0:128], GTs[:, ch, :], Sb[:, :], True, True, [GTs.B, Sb.B], ps[1].B)
                        for hh in range(2):
                            hp = slice(hh * 64, (hh + 1) * 64)
                            V_(lambda e, ch=ch, hp=hp: e.tensor_tensor(out=Sb[hp, hp], in0=ps[1][hp, hp], in1=Hs[hp, ch, :], op=ALU.add), [ps[1].B, Hs.B], [Sb.B])
                    if CUT == 21 and ch == 8:
                        P.disabled = True
                    if ch >= 8:
                        for hh in range(2):
                            V_(lambda e, hh=hh: e.bn_stats(out=gst[:, hh, :], in_=Yt[:, hh * 64:(hh + 1) * 64]), [Yt.B], [gst.B])
                            V_(lambda e, hh=hh: e.bn_aggr(out=gmv[:, hh, :], in_=gst[:, hh, :]), [gst.B], [gmv.B])
                        A_(lambda e: e.activation(out=grs[:], in_=gmv[:, :, 1], func=AF.Sqrt, bias=EPSC[:, 1:2], scale=1.0), [gmv.B, EPSC.B], [grs.B])
                        V_(lambda e: e.reciprocal(out=grs[:], in_=grs[:]), [grs.B], [grs.B])
                        for hh in range(2):
                            hs_ = slice(hh * 64, (hh + 1) * 64)
                            V_(lambda e, hh=hh, hs_=hs_: e.tensor_scalar(out=yn[:, hs_], in0=Yt[:, hs_], scalar1=gmv[:, hh, 0:1], scalar2=grs[:, hh:hh + 1],
                                                                       op0=ALU.subtract, op1=ALU.mult), [Yt.B, gmv.B, grs.B], [yn.B])
                        V_(lambda e, c=c: e.tensor_tensor(out=yn[:], in0=yn[:], in1=pp3[:, 1, :], op=ALU.mult), [yn.B, pp3.B], [yn.B])
                        V_(lambda e, c=c: e.tensor_tensor(out=yn[:], in0=yn[:], in1=pp3[:, 2, :], op=ALU.add), [yn.B, pp3.B], [yn.B])
                        for hh in range(2):
                            hs_ = slice(hh * 64, (hh + 1) * 64)
                            V_(lambda e, hh=hh, hs_=hs_, oc=oc: e.scalar_tensor_tensor(out=yn[:, hs_], in0=Vtm[:, oc, hs_], scalar=sbon[:, oc, hh:hh + 1], in1=yn[:, hs_],
                                                                                      op0=ALU.mult, op1=ALU.add), [Vtm.B, sbon.B, yn.B], [yn.B])
                        V_(lambda e, oc=oc: e.tensor_tensor(out=ybr[:], in0=yn[:], in1=gtm[:, oc, :], op=ALU.mult), [yn.B, gtm.B], [ybr.B])
                        if CUT == 22 and ch == 8:
                            P.disabled = True
                        pv = ps[3].t[:].bitcast(BF16)
                        P.op("tensor", lambda e, pv=pv: e.transpose(out=pv[:, 0:128], in_=ybr[:], identity=identb), reads=[ybr.B, cstb.B], writes=[ps[3].B])
                        if CUT == 23 and ch == 8:
                            P.disabled = True
                        A_(lambda e, pv=pv, c=c, oc=oc: e.activation(out=yT[:, 8 + c, oc * 128:(oc + 1) * 128], in_=pv[:, 0:128], func=AF.Copy), [ps[3].B], [yT.b[8 + c]])
            if CUT == 25:
                P.disabled = True
            if dbg and stop == 3 and CUT == 77:
                d6 = T(P, st, "d6", [128, 1024], F32)
                def dcp(c0, w, ap, bufs):
                    V_(lambda e: e.tensor_copy(out=d6[:, c0:c0 + w], in_=ap), bufs, [d6.B])
                def dsend(sl_):
                    P.dma("sync", lambda e: e.dma_start(out=dbg_d[sl_], in_=d6[:]), d6.B, reads=[d6.B])
                V_(lambda e: e.memset(d6[:], 0.0), [], [d6.B])
                dcp(0, 512, ex[:].rearrange("p a b -> p (a b)"), [ex.B]); dcp(512, 128, Xf[:], [Xf.B]); dcp(640, 128, lwt[:], [lwt.B])
                dcp(768, 128, Yt[:], [Yt.B]); dcp(896, 128, yn[:], [yn.B])
                dsend(6)
                dcp(0, 256, ART[:], [ART.B]); dcp(256, 128, BhT[:], [BhT.B]); dcp(384, 128, KhT[:], [KhT.B])
                dcp(512, 512, TMt[:].rearrange("p a b -> p (a b)"), [TMt.B])
                dsend(7)
                dcp(0, 512, MT[:].rearrange("p a b -> p (a b)"), [MT.B]); dcp(512, 64, GTs[:, 15, 0:64], [GTs.B]); dcp(576, 64, Hs[:, 15, :], [Hs.B])
                dcp(640, 128, QeTs[:, 7, :], [QeTs.B]); dcp(768, 128, Yls[:, 7, :], [Yls.B]); dcp(896, 64, Sb[:, 0:64], [Sb.B])
                dcp(960, 2, sbon[:, 7, :], [sbon.B])
                dsend(8)
                dbg_bufs.append(d6.B)
            dump(st, 4, yT[:, 8, 0:512], [yT.b[8]], 512)
            if not (dbg and stop == 3):
                dump(st, 5, yT[:, 7 + NPAIRS, :], [yT.b[7 + NPAIRS]], 1024)
            if stop == 3:
                P.wait_all("sync", dbg_bufs)
            P.flush()
            if stop == 3:
                P.disabled = True

        stA.close()
        with ExitStack() as st:
            acc = T(P, st, "acc", [128, 8, D], F32, nb=8)
            wt_ = T(P, st, "wt", [128, 8, 32], F32)
            accB = acc.b
            gb2 = [None, None]
            ws = [None, None, None]

            class AccT:
                def __init__(self, i):
                    self.i = i; self.B = accB[i]

                def __getitem__(self, k):
                    return acc[:, self.i, :] if k == slice(None) else acc[:, self.i, k[1]]

            def load_gb(gs, bs):
                P.dma("sync", lambda e: e.dma_start(out=gb2[0][:], in_=gs.partition_broadcast(128)), gb2[0].B, writes=[gb2[0].B])
                if bs is not None:
                    P.dma("sync", lambda e: e.dma_start(out=gb2[1][:], in_=bs.partition_broadcast(128)), gb2[1].B, writes=[gb2[1].B])

            with ExitStack() as st2:
                gb2[0] = T(P, st2, "gb2a0", [128, D], F32); gb2[1] = T(P, st2, "gb2a1", [128, D], F32)
                for i in range(3):
                    ws[i] = T(P, st2, "wsa%d" % i, [128, 16, 512], BF16)
                xt = [T(P, st2, "xt4%d" % i, [128, D], F32) for i in range(2)]
                xn = T(P, st2, "xn4", [128, D], F32)
                stt4 = T(P, st2, "stt4", [128, 4, 6], F32); mv4 = T(P, st2, "mv4", [128, 4], F32)
                load_gb(ln_in_g, ln_in_b)
                for i in range(8):
                    X = xt[i % 2]
                    P.dma("sync", lambda e, X=X, i=i: e.dma_start(out=X[:], in_=xs[(8 + i) * 128:(9 + i) * 128, :]), X.B, writes=[X.B])
                    ln_tile(P, X, xn, stt4, mv4, gb2[0], gb2[1], None, AccT(i))
                    A_(lambda e, i=i: e.activation(out=acc[:, i, :], in_=acc[:, i, :], func=AF.Copy, scale=ALPHA), [accB[i]], [accB[i]])
                w_out_v = w_out.rearrange("(kc p) n -> p kc n", p=128)
                for n in range(4):
                    W_ = ws[n % 3]
                    P.dma("gpsimd", lambda e, W_=W_, n=n: e.dma_start(out=W_[:], in_=w_out_v[:, :, n * 512:(n + 1) * 512]), W_.B, writes=[W_.B])
                    for i in range(8):
                        pst = ps[i % 2]
                        for kc in range(16):
                            mm(pst[:, :], yT[:, kc, i * 128:(i + 1) * 128], W_[:, kc, :], kc == 0, kc == 15, [yT.b[kc], W_.B], pst.B)
                        V_(lambda e, i=i, n=n, pst=pst: e.tensor_tensor(out=acc[:, i, n * 512:(n + 1) * 512], in0=acc[:, i, n * 512:(n + 1) * 512], in1=pst[:, :], op=ALU.add),
                           [accB[i], pst.B], [accB[i]])
                dump(st2, 6, acc[:, 0, 0:1024], [accB[0]], 1024)
                if stop == 4:
                    P.wait_all("sync", dbg_bufs)
                P.flush()
                if stop == 4:
                    P.disabled = True
            hT1 = T(P, st, "hT1", [128, 16, 1024], BF16, nb=8)
            with ExitStack() as st2:
                gb2[0] = T(P, st2, "gb2b0", [128, D], F32); gb2[1] = T(P, st2, "gb2b1", [128, D], F32)
                xn = T(P, st2, "xn4b", [128, D], F32)
                hb4 = T(P, st2, "hb4", [128, D], BF16)
                hlo = T(P, st2, "hlo", [128, D], BF16)
                stt4 = T(P, st2, "stt4b", [128, 4, 6], F32); mv4 = T(P, st2, "mv4b", [128, 4], F32)
                hT1l = T(P, st2, "hT1l", [128, 16, 128], BF16)
                load_gb(ln1_g, ln1_b)
                rwf = T(P, st2, "rwf", [128, 16, 36], F32); rwh = T(P, st2, "rwh", [128, 16, 36], BF16); rwl = T(P, st2, "rwl", [128, 16, 36], BF16)
                rbias = T(P, st2, "rbias", [128, 36], F32)
                P.dma("sync", lambda e: e.dma_start(out=rwf[:, :, 0:4], in_=rgw.rearrange("(kc p) n -> p kc n", p=128)), rwf.B, writes=[rwf.B])
                P.dma("sync", lambda e: e.dma_start(out=rwf[:, :, 4:36], in_=rew.rearrange("(kc p) n -> p kc n", p=128)), rwf.B, writes=[rwf.B])
                P.dma("sync", lambda e: e.dma_start(out=rbias[:, 0:4], in_=rgb.partition_broadcast(128)), rbias.B, writes=[rbias.B])
                P.dma("sync", lambda e: e.dma_start(out=rbias[:, 4:36], in_=reb.partition_broadcast(128)), rbias.B, writes=[rbias.B])
                V_(lambda e: e.tensor_copy(out=rwh[:], in_=rwf[:]), [rwf.B], [rwh.B])
                V_(lambda e: e.tensor_tensor(out=rwf[:], in0=rwf[:], in1=rwh[:], op=ALU.subtract), [rwf.B, rwh.B], [rwf.B])
                V_(lambda e: e.tensor_copy(out=rwl[:], in_=rwf[:]), [rwf.B], [rwl.B])
                L = T(P, st2, "L", [128, 36], F32); r1 = T(P, st2, "r1", [128, 8], F32)
                gm = T(P, st2, "gm", [128, 4], F32); elm = T(P, st2, "elm", [128, 32], F32); ee = T(P, st2, "ee", [128, 32], F32)
                m1 = T(P, st2, "m1", [128, 32], F32); e2 = T(P, st2, "e2", [128, 32], F32); m2 = T(P, st2, "m2", [128, 32], F32)
                for i in range(8):
                    A = AccT(i)
                    ln_tile(P, A, xn, stt4, mv4, gb2[0], gb2[1], hb4, A)
                    transpose_to_fm(P, hb4, hT1, i, ps, identb, cstb.B, pbase=2)
                    V_(lambda e, i=i: e.tensor_tensor(out=xn[:], in0=acc[:, i, :], in1=hb4[:], op=ALU.subtract), [accB[i], hb4.B], [xn.B])
                    A_(lambda e: e.activation(out=hlo[:], in_=xn[:], func=AF.Copy), [xn.B], [hlo.B])
                    transpose_to_fm(P, hlo, hT1l, 0, ps, identb, cstb.B, pbase=4)
                    tsl = slice(i * 128, (i + 1) * 128)
                    n_mm = 0
                    for (hT_, w_, sl_, bi_) in ((hT1, rwh, tsl, i), (hT1l, rwh, slice(0, 128), 0), (hT1, rwl, tsl, i)):
                        for kc in range(16):
                            mm(ps[6][:, 0:36], hT_[:, kc, sl_], w_[:, kc, :], n_mm == 0, n_mm == 47, [hT_.b[bi_], w_.B], ps[6].B)
                            n_mm += 1
                    V_(lambda e: e.tensor_tensor(out=L[:], in0=ps[6][:, 0:36], in1=rbias[:], op=ALU.add), [ps[6].B, rbias.B], [L.B])
                    V_(lambda e: e.reduce_max(out=r1[:, 0:1], in_=L[:, 0:4], axis=AX.X), [L.B], [r1.B])
                    V_(lambda e: e.tensor_scalar(out=gm[:], in0=L[:, 0:4], scalar1=r1[:, 0:1], scalar2=None, op0=ALU.is_ge), [L.B, r1.B], [gm.B])
                    V_(lambda e: e.tensor_scalar(out=r1[:, 1:2], in0=r1[:, 0:1], scalar1=-1.0, scalar2=None, op0=ALU.mult), [r1.B], [r1.B])
                    A_(lambda e: e.activation(out=ee[:, 0:4], in_=L[:, 0:4], func=AF.Exp, bias=r1[:, 1:2], scale=1.0, accum_out=r1[:, 2:3]), [L.B, r1.B], [ee.B, r1.B])
                    V_(lambda e: e.reciprocal(out=r1[:, 3:4], in_=r1[:, 2:3]), [r1.B], [r1.B])
                    V_(lambda e: e.tensor_scalar(out=gm[:], in0=gm[:], scalar1=-1.0, scalar2=-NEG, op0=ALU.add, op1=ALU.mult), [gm.B], [gm.B])
                    V_(lambda e: e.tensor_tensor(out=elm[:].rearrange("p (g e) -> p g e", e=8), in0=L[:, 4:36].rearrange("p (g e) -> p g e", e=8),
                                                 in1=gm[:].unsqueeze(2).to_broadcast([128, 4, 8]), op=ALU.add), [L.B, gm.B], [elm.B])
                    V_(lambda e: e.reduce_max(out=r1[:, 4:5], in_=elm[:], axis=AX.X), [elm.B], [r1.B])
                    V_(lambda e: e.tensor_scalar(out=r1[:, 5:6], in0=r1[:, 4:5], scalar1=-1.0, scalar2=None, op0=ALU.mult), [r1.B], [r1.B])
                    A_(lambda e: e.activation(out=ee[:], in_=elm[:], func=AF.Exp, bias=r1[:, 5:6], scale=1.0), [elm.B, r1.B], [ee.B])
                    V_(lambda e: e.tensor_scalar(out=m1[:], in0=elm[:], scalar1=r1[:, 4:5], scalar2=None, op0=ALU.is_ge), [elm.B, r1.B], [m1.B])
                    V_(lambda e: e.tensor_tensor(out=e2[:], in0=ee[:], in1=m1[:], op=ALU.mult), [ee.B, m1.B], [e2.B])
                    V_(lambda e: e.tensor_tensor(out=e2[:], in0=ee[:], in1=e2[:], op=ALU.subtract), [ee.B, e2.B], [e2.B])
                    V_(lambda e: e.reduce_max(out=r1[:, 6:7], in_=e2[:], axis=AX.X), [e2.B], [r1.B])
                    V_(lambda e: e.tensor_scalar(out=m2[:], in0=e2[:], scalar1=r1[:, 6:7], scalar2=None, op0=ALU.is_ge), [e2.B, r1.B], [m2.B])
                    V_(lambda e: e.tensor_tensor(out=m2[:], in0=m2[:], in1=e2[:], op=ALU.mult), [m2.B, e2.B], [m2.B])
                    V_(lambda e: e.tensor_tensor(out=m1[:], in0=m1[:], in1=m2[:], op=ALU.add), [m1.B, m2.B], [m1.B])
                    V_(lambda e: e.tensor_scalar(out=r1[:, 7:8], in0=r1[:, 6:7], scalar1=1.0, scalar2=None, op0=ALU.add), [r1.B], [r1.B])
                    V_(lambda e: e.reciprocal(out=r1[:, 7:8], in_=r1[:, 7:8]), [r1.B], [r1.B])
                    V_(lambda e, i=i: e.tensor_scalar(out=wt_[:, i, :], in0=m1[:], scalar1=r1[:, 7:8], scalar2=r1[:, 3:4], op0=ALU.mult, op1=ALU.mult), [m1.B, r1.B], [wt_.B])
                    A_(lambda e, i=i: e.activation(out=acc[:, i, :], in_=acc[:, i, :], func=AF.Copy, scale=ALPHA), [accB[i]], [accB[i]])
                dump(st2, 7, wt_[:, 0, :], [wt_.B], 32)
                if stop == 5:
                    P.wait_all("sync", dbg_bufs)
                P.flush()
                if stop == 5:
                    P.disabled = True
            with ExitStack() as st2:
                for i in range(3):
                    ws[i] = T(P, st2, "wsc%d" % i, [128, 16, 512], BF16)
                hid = [T(P, st2, "hid0", [128, 4, 1024], BF16)] * 2
                sg = [T(P, st2, "sg%d" % i, [128, 512], BF16) for i in range(2)]
                nld = 0
                for ex_ in range(NEXP):
                    Wg, Wu, Wd = ws[0], ws[1], ws[2]
                    P.dma("gpsimd", lambda e, ex_=ex_: e.dma_start(out=ws[0][:], in_=wg_d[ex_].rearrange("(kc p) n -> p kc n", p=128)), ws[0].B, writes=[ws[0].B])
                    P.dma("gpsimd", lambda e, ex_=ex_: e.dma_start(out=ws[1][:], in_=wu_d[ex_].rearrange("(kc p) n -> p kc n", p=128)), ws[1].B, writes=[ws[1].B])
                    P.dma("gpsimd", lambda e, ex_=ex_: e.dma_start(out=ws[2][:].rearrange("p (a b) n -> p a b n", b=4), in_=wd_d[ex_].rearrange("(fc p) (b n) -> p fc b n", p=128, n=512)),
                          ws[2].B, writes=[ws[2].B])
                    H_ = hid[ex_ % 2]
                    k_ = 0
                    for tb in range(2):
                        for fc in range(4):
                            pg, pu = ps[(k_ % 2) * 2], ps[(k_ % 2) * 2 + 1]
                            S_ = sg[k_ % 2]
                            for kc in range(16):
                                mm(pg[:, :], Wg[:, kc, fc * 128:(fc + 1) * 128], hT1[:, kc, tb * 512:(tb + 1) * 512], kc == 0, kc == 15, [Wg.B] + hT1.b[tb * 4:(tb + 1) * 4], pg.B)
                            for kc in range(16):
                                mm(pu[:, :], Wu[:, kc, fc * 128:(fc + 1) * 128], hT1[:, kc, tb * 512:(tb + 1) * 512], kc == 0, kc == 15, [Wu.B] + hT1.b[tb * 4:(tb + 1) * 4], pu.B)
                            A_(lambda e, S_=S_, pg=pg: e.activation(out=S_[:], in_=pg[:, :], func=AF.Silu), [pg.B], [S_.B])
                            V_(lambda e, S_=S_, pu=pu, H_=H_, fc=fc, tb=tb: e.tensor_tensor(out=H_[:, fc, tb * 512:(tb + 1) * 512], in0=S_[:], in1=pu[:, :], op=ALU.mult),
                               [S_.B, pu.B], [H_.B])
                            k_ += 1
                    for i in range(8):
                        for n in range(4):
                            po = ps[4 + (i * 4 + n) % 4]
                            for fc in range(4):
                                mm(po[:, :], H_[:, fc, i * 128:(i + 1) * 128], Wd[:, fc * 4 + n, :], fc == 0, fc == 3, [H_.B, Wd.B], po.B)
                            V_(lambda e, i=i, n=n, po=po, ex_=ex_: e.scalar_tensor_tensor(out=acc[:, i, n * 512:(n + 1) * 512], in0=po[:, :], scalar=wt_[:, i, ex_:ex_ + 1],
                                                                                  in1=acc[:, i, n * 512:(n + 1) * 512], op0=ALU.mult, op1=ALU.add), [po.B, wt_.B, accB[i]], [accB[i]])
                dump(st2, 8, acc[:, 0, 0:1024], [accB[0]], 1024)
                if stop == 6:
                    P.wait_all("sync", dbg_bufs)
                P.flush()
                if stop == 6:
                    P.disabled = True
            with ExitStack() as st2:
                gb2[0] = T(P, st2, "gb2d0", [128, D], F32); gb2[1] = T(P, st2, "gb2d1", [128, D], F32)
                ws[0] = T(P, st2, "wsd0", [128, 16, 512], BF16)
                xn = T(P, st2, "xn5", [128, D], F32); hb4 = T(P, st2, "hb5", [128, D], BF16)
                stt4 = T(P, st2, "stt5", [128, 4, 6], F32); mv4 = T(P, st2, "mv5", [128, 4], F32)
                pf = T(P, st2, "pf", [128, 256], F32); pb = T(P, st2, "pb", [128, 256], BF16)
                pT = T(P, st2, "pT", [128, 2, 1024], BF16, nb=8)
                plw = T(P, st2, "plw", [128, 2, D], BF16)
                ssq = T(P, st2, "ssq", [128, 8, 4], F32); rsp = T(P, st2, "rsp", [128, 8], F32)
                sgt = T(P, st2, "sgt", [128, 512], F32); t1_ = T(P, st2, "t1", [128, 512], F32)
                load_gb(ln2_g, ln2_b)
                P.dma("gpsimd", lambda e: e.dma_start(out=plw[:], in_=ple_w.rearrange("(kc p) n -> p kc n", p=128)), plw.B, writes=[plw.B])
                if CUT == 31:
                    P.disabled = True
                for i in range(8):
                    A = AccT(i)
                    ln_tile(P, A, xn, stt4, mv4, gb2[0], gb2[1], hb4, A)
                    transpose_to_fm(P, hb4, hT1, i, ps, identb, cstb.B, pbase=2)
                    P.dma("sync", lambda e, i=i: e.dma_start(out=pf[:], in_=p_in[i * 128:(i + 1) * 128, :]), pf.B, writes=[pf.B])
                    V_(lambda e: e.tensor_copy(out=pb[:], in_=pf[:]), [pf.B], [pb.B])
                    pv = ps[4].t[:].bitcast(BF16)
                    for k in range(2):
                        P.op("tensor", lambda e, k=k, pv=pv: e.transpose(out=pv[:, k * 128:(k + 1) * 128], in_=pb[:, k * 128:(k + 1) * 128], identity=identb), reads=[pb.B, cstb.B], writes=[ps[4].B])
                    V_(lambda e, i=i, pv=pv: e.tensor_copy(out=pT[:, :, i * 128:(i + 1) * 128], in_=pv[:, 0:256].rearrange("p (k t) -> p k t", t=128)), [ps[4].B], [pT.b[i]])
                    for n in range(4):
                        pp = ps[5 + n % 2]
                        for k in range(2):
                            mm(pp[:, :], pT[:, k, i * 128:(i + 1) * 128], plw[:, k, n * 512:(n + 1) * 512], k == 0, k == 1, [pT.b[i], plw.B], pp.B)
                        A_(lambda e, i=i, n=n, pp=pp: e.activation(out=t1_[:], in_=pp[:, :], func=AF.Square, accum_out=ssq[:, i, n:n + 1]), [pp.B], [t1_.B, ssq.B])
                if CUT == 32:
                    P.disabled = True
                V_(lambda e: e.reduce_sum(out=rsp[:], in_=ssq[:], axis=AX.X), [ssq.B], [rsp.B])
                A_(lambda e: e.activation(out=rsp[:], in_=rsp[:], func=AF.Sqrt, bias=EPSC[:, 0:1], scale=1.0 / D), [rsp.B, EPSC.B], [rsp.B])
                V_(lambda e: e.reciprocal(out=rsp[:], in_=rsp[:]), [rsp.B], [rsp.B])
                P.dma("sync", lambda e: e.dma_start(out=gb2[0][:], in_=ple_ng.partition_broadcast(128)), gb2[0].B, writes=[gb2[0].B])
                if CUT == 33:
                    P.disabled = True
                pgw_v = ple_gw.rearrange("(kc p) n -> p kc n", p=128)
                for n in range(4):
                    W_ = ws[0]
                    P.dma("gpsimd", lambda e, W_=W_, n=n: e.dma_start(out=W_[:], in_=pgw_v[:, :, n * 512:(n + 1) * 512]), W_.B, writes=[W_.B])
                    nsl = slice(n * 512, (n + 1) * 512)
                    for i in range(8):
                        pgt, pp = ps[(i % 2) * 2], ps[(i % 2) * 2 + 1]
                        for kc in range(16):
                            mm(pgt[:, :], hT1[:, kc, i * 128:(i + 1) * 128], W_[:, kc, :], kc == 0, kc == 15, [hT1.b[i], W_.B], pgt.B)
                        for k in range(2):
                            mm(pp[:, :], pT[:, k, i * 128:(i + 1) * 128], plw[:, k, nsl], k == 0, k == 1, [pT.b[i], plw.B], pp.B)
                        A_(lambda e, pgt=pgt: e.activation(out=sgt[:], in_=pgt[:, :], func=AF.Sigmoid), [pgt.B], [sgt.B])
                        V_(lambda e, i=i, pp=pp, nsl=nsl: e.scalar_tensor_tensor(out=t1_[:], in0=pp[:, :], scalar=rsp[:, i:i + 1], in1=gb2[0][:, nsl], op0=ALU.mult, op1=ALU.mult),
                           [pp.B, rsp.B, gb2[0].B], [t1_.B])
                        V_(lambda e: e.tensor_tensor(out=t1_[:], in0=t1_[:], in1=sgt[:], op=ALU.mult), [t1_.B, sgt.B], [t1_.B])
                        V_(lambda e, i=i, nsl=nsl: e.tensor_tensor(out=acc[:, i, nsl], in0=acc[:, i, nsl], in1=t1_[:], op=ALU.add), [accB[i], t1_.B], [accB[i]])
                        P.dma("sync", lambda e, i=i, nsl=nsl: e.dma_start(out=out_d[i * 128:(i + 1) * 128, nsl], in_=acc[:, i, nsl]), accB[i], reads=[accB[i]])
                if CUT == 34:
                    P.disabled = True
                P.wait_all("sync", accB + dbg_bufs)
                if stop == 7:
                    P.wait_all("sync", dbg_bufs)
                P.flush()
      except _Stop:
        pass
    return nc


EPSC = None


def rsqrt(P, out, in_, outB, inB, eps_ap):
    P.op("scalar", lambda e: e.activation(out=out, in_=in_, func=AF.Sqrt, bias=eps_ap, scale=1.0), reads=[inB, EPSC.B], writes=[outB])
    P.op("vector", lambda e: e.reciprocal(out=out, in_=out), reads=[outB], writes=[outB])


def ln_tile(P, X, XN, ST, MV, gbc, bbc, out_bf, out_f32, eps=1e-5):
    for j in range(4):
        P.op("vector", lambda e, j=j: e.bn_stats(out=ST[:, j, :], in_=X[:, j * 512:(j + 1) * 512]), reads=[X.B], writes=[ST.B])
    P.op("vector", lambda e: e.bn_aggr(out=MV[:, 0:2], in_=ST[:]), reads=[ST.B], writes=[MV.B])
    rsqrt(P, MV[:, 2:3], MV[:, 1:2], MV.B, MV.B, EPSC[:, 0:1])
    P.op("vector", lambda e: e.scalar_tensor_tensor(out=MV[:, 3:4], in0=MV[:, 0:1], scalar=-1.0, in1=MV[:, 2:3], op0=ALU.mult, op1=ALU.mult),
         reads=[MV.B], writes=[MV.B])
    P.op("scalar", lambda e: e.activation(out=XN[:], in_=X[:], func=AF.Identity, bias=MV[:, 3:4], scale=MV[:, 2:3]),
         reads=[X.B, MV.B], writes=[XN.B])
    P.op("vector", lambda e: e.tensor_tensor(out=XN[:], in0=XN[:], in1=gbc[:], op=ALU.mult), reads=[XN.B, gbc.B], writes=[XN.B])
    if out_f32 is not None:
        P.op("vector", lambda e: e.tensor_tensor(out=out_f32[:], in0=XN[:], in1=bbc[:], op=ALU.add), reads=[XN.B, bbc.B], writes=[out_f32.B])
        if out_bf is not None:
            P.op("scalar", lambda e: e.activation(out=out_bf[:], in_=out_f32[:], func=AF.Copy), reads=[out_f32.B], writes=[out_bf.B])
    else:
        P.op("vector", lambda e: e.tensor_tensor(out=out_bf[:], in0=XN[:], in1=bbc[:], op=ALU.add), reads=[XN.B, bbc.B], writes=[out_bf.B])


def transpose_to_fm(P, HB, dstT, ti, ps, identb, identB, pbase=0):
    for half in range(2):
        pst = ps[pbase + half]
        pv = pst.t[:].bitcast(BF16)
        for j in range(8):
            kc = half * 8 + j
            P.op("tensor", lambda e, kc=kc, j=j, pv=pv: e.transpose(out=pv[:, j * 128:(j + 1) * 128], in_=HB[:, kc * 128:(kc + 1) * 128], identity=identb),
                 reads=[HB.B, identB], writes=[pst.B])
        eng = "scalar" if half == 0 else "vector"
        dst = dstT[:, half * 8:(half + 1) * 8, ti * 128:(ti + 1) * 128]
        src = pv.rearrange("p (j t) -> p j t", t=128)
        if eng == "scalar":
            P.op("scalar", lambda e, dst=dst, src=src: e.activation(out=dst, in_=src, func=AF.Copy), reads=[pst.B], writes=[dstT.b[ti]])
        else:
            P.op("vector", lambda e, dst=dst, src=src: e.tensor_copy(out=dst, in_=src), reads=[pst.B], writes=[dstT.b[ti]])


def make_consts():
    c = np.zeros((8, 128, 128), np.float32)
    i = np.arange(128)
    c[0] = np.eye(128)
    c[1] = (i[:, None] <= i[None, :]) * -EM05
    c[2] = (i[:, None] < i[None, :]) * -EM05
    c[3] = (i[:, None] > i[None, :]) * -EM05
    c[4] = (i[None, :] > i[:, None])
    c[5] = (i[None, :] >= i[:, None])
    c[6] = (i[:, None] > i[None, :])
    c[7] = (i[:, None] // 64 == i[None, :] // 64)
    n = np.arange(-127, 256)
    nn = np.maximum(n, 0)
    nf = np.maximum(nn, 1).astype(np.float32)
    large = 16 + (np.log(nf / 16) / math.log(128 / 16) * 16).astype(np.int32)
    large = np.minimum(large, 31)
    bucket = np.where(nn < 16, nn, large)
    oh = np.zeros((33, 383), np.float32)
    oh[bucket, np.arange(383)] = 1.0
    oh[:32, n < 0] = 0.0
    oh[32, n < 0] = 1.0
    return c, oh


_CACHE = {}


def kernel(**inputs):
    dbg = inputs.pop("_dbg", None)
    stop = inputs.pop("_stop", 99)
    key = ("nc", dbg, stop)
    if key not in _CACHE:
        _CACHE[key] = build(dbg, stop)
    nc = _CACHE[key]
    x = np.asarray(inputs["x"], np.float32)
    p = np.asarray(inputs["p"], np.float32)
    cm, oh = make_consts()
    shared = {"cmat": cm, "onehot": oh}
    for k, v in inputs.items():
        if k in ("x", "p"):
            continue
        a = np.asarray(v, np.float32)
        if a.shape[0] == 1 and k not in ("ln_in_g", "ln_in_b"):
            a = a[0]
        if k in ("moe_w_gate", "moe_w_up", "moe_w_down"):
            a = a.reshape(32, a.shape[-2], a.shape[-1])
            if stop < 6:
                a = a[:1]
        if k in ("rwkv_r_k",):
            a = a.reshape(-1)
        shared[k] = np.ascontiguousarray(a)
    in_maps = []
    for c in range(8):
        b, s = c // 2, c % 2
        seq = np.zeros((2048, 2048), np.float32)
        if s == 1:
            seq[:] = x[b]
        else:
            seq[1024:] = x[b, :1024]
        m = dict(shared)
        m["xs"] = seq
        m["p"] = np.ascontiguousarray(p[0, b, s * 1024:(s + 1) * 1024])
        m["flag"] = np.full((128, 1), float(s), np.float32)
        in_maps.append(m)
    res = run_bass_kernel_spmd(nc, in_maps, core_ids=list(range(8)))
    if dbg:
        return [(r["dbg"], r["out"]) for r in res.results]
    out = np.zeros((4, 2048, 2048), np.float32)
    for c in range(8):
        b, s = c // 2, c % 2
        out[b, s * 1024:(s + 1) * 1024] = res.results[c]["out"]
    return out
```

```python
import math
from contextlib import ExitStack
import numpy as np
import concourse.bass as bass
import concourse.mybir as mybir
from concourse.bass_utils import run_bass_kernel_spmd

F32 = mybir.dt.float32
BF16 = mybir.dt.bfloat16
AF = mybir.ActivationFunctionType
ALU = mybir.AluOpType
AX = mybir.AxisListType
ENGS = ("sync", "scalar", "vector", "gpsimd", "tensor")
ALPHA = 2.0 ** 0.25
LAMBDA_INIT = 0.8 - 0.6
NEG = -1.0e30
EM05 = math.exp(-0.5)
NHEADS = 8
NPAIRS = 8
CUT = 0
NEXP = 32
OUTSEL = tuple(range(8))


class Buf:
    __slots__ = ("name", "w", "r", "dsem", "dcnt")

    def __init__(self, name):
        self.name = name
        self.w = None
        self.r = []
        self.dsem = None
        self.dcnt = 0


class Prog:
    def __init__(self, nc, stack):
        self.nc = nc
        self.stack = stack
        self.ops = {e: [] for e in ENGS}
        self.sem = {e: stack.enter_context(nc.semaphore("c_" + e)) for e in ("scalar", "vector", "gpsimd", "tensor")}
        self.cnt = {e: 0 for e in ENGS}
        self.seen = {e: {} for e in ENGS}
        self.semobj = {}
        self.nbuf = 0
        self.disabled = False
        self.dbufs = []

    def buf(self, name=None):
        self.nbuf += 1
        return Buf(name or ("b%d" % self.nbuf))

    def _deps(self, eng, reads, writes):
        deps = []
        for b in reads:
            if b.w is not None:
                deps.append(b.w)
        for b in writes:
            if b.w is not None:
                deps.append(b.w)
            deps.extend(b.r)
        waits = {}
        for (sid, val, src) in deps:
            if src == "tensor" and eng == "tensor":
                continue
            if self.seen[eng].get(sid, 0) >= val:
                continue
            if waits.get(sid, 0) < val:
                waits[sid] = val
        for sid, val in waits.items():
            self.seen[eng][sid] = val
        return [(self.semobj[sid], val) for sid, val in waits.items()]

    def op(self, eng, fn, reads=(), writes=()):
        if self.disabled:
            return None
        waits = self._deps(eng, reads, writes)
        self.cnt[eng] += 1
        sem = self.sem[eng]
        self.semobj[id(sem)] = sem
        tok = (id(sem), self.cnt[eng], eng)
        self.ops[eng].append((waits, fn, (sem, 1)))
        for b in reads:
            b.r.append(tok)
        for b in writes:
            b.w = tok
            b.r = []
        return tok

    def dma(self, eng, fn, sbuf_buf, reads=(), writes=()):
        if self.disabled:
            return None
        waits = self._deps(eng, reads, writes)
        if sbuf_buf.dsem is None:
            sbuf_buf.dsem = self.stack.enter_context(self.nc.semaphore("d_" + sbuf_buf.name))
            self.semobj[id(sbuf_buf.dsem)] = sbuf_buf.dsem
            self.dbufs.append(sbuf_buf)
        sbuf_buf.dcnt += 16
        tok = (id(sbuf_buf.dsem), sbuf_buf.dcnt, "dma")
        self.ops[eng].append((waits, fn, (sbuf_buf.dsem, 16)))
        for b in reads:
            b.r.append(tok)
        for b in writes:
            b.w = tok
            b.r = []
        return tok

    def wait_all(self, eng, bufs):
        if self.disabled:
            return
        waits = self._deps(eng, bufs, bufs)
        self.ops[eng].append((waits, None, None))

    def barrier(self):
        toks = []
        for x in ("scalar", "vector", "gpsimd", "tensor"):
            if self.cnt[x] > 0:
                self.semobj[id(self.sem[x])] = self.sem[x]
                toks.append((id(self.sem[x]), self.cnt[x]))
        for b in self.dbufs:
            toks.append((id(b.dsem), b.dcnt))
        for eng in ENGS:
            waits = []
            for sid, val in toks:
                if self.seen[eng].get(sid, 0) >= val:
                    continue
                self.seen[eng][sid] = val
                waits.append((self.semobj[sid], val))
            if waits:
                self.ops[eng].append((waits, None, None))

    def flush(self):
        self.barrier()
        nc = self.nc
        ops = self.ops
        self.ops = {e: [] for e in ENGS}

        def replay(lst, e):
            for waits, fn, inc in lst:
                for sem, val in waits:
                    e.wait_ge(sem, val)
                if fn is not None:
                    ins = fn(e)
                    ins.then_inc(inc[0], inc[1])

        with nc.Block() as block:
            @block.sync
            def _(e):
                replay(ops["sync"], e)

            @block.scalar
            def _(e):
                replay(ops["scalar"], e)

            @block.vector
            def _(e):
                replay(ops["vector"], e)

            @block.gpsimd
            def _(e):
                replay(ops["gpsimd"], e)

            @block.tensor
            def _(e):
                replay(ops["tensor"], e)


class T:
    def __init__(self, P, stack, name, shape, dtype, psum=False, nb=1):
        nc = P.nc
        if psum:
            self.t = stack.enter_context(nc.psum_tensor("t_" + name, shape, dtype))
        else:
            self.t = stack.enter_context(nc.sbuf_tensor("t_" + name, shape, dtype))
        self.b = [P.buf(name + "_%d" % i) for i in range(nb)]
        self.B = self.b[0]

    def __getitem__(self, k):
        return self.t[k]


class _Stop(Exception):
    pass


def build(dbg=None, stop=99):
    nc = bass.Bass("TRN2", target_bir_lowering=False)
    D = 2048
    dram = {}

    def din(name, shape):
        dram[name] = nc.dram_tensor(name, list(shape), F32, kind="ExternalInput").ap()
        return dram[name]

    xs = din("xs", [2048, D])
    p_in = din("p", [1024, 256])
    flag_d = din("flag", [128, 1])
    ln_in_g = din("ln_in_g", [D]); ln_in_b = din("ln_in_b", [D])
    rel_bias = din("rel_bias", [32, 8])
    w_in = din("w_in", [D, 6432])
    lamq1 = din("diff_lam_q1", [64]); lamk1 = din("diff_lam_k1", [64])
    lamq2 = din("diff_lam_q2", [64]); lamk2 = din("diff_lam_k2", [64])
    subln_g = din("diff_subln_g", [128])
    rwkv_mu = din("rwkv_mu", [3360])
    rwkv_w0 = din("rwkv_w0", [1024]); rwkv_w2 = din("rwkv_w2", [64, 1024])
    rwkv_a0 = din("rwkv_a0", [1024]); rwkv_a2 = din("rwkv_a2", [64, 1024])
    rwkv_g2 = din("rwkv_g2", [160, 1024])
    rwkv_k_k = din("rwkv_k_k", [1024]); rwkv_k_a = din("rwkv_k_a", [1024])
    rwkv_r_k = din("rwkv_r_k", [1024])
    lnx_g = din("rwkv_lnx_g", [1024]); lnx_b = din("rwkv_lnx_b", [1024])
    w_out = din("w_out", [D, D])
    ln1_g = din("ln1_g", [D]); ln1_b = din("ln1_b", [D])
    rgw = din("router_group_w", [D, 4]); rgb = din("router_group_b", [4])
    rew = din("router_expert_w", [D, 32]); reb = din("router_expert_b", [32])
    nE = 32 if stop >= 6 else 1
    wg_d = din("moe_w_gate", [nE, D, 512]); wu_d = din("moe_w_up", [nE, D, 512]); wd_d = din("moe_w_down", [nE, 512, D])
    ln2_g = din("ln2_g", [D]); ln2_b = din("ln2_b", [D])
    ple_w = din("ple_w", [256, D]); ple_ng = din("ple_norm_g", [D]); ple_gw = din("ple_gate_w", [D, D])
    cmat = din("cmat", [8, 128, 128])
    oh_d = din("onehot", [33, 383])
    out_d = nc.dram_tensor("out", [1024, D], F32, kind="ExternalOutput").ap()
    dbg_d = None
    if dbg:
        dbg_d = nc.dram_tensor("dbg", list(dbg), F32, kind="ExternalOutput").ap()
    trep_d = nc.dram_tensor("trep", [8, 128, 383], F32, kind="Internal").ap()

    stA = ExitStack()
    with ExitStack() as st0:
      try:
        P = Prog(nc, st0)
        stB = st0
        yT = T(P, stB, "yT", [128, 16, 1024], BF16, nb=16)
        cst = T(P, st0, "cst", [128, 8, 128], F32)
        cstb = T(P, st0, "cstb", [128, 8, 128], BF16)
        flag = T(P, st0, "flag", [128, 1], F32)
        pbias = T(P, st0, "pbias", [128, 1], F32)
        ps = [T(P, st0, "ps%d" % i, [128, 512], F32, psum=True) for i in range(8)]
        IDENT, TRI_I, TRI_E, TRI_R, UT_S, UT_I, LT_S, BLK1 = range(8)
        identb = cstb[:, IDENT, :]

        global EPSC
        EPSC = T(P, st0, "epsc", [128, 4], F32)
        P.op("vector", lambda e: e.memset(EPSC[:, 0:1], 1e-5), writes=[EPSC.B])
        P.op("vector", lambda e: e.memset(EPSC[:, 1:2], 64e-5), writes=[EPSC.B])
        P.op("vector", lambda e: e.memset(EPSC[:, 2:3], 1e-24), writes=[EPSC.B])
        P.op("vector", lambda e: e.memset(EPSC[:, 3:4], 0.0), writes=[EPSC.B])
        P.dma("sync", lambda e: e.dma_start(out=cst[:], in_=cmat.rearrange("m p n -> p m n")), cst.B, writes=[cst.B])
        P.dma("sync", lambda e: e.dma_start(out=flag[:], in_=flag_d), flag.B, writes=[flag.B])
        P.op("vector", lambda e: e.tensor_copy(out=cstb[:], in_=cst[:]), reads=[cst.B], writes=[cstb.B])
        P.op("vector", lambda e: e.tensor_scalar(out=pbias[:], in0=flag[:], scalar1=-1.0, scalar2=-NEG, op0=ALU.add, op1=ALU.mult),
             reads=[flag.B], writes=[pbias.B])

        dbg_bufs = []

        def dump(stk, slot, ap, bufs, width, parts=128):
            if not dbg:
                return
            tmp = T(P, stk, "dbg%d" % slot, [128, width], F32)
            P.op("vector", lambda e: e.tensor_copy(out=tmp[0:parts, :], in_=ap), reads=bufs, writes=[tmp.B])
            P.dma("sync", lambda e: e.dma_start(out=dbg_d[slot, 0:parts, 0:width], in_=tmp[0:parts, :]), tmp.B, reads=[tmp.B])
            dbg_bufs.append(tmp.B)

        def mm(out, lhsT, rhs, start, stop, reads, wB, tp=None, sgc=False):
            kw = {}
            if tp is not None:
                kw["tile_position"] = tp
            if sgc:
                kw["skip_group_check"] = True
            P.op("tensor", lambda e: e.matmul(out=out, lhsT=lhsT, rhs=rhs, start=start, stop=stop, **kw), reads=reads, writes=[wB])

        def V_(fn, reads, writes):
            P.op("vector", fn, reads=reads, writes=writes)

        def A_(fn, reads, writes):
            P.op("scalar", fn, reads=reads, writes=writes)

        def small_load(dst_ap, dstB, src_ap):
            P.dma("sync", lambda e: e.dma_start(out=dst_ap, in_=src_ap, allow_slow_non_contiguous=True), dstB, writes=[dstB])

        w_in_v = w_in.rearrange("(kc p) n -> p kc n", p=128)

        def proj_fm(wt, c0, M, tb, pst):
            for kc in range(16):
                mm(pst[0:M, 0:512], wt[:, kc, c0:c0 + M], hT0[:, kc, tb * 512:(tb + 1) * 512], kc == 0, kc == 15,
                   [wt.B] + hT0.b[tb * 4:(tb + 1) * 4], pst.B)

        def shiftmix(pst, M, tb, R, mucol, omucol, muB, out_ap, outB, tmp):
            if tb == 0:
                V_(lambda e: e.memset(R[:], 0.0), [], [R.B])
            else:
                V_(lambda e: e.tensor_copy(out=R[:, 0:1], in_=R[:, 512:513]), [R.B], [R.B])
            if tb < 2:
                A_(lambda e: e.activation(out=R[0:M, 1:513], in_=pst[0:M, 0:512], func=AF.Copy, scale=flag[0:M, 0:1]), [pst.B, flag.B, R.B], [R.B])
            else:
                A_(lambda e: e.activation(out=R[0:M, 1:513], in_=pst[0:M, 0:512], func=AF.Copy), [pst.B, R.B], [R.B])
            V_(lambda e: e.tensor_scalar(out=tmp[0:M, :], in0=R[0:M, 1:513], scalar1=omucol, scalar2=None, op0=ALU.mult), [R.B, muB], [tmp.B])
            V_(lambda e: e.scalar_tensor_tensor(out=out_ap, in0=R[0:M, 0:512], scalar=mucol, in1=tmp[0:M, :], op0=ALU.mult, op1=ALU.add),
               [R.B, muB, tmp.B], [outB])

        mul_ = T(P, st0, "mul", [128, 8], F32)
        murkv = T(P, st0, "murkv", [128, 48], F32)
        st0.enter_context(stA)
        hT0 = T(P, stA, "hT0", [128, 16, 2048], BF16, nb=16)
        txw = T(P, stA, "txw", [64, 2048], BF16)
        xaT = T(P, stA, "xaT", [64, 2048], BF16)
        sxa = T(P, stA, "sxa", [128, 2048], BF16)
        sxb = T(P, stA, "sxb", [32, 2048], BF16)
        V_(lambda e: e.memset(mul_[:], 0.0), [], [mul_.B])
        small_load(mul_[0:64, 0:1], mul_.B, rwkv_mu[3072:3136].rearrange("(p a) -> p a", a=1))
        small_load(mul_[0:64, 1:2], mul_.B, rwkv_mu[3136:3200].rearrange("(p a) -> p a", a=1))
        small_load(mul_[0:128, 2:3], mul_.B, rwkv_mu[3200:3328].rearrange("(p a) -> p a", a=1))
        small_load(mul_[0:32, 3:4], mul_.B, rwkv_mu[3328:3360].rearrange("(p a) -> p a", a=1))
        V_(lambda e: e.tensor_scalar(out=mul_[:, 4:8], in0=mul_[:, 0:4], scalar1=-1.0, scalar2=1.0, op0=ALU.mult, op1=ALU.add), [mul_.B], [mul_.B])
        small_load(murkv[:, 0:24], murkv.B, rwkv_mu[0:3072].rearrange("(a p) -> p a", p=128))
        V_(lambda e: e.tensor_scalar(out=murkv[:, 24:48], in0=murkv[:, 0:24], scalar1=-1.0, scalar2=1.0, op0=ALU.mult, op1=ALU.add), [murkv.B], [murkv.B])

        with ExitStack() as st:
            gbc = T(P, st, "gbc", [128, D], F32); bbc = T(P, st, "bbc", [128, D], F32)
            P.dma("sync", lambda e: e.dma_start(out=gbc[:], in_=ln_in_g.partition_broadcast(128)), gbc.B, writes=[gbc.B])
            P.dma("sync", lambda e: e.dma_start(out=bbc[:], in_=ln_in_b.partition_broadcast(128)), bbc.B, writes=[bbc.B])
            wl = T(P, st, "wl", [128, 16, 288], BF16)
            P.dma("gpsimd", lambda e: e.dma_start(out=wl[:], in_=w_in_v[:, :, 6144:6432]), wl.B, writes=[wl.B])
            xt = [T(P, st, "xt%d" % i, [128, D], F32) for i in range(2)]
            xn = [T(P, st, "xn%d" % i, [128, D], F32) for i in range(2)]
            hb = [T(P, st, "hb%d" % i, [128, D], BF16) for i in range(2)]
            stt = [T(P, st, "stt%d" % i, [128, 4, 6], F32) for i in range(2)]
            mv = [T(P, st, "mv%d" % i, [128, 4], F32) for i in range(2)]
            for i in range(16):
                s = i % 2
                X = xt[s]
                P.dma("sync", lambda e, X=X, i=i: e.dma_start(out=X[:], in_=xs[i * 128:(i + 1) * 128, :]), X.B, writes=[X.B])
                ln_tile(P, X, xn[s], stt[s], mv[s], gbc, bbc, hb[s], None)
                transpose_to_fm(P, hb[s], hT0, i, ps, identb, cstb.B)
            Rl = [T(P, st, "Rl%d" % i, [128, 513], F32) for i in range(4)]
            tmpl = T(P, st, "tmpl", [128, 512], F32)
            xsl = T(P, st, "xsl", [128, 512], F32)
            groups = [(0, 64, txw, AF.Tanh), (64, 64, xaT, AF.Copy), (128, 128, sxa, AF.Sigmoid), (256, 32, sxb, AF.Sigmoid)]
            for tb in range(4):
                for gi, (c0, M, dst, fn_) in enumerate(groups):
                    pst = ps[2 + (gi % 2)]
                    proj_fm(wl, c0, M, tb, pst)
                    shiftmix(pst, M, tb, Rl[gi], mul_[0:M, gi:gi + 1], mul_[0:M, 4 + gi:5 + gi], mul_.B, xsl[0:M, :], xsl.B, tmpl)
                    A_(lambda e, dst=dst, M=M, fn_=fn_, tb=tb: e.activation(out=dst[0:M, tb * 512:(tb + 1) * 512], in_=xsl[0:M, :], func=fn_),
                       [xsl.B], [dst.B])
            dump(st, 0, hT0[:, 3, 0:1024], hT0.b, 1024)
            dump(st, 1, txw[0:64, 1024:2048], [txw.B], 1024, 64)
            if stop == 1:
                P.wait_all("sync", dbg_bufs)
            P.flush()
            if stop == 1:
                P.disabled = True

        with ExitStack() as st:
            rb = T(P, st, "rb", [33, 8], F32)
            oh = T(P, st, "oh", [33, 383], F32)
            P.dma("sync", lambda e: e.dma_start(out=rb[0:32, :], in_=rel_bias), rb.B, writes=[rb.B])
            V_(lambda e: e.memset(rb[32:33, :], NEG), [], [rb.B])
            P.dma("sync", lambda e: e.dma_start(out=oh[:], in_=oh_d), oh.B, writes=[oh.B])
            rbh = T(P, st, "rbh", [33, 128], F32)
            treps = T(P, st, "treps", [128, 383], F32)
            BN = T(P, st, "BN", [128, 8, 256], F32, nb=8)
            farb = T(P, st, "farb", [128, 16], F32)
            drb = [P.buf("drb%d" % h) for h in range(8)]
            for h in range(8):
                V_(lambda e, h=h: e.tensor_copy(out=rbh[:], in_=rb[:, h:h + 1].to_broadcast([33, 128])), [rb.B], [rbh.B])
                mm(ps[0][:, 0:383], rbh[:], oh[:], True, True, [rbh.B, oh.B], ps[0].B)
                V_(lambda e: e.tensor_copy(out=treps[:], in_=ps[0][:, 0:383]), [ps[0].B], [treps.B])
                V_(lambda e, h=h: e.tensor_copy(out=farb[:, h:h + 1], in_=treps[:, 382:383]), [treps.B], [farb.B])
                P.dma("sync", lambda e, h=h: e.dma_start(out=trep_d[h], in_=treps[:]), treps.B, reads=[treps.B], writes=[drb[h]])
                skew = bass.AP(tensor=trep_d.tensor, offset=h * 128 * 383 + 127, ap=[[382, 128], [1, 256]])
                P.dma("sync", lambda e, h=h, skew=skew: e.dma_start(out=BN[:, h, :], in_=skew), BN.b[h], reads=[drb[h]], writes=[BN.b[h]])
            V_(lambda e: e.tensor_scalar(out=farb[:, 8:16], in0=farb[:, 0:8], scalar1=pbias[:, 0:1], scalar2=None, op0=ALU.add), [farb.B, pbias.B], [farb.B])
            if CUT == 1:
                P.disabled = True
            lq = T(P, st, "lq", [1, 4, 64], F32)
            for j, src in enumerate((lamq1, lamk1, lamq2, lamk2)):
                P.dma("sync", lambda e, j=j, src=src: e.dma_start(out=lq[0:1, j, :], in_=src.rearrange("(a n) -> a n", a=1)), lq.B, writes=[lq.B])
            lt = T(P, st, "lt", [1, 8], F32)
            lpr = T(P, st, "lpr", [1, 2, 64], F32)
            V_(lambda e: e.tensor_tensor(out=lpr[0:1, 0, :], in0=lq[0:1, 0, :], in1=lq[0:1, 1, :], op=ALU.mult), [lq.B], [lpr.B])
            V_(lambda e: e.tensor_tensor(out=lpr[0:1, 1, :], in0=lq[0:1, 2, :], in1=lq[0:1, 3, :], op=ALU.mult), [lq.B], [lpr.B])
            V_(lambda e: e.reduce_sum(out=lt[0:1, 0:2], in_=lpr[0:1, :, :], axis=AX.X), [lpr.B], [lt.B])
            A_(lambda e: e.activation(out=lt[0:1, 2:4], in_=lt[0:1, 0:2], func=AF.Exp), [lt.B], [lt.B])
            V_(lambda e: e.tensor_tensor(out=lt[0:1, 4:5], in0=lt[0:1, 3:4], in1=lt[0:1, 2:3], op=ALU.subtract), [lt.B], [lt.B])
            V_(lambda e: e.tensor_scalar(out=lt[0:1, 5:6], in0=lt[0:1, 4:5], scalar1=-LAMBDA_INIT, scalar2=None, op0=ALU.add), [lt.B], [lt.B])
            ones1 = T(P, st, "ones1", [1, 128], F32)
            V_(lambda e: e.memset(ones1[:], 1.0), [], [ones1.B])
            nlam = T(P, st, "nlam", [128, 1], F32)
            mm(ps[1][:, 0:1], ones1[0:1, :], lt[0:1, 5:6], True, True, [ones1.B, lt.B], ps[1].B)
            V_(lambda e: e.tensor_copy(out=nlam[:], in_=ps[1][:, 0:1]), [ps[1].B], [nlam.B])
            gsub = T(P, st, "gsub", [128, 128], F32)
            P.dma("sync", lambda e: e.dma_start(out=gsub[:], in_=subln_g.partition_broadcast(128)), gsub.B, writes=[gsub.B])
            V_(lambda e: e.tensor_scalar(out=gsub[:], in0=gsub[:], scalar1=1.0 - LAMBDA_INIT, scalar2=None, op0=ALU.mult), [gsub.B], [gsub.B])
            if CUT == 2:
                P.disabled = True

            WQ = [T(P, st, "WQ%d" % i, [128, 16, 128], BF16) for i in range(2)]
            WK = [T(P, st, "WK%d" % i, [128, 16, 128], BF16) for i in range(2)]
            WV = [T(P, st, "WV%d" % i, [128, 16, 128], BF16) for i in range(2)]
            qT = T(P, st, "qT", [128, 1024], BF16)
            kT = T(P, st, "kT", [128, 2048], BF16)
            Va = T(P, st, "Va", [128, 16, 129], BF16)
            V_(lambda e: e.memset(Va[:, :, 128:129], 1.0), [], [Va.B])
            E = [[T(P, st, "E%d%d" % (m, j), [128, 512], BF16) for j in range(2)] for m in range(2)]
            TMP = [T(P, st, "TMPa%d" % m, [128, 256], F32) for m in range(2)]
            rc = T(P, st, "rc", [128, 4], F32)
            o2 = T(P, st, "o2", [128, 128], F32)
            oo = T(P, st, "oo", [128, 128], F32)
            junk = T(P, st, "junk", [128, 128], F32)
            ss = T(P, st, "ss", [128, 2], F32)
            yb = T(P, st, "yb", [128, 128], BF16)

            def load_head(h):
                s = h % 2
                for W_, c0 in ((WQ[s], h * 128), (WK[s], 1024 + h * 128), (WV[s], 2048 + h * 128)):
                    P.dma("gpsimd", lambda e, W_=W_, c0=c0: e.dma_start(out=W_[:], in_=w_in_v[:, :, c0:c0 + 128]), W_.B, writes=[W_.B])

            def accap(a, lo, hi):
                return ps[5 + a // 3][:, (a % 3) * 129 + lo:(a % 3) * 129 + hi]

            load_head(0)
            it = 0
            for h in range(NHEADS):
                if h + 1 < NHEADS:
                    load_head(h + 1)
                s = h % 2
                for tb in (2, 3):
                    proj_fm(WQ[s], 0, 128, tb, ps[tb % 2])
                    A_(lambda e, tb=tb: e.activation(out=qT[:, (tb - 2) * 512:(tb - 1) * 512], in_=ps[tb % 2][:, :], func=AF.Copy), [ps[tb % 2].B], [qT.B])
                for tb in range(4):
                    proj_fm(WK[s], 0, 128, tb, ps[2 + tb % 2])
                    V_(lambda e, tb=tb: e.tensor_copy(out=kT[:, tb * 512:(tb + 1) * 512], in_=ps[2 + tb % 2][:, :]), [ps[2 + tb % 2].B], [kT.B])
                for g4 in range(4):
                    pst = ps[g4 % 2]
                    for j in range(4):
                        i = g4 * 4 + j
                        for kc in range(16):
                            mm(pst[:, j * 128:(j + 1) * 128], hT0[:, kc, i * 128:(i + 1) * 128], WV[s][:, kc, :], kc == 0, kc == 15,
                               [WV[s].B, hT0.b[i]], pst.B)
                    V_(lambda e, g4=g4, pst=pst: e.tensor_copy(out=Va[:, g4 * 4:(g4 + 1) * 4, 0:128], in_=pst[:, :].rearrange("p (j t) -> p j t", t=128)),
                       [pst.B], [Va.B])
                if CUT == 3:
                    P.disabled = True
                for g in range(2):
                    nkb = 8 + 4 * g + 4
                    for kb in range(nkb):
                        t = kb - 8 - 4 * g
                        i0 = max(0, t)
                        N = (4 - i0) * 128
                        q0 = g * 512 + i0 * 128
                        if t >= 0:
                            wn = min(256, N); b0 = 0
                        elif t == -1:
                            wn = 128; b0 = 128
                        else:
                            wn = 0; b0 = 0
                        pref = kb < 8
                        for m in range(2):
                            pS = ps[m * 2 + (it % 2)]
                            Em = E[m][it % 2]
                            mm(pS[:, 0:N], kT[m * 64:(m + 1) * 64, kb * 128:(kb + 1) * 128], qT[m * 64:(m + 1) * 64, q0:q0 + N], True, True,
                               [kT.B, qT.B], pS.B)
                            if wn > 0:
                                V_(lambda e, pS=pS, m=m, wn=wn, b0=b0, h=h: e.scalar_tensor_tensor(out=TMP[m][:, 0:wn], in0=pS[:, 0:wn], scalar=0.125,
                                   in1=BN[:, h, b0:b0 + wn], op0=ALU.mult, op1=ALU.add), [pS.B, BN.b[h]], [TMP[m].B])
                                bcol = pbias[:, 0:1] if pref else EPSC[:, 3:4]
                                A_(lambda e, Em=Em, m=m, wn=wn, bcol=bcol: e.activation(out=Em[:, 0:wn], in_=TMP[m][:, 0:wn], func=AF.Exp, bias=bcol, scale=1.0),
                                   [TMP[m].B, pbias.B, EPSC.B], [Em.B])
                            if N > wn:
                                fcol = farb[:, 8 + h:9 + h] if pref else farb[:, h:h + 1]
                                A_(lambda e, Em=Em, pS=pS, wn=wn, N=N, fcol=fcol: e.activation(out=Em[:, wn:N], in_=pS[:, wn:N], func=AF.Exp, bias=fcol, scale=0.125),
                                   [pS.B, farb.B], [Em.B])
                            for li in range(4 - i0):
                                i = i0 + li
                                a = m * 4 + i
                                mm(accap(a, 0, 129), Em[:, li * 128:(li + 1) * 128], Va[:, kb, :], (kb == 0 and a % 3 == 0), False,
                                   [Em.B, Va.B], ps[5 + a // 3].B, sgc=True)
                        it += 1
                    if CUT == 4:
                        P.disabled = True
                    for i in range(4):
                        a1, a2 = i, 4 + i
                        B1, B2 = ps[5 + a1 // 3].B, ps[5 + a2 // 3].B
                        V_(lambda e, a1=a1: e.reciprocal(out=rc[:, 0:1], in_=accap(a1, 128, 129)), [B1], [rc.B])
                        V_(lambda e, a2=a2: e.reciprocal(out=rc[:, 1:2], in_=accap(a2, 128, 129)), [B2], [rc.B])
                        V_(lambda e: e.tensor_tensor(out=rc[:, 2:3], in0=rc[:, 1:2], in1=nlam[:, 0:1], op=ALU.mult), [rc.B, nlam.B], [rc.B])
                        V_(lambda e, a2=a2: e.tensor_scalar(out=o2[:], in0=accap(a2, 0, 128), scalar1=rc[:, 2:3], scalar2=None, op0=ALU.mult), [B2, rc.B], [o2.B])
                        V_(lambda e, a1=a1: e.scalar_tensor_tensor(out=oo[:], in0=accap(a1, 0, 128), scalar=rc[:, 0:1], in1=o2[:], op0=ALU.mult, op1=ALU.add),
                           [B1, rc.B, o2.B], [oo.B])
                        A_(lambda e: e.activation(out=junk[:], in_=oo[:], func=AF.Square, accum_out=ss[:, 0:1]), [oo.B], [junk.B, ss.B])
                        A_(lambda e: e.activation(out=ss[:, 1:2], in_=ss[:, 0:1], func=AF.Sqrt, bias=EPSC[:, 0:1], scale=1.0 / 128), [ss.B, EPSC.B], [ss.B])
                        V_(lambda e: e.reciprocal(out=ss[:, 1:2], in_=ss[:, 1:2]), [ss.B], [ss.B])
                        V_(lambda e: e.scalar_tensor_tensor(out=yb[:], in0=oo[:], scalar=ss[:, 1:2], in1=gsub[:], op0=ALU.mult, op1=ALU.mult),
                           [oo.B, ss.B, gsub.B], [yb.B])
                        pv = ps[4].t[:].bitcast(BF16)
                        P.op("tensor", lambda e, pv=pv: e.transpose(out=pv[:, 0:128], in_=yb[:], identity=identb), reads=[yb.B, cstb.B], writes=[ps[4].B])
                        c0 = g * 512 + i * 128
                        A_(lambda e, pv=pv, h=h, c0=c0: e.activation(out=yT[:, h, c0:c0 + 128], in_=pv[:, 0:128], func=AF.Copy), [ps[4].B], [yT.b[h]])
            dump(st, 2, yT[:, 0, :], [yT.b[0]], 1024)
            dump(st, 3, yT[:, NHEADS - 1, :], [yT.b[NHEADS - 1]], 1024)
            if stop == 2:
                P.wait_all("sync", dbg_bufs)
            P.flush()
            if stop == 2:
                P.disabled = True

        with ExitStack() as st:
            WR = [T(P, st, "WR0", [128, 16, 128], BF16)] * 2
            WKr = [T(P, st, "WKr0", [128, 16, 128], BF16)] * 2
            WVr = [T(P, st, "WVr0", [128, 16, 128], BF16)] * 2
            prm = T(P, st, "prm", [128, 8, 8], F32)
            for j, src in enumerate((rwkv_k_k, rwkv_k_a, rwkv_a0)):
                small_load(prm[:, :, j], prm.B, src.rearrange("(a p) -> p a", p=128))
            V_(lambda e: e.tensor_scalar(out=prm[:, :, 3], in0=prm[:, :, 1], scalar1=-1.0, scalar2=1.0, op0=ALU.mult, op1=ALU.add), [prm.B], [prm.B])
            w2b = T(P, st, "w2b", [64, 1024], BF16); a2b = T(P, st, "a2b", [64, 1024], BF16); g2b = T(P, st, "g2b", [128, 2, 1024], BF16)
            P.dma("gpsimd", lambda e: e.dma_start(out=w2b[:], in_=rwkv_w2), w2b.B, writes=[w2b.B])
            P.dma("gpsimd", lambda e: e.dma_start(out=a2b[:], in_=rwkv_a2), a2b.B, writes=[a2b.B])
            P.dma("gpsimd", lambda e: e.dma_start(out=g2b[:, 0, :], in_=rwkv_g2[0:128, :]), g2b.B, writes=[g2b.B])
            P.dma("gpsimd", lambda e: e.dma_start(out=g2b[0:32, 1, :], in_=rwkv_g2[128:160, :]), g2b.B, writes=[g2b.B])
            pp3 = T(P, st, "pp3", [128, 3, 128], F32)
            rkf = T(P, st, "rkf", [128, 8], F32)
            small_load(rkf[:], rkf.B, rwkv_r_k.rearrange("(a p) -> p a", p=128))
            RK = T(P, st, "RK", [128, 8, 2], BF16)
            V_(lambda e: e.memset(RK[:], 0.0), [], [RK.B])
            V_(lambda e: e.tensor_copy(out=RK[0:64, :, 0], in_=rkf[0:64, :]), [rkf.B], [RK.B])
            V_(lambda e: e.tensor_copy(out=RK[64:128, :, 1], in_=rkf[64:128, :]), [rkf.B], [RK.B])
            Rr = [T(P, st, "Rr%d" % i, [128, 513], F32) for i in range(3)]
            tmpr = T(P, st, "tmpr", [128, 512], F32)
            rs = T(P, st, "rs", [128, 512], F32); ks = T(P, st, "ks", [128, 512], F32); vsb = T(P, st, "vsb", [128, 512], BF16)
            af = T(P, st, "af", [128, 512], F32); kkk = T(P, st, "kkk", [128, 512], F32); sqb = T(P, st, "sqb", [128, 512], BF16)
            rinv = T(P, st, "rinv", [128, 512], F32); kk = T(P, st, "kk", [128, 512], F32); uu = tmpr
            k2 = T(P, st, "k2", [128, 512], F32); bb_ = T(P, st, "bb", [128, 512], F32)
            k2b = T(P, st, "k2b", [128, 512], BF16); bbb = T(P, st, "bbb", [128, 512], BF16); rk2 = T(P, st, "rk2", [128, 512], BF16)
            lwt2 = [T(P, st, "lwt%d" % i, [128, 128], F32) for i in range(2)]
            ex2 = [T(P, st, "ex%d" % i, [128, 4, 128], F32) for i in range(2)]
            ART2 = [T(P, st, "ART%d" % i, [128, 256], BF16) for i in range(2)]
            BhT2 = [T(P, st, "BhT%d" % i, [128, 128], BF16) for i in range(2)]; KhT2 = [T(P, st, "KhT%d" % i, [128, 128], BF16) for i in range(2)]
            TMt2 = [T(P, st, "TMt%d" % i, [128, 4, 128], BF16) for i in range(2)]
            lwt, ex, ART, BhT, KhT, TMt = lwt2[1], ex2[1], ART2[1], BhT2[1], KhT2[1], TMt2[1]
            MTh = [T(P, st, "MT%d" % h_, [128, 2, 256], BF16) for h_ in range(2)]
            NMh = [[T(P, st, "NM%d%d" % (h_, i), [128, 256], BF16) for i in range(2)] for h_ in range(2)]
            Xfh = [T(P, st, "Xf%d" % h_, [128, 128], F32) for h_ in range(2)]; Xbh = [T(P, st, "Xb%d" % h_, [128, 128], BF16) for h_ in range(2)]
            MT, Xf = MTh[1], Xfh[1]
            pBx = [P.buf("pBx%d" % h_) for h_ in range(2)]; pBn = [P.buf("pBn%d" % h_) for h_ in range(2)]; pBq = [P.buf("pBq%d" % h_) for h_ in range(2)]
            GTs = T(P, st, "GTs", [128, 16, 128], BF16); Hs = T(P, st, "Hs", [128, 16, 64], F32)
            QeTs = T(P, st, "QeTs", [128, 8, 128], BF16); Yls = T(P, st, "Yls", [128, 8, 128], F32)
            Vtm = T(P, st, "Vtm", [128, 8, 128], BF16); sbon = T(P, st, "sbon", [128, 8, 2], F32)
            gtm = T(P, st, "gtm", [128, 8, 128], F32)
            Sb = T(P, st, "Sb", [128, 128], BF16)
            Yt = T(P, st, "Yt", [128, 128], F32); yn = T(P, st, "yn", [128, 128], F32); ybr = T(P, st, "ybr", [128, 128], BF16)
            gst = T(P, st, "gst", [128, 2, 6], F32); gmv = T(P, st, "gmv", [128, 2, 2], F32); grs = T(P, st, "grs", [128, 2], F32)
            ident64 = cst[0:64, IDENT, 0:64]
            V_(lambda e: e.memset(GTs[:], 0.0), [], [GTs.B])

            def load_pair(c):
                s = c % 2
                for W_, c0 in ((WR[s], 3072 + c * 128), (WKr[s], 4096 + c * 128), (WVr[s], 5120 + c * 128)):
                    P.dma("gpsimd", lambda e, W_=W_, c0=c0: e.dma_start(out=W_[:], in_=w_in_v[:, :, c0:c0 + 128]), W_.B, writes=[W_.B])

            if CUT == 11:
                P.disabled = True
            load_pair(0)
            for c in range(NPAIRS):
                s = c % 2
                csl = slice(c * 128, (c + 1) * 128)
                for j3, src3 in enumerate((rwkv_w0, lnx_g, lnx_b)):
                    P.dma("sync", lambda e, j3=j3, src3=src3, c=c: e.dma_start(out=pp3[:, j3, :], in_=src3[c * 128:(c + 1) * 128].partition_broadcast(128)), pp3.B, writes=[pp3.B])
                for tb in range(4):
                    for j, (W_, dst, dstB) in enumerate(((WR[s], rs[:, :], rs.B), (WKr[s], ks[:, :], ks.B), (WVr[s], vsb[:, :], vsb.B))):
                        pst = ps[j % 2]
                        proj_fm(W_, 0, 128, tb, pst)
                        shiftmix(pst, 128, tb, Rr[j], murkv[:, j * 8 + c:j * 8 + c + 1], murkv[:, 24 + j * 8 + c:25 + j * 8 + c], murkv.B, dst, dstB, tmpr)
                    mm(ps[0][:, :], a2b[0:64, csl], xaT[0:64, tb * 512:(tb + 1) * 512], True, True, [a2b.B, xaT.B], ps[0].B)
                    A_(lambda e, c=c: e.activation(out=af[:], in_=ps[0][:, :], func=AF.Sigmoid, bias=prm[:, c, 2:3], scale=1.0), [ps[0].B, prm.B], [af.B])
                    V_(lambda e, c=c: e.tensor_scalar(out=kkk[:], in0=ks[:], scalar1=prm[:, c, 0:1], scalar2=None, op0=ALU.mult), [ks.B, prm.B], [kkk.B])
                    A_(lambda e: e.activation(out=sqb[:], in_=kkk[:], func=AF.Square), [kkk.B], [sqb.B])
                    mm(ps[1][:, :], cstb[:, BLK1, :], sqb[:], True, True, [cstb.B, sqb.B], ps[1].B)
                    A_(lambda e: e.activation(out=rinv[:], in_=ps[1][:, :], func=AF.Sqrt, bias=EPSC[:, 2:3], scale=1.0), [ps[1].B, EPSC.B], [rinv.B])
                    V_(lambda e: e.reciprocal(out=rinv[:], in_=rinv[:]), [rinv.B], [rinv.B])
                    V_(lambda e: e.tensor_tensor(out=kk[:], in0=kkk[:], in1=rinv[:], op=ALU.mult), [kkk.B, rinv.B], [kk.B])
                    V_(lambda e, c=c: e.tensor_scalar(out=uu[:], in0=af[:], scalar1=prm[:, c, 1:2], scalar2=prm[:, c, 3:4], op0=ALU.mult, op1=ALU.add), [af.B, prm.B], [uu.B])
                    V_(lambda e: e.tensor_tensor(out=k2[:], in0=ks[:], in1=uu[:], op=ALU.mult), [ks.B, uu.B], [k2.B])
                    V_(lambda e: e.tensor_tensor(out=bb_[:], in0=kk[:], in1=af[:], op=ALU.mult), [kk.B, af.B], [bb_.B])
                    A_(lambda e: e.activation(out=k2b[:], in_=k2[:], func=AF.Copy), [k2.B], [k2b.B])
                    A_(lambda e: e.activation(out=bbb[:], in_=bb_[:], func=AF.Copy), [bb_.B], [bbb.B])
                    V_(lambda e: e.tensor_tensor(out=rk2[:], in0=rs[:], in1=k2[:], op=ALU.mult), [rs.B, k2.B], [rk2.B])
                    if CUT == 12:
                        P.disabled = True
                    def prologue(ch, q4, bs):
                        own = ch >= 8
                        tsl = slice(q4 * 128, (q4 + 1) * 128)
                        gsl = slice(ch * 128, (ch + 1) * 128)
                        lwt, ex, ART, BhT, KhT, TMt = lwt2[bs], ex2[bs], ART2[bs], BhT2[bs], KhT2[bs], TMt2[bs]
                        pz = ps[2]
                        mm(pz[:, 0:128], txw[0:64, gsl], w2b[0:64, csl], True, True, [txw.B, w2b.B], pz.B)
                        yield
                        V_(lambda e: e.tensor_tensor(out=lwt[:], in0=pz[:, 0:128], in1=pp3[:, 0, :], op=ALU.add), [pz.B, pp3.B], [lwt.B])
                        A_(lambda e: e.activation(out=lwt[:], in_=lwt[:], func=AF.Sigmoid), [lwt.B], [lwt.B])
                        yield
                        mm(pz[:, 128:384], lwt[:], cst[:, TRI_I:TRI_E + 1, :].rearrange("p a b -> p (a b)"), True, True, [lwt.B, cst.B], pz.B)
                        mm(pz[:, 384:512], cst[:, TRI_R, :], lwt[:], True, True, [lwt.B, cst.B], pz.B)
                        yield
                        A_(lambda e: e.activation(out=ex[:, 0:2, :].rearrange("p a b -> p (a b)"), in_=pz[:, 128:384], func=AF.Exp), [pz.B], [ex.B])
                        A_(lambda e: e.activation(out=ex[:, 2, :], in_=pz[:, 128:256], func=AF.Exp, scale=-1.0), [pz.B], [ex.B])
                        A_(lambda e: e.activation(out=ex[:, 3, :], in_=pz[:, 384:512], func=AF.Exp), [pz.B], [ex.B])
                        yield
                        V_(lambda e: e.scalar_tensor_tensor(out=ART[:, 0:128], in0=kk[:, tsl], scalar=-1.0, in1=ex[:, 1, :], op0=ALU.mult, op1=ALU.mult), [kk.B, ex.B], [ART.B])
                        V_(lambda e: e.tensor_tensor(out=ART[:, 128:256], in0=rs[:, tsl], in1=ex[:, 0, :], op=ALU.mult), [rs.B, ex.B], [ART.B])
                        V_(lambda e: e.tensor_tensor(out=BhT[:], in0=bb_[:, tsl], in1=ex[:, 2, :], op=ALU.mult), [bb_.B, ex.B], [BhT.B])
                        V_(lambda e: e.tensor_tensor(out=KhT[:], in0=k2[:, tsl], in1=ex[:, 2, :], op=ALU.mult), [k2.B, ex.B], [KhT.B])
                        yield
                        pt = ps[3]
                        ptv = pt.t[:].bitcast(BF16)
                        for j, (src, sB) in enumerate(((bbb[:, tsl], bbb.B), (k2b[:, tsl], k2b.B), (vsb[:, tsl], vsb.B), (ART[:, 0:128], ART.B))):
                            P.op("tensor", lambda e, j=j, src=src, ptv=ptv: e.transpose(out=ptv[:, j * 128:(j + 1) * 128], in_=src, identity=identb), reads=[sB, cstb.B], writes=[pt.B])
                        yield
                        V_(lambda e: e.tensor_tensor(out=TMt[:, 0:2, :], in0=ptv[:, 0:256].rearrange("p (a b) -> p a b", b=128),
                                                     in1=ex[:, 3:4, :].to_broadcast([128, 2, 128]), op=ALU.mult), [pt.B, ex.B], [TMt.B])
                        A_(lambda e: e.activation(out=TMt[:, 2:4, :].rearrange("p a b -> p (a b)"), in_=ptv[:, 256:512], func=AF.Copy), [pt.B], [TMt.B])
                        if own:
                            oc = ch - 8
                            yield
                            A_(lambda e: e.activation(out=Vtm[:, oc, :], in_=TMt[:, 2, :], func=AF.Copy), [TMt.B], [Vtm.B])
                            mm(ps[3][:, 256:258], rk2[:, tsl], RK[:, c, :], True, True, [rk2.B, RK.B], ps[3].B)
                            mm(ps[3][:, 384:512], sxa[:, gsl], g2b[:, 0, csl], True, False, [sxa.B, g2b.B], ps[3].B)
                            mm(ps[3][:, 384:512], sxb[0:32, gsl], g2b[0:32, 1, csl], False, True, [sxb.B, g2b.B], ps[3].B)
                            yield
                            V_(lambda e: e.tensor_copy(out=sbon[:, oc, :], in_=ps[3][:, 256:258]), [ps[3].B], [sbon.B])
                            V_(lambda e: e.tensor_copy(out=gtm[:, oc, :], in_=ps[3][:, 384:512]), [ps[3].B], [gtm.B])

                    def chain(ch, hh, own, tsl, bs):
                        hp = slice(hh * 64, (hh + 1) * 64)
                        tpk = (hh * 64, 0)
                        tpo = (0, hh * 64)
                        ex, ART, BhT, KhT, TMt = ex2[bs], ART2[bs], BhT2[bs], KhT2[bs], TMt2[bs]
                        pA, pB = ps[4 + hh], ps[6 + hh]
                        bX = bN = bQ = pB.B
                        NM, Xf, Xb, MT = NMh[hh], Xfh[hh], Xbh[hh], MTh[hh]
                        mm(pA[:, 0:256], BhT[hp, :], ART[hp, :], True, True, [BhT.B, ART.B], pA.B, tpk)
                        mm(pA[:, 256:512], KhT[hp, :], ART[hp, :], True, True, [KhT.B, ART.B], pA.B, tpk)
                        mm(pB[:, 384:512], ART[hp, 0:128], BhT[hp, :], True, True, [BhT.B, ART.B], bQ, tpk)
                        yield
                        V_(lambda e: e.tensor_tensor(out=MT[:, :, :], in0=pA[:, :].rearrange("p (a b) -> p a b", b=256),
                                                     in1=cst[:, UT_S:UT_I + 1, :].rearrange("p a b -> p (a b)").unsqueeze(1).to_broadcast([128, 2, 256]), op=ALU.mult),
                           [pA.B, cst.B], [MT.B])
                        V_(lambda e: e.tensor_tensor(out=NM[0][:, 0:128], in0=pA[:, 0:128], in1=cst[:, UT_S, :], op=ALU.mult), [pA.B, cst.B], [NM[0].B])
                        V_(lambda e: e.tensor_tensor(out=NM[0][:, 128:256], in0=pB[:, 384:512], in1=cst[:, LT_S, :], op=ALU.mult), [bQ, cst.B], [NM[0].B])
                        A_(lambda e: e.activation(out=Xb[:, 0:64], in_=TMt[:, 3, hp], func=AF.Copy), [TMt.B], [Xb.B])
                        A_(lambda e: e.activation(out=Xf[:, 0:64], in_=TMt[:, 3, hp], func=AF.Copy), [TMt.B], [Xf.B])
                        yield
                        mm(pA[:, 0:64], MT[:, 1, 0:128], TMt[:, 2, hp], True, True, [MT.B, TMt.B], pA.B)
                        yield
                        V_(lambda e: e.tensor_copy(out=Xb[:, 64:128], in_=pA[:, 0:64]), [pA.B], [Xb.B])
                        V_(lambda e: e.tensor_copy(out=Xf[:, 64:128], in_=pA[:, 0:64]), [pA.B], [Xf.B])
                        yield
                        for lv in range(7):
                            cur, nxt = NM[lv % 2], NM[(lv + 1) % 2]
                            mm(pB[:, 0:128], cur[:, 0:128], Xb[:], True, True, [cur.B, Xb.B], bX)
                            if lv < 6:
                                mm(pB[:, 128:256], cur[:, 128:256], cur[:, 0:128], True, True, [cur.B], bN)
                                mm(pB[:, 256:384], cur[:, 0:128], cur[:, 128:256], True, True, [cur.B], bN)
                            yield
                            V_(lambda e: e.tensor_tensor(out=Xb[:], in0=Xf[:], in1=pB[:, 0:128], op=ALU.add), [Xf.B, bX], [Xb.B])
                            if lv < 6:
                                V_(lambda e, nxt=nxt: e.tensor_copy(out=nxt[:], in_=pB[:, 128:384]), [bN], [nxt.B])
                                V_(lambda e: e.tensor_tensor(out=Xf[:], in0=Xf[:], in1=pB[:, 0:128], op=ALU.add), [Xf.B, bX], [Xf.B])
                            yield
                        mm(pA[hp, 64:128], Xb[:, 0:64], TMt[:, 0, hp], True, True, [Xb.B, TMt.B], pA.B, tpo)
                        mm(pA[hp, 128:192], TMt[:, 0, hp], Xb[:, 64:128], True, False, [Xb.B, TMt.B], pA.B, tpo)
                        mm(pA[hp, 128:192], TMt[:, 1, hp], TMt[:, 2, hp], False, True, [TMt.B], pA.B, tpo)
                        if own:
                            mm(pB[hp, 384:512], Xb[:, 0:64], MT[:, 0, 128:256], True, True, [Xb.B, MT.B], bQ, tpo)
                            mm(pA[:, 192:256], MT[:, 0, 128:256], Xb[:, 64:128], True, False, [Xb.B, MT.B], pA.B)
                            mm(pA[:, 192:256], MT[:, 1, 128:256], TMt[:, 2, hp], False, True, [TMt.B, MT.B], pA.B)
                        yield
                        V_(lambda e: e.scalar_tensor_tensor(out=GTs[hp, ch, hh * 64:(hh + 1) * 64], in0=cst[hp, IDENT, hh * 64:(hh + 1) * 64], scalar=ex[hp, 0, 127:128],
                                                            in1=pA[hp, 64:128], op0=ALU.mult, op1=ALU.add), [cst.B, ex.B, pA.B], [GTs.B])
                        V_(lambda e: e.tensor_copy(out=Hs[hp, ch, :], in_=pA[hp, 128:192]), [pA.B], [Hs.B])
                        if own:
                            oc = ch - 8
                            V_(lambda e: e.tensor_tensor(out=QeTs[hp, oc, :], in0=pB[hp, 384:512], in1=ART[hp, 128:256], op=ALU.add),
                               [bQ, ART.B], [QeTs.B])
                            V_(lambda e: e.tensor_copy(out=Yls[:, oc, hp], in_=pA[:, 192:256]), [pA.B], [Yls.B])

                    def run_rr(gens):
                        while gens:
                            for g_ in list(gens):
                                try:
                                    next(g_)
                                except StopIteration:
                                    gens.remove(g_)

                    run_rr([prologue(tb * 4, 0, 0)])
                    for q4 in range(4):
                        ch = tb * 4 + q4
                        own = ch >= 8
                        tsl = slice(q4 * 128, (q4 + 1) * 128)
                        gens = [chain(ch, 0, own, tsl, q4 % 2), chain(ch, 1, own, tsl, q4 % 2)]
                        if q4 < 3:
                            gens.append(prologue(ch + 1, q4 + 1, (q4 + 1) % 2))
                        run_rr(gens)
                if c + 1 < NPAIRS:
                    load_pair(c + 1)
                if CUT == 17:
                    P.disabled = True
                V_(lambda e: e.memset(Sb[:], 0.0), [], [Sb.B])
                for ch in range(16):
                    if CUT == 19 and ch == 8:
                        P.disabled = True
                    if ch >= 8:
                        oc = ch - 8
                        mm(ps[0][:, 0:128], QeTs[:, oc, :], Sb[:, :], True, True, [QeTs.B, Sb.B], ps[0].B)
                        V_(lambda e, oc=oc: e.tensor_tensor(out=Yt[:], in0=ps[0][:, 0:128], in1=Yls[:, oc, :], op=ALU.add), [ps[0].B, Yls.B], [Yt.B])
                    if CUT == 20 and ch == 8:
                        P.disabled = True
                    if ch < 15:
                        mm(ps[1][:, 0:128], GTs[:, ch, :], Sb[:, :], True, True, [GTs.B, Sb.B], ps[1].B)
                        for hh in range(2):
                            hp = slice(hh * 64, (hh + 1) * 64)
                            V_(lambda e, ch=ch, hp=hp: e.tensor_tensor(out=Sb[hp, hp], in0=ps[1][hp, hp], in1=Hs[hp, ch, :], op=ALU.add), [ps[1].B, Hs.B], [Sb.B])
                    if CUT == 21 and ch == 8:
                        P.disabled = True
                    if ch >= 8:
                        for hh in range(2):
                            V_(lambda e, hh=hh: e.bn_stats(out=gst[:, hh, :], in_=Yt[:, hh * 64:(hh + 1) * 64]), [Yt.B], [gst.B])
                            V_(lambda e, hh=hh: e.bn_aggr(out=gmv[:, hh, :], in_=gst[:, hh, :]), [gst.B], [gmv.B])
                        A_(lambda e: e.activation(out=grs[:], in_=gmv[:, :, 1], func=AF.Sqrt, bias=EPSC[:, 1:2], scale=1.0), [gmv.B, EPSC.B], [grs.B])
                        V_(lambda e: e.reciprocal(out=grs[:], in_=grs[:]), [grs.B], [grs.B])
                        for hh in range(2):
                            hs_ = slice(hh * 64, (hh + 1) * 64)
                            V_(lambda e, hh=hh, hs_=hs_: e.tensor_scalar(out=yn[:, hs_], in0=Yt[:, hs_], scalar1=gmv[:, hh, 0:1], scalar2=grs[:, hh:hh + 1],
                                                                       op0=ALU.subtract, op1=ALU.mult), [Yt.B, gmv.B, grs.B], [yn.B])
                        V_(lambda e, c=c: e.tensor_tensor(out=yn[:], in0=yn[:], in1=pp3[:, 1, :], op=ALU.mult), [yn.B, pp3.B], [yn.B])
                        V_(lambda e, c=c: e.tensor_tensor(out=yn[:], in0=yn[:], in1=pp3[:, 2, :], op=ALU.add), [yn.B, pp3.B], [yn.B])
                        for hh in range(2):
                            hs_ = slice(hh * 64, (hh + 1) * 64)
                            V_(lambda e, hh=hh, hs_=hs_, oc=oc: e.scalar_tensor_tensor(out=yn[:, hs_], in0=Vtm[:, oc, hs_], scalar=sbon[:, oc, hh:hh + 1], in1=yn[:, hs_],
                                                                                      op0=ALU.mult, op1=ALU.add), [Vtm.B, sbon.B, yn.B], [yn.B])
                        V_(lambda e, oc=oc: e.tensor_tensor(out=ybr[:], in0=yn[:], in1=gtm[:, oc, :], op=ALU.mult), [yn.B, gtm.B], [ybr.B])
                        if CUT == 22 and ch == 8:
                            P.disabled = True
                        pv = ps[3].t[:].bitcast(BF16)
                        P.op("tensor", lambda e, pv=pv: e.transpose(out=pv[:, 0:128], in_=ybr[:], identity=identb), reads=[ybr.B, cstb.B], writes=[ps[3].B])
                        if CUT == 23 and ch == 8:
                            P.disabled = True
                        A_(lambda e, pv=pv, c=c, oc=oc: e.activation(out=yT[:, 8 + c, oc * 128:(oc + 1) * 128], in_=pv[:, 0:128], func=AF.Copy), [ps[3].B], [yT.b[8 + c]])
            if CUT == 25:
                P.disabled = True
            if dbg and stop == 3 and CUT == 77:
                d6 = T(P, st, "d6", [128, 1024], F32)
                def dcp(c0, w, ap, bufs):
                    V_(lambda e: e.tensor_copy(out=d6[:, c0:c0 + w], in_=ap), bufs, [d6.B])
                def dsend(sl_):
                    P.dma("sync", lambda e: e.dma_start(out=dbg_d[sl_], in_=d6[:]), d6.B, reads=[d6.B])
                V_(lambda e: e.memset(d6[:], 0.0), [], [d6.B])
                dcp(0, 512, ex[:].rearrange("p a b -> p (a b)"), [ex.B]); dcp(512, 128, Xf[:], [Xf.B]); dcp(640, 128, lwt[:], [lwt.B])
                dcp(768, 128, Yt[:], [Yt.B]); dcp(896, 128, yn[:], [yn.B])
                dsend(6)
                dcp(0, 256, ART[:], [ART.B]); dcp(256, 128, BhT[:], [BhT.B]); dcp(384, 128, KhT[:], [KhT.B])
                dcp(512, 512, TMt[:].rearrange("p a b -> p (a b)"), [TMt.B])
                dsend(7)
                dcp(0, 512, MT[:].rearrange("p a b -> p (a b)"), [MT.B]); dcp(512, 64, GTs[:, 15, 0:64], [GTs.B]); dcp(576, 64, Hs[:, 15, :], [Hs.B])
                dcp(640, 128, QeTs[:, 7, :], [QeTs.B]); dcp(768, 128, Yls[:, 7, :], [Yls.B]); dcp(896, 64, Sb[:, 0:64], [Sb.B])
                dcp(960, 2, sbon[:, 7, :], [sbon.B])
                dsend(8)
                dbg_bufs.append(d6.B)
            dump(st, 4, yT[:, 8, 0:512], [yT.b[8]], 512)
            if not (dbg and stop == 3):
                dump(st, 5, yT[:, 7 + NPAIRS, :], [yT.b[7 + NPAIRS]], 1024)
            if stop == 3:
                P.wait_all("sync", dbg_bufs)
            P.flush()
            if stop == 3:
                P.disabled = True

        stA.close()
        with ExitStack() as st:
            acc = T(P, st, "acc", [128, 8, D], F32, nb=8)
            wt_ = T(P, st, "wt", [128, 8, 32], F32)
            accB = acc.b
            gb2 = [None, None]
            ws = [None, None, None]

            class AccT:
                def __init__(self, i):
                    self.i = i; self.B = accB[i]

                def __getitem__(self, k):
                    return acc[:, self.i, :] if k == slice(None) else acc[:, self.i, k[1]]

            def load_gb(gs, bs):
                P.dma("sync", lambda e: e.dma_start(out=gb2[0][:], in_=gs.partition_broadcast(128)), gb2[0].B, writes=[gb2[0].B])
                if bs is not None:
                    P.dma("sync", lambda e: e.dma_start(out=gb2[1][:], in_=bs.partition_broadcast(128)), gb2[1].B, writes=[gb2[1].B])

            with ExitStack() as st2:
                gb2[0] = T(P, st2, "gb2a0", [128, D], F32); gb2[1] = T(P, st2, "gb2a1", [128, D], F32)
                for i in range(3):
                    ws[i] = T(P, st2, "wsa%d" % i, [128, 16, 512], BF16)
                xt = [T(P, st2, "xt4%d" % i, [128, D], F32) for i in range(2)]
                xn = T(P, st2, "xn4", [128, D], F32)
                stt4 = T(P, st2, "stt4", [128, 4, 6], F32); mv4 = T(P, st2, "mv4", [128, 4], F32)
                load_gb(ln_in_g, ln_in_b)
                for i in range(8):
                    X = xt[i % 2]
                    P.dma("sync", lambda e, X=X, i=i: e.dma_start(out=X[:], in_=xs[(8 + i) * 128:(9 + i) * 128, :]), X.B, writes=[X.B])
                    ln_tile(P, X, xn, stt4, mv4, gb2[0], gb2[1], None, AccT(i))
                    A_(lambda e, i=i: e.activation(out=acc[:, i, :], in_=acc[:, i, :], func=AF.Copy, scale=ALPHA), [accB[i]], [accB[i]])
                w_out_v = w_out.rearrange("(kc p) n -> p kc n", p=128)
                for n in range(4):
                    W_ = ws[n % 3]
                    P.dma("gpsimd", lambda e, W_=W_, n=n: e.dma_start(out=W_[:], in_=w_out_v[:, :, n * 512:(n + 1) * 512]), W_.B, writes=[W_.B])
                    for i in range(8):
                        pst = ps[i % 2]
                        for kc in range(16):
                            mm(pst[:, :], yT[:, kc, i * 128:(i + 1) * 128], W_[:, kc, :], kc == 0, kc == 15, [yT.b[kc], W_.B], pst.B)
                        V_(lambda e, i=i, n=n, pst=pst: e.tensor_tensor(out=acc[:, i, n * 512:(n + 1) * 512], in0=acc[:, i, n * 512:(n + 1) * 512], in1=pst[:, :], op=ALU.add),
                           [accB[i], pst.B], [accB[i]])
                dump(st2, 6, acc[:, 0, 0:1024], [accB[0]], 1024)
                if stop == 4:
                    P.wait_all("sync", dbg_bufs)
                P.flush()
                if stop == 4:
                    P.disabled = True
            hT1 = T(P, st, "hT1", [128, 16, 1024], BF16, nb=8)
            with ExitStack() as st2:
                gb2[0] = T(P, st2, "gb2b0", [128, D], F32); gb2[1] = T(P, st2, "gb2b1", [128, D], F32)
                xn = T(P, st2, "xn4b", [128, D], F32)
                hb4 = T(P, st2, "hb4", [128, D], BF16)
                hlo = T(P, st2, "hlo", [128, D], BF16)
                stt4 = T(P, st2, "stt4b", [128, 4, 6], F32); mv4 = T(P, st2, "mv4b", [128, 4], F32)
                hT1l = T(P, st2, "hT1l", [128, 16, 128], BF16)
                load_gb(ln1_g, ln1_b)
                rwf = T(P, st2, "rwf", [128, 16, 36], F32); rwh = T(P, st2, "rwh", [128, 16, 36], BF16); rwl = T(P, st2, "rwl", [128, 16, 36], BF16)
                rbias = T(P, st2, "rbias", [128, 36], F32)
                P.dma("sync", lambda e: e.dma_start(out=rwf[:, :, 0:4], in_=rgw.rearrange("(kc p) n -> p kc n", p=128)), rwf.B, writes=[rwf.B])
                P.dma("sync", lambda e: e.dma_start(out=rwf[:, :, 4:36], in_=rew.rearrange("(kc p) n -> p kc n", p=128)), rwf.B, writes=[rwf.B])
                P.dma("sync", lambda e: e.dma_start(out=rbias[:, 0:4], in_=rgb.partition_broadcast(128)), rbias.B, writes=[rbias.B])
                P.dma("sync", lambda e: e.dma_start(out=rbias[:, 4:36], in_=reb.partition_broadcast(128)), rbias.B, writes=[rbias.B])
                V_(lambda e: e.tensor_copy(out=rwh[:], in_=rwf[:]), [rwf.B], [rwh.B])
                V_(lambda e: e.tensor_tensor(out=rwf[:], in0=rwf[:], in1=rwh[:], op=ALU.subtract), [rwf.B, rwh.B], [rwf.B])
                V_(lambda e: e.tensor_copy(out=rwl[:], in_=rwf[:]), [rwf.B], [rwl.B])
                L = T(P, st2, "L", [128, 36], F32); r1 = T(P, st2, "r1", [128, 8], F32)
                gm = T(P, st2, "gm", [128, 4], F32); elm = T(P, st2, "elm", [128, 32], F32); ee = T(P, st2, "ee", [128, 32], F32)
                m1 = T(P, st2, "m1", [128, 32], F32); e2 = T(P, st2, "e2", [128, 32], F32); m2 = T(P, st2, "m2", [128, 32], F32)
                for i in range(8):
                    A = AccT(i)
                    ln_tile(P, A, xn, stt4, mv4, gb2[0], gb2[1], hb4, A)
                    transpose_to_fm(P, hb4, hT1, i, ps, identb, cstb.B, pbase=2)
                    V_(lambda e, i=i: e.tensor_tensor(out=xn[:], in0=acc[:, i, :], in1=hb4[:], op=ALU.subtract), [accB[i], hb4.B], [xn.B])
                    A_(lambda e: e.activation(out=hlo[:], in_=xn[:], func=AF.Copy), [xn.B], [hlo.B])
                    transpose_to_fm(P, hlo, hT1l, 0, ps, identb, cstb.B, pbase=4)
                    tsl = slice(i * 128, (i + 1) * 128)
                    n_mm = 0
                    for (hT_, w_, sl_, bi_) in ((hT1, rwh, tsl, i), (hT1l, rwh, slice(0, 128), 0), (hT1, rwl, tsl, i)):
                        for kc in range(16):
                            mm(ps[6][:, 0:36], hT_[:, kc, sl_], w_[:, kc, :], n_mm == 0, n_mm == 47, [hT_.b[bi_], w_.B], ps[6].B)
                            n_mm += 1
                    V_(lambda e: e.tensor_tensor(out=L[:], in0=ps[6][:, 0:36], in1=rbias[:], op=ALU.add), [ps[6].B, rbias.B], [L.B])
                    V_(lambda e: e.reduce_max(out=r1[:, 0:1], in_=L[:, 0:4], axis=AX.X), [L.B], [r1.B])
                    V_(lambda e: e.tensor_scalar(out=gm[:], in0=L[:, 0:4], scalar1=r1[:, 0:1], scalar2=None, op0=ALU.is_ge), [L.B, r1.B], [gm.B])
                    V_(lambda e: e.tensor_scalar(out=r1[:, 1:2], in0=r1[:, 0:1], scalar1=-1.0, scalar2=None, op0=ALU.mult), [r1.B], [r1.B])
                    A_(lambda e: e.activation(out=ee[:, 0:4], in_=L[:, 0:4], func=AF.Exp, bias=r1[:, 1:2], scale=1.0, accum_out=r1[:, 2:3]), [L.B, r1.B], [ee.B, r1.B])
                    V_(lambda e: e.reciprocal(out=r1[:, 3:4], in_=r1[:, 2:3]), [r1.B], [r1.B])
                    V_(lambda e: e.tensor_scalar(out=gm[:], in0=gm[:], scalar1=-1.0, scalar2=-NEG, op0=ALU.add, op1=ALU.mult), [gm.B], [gm.B])
                    V_(lambda e: e.tensor_tensor(out=elm[:].rearrange("p (g e) -> p g e", e=8), in0=L[:, 4:36].rearrange("p (g e) -> p g e", e=8),
                                                 in1=gm[:].unsqueeze(2).to_broadcast([128, 4, 8]), op=ALU.add), [L.B, gm.B], [elm.B])
                    V_(lambda e: e.reduce_max(out=r1[:, 4:5], in_=elm[:], axis=AX.X), [elm.B], [r1.B])
                    V_(lambda e: e.tensor_scalar(out=r1[:, 5:6], in0=r1[:, 4:5], scalar1=-1.0, scalar2=None, op0=ALU.mult), [r1.B], [r1.B])
                    A_(lambda e: e.activation(out=ee[:], in_=elm[:], func=AF.Exp, bias=r1[:, 5:6], scale=1.0), [elm.B, r1.B], [ee.B])
                    V_(lambda e: e.tensor_scalar(out=m1[:], in0=elm[:], scalar1=r1[:, 4:5], scalar2=None, op0=ALU.is_ge), [elm.B, r1.B], [m1.B])
                    V_(lambda e: e.tensor_tensor(out=e2[:], in0=ee[:], in1=m1[:], op=ALU.mult), [ee.B, m1.B], [e2.B])
                    V_(lambda e: e.tensor_tensor(out=e2[:], in0=ee[:], in1=e2[:], op=ALU.subtract), [ee.B, e2.B], [e2.B])
                    V_(lambda e: e.reduce_max(out=r1[:, 6:7], in_=e2[:], axis=AX.X), [e2.B], [r1.B])
                    V_(lambda e: e.tensor_scalar(out=m2[:], in0=e2[:], scalar1=r1[:, 6:7], scalar2=None, op0=ALU.is_ge), [e2.B, r1.B], [m2.B])
                    V_(lambda e: e.tensor_tensor(out=m2[:], in0=m2[:], in1=e2[:], op=ALU.mult), [m2.B, e2.B], [m2.B])
                    V_(lambda e: e.tensor_tensor(out=m1[:], in0=m1[:], in1=m2[:], op=ALU.add), [m1.B, m2.B], [m1.B])
                    V_(lambda e: e.tensor_scalar(out=r1[:, 7:8], in0=r1[:, 6:7], scalar1=1.0, scalar2=None, op0=ALU.add), [r1.B], [r1.B])
                    V_(lambda e: e.reciprocal(out=r1[:, 7:8], in_=r1[:, 7:8]), [r1.B], [r1.B])
                    V_(lambda e, i=i: e.tensor_scalar(out=wt_[:, i, :], in0=m1[:], scalar1=r1[:, 7:8], scalar2=r1[:, 3:4], op0=ALU.mult, op1=ALU.mult), [m1.B, r1.B], [wt_.B])
                    A_(lambda e, i=i: e.activation(out=acc[:, i, :], in_=acc[:, i, :], func=AF.Copy, scale=ALPHA), [accB[i]], [accB[i]])
                dump(st2, 7, wt_[:, 0, :], [wt_.B], 32)
                if stop == 5:
                    P.wait_all("sync", dbg_bufs)
                P.flush()
                if stop == 5:
                    P.disabled = True
            with ExitStack() as st2:
                for i in range(3):
                    ws[i] = T(P, st2, "wsc%d" % i, [128, 16, 512], BF16)
                hid = [T(P, st2, "hid0", [128, 4, 1024], BF16)] * 2
                sg = [T(P, st2, "sg%d" % i, [128, 512], BF16) for i in range(2)]
                nld = 0
                for ex_ in range(NEXP):
                    Wg, Wu, Wd = ws[0], ws[1], ws[2]
                    P.dma("gpsimd", lambda e, ex_=ex_: e.dma_start(out=ws[0][:], in_=wg_d[ex_].rearrange("(kc p) n -> p kc n", p=128)), ws[0].B, writes=[ws[0].B])
                    P.dma("gpsimd", lambda e, ex_=ex_: e.dma_start(out=ws[1][:], in_=wu_d[ex_].rearrange("(kc p) n -> p kc n", p=128)), ws[1].B, writes=[ws[1].B])
                    P.dma("gpsimd", lambda e, ex_=ex_: e.dma_start(out=ws[2][:].rearrange("p (a b) n -> p a b n", b=4), in_=wd_d[ex_].rearrange("(fc p) (b n) -> p fc b n", p=128, n=512)),
                          ws[2].B, writes=[ws[2].B])
                    H_ = hid[ex_ % 2]
                    k_ = 0
                    for tb in range(2):
                        for fc in range(4):
                            pg, pu = ps[(k_ % 2) * 2], ps[(k_ % 2) * 2 + 1]
                            S_ = sg[k_ % 2]
                            for kc in range(16):
                                mm(pg[:, :], Wg[:, kc, fc * 128:(fc + 1) * 128], hT1[:, kc, tb * 512:(tb + 1) * 512], kc == 0, kc == 15, [Wg.B] + hT1.b[tb * 4:(tb + 1) * 4], pg.B)
                            for kc in range(16):
                                mm(pu[:, :], Wu[:, kc, fc * 128:(fc + 1) * 128], hT1[:, kc, tb * 512:(tb + 1) * 512], kc == 0, kc == 15, [Wu.B] + hT1.b[tb * 4:(tb + 1) * 4], pu.B)
                            A_(lambda e, S_=S_, pg=pg: e.activation(out=S_[:], in_=pg[:, :], func=AF.Silu), [pg.B], [S_.B])
                            V_(lambda e, S_=S_, pu=pu, H_=H_, fc=fc, tb=tb: e.tensor_tensor(out=H_[:, fc, tb * 512:(tb + 1) * 512], in0=S_[:], in1=pu[:, :], op=ALU.mult),
                               [S_.B, pu.B], [H_.B])
                            k_ += 1
                    for i in range(8):
                        for n in range(4):
                            po = ps[4 + (i * 4 + n) % 4]
                            for fc in range(4):
                                mm(po[:, :], H_[:, fc, i * 128:(i + 1) * 128], Wd[:, fc * 4 + n, :], fc == 0, fc == 3, [H_.B, Wd.B], po.B)
                            V_(lambda e, i=i, n=n, po=po, ex_=ex_: e.scalar_tensor_tensor(out=acc[:, i, n * 512:(n + 1) * 512], in0=po[:, :], scalar=wt_[:, i, ex_:ex_ + 1],
                                                                                  in1=acc[:, i, n * 512:(n + 1) * 512], op0=ALU.mult, op1=ALU.add), [po.B, wt_.B, accB[i]], [accB[i]])
                dump(st2, 8, acc[:, 0, 0:1024], [accB[0]], 1024)
                if stop == 6:
                    P.wait_all("sync", dbg_bufs)
                P.flush()
                if stop == 6:
                    P.disabled = True
            with ExitStack() as st2:
                gb2[0] = T(P, st2, "gb2d0", [128, D], F32); gb2[1] = T(P, st2, "gb2d1", [128, D], F32)
                ws[0] = T(P, st2, "wsd0", [128, 16, 512], BF16)
                xn = T(P, st2, "xn5", [128, D], F32); hb4 = T(P, st2, "hb5", [128, D], BF16)
                stt4 = T(P, st2, "stt5", [128, 4, 6], F32); mv4 = T(P, st2, "mv5", [128, 4], F32)
                pf = T(P, st2, "pf", [128, 256], F32); pb = T(P, st2, "pb", [128, 256], BF16)
                pT = T(P, st2, "pT", [128, 2, 1024], BF16, nb=8)
                plw = T(P, st2, "plw", [128, 2, D], BF16)
                ssq = T(P, st2, "ssq", [128, 8, 4], F32); rsp = T(P, st2, "rsp", [128, 8], F32)
                sgt = T(P, st2, "sgt", [128, 512], F32); t1_ = T(P, st2, "t1", [128, 512], F32)
                load_gb(ln2_g, ln2_b)
                P.dma("gpsimd", lambda e: e.dma_start(out=plw[:], in_=ple_w.rearrange("(kc p) n -> p kc n", p=128)), plw.B, writes=[plw.B])
                if CUT == 31:
                    P.disabled = True
                for i in range(8):
                    A = AccT(i)
                    ln_tile(P, A, xn, stt4, mv4, gb2[0], gb2[1], hb4, A)
                    transpose_to_fm(P, hb4, hT1, i, ps, identb, cstb.B, pbase=2)
                    P.dma("sync", lambda e, i=i: e.dma_start(out=pf[:], in_=p_in[i * 128:(i + 1) * 128, :]), pf.B, writes=[pf.B])
                    V_(lambda e: e.tensor_copy(out=pb[:], in_=pf[:]), [pf.B], [pb.B])
                    pv = ps[4].t[:].bitcast(BF16)
                    for k in range(2):
                        P.op("tensor", lambda e, k=k, pv=pv: e.transpose(out=pv[:, k * 128:(k + 1) * 128], in_=pb[:, k * 128:(k + 1) * 128], identity=identb), reads=[pb.B, cstb.B], writes=[ps[4].B])
                    V_(lambda e, i=i, pv=pv: e.tensor_copy(out=pT[:, :, i * 128:(i + 1) * 128], in_=pv[:, 0:256].rearrange("p (k t) -> p k t", t=128)), [ps[4].B], [pT.b[i]])
                    for n in range(4):
                        pp = ps[5 + n % 2]
                        for k in range(2):
                            mm(pp[:, :], pT[:, k, i * 128:(i + 1) * 128], plw[:, k, n * 512:(n + 1) * 512], k == 0, k == 1, [pT.b[i], plw.B], pp.B)
                        A_(lambda e, i=i, n=n, pp=pp: e.activation(out=t1_[:], in_=pp[:, :], func=AF.Square, accum_out=ssq[:, i, n:n + 1]), [pp.B], [t1_.B, ssq.B])
                if CUT == 32:
                    P.disabled = True
                V_(lambda e: e.reduce_sum(out=rsp[:], in_=ssq[:], axis=AX.X), [ssq.B], [rsp.B])
                A_(lambda e: e.activation(out=rsp[:], in_=rsp[:], func=AF.Sqrt, bias=EPSC[:, 0:1], scale=1.0 / D), [rsp.B, EPSC.B], [rsp.B])
                V_(lambda e: e.reciprocal(out=rsp[:], in_=rsp[:]), [rsp.B], [rsp.B])
                P.dma("sync", lambda e: e.dma_start(out=gb2[0][:], in_=ple_ng.partition_broadcast(128)), gb2[0].B, writes=[gb2[0].B])
                if CUT == 33:
                    P.disabled = True
                pgw_v = ple_gw.rearrange("(kc p) n -> p kc n", p=128)
                for n in range(4):
                    W_ = ws[0]
                    P.dma("gpsimd", lambda e, W_=W_, n=n: e.dma_start(out=W_[:], in_=pgw_v[:, :, n * 512:(n + 1) * 512]), W_.B, writes=[W_.B])
                    nsl = slice(n * 512, (n + 1) * 512)
                    for i in range(8):
                        pgt, pp = ps[(i % 2) * 2], ps[(i % 2) * 2 + 1]
                        for kc in range(16):
                            mm(pgt[:, :], hT1[:, kc, i * 128:(i + 1) * 128], W_[:, kc, :], kc == 0, kc == 15, [hT1.b[i], W_.B], pgt.B)
                        for k in range(2):
                            mm(pp[:, :], pT[:, k, i * 128:(i + 1) * 128], plw[:, k, nsl], k == 0, k == 1, [pT.b[i], plw.B], pp.B)
                        A_(lambda e, pgt=pgt: e.activation(out=sgt[:], in_=pgt[:, :], func=AF.Sigmoid), [pgt.B], [sgt.B])
                        V_(lambda e, i=i, pp=pp, nsl=nsl: e.scalar_tensor_tensor(out=t1_[:], in0=pp[:, :], scalar=rsp[:, i:i + 1], in1=gb2[0][:, nsl], op0=ALU.mult, op1=ALU.mult),
                           [pp.B, rsp.B, gb2[0].B], [t1_.B])
                        V_(lambda e: e.tensor_tensor(out=t1_[:], in0=t1_[:], in1=sgt[:], op=ALU.mult), [t1_.B, sgt.B], [t1_.B])
                        V_(lambda e, i=i, nsl=nsl: e.tensor_tensor(out=acc[:, i, nsl], in0=acc[:, i, nsl], in1=t1_[:], op=ALU.add), [accB[i], t1_.B], [accB[i]])
                        P.dma("sync", lambda e, i=i, nsl=nsl: e.dma_start(out=out_d[i * 128:(i + 1) * 128, nsl], in_=acc[:, i, nsl]), accB[i], reads=[accB[i]])
                if CUT == 34:
                    P.disabled = True
                P.wait_all("sync", accB + dbg_bufs)
                if stop == 7:
                    P.wait_all("sync", dbg_bufs)
                P.flush()
      except _Stop:
        pass
    return nc


EPSC = None


def rsqrt(P, out, in_, outB, inB, eps_ap):
    P.op("scalar", lambda e: e.activation(out=out, in_=in_, func=AF.Sqrt, bias=eps_ap, scale=1.0), reads=[inB, EPSC.B], writes=[outB])
    P.op("vector", lambda e: e.reciprocal(out=out, in_=out), reads=[outB], writes=[outB])


def ln_tile(P, X, XN, ST, MV, gbc, bbc, out_bf, out_f32, eps=1e-5):
    for j in range(4):
        P.op("vector", lambda e, j=j: e.bn_stats(out=ST[:, j, :], in_=X[:, j * 512:(j + 1) * 512]), reads=[X.B], writes=[ST.B])
    P.op("vector", lambda e: e.bn_aggr(out=MV[:, 0:2], in_=ST[:]), reads=[ST.B], writes=[MV.B])
    rsqrt(P, MV[:, 2:3], MV[:, 1:2], MV.B, MV.B, EPSC[:, 0:1])
    P.op("vector", lambda e: e.scalar_tensor_tensor(out=MV[:, 3:4], in0=MV[:, 0:1], scalar=-1.0, in1=MV[:, 2:3], op0=ALU.mult, op1=ALU.mult),
         reads=[MV.B], writes=[MV.B])
    P.op("scalar", lambda e: e.activation(out=XN[:], in_=X[:], func=AF.Identity, bias=MV[:, 3:4], scale=MV[:, 2:3]),
         reads=[X.B, MV.B], writes=[XN.B])
    P.op("vector", lambda e: e.tensor_tensor(out=XN[:], in0=XN[:], in1=gbc[:], op=ALU.mult), reads=[XN.B, gbc.B], writes=[XN.B])
    if out_f32 is not None:
        P.op("vector", lambda e: e.tensor_tensor(out=out_f32[:], in0=XN[:], in1=bbc[:], op=ALU.add), reads=[XN.B, bbc.B], writes=[out_f32.B])
        if out_bf is not None:
            P.op("scalar", lambda e: e.activation(out=out_bf[:], in_=out_f32[:], func=AF.Copy), reads=[out_f32.B], writes=[out_bf.B])
    else:
        P.op("vector", lambda e: e.tensor_tensor(out=out_bf[:], in0=XN[:], in1=bbc[:], op=ALU.add), reads=[XN.B, bbc.B], writes=[out_bf.B])


def transpose_to_fm(P, HB, dstT, ti, ps, identb, identB, pbase=0):
    for half in range(2):
        pst = ps[pbase + half]
        pv = pst.t[:].bitcast(BF16)
        for j in range(8):
            kc = half * 8 + j
            P.op("tensor", lambda e, kc=kc, j=j, pv=pv: e.transpose(out=pv[:, j * 128:(j + 1) * 128], in_=HB[:, kc * 128:(kc + 1) * 128], identity=identb),
                 reads=[HB.B, identB], writes=[pst.B])
        eng = "scalar" if half == 0 else "vector"
        dst = dstT[:, half * 8:(half + 1) * 8, ti * 128:(ti + 1) * 128]
        src = pv.rearrange("p (j t) -> p j t", t=128)
        if eng == "scalar":
            P.op("scalar", lambda e, dst=dst, src=src: e.activation(out=dst, in_=src, func=AF.Copy), reads=[pst.B], writes=[dstT.b[ti]])
        else:
            P.op("vector", lambda e, dst=dst, src=src: e.tensor_copy(out=dst, in_=src), reads=[pst.B], writes=[dstT.b[ti]])


def make_consts():
    c = np.zeros((8, 128, 128), np.float32)
    i = np.arange(128)
    c[0] = np.eye(128)
    c[1] = (i[:, None] <= i[None, :]) * -EM05
    c[2] = (i[:, None] < i[None, :]) * -EM05
    c[3] = (i[:, None] > i[None, :]) * -EM05
    c[4] = (i[None, :] > i[:, None])
    c[5] = (i[None, :] >= i[:, None])
    c[6] = (i[:, None] > i[None, :])
    c[7] = (i[:, None] // 64 == i[None, :] // 64)
    n = np.arange(-127, 256)
    nn = np.maximum(n, 0)
    nf = np.maximum(nn, 1).astype(np.float32)
    large = 16 + (np.log(nf / 16) / math.log(128 / 16) * 16).astype(np.int32)
    large = np.minimum(large, 31)
    bucket = np.where(nn < 16, nn, large)
    oh = np.zeros((33, 383), np.float32)
    oh[bucket, np.arange(383)] = 1.0
    oh[:32, n < 0] = 0.0
    oh[32, n < 0] = 1.0
    return c, oh


_CACHE = {}


def kernel(**inputs):
    dbg = inputs.pop("_dbg", None)
    stop = inputs.pop("_stop", 99)
    key = ("nc", dbg, stop)
    if key not in _CACHE:
        _CACHE[key] = build(dbg, stop)
    nc = _CACHE[key]
    x = np.asarray(inputs["x"], np.float32)
    p = np.asarray(inputs["p"], np.float32)
    cm, oh = make_consts()
    shared = {"cmat": cm, "onehot": oh}
    for k, v in inputs.items():
        if k in ("x", "p"):
            continue
        a = np.asarray(v, np.float32)
        if a.shape[0] == 1 and k not in ("ln_in_g", "ln_in_b"):
            a = a[0]
        if k in ("moe_w_gate", "moe_w_up", "moe_w_down"):
            a = a.reshape(32, a.shape[-2], a.shape[-1])
            if stop < 6:
                a = a[:1]
        if k in ("rwkv_r_k",):
            a = a.reshape(-1)
        shared[k] = np.ascontiguousarray(a)
    in_maps = []
    for c in range(8):
        b, s = c // 2, c % 2
        seq = np.zeros((2048, 2048), np.float32)
        if s == 1:
            seq[:] = x[b]
        else:
            seq[1024:] = x[b, :1024]
        m = dict(shared)
        m["xs"] = seq
        m["p"] = np.ascontiguousarray(p[0, b, s * 1024:(s + 1) * 1024])
        m["flag"] = np.full((128, 1), float(s), np.float32)
        in_maps.append(m)
    res = run_bass_kernel_spmd(nc, in_maps, core_ids=list(range(8)))
    if dbg:
        return [(r["dbg"], r["out"]) for r in res.results]
    out = np.zeros((4, 2048, 2048), np.float32)
    for c in range(8):
        b, s = c // 2, c % 2
        out[b, s * 1024:(s + 1) * 1024] = res.results[c]["out"]
    return out
```

```python
import math
from contextlib import ExitStack
import numpy as np
import concourse.bass as bass
import concourse.mybir as mybir
from concourse.bass_utils import run_bass_kernel_spmd

F32 = mybir.dt.float32
BF16 = mybir.dt.bfloat16
AF = mybir.ActivationFunctionType
ALU = mybir.AluOpType
AX = mybir.AxisListType
ENGS = ("sync", "scalar", "vector", "gpsimd", "tensor")
ALPHA = 2.0 ** 0.25
LAMBDA_INIT = 0.8 - 0.6
NEG = -1.0e30
EM05 = math.exp(-0.5)
NHEADS = 8
NPAIRS = 8
CUT = 0
NEXP = 32
OUTSEL = tuple(range(8))


class Buf:
    __slots__ = ("name", "w", "r", "dsem", "dcnt")

    def __init__(self, name):
        self.name = name
        self.w = None
        self.r = []
        self.dsem = None
        self.dcnt = 0


class Prog:
    def __init__(self, nc, stack):
        self.nc = nc
        self.stack = stack
        self.ops = {e: [] for e in ENGS}
        self.sem = {e: stack.enter_context(nc.semaphore("c_" + e)) for e in ("scalar", "vector", "gpsimd", "tensor")}
        self.cnt = {e: 0 for e in ENGS}
        self.seen = {e: {} for e in ENGS}
        self.semobj = {}
        self.nbuf = 0
        self.disabled = False
        self.dbufs = []

    def buf(self, name=None):
        self.nbuf += 1
        return Buf(name or ("b%d" % self.nbuf))

    def _deps(self, eng, reads, writes):
        deps = []
        for b in reads:
            if b.w is not None:
                deps.append(b.w)
        for b in writes:
            if b.w is not None:
                deps.append(b.w)
            deps.extend(b.r)
        waits = {}
        for (sid, val, src) in deps:
            if src == "tensor" and eng == "tensor":
                continue
            if self.seen[eng].get(sid, 0) >= val:
                continue
            if waits.get(sid, 0) < val:
                waits[sid] = val
        for sid, val in waits.items():
            self.seen[eng][sid] = val
        return [(self.semobj[sid], val) for sid, val in waits.items()]

    def op(self, eng, fn, reads=(), writes=()):
        if self.disabled:
            return None
        waits = self._deps(eng, reads, writes)
        self.cnt[eng] += 1
        sem = self.sem[eng]
        self.semobj[id(sem)] = sem
        tok = (id(sem), self.cnt[eng], eng)
        self.ops[eng].append((waits, fn, (sem, 1)))
        for b in reads:
            b.r.append(tok)
        for b in writes:
            b.w = tok
            b.r = []
        return tok

    def dma(self, eng, fn, sbuf_buf, reads=(), writes=()):
        if self.disabled:
            return None
        waits = self._deps(eng, reads, writes)
        if sbuf_buf.dsem is None:
            sbuf_buf.dsem = self.stack.enter_context(self.nc.semaphore("d_" + sbuf_buf.name))
            self.semobj[id(sbuf_buf.dsem)] = sbuf_buf.dsem
            self.dbufs.append(sbuf_buf)
        sbuf_buf.dcnt += 16
        tok = (id(sbuf_buf.dsem), sbuf_buf.dcnt, "dma")
        self.ops[eng].append((waits, fn, (sbuf_buf.dsem, 16)))
        for b in reads:
            b.r.append(tok)
        for b in writes:
            b.w = tok
            b.r = []
        return tok

    def wait_all(self, eng, bufs):
        if self.disabled:
            return
        waits = self._deps(eng, bufs, bufs)
        self.ops[eng].append((waits, None, None))

    def barrier(self):
        toks = []
        for x in ("scalar", "vector", "gpsimd", "tensor"):
            if self.cnt[x] > 0:
                self.semobj[id(self.sem[x])] = self.sem[x]
                toks.append((id(self.sem[x]), self.cnt[x]))
        for b in self.dbufs:
            toks.append((id(b.dsem), b.dcnt))
        for eng in ENGS:
            waits = []
            for sid, val in toks:
                if self.seen[eng].get(sid, 0) >= val:
                    continue
                self.seen[eng][sid] = val
                waits.append((self.semobj[sid], val))
            if waits:
                self.ops[eng].append((waits, None, None))

    def flush(self):
        self.barrier()
        nc = self.nc
        ops = self.ops
        self.ops = {e: [] for e in ENGS}

        def replay(lst, e):
            for waits, fn, inc in lst:
                for sem, val in waits:
                    e.wait_ge(sem, val)
                if fn is not None:
                    ins = fn(e)
                    ins.then_inc(inc[0], inc[1])

        with nc.Block() as block:
            @block.sync
            def _(e):
                replay(ops["sync"], e)

            @block.scalar
            def _(e):
                replay(ops["scalar"], e)

            @block.vector
            def _(e):
                replay(ops["vector"], e)

            @block.gpsimd
            def _(e):
                replay(ops["gpsimd"], e)

            @block.tensor
            def _(e):
                replay(ops["tensor"], e)


class T:
    def __init__(self, P, stack, name, shape, dtype, psum=False, nb=1):
        nc = P.nc
        if psum:
            self.t = stack.enter_context(nc.psum_tensor("t_" + name, shape, dtype))
        else:
            self.t = stack.enter_context(nc.sbuf_tensor("t_" + name, shape, dtype))
        self.b = [P.buf(name + "_%d" % i) for i in range(nb)]
        self.B = self.b[0]

    def __getitem__(self, k):
        return self.t[k]


class _Stop(Exception):
    pass


def build(dbg=None, stop=99):
    nc = bass.Bass("TRN2", target_bir_lowering=False)
    D = 2048
    dram = {}

    def din(name, shape):
        dram[name] = nc.dram_tensor(name, list(shape), F32, kind="ExternalInput").ap()
        return dram[name]

    xs = din("xs", [2048, D])
    p_in = din("p", [1024, 256])
    flag_d = din("flag", [128, 1])
    ln_in_g = din("ln_in_g", [D]); ln_in_b = din("ln_in_b", [D])
    rel_bias = din("rel_bias", [32, 8])
    w_in = din("w_in", [D, 6432])
    lamq1 = din("diff_lam_q1", [64]); lamk1 = din("diff_lam_k1", [64])
    lamq2 = din("diff_lam_q2", [64]); lamk2 = din("diff_lam_k2", [64])
    subln_g = din("diff_subln_g", [128])
    rwkv_mu = din("rwkv_mu", [3360])
    rwkv_w0 = din("rwkv_w0", [1024]); rwkv_w2 = din("rwkv_w2", [64, 1024])
    rwkv_a0 = din("rwkv_a0", [1024]); rwkv_a2 = din("rwkv_a2", [64, 1024])
    rwkv_g2 = din("rwkv_g2", [160, 1024])
    rwkv_k_k = din("rwkv_k_k", [1024]); rwkv_k_a = din("rwkv_k_a", [1024])
    rwkv_r_k = din("rwkv_r_k", [1024])
    lnx_g = din("rwkv_lnx_g", [1024]); lnx_b = din("rwkv_lnx_b", [1024])
    w_out = din("w_out", [D, D])
    ln1_g = din("ln1_g", [D]); ln1_b = din("ln1_b", [D])
    rgw = din("router_group_w", [D, 4]); rgb = din("router_group_b", [4])
    rew = din("router_expert_w", [D, 32]); reb = din("router_expert_b", [32])
    nE = 32 if stop >= 6 else 1
    wg_d = din("moe_w_gate", [nE, D, 512]); wu_d = din("moe_w_up", [nE, D, 512]); wd_d = din("moe_w_down", [nE, 512, D])
    ln2_g = din("ln2_g", [D]); ln2_b = din("ln2_b", [D])
    ple_w = din("ple_w", [256, D]); ple_ng = din("ple_norm_g", [D]); ple_gw = din("ple_gate_w", [D, D])
    cmat = din("cmat", [8, 128, 128])
    oh_d = din("onehot", [33, 383])
    out_d = nc.dram_tensor("out", [1024, D], F32, kind="ExternalOutput").ap()
    dbg_d = None
    if dbg:
        dbg_d = nc.dram_tensor("dbg", list(dbg), F32, kind="ExternalOutput").ap()
    trep_d = nc.dram_tensor("trep", [8, 128, 383], F32, kind="Internal").ap()

    stA = ExitStack()
    with ExitStack() as st0:
      try:
        P = Prog(nc, st0)
        stB = st0
        yT = T(P, stB, "yT", [128, 16, 1024], BF16, nb=16)
        cst = T(P, st0, "cst", [128, 8, 128], F32)
        cstb = T(P, st0, "cstb", [128, 8, 128], BF16)
        flag = T(P, st0, "flag", [128, 1], F32)
        pbias = T(P, st0, "pbias", [128, 1], F32)
        ps = [T(P, st0, "ps%d" % i, [128, 512], F32, psum=True) for i in range(8)]
        IDENT, TRI_I, TRI_E, TRI_R, UT_S, UT_I, LT_S, BLK1 = range(8)
        identb = cstb[:, IDENT, :]

        global EPSC
        EPSC = T(P, st0, "epsc", [128, 4], F32)
        P.op("vector", lambda e: e.memset(EPSC[:, 0:1], 1e-5), writes=[EPSC.B])
        P.op("vector", lambda e: e.memset(EPSC[:, 1:2], 64e-5), writes=[EPSC.B])
        P.op("vector", lambda e: e.memset(EPSC[:, 2:3], 1e-24), writes=[EPSC.B])
        P.op("vector", lambda e: e.memset(EPSC[:, 3:4], 0.0), writes=[EPSC.B])
        P.dma("sync", lambda e: e.dma_start(out=cst[:], in_=cmat.rearrange("m p n -> p m n")), cst.B, writes=[cst.B])
        P.dma("sync", lambda e: e.dma_start(out=flag[:], in_=flag_d), flag.B, writes=[flag.B])
        P.op("vector", lambda e: e.tensor_copy(out=cstb[:], in_=cst[:]), reads=[cst.B], writes=[cstb.B])
        P.op("vector", lambda e: e.tensor_scalar(out=pbias[:], in0=flag[:], scalar1=-1.0, scalar2=-NEG, op0=ALU.add, op1=ALU.mult),
             reads=[flag.B], writes=[pbias.B])

        dbg_bufs = []

        def dump(stk, slot, ap, bufs, width, parts=128):
            if not dbg or P.disabled:
                return
            tmp = T(P, stk, "dbg%d" % slot, [128, width], F32)
            P.op("vector", lambda e: e.tensor_copy(out=tmp[0:parts, :], in_=ap), reads=bufs, writes=[tmp.B])
            P.dma("sync", lambda e: e.dma_start(out=dbg_d[slot, 0:parts, 0:width], in_=tmp[0:parts, :]), tmp.B, reads=[tmp.B])
            dbg_bufs.append(tmp.B)

        def mm(out, lhsT, rhs, start, stop, reads, wB, tp=None, sgc=False):
            kw = {}
            if tp is not None:
                kw["tile_position"] = tp
            if sgc:
                kw["skip_group_check"] = True
            P.op("tensor", lambda e: e.matmul(out=out, lhsT=lhsT, rhs=rhs, start=start, stop=stop, **kw), reads=reads, writes=[wB])

        def V_(fn, reads, writes):
            P.op("vector", fn, reads=reads, writes=writes)

        def A_(fn, reads, writes):
            P.op("scalar", fn, reads=reads, writes=writes)

        def small_load(dst_ap, dstB, src_ap):
            P.dma("sync", lambda e: e.dma_start(out=dst_ap, in_=src_ap, allow_slow_non_contiguous=True), dstB, writes=[dstB])

        w_in_v = w_in.rearrange("(kc p) n -> p kc n", p=128)

        def proj_fm(wt, c0, M, tb, pst):
            for kc in range(16):
                mm(pst[0:M, 0:512], wt[:, kc, c0:c0 + M], hT0[:, kc, tb * 512:(tb + 1) * 512], kc == 0, kc == 15,
                   [wt.B] + hT0.b[tb * 4:(tb + 1) * 4], pst.B)

        def shiftmix(pst, M, tb, R, mucol, omucol, muB, out_ap, outB, tmp):
            if tb == 0:
                V_(lambda e: e.memset(R[:], 0.0), [], [R.B])
            else:
                V_(lambda e: e.tensor_copy(out=R[:, 0:1], in_=R[:, 512:513]), [R.B], [R.B])
            if tb < 2:
                A_(lambda e: e.activation(out=R[0:M, 1:513], in_=pst[0:M, 0:512], func=AF.Copy, scale=flag[0:M, 0:1]), [pst.B, flag.B, R.B], [R.B])
            else:
                A_(lambda e: e.activation(out=R[0:M, 1:513], in_=pst[0:M, 0:512], func=AF.Copy), [pst.B, R.B], [R.B])
            V_(lambda e: e.tensor_scalar(out=tmp[0:M, :], in0=R[0:M, 1:513], scalar1=omucol, scalar2=None, op0=ALU.mult), [R.B, muB], [tmp.B])
            V_(lambda e: e.scalar_tensor_tensor(out=out_ap, in0=R[0:M, 0:512], scalar=mucol, in1=tmp[0:M, :], op0=ALU.mult, op1=ALU.add),
               [R.B, muB, tmp.B], [outB])

        mul_ = T(P, st0, "mul", [128, 8], F32)
        murkv = T(P, st0, "murkv", [128, 48], F32)
        st0.enter_context(stA)
        hT0 = T(P, stA, "hT0", [128, 16, 2048], BF16, nb=16)
        txw = T(P, stA, "txw", [64, 2048], BF16)
        xaT = T(P, stA, "xaT", [64, 2048], BF16)
        sxa = T(P, stA, "sxa", [128, 2048], BF16)
        sxb = T(P, stA, "sxb", [32, 2048], BF16)
        V_(lambda e: e.memset(mul_[:], 0.0), [], [mul_.B])
        small_load(mul_[0:64, 0:1], mul_.B, rwkv_mu[3072:3136].rearrange("(p a) -> p a", a=1))
        small_load(mul_[0:64, 1:2], mul_.B, rwkv_mu[3136:3200].rearrange("(p a) -> p a", a=1))
        small_load(mul_[0:128, 2:3], mul_.B, rwkv_mu[3200:3328].rearrange("(p a) -> p a", a=1))
        small_load(mul_[0:32, 3:4], mul_.B, rwkv_mu[3328:3360].rearrange("(p a) -> p a", a=1))
        V_(lambda e: e.tensor_scalar(out=mul_[:, 4:8], in0=mul_[:, 0:4], scalar1=-1.0, scalar2=1.0, op0=ALU.mult, op1=ALU.add), [mul_.B], [mul_.B])
        small_load(murkv[:, 0:24], murkv.B, rwkv_mu[0:3072].rearrange("(a p) -> p a", p=128))
        V_(lambda e: e.tensor_scalar(out=murkv[:, 24:48], in0=murkv[:, 0:24], scalar1=-1.0, scalar2=1.0, op0=ALU.mult, op1=ALU.add), [murkv.B], [murkv.B])

        with ExitStack() as st:
            gbc = T(P, st, "gbc", [128, D], F32); bbc = T(P, st, "bbc", [128, D], F32)
            P.dma("sync", lambda e: e.dma_start(out=gbc[:], in_=ln_in_g.partition_broadcast(128)), gbc.B, writes=[gbc.B])
            P.dma("sync", lambda e: e.dma_start(out=bbc[:], in_=ln_in_b.partition_broadcast(128)), bbc.B, writes=[bbc.B])
            wl = T(P, st, "wl", [128, 16, 288], BF16)
            P.dma("gpsimd", lambda e: e.dma_start(out=wl[:], in_=w_in_v[:, :, 6144:6432]), wl.B, writes=[wl.B])
            xt = [T(P, st, "xt%d" % i, [128, D], F32) for i in range(2)]
            xn = [T(P, st, "xn%d" % i, [128, D], F32) for i in range(2)]
            hb = [T(P, st, "hb%d" % i, [128, D], BF16) for i in range(2)]
            stt = [T(P, st, "stt%d" % i, [128, 4, 6], F32) for i in range(2)]
            mv = [T(P, st, "mv%d" % i, [128, 4], F32) for i in range(2)]
            for i in range(16):
                s = i % 2
                X = xt[s]
                P.dma("sync", lambda e, X=X, i=i: e.dma_start(out=X[:], in_=xs[i * 128:(i + 1) * 128, :]), X.B, writes=[X.B])
                ln_tile(P, X, xn[s], stt[s], mv[s], gbc, bbc, hb[s], None)
                transpose_to_fm(P, hb[s], hT0, i, ps, identb, cstb.B)
            Rl = [T(P, st, "Rl%d" % i, [128, 513], F32) for i in range(4)]
            tmpl = T(P, st, "tmpl", [128, 512], F32)
            xsl = T(P, st, "xsl", [128, 512], F32)
            groups = [(0, 64, txw, AF.Tanh), (64, 64, xaT, AF.Copy), (128, 128, sxa, AF.Sigmoid), (256, 32, sxb, AF.Sigmoid)]
            for tb in range(4):
                for gi, (c0, M, dst, fn_) in enumerate(groups):
                    pst = ps[2 + (gi % 2)]
                    proj_fm(wl, c0, M, tb, pst)
                    shiftmix(pst, M, tb, Rl[gi], mul_[0:M, gi:gi + 1], mul_[0:M, 4 + gi:5 + gi], mul_.B, xsl[0:M, :], xsl.B, tmpl)
                    A_(lambda e, dst=dst, M=M, fn_=fn_, tb=tb: e.activation(out=dst[0:M, tb * 512:(tb + 1) * 512], in_=xsl[0:M, :], func=fn_),
                       [xsl.B], [dst.B])
            dump(st, 0, hT0[:, 3, 0:1024], hT0.b, 1024)
            dump(st, 1, txw[0:64, 1024:2048], [txw.B], 1024, 64)
            if stop == 1:
                P.wait_all("sync", dbg_bufs)
            P.flush()
            if stop == 1:
                P.disabled = True

        with ExitStack() as st:
            rb = T(P, st, "rb", [33, 8], F32)
            oh = T(P, st, "oh", [33, 383], F32)
            P.dma("sync", lambda e: e.dma_start(out=rb[0:32, :], in_=rel_bias), rb.B, writes=[rb.B])
            V_(lambda e: e.memset(rb[32:33, :], NEG), [], [rb.B])
            P.dma("sync", lambda e: e.dma_start(out=oh[:], in_=oh_d), oh.B, writes=[oh.B])
            rbh = T(P, st, "rbh", [33, 128], F32)
            treps = T(P, st, "treps", [128, 383], F32)
            BN = T(P, st, "BN", [128, 8, 256], F32, nb=8)
            farb = T(P, st, "farb", [128, 16], F32)
            drb = [P.buf("drb%d" % h) for h in range(8)]
            for h in range(8):
                V_(lambda e, h=h: e.tensor_copy(out=rbh[:], in_=rb[:, h:h + 1].to_broadcast([33, 128])), [rb.B], [rbh.B])
                mm(ps[0][:, 0:383], rbh[:], oh[:], True, True, [rbh.B, oh.B], ps[0].B)
                V_(lambda e: e.tensor_copy(out=treps[:], in_=ps[0][:, 0:383]), [ps[0].B], [treps.B])
                V_(lambda e, h=h: e.tensor_copy(out=farb[:, h:h + 1], in_=treps[:, 382:383]), [treps.B], [farb.B])
                P.dma("sync", lambda e, h=h: e.dma_start(out=trep_d[h], in_=treps[:]), treps.B, reads=[treps.B], writes=[drb[h]])
                skew = bass.AP(tensor=trep_d.tensor, offset=h * 128 * 383 + 127, ap=[[382, 128], [1, 256]])
                P.dma("sync", lambda e, h=h, skew=skew: e.dma_start(out=BN[:, h, :], in_=skew), BN.b[h], reads=[drb[h]], writes=[BN.b[h]])
            V_(lambda e: e.tensor_scalar(out=farb[:, 8:16], in0=farb[:, 0:8], scalar1=pbias[:, 0:1], scalar2=None, op0=ALU.add), [farb.B, pbias.B], [farb.B])
            if CUT == 1:
                P.disabled = True
            lq = T(P, st, "lq", [1, 4, 64], F32)
            for j, src in enumerate((lamq1, lamk1, lamq2, lamk2)):
                P.dma("sync", lambda e, j=j, src=src: e.dma_start(out=lq[0:1, j, :], in_=src.rearrange("(a n) -> a n", a=1)), lq.B, writes=[lq.B])
            lt = T(P, st, "lt", [1, 8], F32)
            lpr = T(P, st, "lpr", [1, 2, 64], F32)
            V_(lambda e: e.tensor_tensor(out=lpr[0:1, 0, :], in0=lq[0:1, 0, :], in1=lq[0:1, 1, :], op=ALU.mult), [lq.B], [lpr.B])
            V_(lambda e: e.tensor_tensor(out=lpr[0:1, 1, :], in0=lq[0:1, 2, :], in1=lq[0:1, 3, :], op=ALU.mult), [lq.B], [lpr.B])
            V_(lambda e: e.reduce_sum(out=lt[0:1, 0:2], in_=lpr[0:1, :, :], axis=AX.X), [lpr.B], [lt.B])
            A_(lambda e: e.activation(out=lt[0:1, 2:4], in_=lt[0:1, 0:2], func=AF.Exp), [lt.B], [lt.B])
            V_(lambda e: e.tensor_tensor(out=lt[0:1, 4:5], in0=lt[0:1, 3:4], in1=lt[0:1, 2:3], op=ALU.subtract), [lt.B], [lt.B])
            V_(lambda e: e.tensor_scalar(out=lt[0:1, 5:6], in0=lt[0:1, 4:5], scalar1=-LAMBDA_INIT, scalar2=None, op0=ALU.add), [lt.B], [lt.B])
            ones1 = T(P, st, "ones1", [1, 128], F32)
            V_(lambda e: e.memset(ones1[:], 1.0), [], [ones1.B])
            nlam = T(P, st, "nlam", [128, 1], F32)
            mm(ps[1][:, 0:1], ones1[0:1, :], lt[0:1, 5:6], True, True, [ones1.B, lt.B], ps[1].B)
            V_(lambda e: e.tensor_copy(out=nlam[:], in_=ps[1][:, 0:1]), [ps[1].B], [nlam.B])
            gsub = T(P, st, "gsub", [128, 128], F32)
            P.dma("sync", lambda e: e.dma_start(out=gsub[:], in_=subln_g.partition_broadcast(128)), gsub.B, writes=[gsub.B])
            V_(lambda e: e.tensor_scalar(out=gsub[:], in0=gsub[:], scalar1=1.0 - LAMBDA_INIT, scalar2=None, op0=ALU.mult), [gsub.B], [gsub.B])
            if CUT == 2:
                P.disabled = True

            WQ = [T(P, st, "WQ%d" % i, [128, 16, 128], BF16) for i in range(2)]
            WK = [T(P, st, "WK%d" % i, [128, 16, 128], BF16) for i in range(2)]
            WV = [T(P, st, "WV%d" % i, [128, 16, 128], BF16) for i in range(2)]
            qT = T(P, st, "qT", [128, 1024], BF16)
            kT = T(P, st, "kT", [128, 2048], BF16)
            Va = T(P, st, "Va", [128, 16, 129], BF16)
            V_(lambda e: e.memset(Va[:, :, 128:129], 1.0), [], [Va.B])
            E = [[T(P, st, "E%d%d" % (m, j), [128, 512], BF16) for j in range(2)] for m in range(2)]
            TMP = [[T(P, st, "TMPa%d%d" % (m, j), [128, 256], F32) for j in range(2)] for m in range(2)]
            rc = T(P, st, "rc", [128, 4], F32)
            o2 = T(P, st, "o2", [128, 128], F32)
            oo = T(P, st, "oo", [128, 128], F32)
            junk = T(P, st, "junk", [128, 128], F32)
            ss = T(P, st, "ss", [128, 2], F32)
            yb = T(P, st, "yb", [128, 128], BF16)

            def load_head(h):
                s = h % 2
                for W_, c0 in ((WQ[s], h * 128), (WK[s], 1024 + h * 128), (WV[s], 2048 + h * 128)):
                    P.dma("gpsimd", lambda e, W_=W_, c0=c0: e.dma_start(out=W_[:], in_=w_in_v[:, :, c0:c0 + 128]), W_.B, writes=[W_.B])

            def accap(a, lo, hi):
                return ps[5 + a // 3][:, (a % 3) * 129 + lo:(a % 3) * 129 + hi]

            load_head(0)
            it = 0
            for h in range(NHEADS):
                if h + 1 < NHEADS:
                    load_head(h + 1)
                s = h % 2
                for tb in (2, 3):
                    proj_fm(WQ[s], 0, 128, tb, ps[tb % 2])
                    A_(lambda e, tb=tb: e.activation(out=qT[:, (tb - 2) * 512:(tb - 1) * 512], in_=ps[tb % 2][:, :], func=AF.Copy), [ps[tb % 2].B], [qT.B])
                for tb in range(4):
                    proj_fm(WK[s], 0, 128, tb, ps[2 + tb % 2])
                    V_(lambda e, tb=tb: e.tensor_copy(out=kT[:, tb * 512:(tb + 1) * 512], in_=ps[2 + tb % 2][:, :]), [ps[2 + tb % 2].B], [kT.B])
                for g4 in range(4):
                    pst = ps[g4 % 2]
                    for j in range(4):
                        i = g4 * 4 + j
                        for kc in range(16):
                            mm(pst[:, j * 128:(j + 1) * 128], hT0[:, kc, i * 128:(i + 1) * 128], WV[s][:, kc, :], kc == 0, kc == 15,
                               [WV[s].B, hT0.b[i]], pst.B)
                    V_(lambda e, g4=g4, pst=pst: e.tensor_copy(out=Va[:, g4 * 4:(g4 + 1) * 4, 0:128], in_=pst[:, :].rearrange("p (j t) -> p j t", t=128)),
                       [pst.B], [Va.B])
                if CUT == 3:
                    P.disabled = True
                for g in range(2):
                    nkb = 8 + 4 * g + 4
                    steps = []
                    for kb in range(nkb):
                        t = kb - 8 - 4 * g
                        i0 = max(0, t)
                        N = (4 - i0) * 128
                        q0 = g * 512 + i0 * 128
                        if t >= 0:
                            wn = min(256, N); b0 = 0
                        elif t == -1:
                            wn = 128; b0 = 128
                        else:
                            wn = 0; b0 = 0
                        steps.append((kb, i0, N, q0, wn, b0, kb < 8, it))
                        it += 1

                    def emit_scores(stp, h=h):
                        kb, i0, N, q0, wn, b0, pref, it_ = stp
                        for m in range(2):
                            pS = ps[m * 2 + (it_ % 2)]
                            Em = E[m][it_ % 2]
                            Tm = TMP[m][it_ % 2]
                            mm(pS[:, 0:N], kT[m * 64:(m + 1) * 64, kb * 128:(kb + 1) * 128], qT[m * 64:(m + 1) * 64, q0:q0 + N], True, True,
                               [kT.B, qT.B], pS.B)
                            if wn > 0:
                                V_(lambda e, pS=pS, Tm=Tm, wn=wn, b0=b0, h=h: e.scalar_tensor_tensor(out=Tm[:, 0:wn], in0=pS[:, 0:wn], scalar=0.125,
                                   in1=BN[:, h, b0:b0 + wn], op0=ALU.mult, op1=ALU.add), [pS.B, BN.b[h]], [Tm.B])
                                bcol = pbias[:, 0:1] if pref else EPSC[:, 3:4]
                                A_(lambda e, Em=Em, Tm=Tm, wn=wn, bcol=bcol: e.activation(out=Em[:, 0:wn], in_=Tm[:, 0:wn], func=AF.Exp, bias=bcol, scale=1.0),
                                   [Tm.B, pbias.B, EPSC.B], [Em.B])
                            if N > wn:
                                fcol = farb[:, 8 + h:9 + h] if pref else farb[:, h:h + 1]
                                A_(lambda e, Em=Em, pS=pS, wn=wn, N=N, fcol=fcol: e.activation(out=Em[:, wn:N], in_=pS[:, wn:N], func=AF.Exp, bias=fcol, scale=0.125),
                                   [pS.B, farb.B], [Em.B])

                    def emit_pv(stp):
                        kb, i0, N, q0, wn, b0, pref, it_ = stp
                        for m in range(2):
                            Em = E[m][it_ % 2]
                            for li in range(4 - i0):
                                i = i0 + li
                                a = m * 4 + i
                                mm(accap(a, 0, 129), Em[:, li * 128:(li + 1) * 128], Va[:, kb, :], (kb == 0 and a % 3 == 0), False,
                                   [Em.B, Va.B], ps[5 + a // 3].B, sgc=True)

                    emit_scores(steps[0])
                    for si, stp in enumerate(steps):
                        if si + 1 < len(steps):
                            emit_scores(steps[si + 1])
                        emit_pv(stp)
                    if CUT == 4:
                        P.disabled = True
                    for i in range(4):
                        a1, a2 = i, 4 + i
                        B1, B2 = ps[5 + a1 // 3].B, ps[5 + a2 // 3].B
                        V_(lambda e, a1=a1: e.reciprocal(out=rc[:, 0:1], in_=accap(a1, 128, 129)), [B1], [rc.B])
                        V_(lambda e, a2=a2: e.reciprocal(out=rc[:, 1:2], in_=accap(a2, 128, 129)), [B2], [rc.B])
                        V_(lambda e: e.tensor_tensor(out=rc[:, 2:3], in0=rc[:, 1:2], in1=nlam[:, 0:1], op=ALU.mult), [rc.B, nlam.B], [rc.B])
                        V_(lambda e, a2=a2: e.tensor_scalar(out=o2[:], in0=accap(a2, 0, 128), scalar1=rc[:, 2:3], scalar2=None, op0=ALU.mult), [B2, rc.B], [o2.B])
                        V_(lambda e, a1=a1: e.scalar_tensor_tensor(out=oo[:], in0=accap(a1, 0, 128), scalar=rc[:, 0:1], in1=o2[:], op0=ALU.mult, op1=ALU.add),
                           [B1, rc.B, o2.B], [oo.B])
                        A_(lambda e: e.activation(out=junk[:], in_=oo[:], func=AF.Square, accum_out=ss[:, 0:1]), [oo.B], [junk.B, ss.B])
                        A_(lambda e: e.activation(out=ss[:, 1:2], in_=ss[:, 0:1], func=AF.Sqrt, bias=EPSC[:, 0:1], scale=1.0 / 128), [ss.B, EPSC.B], [ss.B])
                        V_(lambda e: e.reciprocal(out=ss[:, 1:2], in_=ss[:, 1:2]), [ss.B], [ss.B])
                        V_(lambda e: e.scalar_tensor_tensor(out=yb[:], in0=oo[:], scalar=ss[:, 1:2], in1=gsub[:], op0=ALU.mult, op1=ALU.mult),
                           [oo.B, ss.B, gsub.B], [yb.B])
                        pv = ps[4].t[:].bitcast(BF16)
                        P.op("tensor", lambda e, pv=pv: e.transpose(out=pv[:, 0:128], in_=yb[:], identity=identb), reads=[yb.B, cstb.B], writes=[ps[4].B])
                        c0 = g * 512 + i * 128
                        A_(lambda e, pv=pv, h=h, c0=c0: e.activation(out=yT[:, h, c0:c0 + 128], in_=pv[:, 0:128], func=AF.Copy), [ps[4].B], [yT.b[h]])
            dump(st, 2, yT[:, 0, :], [yT.b[0]], 1024)
            dump(st, 3, yT[:, NHEADS - 1, :], [yT.b[NHEADS - 1]], 1024)
            if stop == 2:
                P.wait_all("sync", dbg_bufs)
            P.flush()
            if stop == 2:
                P.disabled = True

        with ExitStack() as st:
            WR = [T(P, st, "WR0", [128, 16, 128], BF16)] * 2
            WKr = [T(P, st, "WKr0", [128, 16, 128], BF16)] * 2
            WVr = [T(P, st, "WVr0", [128, 16, 128], BF16)] * 2
            prm = T(P, st, "prm", [128, 8, 8], F32)
            for j, src in enumerate((rwkv_k_k, rwkv_k_a, rwkv_a0)):
                small_load(prm[:, :, j], prm.B, src.rearrange("(a p) -> p a", p=128))
            V_(lambda e: e.tensor_scalar(out=prm[:, :, 3], in0=prm[:, :, 1], scalar1=-1.0, scalar2=1.0, op0=ALU.mult, op1=ALU.add), [prm.B], [prm.B])
            w2b = T(P, st, "w2b", [64, 1024], BF16); a2b = T(P, st, "a2b", [64, 1024], BF16); g2b = T(P, st, "g2b", [128, 2, 1024], BF16)
            P.dma("gpsimd", lambda e: e.dma_start(out=w2b[:], in_=rwkv_w2), w2b.B, writes=[w2b.B])
            P.dma("gpsimd", lambda e: e.dma_start(out=a2b[:], in_=rwkv_a2), a2b.B, writes=[a2b.B])
            P.dma("gpsimd", lambda e: e.dma_start(out=g2b[:, 0, :], in_=rwkv_g2[0:128, :]), g2b.B, writes=[g2b.B])
            P.dma("gpsimd", lambda e: e.dma_start(out=g2b[0:32, 1, :], in_=rwkv_g2[128:160, :]), g2b.B, writes=[g2b.B])
            pp3 = T(P, st, "pp3", [128, 3, 128], F32)
            rkf = T(P, st, "rkf", [128, 8], F32)
            small_load(rkf[:], rkf.B, rwkv_r_k.rearrange("(a p) -> p a", p=128))
            RK = T(P, st, "RK", [128, 8, 2], BF16)
            V_(lambda e: e.memset(RK[:], 0.0), [], [RK.B])
            V_(lambda e: e.tensor_copy(out=RK[0:64, :, 0], in_=rkf[0:64, :]), [rkf.B], [RK.B])
            V_(lambda e: e.tensor_copy(out=RK[64:128, :, 1], in_=rkf[64:128, :]), [rkf.B], [RK.B])
            Rr = [T(P, st, "Rr%d" % i, [128, 513], F32) for i in range(3)]
            tmpr = T(P, st, "tmpr", [128, 512], F32)
            rs = T(P, st, "rs", [128, 512], F32); ks = T(P, st, "ks", [128, 512], F32); vsb = T(P, st, "vsb", [128, 512], BF16)
            af = T(P, st, "af", [128, 512], F32); kkk = T(P, st, "kkk", [128, 512], F32); sqb = T(P, st, "sqb", [128, 512], BF16)
            rinv = T(P, st, "rinv", [128, 512], F32); kk = T(P, st, "kk", [128, 512], F32); uu = tmpr
            k2 = T(P, st, "k2", [128, 512], F32); bb_ = T(P, st, "bb", [128, 512], F32)
            k2b = T(P, st, "k2b", [128, 512], BF16); bbb = T(P, st, "bbb", [128, 512], BF16); rk2 = T(P, st, "rk2", [128, 512], BF16)
            lwt2 = [T(P, st, "lwt%d" % i, [128, 128], F32) for i in range(2)]
            ex2 = [T(P, st, "ex%d" % i, [128, 4, 128], F32) for i in range(2)]
            ART2 = [T(P, st, "ART%d" % i, [128, 256], BF16) for i in range(2)]
            BhT2 = [T(P, st, "BhT%d" % i, [128, 128], BF16) for i in range(2)]; KhT2 = [T(P, st, "KhT%d" % i, [128, 128], BF16) for i in range(2)]
            TMt2 = [T(P, st, "TMt%d" % i, [128, 4, 128], BF16) for i in range(2)]
            lwt, ex, ART, BhT, KhT, TMt = lwt2[1], ex2[1], ART2[1], BhT2[1], KhT2[1], TMt2[1]
            MTh = [T(P, st, "MT%d" % h_, [128, 2, 256], BF16) for h_ in range(2)]
            NMh = [[T(P, st, "NM%d%d" % (h_, i), [128, 256], BF16) for i in range(2)] for h_ in range(2)]
            Xfh = [T(P, st, "Xf%d" % h_, [128, 128], F32) for h_ in range(2)]; Xbh = [T(P, st, "Xb%d" % h_, [128, 128], BF16) for h_ in range(2)]
            MT, Xf = MTh[1], Xfh[1]
            pBx = [P.buf("pBx%d" % h_) for h_ in range(2)]; pBn = [P.buf("pBn%d" % h_) for h_ in range(2)]; pBq = [P.buf("pBq%d" % h_) for h_ in range(2)]
            GTs = T(P, st, "GTs", [128, 16, 128], BF16); Hs = T(P, st, "Hs", [128, 16, 64], F32)
            QeTs = T(P, st, "QeTs", [128, 8, 128], BF16); Yls = T(P, st, "Yls", [128, 8, 128], F32)
            Vtm = T(P, st, "Vtm", [128, 8, 128], BF16); sbon = T(P, st, "sbon", [128, 8, 2], F32)
            gtm = T(P, st, "gtm", [128, 8, 128], F32)
            Sb = T(P, st, "Sb", [128, 128], BF16)
            Yt = T(P, st, "Yt", [128, 128], F32); yn = T(P, st, "yn", [128, 128], F32); ybr = T(P, st, "ybr", [128, 128], BF16)
            gst = T(P, st, "gst", [128, 2, 6], F32); gmv = T(P, st, "gmv", [128, 2, 2], F32); grs = T(P, st, "grs", [128, 2], F32)
            ident64 = cst[0:64, IDENT, 0:64]
            V_(lambda e: e.memset(GTs[:], 0.0), [], [GTs.B])

            def load_pair(c):
                s = c % 2
                for W_, c0 in ((WR[s], 3072 + c * 128), (WKr[s], 4096 + c * 128), (WVr[s], 5120 + c * 128)):
                    P.dma("gpsimd", lambda e, W_=W_, c0=c0: e.dma_start(out=W_[:], in_=w_in_v[:, :, c0:c0 + 128]), W_.B, writes=[W_.B])

            if CUT == 11:
                P.disabled = True
            load_pair(0)
            for c in range(NPAIRS):
                s = c % 2
                csl = slice(c * 128, (c + 1) * 128)
                for j3, src3 in enumerate((rwkv_w0, lnx_g, lnx_b)):
                    P.dma("sync", lambda e, j3=j3, src3=src3, c=c: e.dma_start(out=pp3[:, j3, :], in_=src3[c * 128:(c + 1) * 128].partition_broadcast(128)), pp3.B, writes=[pp3.B])
                for tb in range(4):
                    for j, (W_, dst, dstB) in enumerate(((WR[s], rs[:, :], rs.B), (WKr[s], ks[:, :], ks.B), (WVr[s], vsb[:, :], vsb.B))):
                        pst = ps[j % 2]
                        proj_fm(W_, 0, 128, tb, pst)
                        shiftmix(pst, 128, tb, Rr[j], murkv[:, j * 8 + c:j * 8 + c + 1], murkv[:, 24 + j * 8 + c:25 + j * 8 + c], murkv.B, dst, dstB, tmpr)
                    mm(ps[0][:, :], a2b[0:64, csl], xaT[0:64, tb * 512:(tb + 1) * 512], True, True, [a2b.B, xaT.B], ps[0].B)
                    A_(lambda e, c=c: e.activation(out=af[:], in_=ps[0][:, :], func=AF.Sigmoid, bias=prm[:, c, 2:3], scale=1.0), [ps[0].B, prm.B], [af.B])
                    V_(lambda e, c=c: e.tensor_scalar(out=kkk[:], in0=ks[:], scalar1=prm[:, c, 0:1], scalar2=None, op0=ALU.mult), [ks.B, prm.B], [kkk.B])
                    A_(lambda e: e.activation(out=sqb[:], in_=kkk[:], func=AF.Square), [kkk.B], [sqb.B])
                    mm(ps[1][:, :], cstb[:, BLK1, :], sqb[:], True, True, [cstb.B, sqb.B], ps[1].B)
                    A_(lambda e: e.activation(out=rinv[:], in_=ps[1][:, :], func=AF.Sqrt, bias=EPSC[:, 2:3], scale=1.0), [ps[1].B, EPSC.B], [rinv.B])
                    V_(lambda e: e.reciprocal(out=rinv[:], in_=rinv[:]), [rinv.B], [rinv.B])
                    V_(lambda e: e.tensor_tensor(out=kk[:], in0=kkk[:], in1=rinv[:], op=ALU.mult), [kkk.B, rinv.B], [kk.B])
                    V_(lambda e, c=c: e.tensor_scalar(out=uu[:], in0=af[:], scalar1=prm[:, c, 1:2], scalar2=prm[:, c, 3:4], op0=ALU.mult, op1=ALU.add), [af.B, prm.B], [uu.B])
                    V_(lambda e: e.tensor_tensor(out=k2[:], in0=ks[:], in1=uu[:], op=ALU.mult), [ks.B, uu.B], [k2.B])
                    V_(lambda e: e.tensor_tensor(out=bb_[:], in0=kk[:], in1=af[:], op=ALU.mult), [kk.B, af.B], [bb_.B])
                    A_(lambda e: e.activation(out=k2b[:], in_=k2[:], func=AF.Copy), [k2.B], [k2b.B])
                    A_(lambda e: e.activation(out=bbb[:], in_=bb_[:], func=AF.Copy), [bb_.B], [bbb.B])
                    V_(lambda e: e.tensor_tensor(out=rk2[:], in0=rs[:], in1=k2[:], op=ALU.mult), [rs.B, k2.B], [rk2.B])
                    if CUT == 12:
                        P.disabled = True
                    def prologue(ch, q4, bs):
                        own = ch >= 8
                        tsl = slice(q4 * 128, (q4 + 1) * 128)
                        gsl = slice(ch * 128, (ch + 1) * 128)
                        lwt, ex, ART, BhT, KhT, TMt = lwt2[bs], ex2[bs], ART2[bs], BhT2[bs], KhT2[bs], TMt2[bs]
                        pz = ps[2]
                        mm(pz[:, 0:128], txw[0:64, gsl], w2b[0:64, csl], True, True, [txw.B, w2b.B], pz.B)
                        yield
                        V_(lambda e: e.tensor_tensor(out=lwt[:], in0=pz[:, 0:128], in1=pp3[:, 0, :], op=ALU.add), [pz.B, pp3.B], [lwt.B])
                        A_(lambda e: e.activation(out=lwt[:], in_=lwt[:], func=AF.Sigmoid), [lwt.B], [lwt.B])
                        yield
                        mm(pz[:, 128:384], lwt[:], cst[:, TRI_I:TRI_E + 1, :].rearrange("p a b -> p (a b)"), True, True, [lwt.B, cst.B], pz.B)
                        mm(pz[:, 384:512], cst[:, TRI_R, :], lwt[:], True, True, [lwt.B, cst.B], pz.B)
                        yield
                        A_(lambda e: e.activation(out=ex[:, 0:2, :].rearrange("p a b -> p (a b)"), in_=pz[:, 128:384], func=AF.Exp), [pz.B], [ex.B])
                        A_(lambda e: e.activation(out=ex[:, 2, :], in_=pz[:, 128:256], func=AF.Exp, scale=-1.0), [pz.B], [ex.B])
                        A_(lambda e: e.activation(out=ex[:, 3, :], in_=pz[:, 384:512], func=AF.Exp), [pz.B], [ex.B])
                        yield
                        V_(lambda e: e.scalar_tensor_tensor(out=ART[:, 0:128], in0=kk[:, tsl], scalar=-1.0, in1=ex[:, 1, :], op0=ALU.mult, op1=ALU.mult), [kk.B, ex.B], [ART.B])
                        V_(lambda e: e.tensor_tensor(out=ART[:, 128:256], in0=rs[:, tsl], in1=ex[:, 0, :], op=ALU.mult), [rs.B, ex.B], [ART.B])
                        V_(lambda e: e.tensor_tensor(out=BhT[:], in0=bb_[:, tsl], in1=ex[:, 2, :], op=ALU.mult), [bb_.B, ex.B], [BhT.B])
                        V_(lambda e: e.tensor_tensor(out=KhT[:], in0=k2[:, tsl], in1=ex[:, 2, :], op=ALU.mult), [k2.B, ex.B], [KhT.B])
                        yield
                        pt = ps[3]
                        ptv = pt.t[:].bitcast(BF16)
                        for j, (src, sB) in enumerate(((bbb[:, tsl], bbb.B), (k2b[:, tsl], k2b.B), (vsb[:, tsl], vsb.B), (ART[:, 0:128], ART.B))):
                            P.op("tensor", lambda e, j=j, src=src, ptv=ptv: e.transpose(out=ptv[:, j * 128:(j + 1) * 128], in_=src, identity=identb), reads=[sB, cstb.B], writes=[pt.B])
                        yield
                        V_(lambda e: e.tensor_tensor(out=TMt[:, 0:2, :], in0=ptv[:, 0:256].rearrange("p (a b) -> p a b", b=128),
                                                     in1=ex[:, 3:4, :].to_broadcast([128, 2, 128]), op=ALU.mult), [pt.B, ex.B], [TMt.B])
                        A_(lambda e: e.activation(out=TMt[:, 2:4, :].rearrange("p a b -> p (a b)"), in_=ptv[:, 256:512], func=AF.Copy), [pt.B], [TMt.B])
                        if own:
                            oc = ch - 8
                            yield
                            A_(lambda e: e.activation(out=Vtm[:, oc, :], in_=TMt[:, 2, :], func=AF.Copy), [TMt.B], [Vtm.B])
                            mm(ps[3][:, 256:258], rk2[:, tsl], RK[:, c, :], True, True, [rk2.B, RK.B], ps[3].B)
                            mm(ps[3][:, 384:512], sxa[:, gsl], g2b[:, 0, csl], True, False, [sxa.B, g2b.B], ps[3].B)
                            mm(ps[3][:, 384:512], sxb[0:32, gsl], g2b[0:32, 1, csl], False, True, [sxb.B, g2b.B], ps[3].B)
                            yield
                            V_(lambda e: e.tensor_copy(out=sbon[:, oc, :], in_=ps[3][:, 256:258]), [ps[3].B], [sbon.B])
                            V_(lambda e: e.tensor_copy(out=gtm[:, oc, :], in_=ps[3][:, 384:512]), [ps[3].B], [gtm.B])

                    def chain(ch, hh, own, tsl, bs):
                        hp = slice(hh * 64, (hh + 1) * 64)
                        tpk = (hh * 64, 0)
                        tpo = (0, hh * 64)
                        ex, ART, BhT, KhT, TMt = ex2[bs], ART2[bs], BhT2[bs], KhT2[bs], TMt2[bs]
                        pA, pB = ps[4 + hh], ps[6 + hh]
                        bX = bN = bQ = pB.B
                        NM, Xf, Xb, MT = NMh[hh], Xfh[hh], Xbh[hh], MTh[hh]
                        mm(pA[:, 0:256], BhT[hp, :], ART[hp, :], True, True, [BhT.B, ART.B], pA.B, tpk)
                        mm(pA[:, 256:512], KhT[hp, :], ART[hp, :], True, True, [KhT.B, ART.B], pA.B, tpk)
                        mm(pB[:, 384:512], ART[hp, 0:128], BhT[hp, :], True, True, [BhT.B, ART.B], bQ, tpk)
                        yield
                        V_(lambda e: e.tensor_tensor(out=MT[:, :, :], in0=pA[:, :].rearrange("p (a b) -> p a b", b=256),
                                                     in1=cst[:, UT_S:UT_I + 1, :].rearrange("p a b -> p (a b)").unsqueeze(1).to_broadcast([128, 2, 256]), op=ALU.mult),
                           [pA.B, cst.B], [MT.B])
                        V_(lambda e: e.tensor_tensor(out=NM[0][:, 0:128], in0=pA[:, 0:128], in1=cst[:, UT_S, :], op=ALU.mult), [pA.B, cst.B], [NM[0].B])
                        V_(lambda e: e.tensor_tensor(out=NM[0][:, 128:256], in0=pB[:, 384:512], in1=cst[:, LT_S, :], op=ALU.mult), [bQ, cst.B], [NM[0].B])
                        A_(lambda e: e.activation(out=Xb[:, 0:64], in_=TMt[:, 3, hp], func=AF.Copy), [TMt.B], [Xb.B])
                        A_(lambda e: e.activation(out=Xf[:, 0:64], in_=TMt[:, 3, hp], func=AF.Copy), [TMt.B], [Xf.B])
                        yield
                        mm(pA[:, 0:64], MT[:, 1, 0:128], TMt[:, 2, hp], True, True, [MT.B, TMt.B], pA.B)
                        yield
                        V_(lambda e: e.tensor_copy(out=Xb[:, 64:128], in_=pA[:, 0:64]), [pA.B], [Xb.B])
                        V_(lambda e: e.tensor_copy(out=Xf[:, 64:128], in_=pA[:, 0:64]), [pA.B], [Xf.B])
                        yield
                        for lv in range(7):
                            cur, nxt = NM[lv % 2], NM[(lv + 1) % 2]
                            mm(pB[:, 0:128], cur[:, 0:128], Xb[:], True, True, [cur.B, Xb.B], bX)
                            if lv < 6:
                                mm(pB[:, 128:256], cur[:, 128:256], cur[:, 0:128], True, True, [cur.B], bN)
                                mm(pB[:, 256:384], cur[:, 0:128], cur[:, 128:256], True, True, [cur.B], bN)
                            yield
                            V_(lambda e: e.tensor_tensor(out=Xb[:], in0=Xf[:], in1=pB[:, 0:128], op=ALU.add), [Xf.B, bX], [Xb.B])
                            if lv < 6:
                                V_(lambda e, nxt=nxt: e.tensor_copy(out=nxt[:], in_=pB[:, 128:384]), [bN], [nxt.B])
                                V_(lambda e: e.tensor_tensor(out=Xf[:], in0=Xf[:], in1=pB[:, 0:128], op=ALU.add), [Xf.B, bX], [Xf.B])
                            yield
                        mm(pA[hp, 64:128], Xb[:, 0:64], TMt[:, 0, hp], True, True, [Xb.B, TMt.B], pA.B, tpo)
                        mm(pA[hp, 128:192], TMt[:, 0, hp], Xb[:, 64:128], True, False, [Xb.B, TMt.B], pA.B, tpo)
                        mm(pA[hp, 128:192], TMt[:, 1, hp], TMt[:, 2, hp], False, True, [TMt.B], pA.B, tpo)
                        if own:
                            mm(pB[hp, 384:512], Xb[:, 0:64], MT[:, 0, 128:256], True, True, [Xb.B, MT.B], bQ, tpo)
                            mm(pA[:, 192:256], MT[:, 0, 128:256], Xb[:, 64:128], True, False, [Xb.B, MT.B], pA.B)
                            mm(pA[:, 192:256], MT[:, 1, 128:256], TMt[:, 2, hp], False, True, [TMt.B, MT.B], pA.B)
                        yield
                        V_(lambda e: e.scalar_tensor_tensor(out=GTs[hp, ch, hh * 64:(hh + 1) * 64], in0=cst[hp, IDENT, hh * 64:(hh + 1) * 64], scalar=ex[hp, 0, 127:128],
                                                            in1=pA[hp, 64:128], op0=ALU.mult, op1=ALU.add), [cst.B, ex.B, pA.B], [GTs.B])
                        V_(lambda e: e.tensor_copy(out=Hs[hp, ch, :], in_=pA[hp, 128:192]), [pA.B], [Hs.B])
                        if own:
                            oc = ch - 8
                            V_(lambda e: e.tensor_tensor(out=QeTs[hp, oc, :], in0=pB[hp, 384:512], in1=ART[hp, 128:256], op=ALU.add),
                               [bQ, ART.B], [QeTs.B])
                            V_(lambda e: e.tensor_copy(out=Yls[:, oc, hp], in_=pA[:, 192:256]), [pA.B], [Yls.B])

                    def run_rr(gens):
                        while gens:
                            for g_ in list(gens):
                                try:
                                    next(g_)
                                except StopIteration:
                                    gens.remove(g_)

                    run_rr([prologue(tb * 4, 0, 0)])
                    for q4 in range(4):
                        ch = tb * 4 + q4
                        own = ch >= 8
                        tsl = slice(q4 * 128, (q4 + 1) * 128)
                        gens = [chain(ch, 0, own, tsl, q4 % 2), chain(ch, 1, own, tsl, q4 % 2)]
                        if q4 < 3:
                            gens.append(prologue(ch + 1, q4 + 1, (q4 + 1) % 2))
                        run_rr(gens)
                if c + 1 < NPAIRS:
                    load_pair(c + 1)
                if CUT == 17:
                    P.disabled = True
                V_(lambda e: e.memset(Sb[:], 0.0), [], [Sb.B])
                for ch in range(16):
                    if CUT == 19 and ch == 8:
                        P.disabled = True
                    if ch >= 8:
                        oc = ch - 8
                        mm(ps[0][:, 0:128], QeTs[:, oc, :], Sb[:, :], True, True, [QeTs.B, Sb.B], ps[0].B)
                        V_(lambda e, oc=oc: e.tensor_tensor(out=Yt[:], in0=ps[0][:, 0:128], in1=Yls[:, oc, :], op=ALU.add), [ps[0].B, Yls.B], [Yt.B])
                    if CUT == 20 and ch == 8:
                        P.disabled = True
                    if ch < 15:
                        mm(ps[1][:, 0:128], GTs[:, ch, :], Sb[:, :], True, True, [GTs.B, Sb.B], ps[1].B)
                        for hh in range(2):
                            hp = slice(hh * 64, (hh + 1) * 64)
                            V_(lambda e, ch=ch, hp=hp: e.tensor_tensor(out=Sb[hp, hp], in0=ps[1][hp, hp], in1=Hs[hp, ch, :], op=ALU.add), [ps[1].B, Hs.B], [Sb.B])
                    if CUT == 21 and ch == 8:
                        P.disabled = True
                    if ch >= 8:
                        for hh in range(2):
                            V_(lambda e, hh=hh: e.bn_stats(out=gst[:, hh, :], in_=Yt[:, hh * 64:(hh + 1) * 64]), [Yt.B], [gst.B])
                            V_(lambda e, hh=hh: e.bn_aggr(out=gmv[:, hh, :], in_=gst[:, hh, :]), [gst.B], [gmv.B])
                        A_(lambda e: e.activation(out=grs[:], in_=gmv[:, :, 1], func=AF.Sqrt, bias=EPSC[:, 1:2], scale=1.0), [gmv.B, EPSC.B], [grs.B])
                        V_(lambda e: e.reciprocal(out=grs[:], in_=grs[:]), [grs.B], [grs.B])
                        for hh in range(2):
                            hs_ = slice(hh * 64, (hh + 1) * 64)
                            V_(lambda e, hh=hh, hs_=hs_: e.tensor_scalar(out=yn[:, hs_], in0=Yt[:, hs_], scalar1=gmv[:, hh, 0:1], scalar2=grs[:, hh:hh + 1],
                                                                       op0=ALU.subtract, op1=ALU.mult), [Yt.B, gmv.B, grs.B], [yn.B])
                        V_(lambda e, c=c: e.tensor_tensor(out=yn[:], in0=yn[:], in1=pp3[:, 1, :], op=ALU.mult), [yn.B, pp3.B], [yn.B])
                        V_(lambda e, c=c: e.tensor_tensor(out=yn[:], in0=yn[:], in1=pp3[:, 2, :], op=ALU.add), [yn.B, pp3.B], [yn.B])
                        for hh in range(2):
                            hs_ = slice(hh * 64, (hh + 1) * 64)
                            V_(lambda e, hh=hh, hs_=hs_, oc=oc: e.scalar_tensor_tensor(out=yn[:, hs_], in0=Vtm[:, oc, hs_], scalar=sbon[:, oc, hh:hh + 1], in1=yn[:, hs_],
                                                                                      op0=ALU.mult, op1=ALU.add), [Vtm.B, sbon.B, yn.B], [yn.B])
                        V_(lambda e, oc=oc: e.tensor_tensor(out=ybr[:], in0=yn[:], in1=gtm[:, oc, :], op=ALU.mult), [yn.B, gtm.B], [ybr.B])
                        if CUT == 22 and ch == 8:
                            P.disabled = True
                        pv = ps[3].t[:].bitcast(BF16)
                        P.op("tensor", lambda e, pv=pv: e.transpose(out=pv[:, 0:128], in_=ybr[:], identity=identb), reads=[ybr.B, cstb.B], writes=[ps[3].B])
                        if CUT == 23 and ch == 8:
                            P.disabled = True
                        A_(lambda e, pv=pv, c=c, oc=oc: e.activation(out=yT[:, 8 + c, oc * 128:(oc + 1) * 128], in_=pv[:, 0:128], func=AF.Copy), [ps[3].B], [yT.b[8 + c]])
            if CUT == 25:
                P.disabled = True
            if dbg and stop == 3 and CUT == 77:
                d6 = T(P, st, "d6", [128, 1024], F32)
                def dcp(c0, w, ap, bufs):
                    V_(lambda e: e.tensor_copy(out=d6[:, c0:c0 + w], in_=ap), bufs, [d6.B])
                def dsend(sl_):
                    P.dma("sync", lambda e: e.dma_start(out=dbg_d[sl_], in_=d6[:]), d6.B, reads=[d6.B])
                V_(lambda e: e.memset(d6[:], 0.0), [], [d6.B])
                dcp(0, 512, ex[:].rearrange("p a b -> p (a b)"), [ex.B]); dcp(512, 128, Xf[:], [Xf.B]); dcp(640, 128, lwt[:], [lwt.B])
                dcp(768, 128, Yt[:], [Yt.B]); dcp(896, 128, yn[:], [yn.B])
                dsend(6)
                dcp(0, 256, ART[:], [ART.B]); dcp(256, 128, BhT[:], [BhT.B]); dcp(384, 128, KhT[:], [KhT.B])
                dcp(512, 512, TMt[:].rearrange("p a b -> p (a b)"), [TMt.B])
                dsend(7)
                dcp(0, 512, MT[:].rearrange("p a b -> p (a b)"), [MT.B]); dcp(512, 64, GTs[:, 15, 0:64], [GTs.B]); dcp(576, 64, Hs[:, 15, :], [Hs.B])
                dcp(640, 128, QeTs[:, 7, :], [QeTs.B]); dcp(768, 128, Yls[:, 7, :], [Yls.B]); dcp(896, 64, Sb[:, 0:64], [Sb.B])
                dcp(960, 2, sbon[:, 7, :], [sbon.B])
                dsend(8)
                dbg_bufs.append(d6.B)
            dump(st, 4, yT[:, 8, 0:512], [yT.b[8]], 512)
            if not (dbg and stop == 3):
                dump(st, 5, yT[:, 7 + NPAIRS, :], [yT.b[7 + NPAIRS]], 1024)
            if stop == 3:
                P.wait_all("sync", dbg_bufs)
            P.flush()
            if stop == 3:
                P.disabled = True

        stA.close()
        with ExitStack() as st:
            acc = T(P, st, "acc", [128, 8, D], F32, nb=8)
            wt_ = T(P, st, "wt", [128, 8, 32], F32)
            accB = acc.b
            gb2 = [None, None]
            ws = [None, None, None]

            class AccT:
                def __init__(self, i):
                    self.i = i; self.B = accB[i]

                def __getitem__(self, k):
                    return acc[:, self.i, :] if k == slice(None) else acc[:, self.i, k[1]]

            def load_gb(gs, bs):
                P.dma("sync", lambda e: e.dma_start(out=gb2[0][:], in_=gs.partition_broadcast(128)), gb2[0].B, writes=[gb2[0].B])
                if bs is not None:
                    P.dma("sync", lambda e: e.dma_start(out=gb2[1][:], in_=bs.partition_broadcast(128)), gb2[1].B, writes=[gb2[1].B])

            with ExitStack() as st2:
                gb2[0] = T(P, st2, "gb2a0", [128, D], F32); gb2[1] = T(P, st2, "gb2a1", [128, D], F32)
                for i in range(3):
                    ws[i] = T(P, st2, "wsa%d" % i, [128, 16, 512], BF16)
                xt = [T(P, st2, "xt4%d" % i, [128, D], F32) for i in range(2)]
                xn = T(P, st2, "xn4", [128, D], F32)
                stt4 = T(P, st2, "stt4", [128, 4, 6], F32); mv4 = T(P, st2, "mv4", [128, 4], F32)
                load_gb(ln_in_g, ln_in_b)
                for i in range(8):
                    X = xt[i % 2]
                    P.dma("sync", lambda e, X=X, i=i: e.dma_start(out=X[:], in_=xs[(8 + i) * 128:(9 + i) * 128, :]), X.B, writes=[X.B])
                    ln_tile(P, X, xn, stt4, mv4, gb2[0], gb2[1], None, AccT(i))
                    A_(lambda e, i=i: e.activation(out=acc[:, i, :], in_=acc[:, i, :], func=AF.Copy, scale=ALPHA), [accB[i]], [accB[i]])
                w_out_v = w_out.rearrange("(kc p) n -> p kc n", p=128)
                for n in range(4):
                    W_ = ws[n % 3]
                    P.dma("gpsimd", lambda e, W_=W_, n=n: e.dma_start(out=W_[:], in_=w_out_v[:, :, n * 512:(n + 1) * 512]), W_.B, writes=[W_.B])
                    for i in range(8):
                        pst = ps[i % 2]
                        for kc in range(16):
                            mm(pst[:, :], yT[:, kc, i * 128:(i + 1) * 128], W_[:, kc, :], kc == 0, kc == 15, [yT.b[kc], W_.B], pst.B)
                        V_(lambda e, i=i, n=n, pst=pst: e.tensor_tensor(out=acc[:, i, n * 512:(n + 1) * 512], in0=acc[:, i, n * 512:(n + 1) * 512], in1=pst[:, :], op=ALU.add),
                           [accB[i], pst.B], [accB[i]])
                dump(st2, 6, acc[:, 0, 0:1024], [accB[0]], 1024)
                if stop == 4:
                    P.wait_all("sync", dbg_bufs)
                P.flush()
                if stop == 4:
                    P.disabled = True
            hT1 = T(P, st, "hT1", [128, 16, 1024], BF16, nb=8)
            with ExitStack() as st2:
                gb2[0] = T(P, st2, "gb2b0", [128, D], F32); gb2[1] = T(P, st2, "gb2b1", [128, D], F32)
                xn = T(P, st2, "xn4b", [128, D], F32)
                hb4 = T(P, st2, "hb4", [128, D], BF16)
                hlo = T(P, st2, "hlo", [128, D], BF16)
                stt4 = T(P, st2, "stt4b", [128, 4, 6], F32); mv4 = T(P, st2, "mv4b", [128, 4], F32)
                hT1l = T(P, st2, "hT1l", [128, 16, 128], BF16)
                load_gb(ln1_g, ln1_b)
                rwf = T(P, st2, "rwf", [128, 16, 36], F32); rwh = T(P, st2, "rwh", [128, 16, 36], BF16); rwl = T(P, st2, "rwl", [128, 16, 36], BF16)
                rbias = T(P, st2, "rbias", [128, 36], F32)
                P.dma("sync", lambda e: e.dma_start(out=rwf[:, :, 0:4], in_=rgw.rearrange("(kc p) n -> p kc n", p=128)), rwf.B, writes=[rwf.B])
                P.dma("sync", lambda e: e.dma_start(out=rwf[:, :, 4:36], in_=rew.rearrange("(kc p) n -> p kc n", p=128)), rwf.B, writes=[rwf.B])
                P.dma("sync", lambda e: e.dma_start(out=rbias[:, 0:4], in_=rgb.partition_broadcast(128)), rbias.B, writes=[rbias.B])
                P.dma("sync", lambda e: e.dma_start(out=rbias[:, 4:36], in_=reb.partition_broadcast(128)), rbias.B, writes=[rbias.B])
                V_(lambda e: e.tensor_copy(out=rwh[:], in_=rwf[:]), [rwf.B], [rwh.B])
                V_(lambda e: e.tensor_tensor(out=rwf[:], in0=rwf[:], in1=rwh[:], op=ALU.subtract), [rwf.B, rwh.B], [rwf.B])
                V_(lambda e: e.tensor_copy(out=rwl[:], in_=rwf[:]), [rwf.B], [rwl.B])
                L = T(P, st2, "L", [128, 36], F32); r1 = T(P, st2, "r1", [128, 8], F32)
                gm = T(P, st2, "gm", [128, 4], F32); elm = T(P, st2, "elm", [128, 32], F32); ee = T(P, st2, "ee", [128, 32], F32)
                m1 = T(P, st2, "m1", [128, 32], F32); e2 = T(P, st2, "e2", [128, 32], F32); m2 = T(P, st2, "m2", [128, 32], F32)
                for i in range(8):
                    A = AccT(i)
                    ln_tile(P, A, xn, stt4, mv4, gb2[0], gb2[1], hb4, A)
                    transpose_to_fm(P, hb4, hT1, i, ps, identb, cstb.B, pbase=2)
                    V_(lambda e, i=i: e.tensor_tensor(out=xn[:], in0=acc[:, i, :], in1=hb4[:], op=ALU.subtract), [accB[i], hb4.B], [xn.B])
                    A_(lambda e: e.activation(out=hlo[:], in_=xn[:], func=AF.Copy), [xn.B], [hlo.B])
                    transpose_to_fm(P, hlo, hT1l, 0, ps, identb, cstb.B, pbase=4)
                    tsl = slice(i * 128, (i + 1) * 128)
                    n_mm = 0
                    for (hT_, w_, sl_, bi_) in ((hT1, rwh, tsl, i), (hT1l, rwh, slice(0, 128), 0), (hT1, rwl, tsl, i)):
                        for kc in range(16):
                            mm(ps[6][:, 0:36], hT_[:, kc, sl_], w_[:, kc, :], n_mm == 0, n_mm == 47, [hT_.b[bi_], w_.B], ps[6].B)
                            n_mm += 1
                    V_(lambda e: e.tensor_tensor(out=L[:], in0=ps[6][:, 0:36], in1=rbias[:], op=ALU.add), [ps[6].B, rbias.B], [L.B])
                    V_(lambda e: e.reduce_max(out=r1[:, 0:1], in_=L[:, 0:4], axis=AX.X), [L.B], [r1.B])
                    V_(lambda e: e.tensor_scalar(out=gm[:], in0=L[:, 0:4], scalar1=r1[:, 0:1], scalar2=None, op0=ALU.is_ge), [L.B, r1.B], [gm.B])
                    V_(lambda e: e.tensor_scalar(out=r1[:, 1:2], in0=r1[:, 0:1], scalar1=-1.0, scalar2=None, op0=ALU.mult), [r1.B], [r1.B])
                    A_(lambda e: e.activation(out=ee[:, 0:4], in_=L[:, 0:4], func=AF.Exp, bias=r1[:, 1:2], scale=1.0, accum_out=r1[:, 2:3]), [L.B, r1.B], [ee.B, r1.B])
                    V_(lambda e: e.reciprocal(out=r1[:, 3:4], in_=r1[:, 2:3]), [r1.B], [r1.B])
                    V_(lambda e: e.tensor_scalar(out=gm[:], in0=gm[:], scalar1=-1.0, scalar2=-NEG, op0=ALU.add, op1=ALU.mult), [gm.B], [gm.B])
                    V_(lambda e: e.tensor_tensor(out=elm[:].rearrange("p (g e) -> p g e", e=8), in0=L[:, 4:36].rearrange("p (g e) -> p g e", e=8),
                                                 in1=gm[:].unsqueeze(2).to_broadcast([128, 4, 8]), op=ALU.add), [L.B, gm.B], [elm.B])
                    V_(lambda e: e.reduce_max(out=r1[:, 4:5], in_=elm[:], axis=AX.X), [elm.B], [r1.B])
                    V_(lambda e: e.tensor_scalar(out=r1[:, 5:6], in0=r1[:, 4:5], scalar1=-1.0, scalar2=None, op0=ALU.mult), [r1.B], [r1.B])
                    A_(lambda e: e.activation(out=ee[:], in_=elm[:], func=AF.Exp, bias=r1[:, 5:6], scale=1.0), [elm.B, r1.B], [ee.B])
                    V_(lambda e: e.tensor_scalar(out=m1[:], in0=elm[:], scalar1=r1[:, 4:5], scalar2=None, op0=ALU.is_ge), [elm.B, r1.B], [m1.B])
                    V_(lambda e: e.tensor_tensor(out=e2[:], in0=ee[:], in1=m1[:], op=ALU.mult), [ee.B, m1.B], [e2.B])
                    V_(lambda e: e.tensor_tensor(out=e2[:], in0=ee[:], in1=e2[:], op=ALU.subtract), [ee.B, e2.B], [e2.B])
                    V_(lambda e: e.reduce_max(out=r1[:, 6:7], in_=e2[:], axis=AX.X), [e2.B], [r1.B])
                    V_(lambda e: e.tensor_scalar(out=m2[:], in0=e2[:], scalar1=r1[:, 6:7], scalar2=None, op0=ALU.is_ge), [e2.B, r1.B], [m2.B])
                    V_(lambda e: e.tensor_tensor(out=m2[:], in0=m2[:], in1=e2[:], op=ALU.mult), [m2.B, e2.B], [m2.B])
                    V_(lambda e: e.tensor_tensor(out=m1[:], in0=m1[:], in1=m2[:], op=ALU.add), [m1.B, m2.B], [m1.B])
                    V_(lambda e: e.tensor_scalar(out=r1[:, 7:8], in0=r1[:, 6:7], scalar1=1.0, scalar2=None, op0=ALU.add), [r1.B], [r1.B])
                    V_(lambda e: e.reciprocal(out=r1[:, 7:8], in_=r1[:, 7:8]), [r1.B], [r1.B])
                    V_(lambda e, i=i: e.tensor_scalar(out=wt_[:, i, :], in0=m1[:], scalar1=r1[:, 7:8], scalar2=r1[:, 3:4], op0=ALU.mult, op1=ALU.mult), [m1.B, r1.B], [wt_.B])
                    A_(lambda e, i=i: e.activation(out=acc[:, i, :], in_=acc[:, i, :], func=AF.Copy, scale=ALPHA), [accB[i]], [accB[i]])
                dump(st2, 7, wt_[:, 0, :], [wt_.B], 32)
                if stop == 5:
                    P.wait_all("sync", dbg_bufs)
                P.flush()
                if stop == 5:
                    P.disabled = True
            with ExitStack() as st2:
                for i in range(3):
                    ws[i] = T(P, st2, "wsc%d" % i, [128, 16, 512], BF16)
                hid = [T(P, st2, "hid0", [128, 4, 1024], BF16)] * 2
                sg = [T(P, st2, "sg%d" % i, [128, 512], BF16) for i in range(2)]
                nld = 0
                for ex_ in range(NEXP):
                    Wg, Wu, Wd = ws[0], ws[1], ws[2]
                    P.dma("gpsimd", lambda e, ex_=ex_: e.dma_start(out=ws[0][:], in_=wg_d[ex_].rearrange("(kc p) n -> p kc n", p=128)), ws[0].B, writes=[ws[0].B])
                    P.dma("gpsimd", lambda e, ex_=ex_: e.dma_start(out=ws[1][:], in_=wu_d[ex_].rearrange("(kc p) n -> p kc n", p=128)), ws[1].B, writes=[ws[1].B])
                    P.dma("gpsimd", lambda e, ex_=ex_: e.dma_start(out=ws[2][:].rearrange("p (a b) n -> p a b n", b=4), in_=wd_d[ex_].rearrange("(fc p) (b n) -> p fc b n", p=128, n=512)),
                          ws[2].B, writes=[ws[2].B])
                    H_ = hid[ex_ % 2]
                    k_ = 0
                    for tb in range(2):
                        for fc in range(4):
                            pg, pu = ps[(k_ % 2) * 2], ps[(k_ % 2) * 2 + 1]
                            S_ = sg[k_ % 2]
                            for kc in range(16):
                                mm(pg[:, :], Wg[:, kc, fc * 128:(fc + 1) * 128], hT1[:, kc, tb * 512:(tb + 1) * 512], kc == 0, kc == 15, [Wg.B] + hT1.b[tb * 4:(tb + 1) * 4], pg.B)
                            for kc in range(16):
                                mm(pu[:, :], Wu[:, kc, fc * 128:(fc + 1) * 128], hT1[:, kc, tb * 512:(tb + 1) * 512], kc == 0, kc == 15, [Wu.B] + hT1.b[tb * 4:(tb + 1) * 4], pu.B)
                            A_(lambda e, S_=S_, pg=pg: e.activation(out=S_[:], in_=pg[:, :], func=AF.Silu), [pg.B], [S_.B])
                            V_(lambda e, S_=S_, pu=pu, H_=H_, fc=fc, tb=tb: e.tensor_tensor(out=H_[:, fc, tb * 512:(tb + 1) * 512], in0=S_[:], in1=pu[:, :], op=ALU.mult),
                               [S_.B, pu.B], [H_.B])
                            k_ += 1
                    for i in range(8):
                        for n in range(4):
                            po = ps[4 + (i * 4 + n) % 4]
                            for fc in range(4):
                                mm(po[:, :], H_[:, fc, i * 128:(i + 1) * 128], Wd[:, fc * 4 + n, :], fc == 0, fc == 3, [H_.B, Wd.B], po.B)
                            V_(lambda e, i=i, n=n, po=po, ex_=ex_: e.scalar_tensor_tensor(out=acc[:, i, n * 512:(n + 1) * 512], in0=po[:, :], scalar=wt_[:, i, ex_:ex_ + 1],
                                                                                  in1=acc[:, i, n * 512:(n + 1) * 512], op0=ALU.mult, op1=ALU.add), [po.B, wt_.B, accB[i]], [accB[i]])
                dump(st2, 8, acc[:, 0, 0:1024], [accB[0]], 1024)
                if stop == 6:
                    P.wait_all("sync", dbg_bufs)
                P.flush()
                if stop == 6:
                    P.disabled = True
            with ExitStack() as st2:
                gb2[0] = T(P, st2, "gb2d0", [128, D], F32); gb2[1] = T(P, st2, "gb2d1", [128, D], F32)
                ws[0] = T(P, st2, "wsd0", [128, 16, 512], BF16)
                xn = T(P, st2, "xn5", [128, D], F32); hb4 = T(P, st2, "hb5", [128, D], BF16)
                stt4 = T(P, st2, "stt5", [128, 4, 6], F32); mv4 = T(P, st2, "mv5", [128, 4], F32)
                pf = T(P, st2, "pf", [128, 256], F32); pb = T(P, st2, "pb", [128, 256], BF16)
                pT = T(P, st2, "pT", [128, 2, 1024], BF16, nb=8)
                plw = T(P, st2, "plw", [128, 2, D], BF16)
                ssq = T(P, st2, "ssq", [128, 8, 4], F32); rsp = T(P, st2, "rsp", [128, 8], F32)
                sgt = T(P, st2, "sgt", [128, 512], F32); t1_ = T(P, st2, "t1", [128, 512], F32)
                load_gb(ln2_g, ln2_b)
                P.dma("gpsimd", lambda e: e.dma_start(out=plw[:], in_=ple_w.rearrange("(kc p) n -> p kc n", p=128)), plw.B, writes=[plw.B])
                if CUT == 31:
                    P.disabled = True
                for i in range(8):
                    A = AccT(i)
                    ln_tile(P, A, xn, stt4, mv4, gb2[0], gb2[1], hb4, A)
                    transpose_to_fm(P, hb4, hT1, i, ps, identb, cstb.B, pbase=2)
                    P.dma("sync", lambda e, i=i: e.dma_start(out=pf[:], in_=p_in[i * 128:(i + 1) * 128, :]), pf.B, writes=[pf.B])
                    V_(lambda e: e.tensor_copy(out=pb[:], in_=pf[:]), [pf.B], [pb.B])
                    pv = ps[4].t[:].bitcast(BF16)
                    for k in range(2):
                        P.op("tensor", lambda e, k=k, pv=pv: e.transpose(out=pv[:, k * 128:(k + 1) * 128], in_=pb[:, k * 128:(k + 1) * 128], identity=identb), reads=[pb.B, cstb.B], writes=[ps[4].B])
                    V_(lambda e, i=i, pv=pv: e.tensor_copy(out=pT[:, :, i * 128:(i + 1) * 128], in_=pv[:, 0:256].rearrange("p (k t) -> p k t", t=128)), [ps[4].B], [pT.b[i]])
                    for n in range(4):
                        pp = ps[5 + n % 2]
                        for k in range(2):
                            mm(pp[:, :], pT[:, k, i * 128:(i + 1) * 128], plw[:, k, n * 512:(n + 1) * 512], k == 0, k == 1, [pT.b[i], plw.B], pp.B)
                        A_(lambda e, i=i, n=n, pp=pp: e.activation(out=t1_[:], in_=pp[:, :], func=AF.Square, accum_out=ssq[:, i, n:n + 1]), [pp.B], [t1_.B, ssq.B])
                if CUT == 32:
                    P.disabled = True
                V_(lambda e: e.reduce_sum(out=rsp[:], in_=ssq[:], axis=AX.X), [ssq.B], [rsp.B])
                A_(lambda e: e.activation(out=rsp[:], in_=rsp[:], func=AF.Sqrt, bias=EPSC[:, 0:1], scale=1.0 / D), [rsp.B, EPSC.B], [rsp.B])
                V_(lambda e: e.reciprocal(out=rsp[:], in_=rsp[:]), [rsp.B], [rsp.B])
                P.dma("sync", lambda e: e.dma_start(out=gb2[0][:], in_=ple_ng.partition_broadcast(128)), gb2[0].B, writes=[gb2[0].B])
                if CUT == 33:
                    P.disabled = True
                pgw_v = ple_gw.rearrange("(kc p) n -> p kc n", p=128)
                for n in range(4):
                    W_ = ws[0]
                    P.dma("gpsimd", lambda e, W_=W_, n=n: e.dma_start(out=W_[:], in_=pgw_v[:, :, n * 512:(n + 1) * 512]), W_.B, writes=[W_.B])
                    nsl = slice(n * 512, (n + 1) * 512)
                    for i in range(8):
                        pgt, pp = ps[(i % 2) * 2], ps[(i % 2) * 2 + 1]
                        for kc in range(16):
                            mm(pgt[:, :], hT1[:, kc, i * 128:(i + 1) * 128], W_[:, kc, :], kc == 0, kc == 15, [hT1.b[i], W_.B], pgt.B)
                        for k in range(2):
                            mm(pp[:, :], pT[:, k, i * 128:(i + 1) * 128], plw[:, k, nsl], k == 0, k == 1, [pT.b[i], plw.B], pp.B)
                        A_(lambda e, pgt=pgt: e.activation(out=sgt[:], in_=pgt[:, :], func=AF.Sigmoid), [pgt.B], [sgt.B])
                        V_(lambda e, i=i, pp=pp, nsl=nsl: e.scalar_tensor_tensor(out=t1_[:], in0=pp[:, :], scalar=rsp[:, i:i + 1], in1=gb2[0][:, nsl], op0=ALU.mult, op1=ALU.mult),
                           [pp.B, rsp.B, gb2[0].B], [t1_.B])
                        V_(lambda e: e.tensor_tensor(out=t1_[:], in0=t1_[:], in1=sgt[:], op=ALU.mult), [t1_.B, sgt.B], [t1_.B])
                        V_(lambda e, i=i, nsl=nsl: e.tensor_tensor(out=acc[:, i, nsl], in0=acc[:, i, nsl], in1=t1_[:], op=ALU.add), [accB[i], t1_.B], [accB[i]])
                        P.dma("sync", lambda e, i=i, nsl=nsl: e.dma_start(out=out_d[i * 128:(i + 1) * 128, nsl], in_=acc[:, i, nsl]), accB[i], reads=[accB[i]])
                if CUT == 34:
                    P.disabled = True
                P.wait_all("sync", accB + dbg_bufs)
                if stop == 7:
                    P.wait_all("sync", dbg_bufs)
                P.flush()
      except _Stop:
        pass
    return nc


EPSC = None


def rsqrt(P, out, in_, outB, inB, eps_ap):
    P.op("scalar", lambda e: e.activation(out=out, in_=in_, func=AF.Sqrt, bias=eps_ap, scale=1.0), reads=[inB, EPSC.B], writes=[outB])
    P.op("vector", lambda e: e.reciprocal(out=out, in_=out), reads=[outB], writes=[outB])


def ln_tile(P, X, XN, ST, MV, gbc, bbc, out_bf, out_f32, eps=1e-5):
    for j in range(4):
        P.op("vector", lambda e, j=j: e.bn_stats(out=ST[:, j, :], in_=X[:, j * 512:(j + 1) * 512]), reads=[X.B], writes=[ST.B])
    P.op("vector", lambda e: e.bn_aggr(out=MV[:, 0:2], in_=ST[:]), reads=[ST.B], writes=[MV.B])
    rsqrt(P, MV[:, 2:3], MV[:, 1:2], MV.B, MV.B, EPSC[:, 0:1])
    P.op("vector", lambda e: e.scalar_tensor_tensor(out=MV[:, 3:4], in0=MV[:, 0:1], scalar=-1.0, in1=MV[:, 2:3], op0=ALU.mult, op1=ALU.mult),
         reads=[MV.B], writes=[MV.B])
    P.op("scalar", lambda e: e.activation(out=XN[:], in_=X[:], func=AF.Identity, bias=MV[:, 3:4], scale=MV[:, 2:3]),
         reads=[X.B, MV.B], writes=[XN.B])
    P.op("vector", lambda e: e.tensor_tensor(out=XN[:], in0=XN[:], in1=gbc[:], op=ALU.mult), reads=[XN.B, gbc.B], writes=[XN.B])
    if out_f32 is not None:
        P.op("vector", lambda e: e.tensor_tensor(out=out_f32[:], in0=XN[:], in1=bbc[:], op=ALU.add), reads=[XN.B, bbc.B], writes=[out_f32.B])
        if out_bf is not None:
            P.op("scalar", lambda e: e.activation(out=out_bf[:], in_=out_f32[:], func=AF.Copy), reads=[out_f32.B], writes=[out_bf.B])
    else:
        P.op("vector", lambda e: e.tensor_tensor(out=out_bf[:], in0=XN[:], in1=bbc[:], op=ALU.add), reads=[XN.B, bbc.B], writes=[out_bf.B])


def transpose_to_fm(P, HB, dstT, ti, ps, identb, identB, pbase=0):
    for half in range(2):
        pst = ps[pbase + half]
        pv = pst.t[:].bitcast(BF16)
        for j in range(8):
            kc = half * 8 + j
            P.op("tensor", lambda e, kc=kc, j=j, pv=pv: e.transpose(out=pv[:, j * 128:(j + 1) * 128], in_=HB[:, kc * 128:(kc + 1) * 128], identity=identb),
                 reads=[HB.B, identB], writes=[pst.B])
        eng = "scalar" if half == 0 else "vector"
        dst = dstT[:, half * 8:(half + 1) * 8, ti * 128:(ti + 1) * 128]
        src = pv.rearrange("p (j t) -> p j t", t=128)
        if eng == "scalar":
            P.op("scalar", lambda e, dst=dst, src=src: e.activation(out=dst, in_=src, func=AF.Copy), reads=[pst.B], writes=[dstT.b[ti]])
        else:
            P.op("vector", lambda e, dst=dst, src=src: e.tensor_copy(out=dst, in_=src), reads=[pst.B], writes=[dstT.b[ti]])


def make_consts():
    c = np.zeros((8, 128, 128), np.float32)
    i = np.arange(128)
    c[0] = np.eye(128)
    c[1] = (i[:, None] <= i[None, :]) * -EM05
    c[2] = (i[:, None] < i[None, :]) * -EM05
    c[3] = (i[:, None] > i[None, :]) * -EM05
    c[4] = (i[None, :] > i[:, None])
    c[5] = (i[None, :] >= i[:, None])
    c[6] = (i[:, None] > i[None, :])
    c[7] = (i[:, None] // 64 == i[None, :] // 64)
    n = np.arange(-127, 256)
    nn = np.maximum(n, 0)
    nf = np.maximum(nn, 1).astype(np.float32)
    large = 16 + (np.log(nf / 16) / math.log(128 / 16) * 16).astype(np.int32)
    large = np.minimum(large, 31)
    bucket = np.where(nn < 16, nn, large)
    oh = np.zeros((33, 383), np.float32)
    oh[bucket, np.arange(383)] = 1.0
    oh[:32, n < 0] = 0.0
    oh[32, n < 0] = 1.0
    return c, oh


_CACHE = {}


def kernel(**inputs):
    dbg = inputs.pop("_dbg", None)
    stop = inputs.pop("_stop", 99)
    key = ("nc", dbg, stop)
    if key not in _CACHE:
        _CACHE[key] = build(dbg, stop)
    nc = _CACHE[key]
    x = np.asarray(inputs["x"], np.float32)
    p = np.asarray(inputs["p"], np.float32)
    cm, oh = make_consts()
    shared = {"cmat": cm, "onehot": oh}
    for k, v in inputs.items():
        if k in ("x", "p"):
            continue
        a = np.asarray(v, np.float32)
        if a.shape[0] == 1 and k not in ("ln_in_g", "ln_in_b"):
            a = a[0]
        if k in ("moe_w_gate", "moe_w_up", "moe_w_down"):
            a = a.reshape(32, a.shape[-2], a.shape[-1])
            if stop < 6:
                a = a[:1]
        if k in ("rwkv_r_k",):
            a = a.reshape(-1)
        shared[k] = np.ascontiguousarray(a)
    in_maps = []
    for c in range(8):
        b, s = c // 2, c % 2
        seq = np.zeros((2048, 2048), np.float32)
        if s == 1:
            seq[:] = x[b]
        else:
            seq[1024:] = x[b, :1024]
        m = dict(shared)
        m["xs"] = seq
        m["p"] = np.ascontiguousarray(p[0, b, s * 1024:(s + 1) * 1024])
        m["flag"] = np.full((128, 1), float(s), np.float32)
        in_maps.append(m)
    res = run_bass_kernel_spmd(nc, in_maps, core_ids=list(range(8)))
    if dbg:
        return [(r["dbg"], r["out"]) for r in res.results]
    out = np.zeros((4, 2048, 2048), np.float32)
    for c in range(8):
        b, s = c // 2, c % 2
        out[b, s * 1024:(s + 1) * 1024] = res.results[c]["out"]
    return out
```

```python
import math
from contextlib import ExitStack
import numpy as np
import concourse.bass as bass
import concourse.mybir as mybir
from concourse.bass_utils import run_bass_kernel_spmd

F32 = mybir.dt.float32
BF16 = mybir.dt.bfloat16
AF = mybir.ActivationFunctionType
ALU = mybir.AluOpType
AX = mybir.AxisListType
ENGS = ("sync", "scalar", "vector", "gpsimd", "tensor")
ALPHA = 2.0 ** 0.25
LAMBDA_INIT = 0.8 - 0.6
NEG = -1.0e30
EM05 = math.exp(-0.5)
NHEADS = 8
NPAIRS = 8
CUT = 0
NEXP = 32
OUTSEL = tuple(range(8))


class Buf:
    __slots__ = ("name", "w", "r", "dsem", "dcnt")

    def __init__(self, name):
        self.name = name
        self.w = None
        self.r = []
        self.dsem = None
        self.dcnt = 0


class Prog:
    def __init__(self, nc, stack):
        self.nc = nc
        self.stack = stack
        self.ops = {e: [] for e in ENGS}
        self.sem = {e: stack.enter_context(nc.semaphore("c_" + e)) for e in ("scalar", "vector", "gpsimd", "tensor")}
        self.cnt = {e: 0 for e in ENGS}
        self.seen = {e: {} for e in ENGS}
        self.semobj = {}
        self.nbuf = 0
        self.disabled = False
        self.dbufs = []

    def buf(self, name=None):
        self.nbuf += 1
        return Buf(name or ("b%d" % self.nbuf))

    def _deps(self, eng, reads, writes):
        deps = []
        for b in reads:
            if b.w is not None:
                deps.append(b.w)
        for b in writes:
            if b.w is not None:
                deps.append(b.w)
            deps.extend(b.r)
        waits = {}
        for (sid, val, src) in deps:
            if src == "tensor" and eng == "tensor":
                continue
            if self.seen[eng].get(sid, 0) >= val:
                continue
            if waits.get(sid, 0) < val:
                waits[sid] = val
        for sid, val in waits.items():
            self.seen[eng][sid] = val
        return [(self.semobj[sid], val) for sid, val in waits.items()]

    def op(self, eng, fn, reads=(), writes=()):
        if self.disabled:
            return None
        waits = self._deps(eng, reads, writes)
        self.cnt[eng] += 1
        sem = self.sem[eng]
        self.semobj[id(sem)] = sem
        tok = (id(sem), self.cnt[eng], eng)
        self.ops[eng].append((waits, fn, (sem, 1)))
        for b in reads:
            b.r.append(tok)
        for b in writes:
            b.w = tok
            b.r = []
        return tok

    def dma(self, eng, fn, sbuf_buf, reads=(), writes=()):
        if self.disabled:
            return None
        waits = self._deps(eng, reads, writes)
        if sbuf_buf.dsem is None:
            sbuf_buf.dsem = self.stack.enter_context(self.nc.semaphore("d_" + sbuf_buf.name))
            self.semobj[id(sbuf_buf.dsem)] = sbuf_buf.dsem
            self.dbufs.append(sbuf_buf)
        sbuf_buf.dcnt += 16
        tok = (id(sbuf_buf.dsem), sbuf_buf.dcnt, "dma")
        self.ops[eng].append((waits, fn, (sbuf_buf.dsem, 16)))
        for b in reads:
            b.r.append(tok)
        for b in writes:
            b.w = tok
            b.r = []
        return tok

    def wait_all(self, eng, bufs):
        if self.disabled:
            return
        waits = self._deps(eng, bufs, bufs)
        self.ops[eng].append((waits, None, None))

    def barrier(self):
        toks = []
        for x in ("scalar", "vector", "gpsimd", "tensor"):
            if self.cnt[x] > 0:
                self.semobj[id(self.sem[x])] = self.sem[x]
                toks.append((id(self.sem[x]), self.cnt[x]))
        for b in self.dbufs:
            toks.append((id(b.dsem), b.dcnt))
        for eng in ENGS:
            waits = []
            for sid, val in toks:
                if self.seen[eng].get(sid, 0) >= val:
                    continue
                self.seen[eng][sid] = val
                waits.append((self.semobj[sid], val))
            if waits:
                self.ops[eng].append((waits, None, None))

    def flush(self):
        self.barrier()
        nc = self.nc
        ops = self.ops
        self.ops = {e: [] for e in ENGS}

        def replay(lst, e):
            for waits, fn, inc in lst:
                for sem, val in waits:
                    e.wait_ge(sem, val)
                if fn is not None:
                    ins = fn(e)
                    ins.then_inc(inc[0], inc[1])

        with nc.Block() as block:
            @block.sync
            def _(e):
                replay(ops["sync"], e)

            @block.scalar
            def _(e):
                replay(ops["scalar"], e)

            @block.vector
            def _(e):
                replay(ops["vector"], e)

            @block.gpsimd
            def _(e):
                replay(ops["gpsimd"], e)

            @block.tensor
            def _(e):
                replay(ops["tensor"], e)


class T:
    def __init__(self, P, stack, name, shape, dtype, psum=False, nb=1):
        nc = P.nc
        if psum:
            self.t = stack.enter_context(nc.psum_tensor("t_" + name, shape, dtype))
        else:
            self.t = stack.enter_context(nc.sbuf_tensor("t_" + name, shape, dtype))
        self.b = [P.buf(name + "_%d" % i) for i in range(nb)]
        self.B = self.b[0]

    def __getitem__(self, k):
        return self.t[k]


class _Stop(Exception):
    pass


def build(dbg=None, stop=99):
    nc = bass.Bass("TRN2", target_bir_lowering=False)
    D = 2048
    dram = {}

    def din(name, shape):
        dram[name] = nc.dram_tensor(name, list(shape), F32, kind="ExternalInput").ap()
        return dram[name]

    xs = din("xs", [2048, D])
    p_in = din("p", [1024, 256])
    flag_d = din("flag", [128, 1])
    ln_in_g = din("ln_in_g", [D]); ln_in_b = din("ln_in_b", [D])
    rel_bias = din("rel_bias", [32, 8])
    w_in = din("w_in", [D, 6432])
    lamq1 = din("diff_lam_q1", [64]); lamk1 = din("diff_lam_k1", [64])
    lamq2 = din("diff_lam_q2", [64]); lamk2 = din("diff_lam_k2", [64])
    subln_g = din("diff_subln_g", [128])
    rwkv_mu = din("rwkv_mu", [3360])
    rwkv_w0 = din("rwkv_w0", [1024]); rwkv_w2 = din("rwkv_w2", [64, 1024])
    rwkv_a0 = din("rwkv_a0", [1024]); rwkv_a2 = din("rwkv_a2", [64, 1024])
    rwkv_g2 = din("rwkv_g2", [160, 1024])
    rwkv_k_k = din("rwkv_k_k", [1024]); rwkv_k_a = din("rwkv_k_a", [1024])
    rwkv_r_k = din("rwkv_r_k", [1024])
    lnx_g = din("rwkv_lnx_g", [1024]); lnx_b = din("rwkv_lnx_b", [1024])
    w_out = din("w_out", [D, D])
    ln1_g = din("ln1_g", [D]); ln1_b = din("ln1_b", [D])
    rgw = din("router_group_w", [D, 4]); rgb = din("router_group_b", [4])
    rew = din("router_expert_w", [D, 32]); reb = din("router_expert_b", [32])
    nE = 32 if stop >= 6 else 1
    wg_d = din("moe_w_gate", [nE, D, 512]); wu_d = din("moe_w_up", [nE, D, 512]); wd_d = din("moe_w_down", [nE, 512, D])
    ln2_g = din("ln2_g", [D]); ln2_b = din("ln2_b", [D])
    ple_w = din("ple_w", [256, D]); ple_ng = din("ple_norm_g", [D]); ple_gw = din("ple_gate_w", [D, D])
    cmat = din("cmat", [8, 128, 128])
    oh_d = din("onehot", [33, 383])
    out_d = nc.dram_tensor("out", [1024, D], F32, kind="ExternalOutput").ap()
    dbg_d = None
    if dbg:
        dbg_d = nc.dram_tensor("dbg", list(dbg), F32, kind="ExternalOutput").ap()
    trep_d = nc.dram_tensor("trep", [8, 128, 383], F32, kind="Internal").ap()

    stA = ExitStack()
    with ExitStack() as st0:
      try:
        P = Prog(nc, st0)
        stB = st0
        yT = T(P, stB, "yT", [128, 16, 1024], BF16, nb=16)
        cst = T(P, st0, "cst", [128, 8, 128], F32)
        cstb = T(P, st0, "cstb", [128, 8, 128], BF16)
        flag = T(P, st0, "flag", [128, 1], F32)
        pbias = T(P, st0, "pbias", [128, 1], F32)
        ps = [T(P, st0, "ps%d" % i, [128, 512], F32, psum=True) for i in range(8)]
        IDENT, TRI_I, TRI_E, TRI_R, UT_S, UT_I, LT_S, BLK1 = range(8)
        identb = cstb[:, IDENT, :]

        global EPSC
        EPSC = T(P, st0, "epsc", [128, 4], F32)
        P.op("vector", lambda e: e.memset(EPSC[:, 0:1], 1e-5), writes=[EPSC.B])
        P.op("vector", lambda e: e.memset(EPSC[:, 1:2], 64e-5), writes=[EPSC.B])
        P.op("vector", lambda e: e.memset(EPSC[:, 2:3], 1e-24), writes=[EPSC.B])
        P.op("vector", lambda e: e.memset(EPSC[:, 3:4], 0.0), writes=[EPSC.B])
        P.dma("sync", lambda e: e.dma_start(out=cst[:], in_=cmat.rearrange("m p n -> p m n")), cst.B, writes=[cst.B])
        P.dma("sync", lambda e: e.dma_start(out=flag[:], in_=flag_d), flag.B, writes=[flag.B])
        P.op("vector", lambda e: e.tensor_copy(out=cstb[:], in_=cst[:]), reads=[cst.B], writes=[cstb.B])
        P.op("vector", lambda e: e.tensor_scalar(out=pbias[:], in0=flag[:], scalar1=-1.0, scalar2=-NEG, op0=ALU.add, op1=ALU.mult),
             reads=[flag.B], writes=[pbias.B])

        dbg_bufs = []

        def dump(stk, slot, ap, bufs, width, parts=128):
            if not dbg or P.disabled:
                return
            tmp = T(P, stk, "dbg%d" % slot, [128, width], F32)
            P.op("vector", lambda e: e.tensor_copy(out=tmp[0:parts, :], in_=ap), reads=bufs, writes=[tmp.B])
            P.dma("sync", lambda e: e.dma_start(out=dbg_d[slot, 0:parts, 0:width], in_=tmp[0:parts, :]), tmp.B, reads=[tmp.B])
            dbg_bufs.append(tmp.B)

        def mm(out, lhsT, rhs, start, stop, reads, wB, tp=None, sgc=False):
            kw = {}
            if tp is not None:
                kw["tile_position"] = tp
            if sgc:
                kw["skip_group_check"] = True
            P.op("tensor", lambda e: e.matmul(out=out, lhsT=lhsT, rhs=rhs, start=start, stop=stop, **kw), reads=reads, writes=[wB])

        def V_(fn, reads, writes):
            P.op("vector", fn, reads=reads, writes=writes)

        def A_(fn, reads, writes):
            P.op("scalar", fn, reads=reads, writes=writes)

        def small_load(dst_ap, dstB, src_ap):
            P.dma("sync", lambda e: e.dma_start(out=dst_ap, in_=src_ap, allow_slow_non_contiguous=True), dstB, writes=[dstB])

        w_in_v = w_in.rearrange("(kc p) n -> p kc n", p=128)

        def proj_fm(wt, c0, M, tb, pst):
            for kc in range(16):
                mm(pst[0:M, 0:512], wt[:, kc, c0:c0 + M], hT0[:, kc, tb * 512:(tb + 1) * 512], kc == 0, kc == 15,
                   [wt.B] + hT0.b[tb * 4:(tb + 1) * 4], pst.B)

        def shiftmix(pst, M, tb, R, mucol, omucol, muB, out_ap, outB, tmp):
            if tb == 0:
                V_(lambda e: e.memset(R[:], 0.0), [], [R.B])
            else:
                V_(lambda e: e.tensor_copy(out=R[:, 0:1], in_=R[:, 512:513]), [R.B], [R.B])
            if tb < 2:
                A_(lambda e: e.activation(out=R[0:M, 1:513], in_=pst[0:M, 0:512], func=AF.Copy, scale=flag[0:M, 0:1]), [pst.B, flag.B, R.B], [R.B])
            else:
                A_(lambda e: e.activation(out=R[0:M, 1:513], in_=pst[0:M, 0:512], func=AF.Copy), [pst.B, R.B], [R.B])
            V_(lambda e: e.tensor_scalar(out=tmp[0:M, :], in0=R[0:M, 1:513], scalar1=omucol, scalar2=None, op0=ALU.mult), [R.B, muB], [tmp.B])
            V_(lambda e: e.scalar_tensor_tensor(out=out_ap, in0=R[0:M, 0:512], scalar=mucol, in1=tmp[0:M, :], op0=ALU.mult, op1=ALU.add),
               [R.B, muB, tmp.B], [outB])

        mul_ = T(P, st0, "mul", [128, 8], F32)
        murkv = T(P, st0, "murkv", [128, 48], F32)
        st0.enter_context(stA)
        hT0 = T(P, stA, "hT0", [128, 16, 2048], BF16, nb=16)
        txw = T(P, stA, "txw", [64, 2048], BF16)
        xaT = T(P, stA, "xaT", [64, 2048], BF16)
        sxa = T(P, stA, "sxa", [128, 2048], BF16)
        sxb = T(P, stA, "sxb", [32, 2048], BF16)
        V_(lambda e: e.memset(mul_[:], 0.0), [], [mul_.B])
        small_load(mul_[0:64, 0:1], mul_.B, rwkv_mu[3072:3136].rearrange("(p a) -> p a", a=1))
        small_load(mul_[0:64, 1:2], mul_.B, rwkv_mu[3136:3200].rearrange("(p a) -> p a", a=1))
        small_load(mul_[0:128, 2:3], mul_.B, rwkv_mu[3200:3328].rearrange("(p a) -> p a", a=1))
        small_load(mul_[0:32, 3:4], mul_.B, rwkv_mu[3328:3360].rearrange("(p a) -> p a", a=1))
        V_(lambda e: e.tensor_scalar(out=mul_[:, 4:8], in0=mul_[:, 0:4], scalar1=-1.0, scalar2=1.0, op0=ALU.mult, op1=ALU.add), [mul_.B], [mul_.B])
        small_load(murkv[:, 0:24], murkv.B, rwkv_mu[0:3072].rearrange("(a p) -> p a", p=128))
        V_(lambda e: e.tensor_scalar(out=murkv[:, 24:48], in0=murkv[:, 0:24], scalar1=-1.0, scalar2=1.0, op0=ALU.mult, op1=ALU.add), [murkv.B], [murkv.B])

        with ExitStack() as st:
            gbc = T(P, st, "gbc", [128, D], F32); bbc = T(P, st, "bbc", [128, D], F32)
            P.dma("sync", lambda e: e.dma_start(out=gbc[:], in_=ln_in_g.partition_broadcast(128)), gbc.B, writes=[gbc.B])
            P.dma("sync", lambda e: e.dma_start(out=bbc[:], in_=ln_in_b.partition_broadcast(128)), bbc.B, writes=[bbc.B])
            wl = T(P, st, "wl", [128, 16, 288], BF16)
            P.dma("gpsimd", lambda e: e.dma_start(out=wl[:], in_=w_in_v[:, :, 6144:6432]), wl.B, writes=[wl.B])
            xt = [T(P, st, "xt%d" % i, [128, D], F32) for i in range(2)]
            xn = [T(P, st, "xn%d" % i, [128, D], F32) for i in range(2)]
            hb = [T(P, st, "hb%d" % i, [128, D], BF16) for i in range(2)]
            stt = [T(P, st, "stt%d" % i, [128, 4, 6], F32) for i in range(2)]
            mv = [T(P, st, "mv%d" % i, [128, 4], F32) for i in range(2)]
            for i in range(16):
                s = i % 2
                X = xt[s]
                P.dma("sync", lambda e, X=X, i=i: e.dma_start(out=X[:], in_=xs[i * 128:(i + 1) * 128, :]), X.B, writes=[X.B])
                ln_tile(P, X, xn[s], stt[s], mv[s], gbc, bbc, hb[s], None)
                transpose_to_fm(P, hb[s], hT0, i, ps, identb, cstb.B)
            Rl = [T(P, st, "Rl%d" % i, [128, 513], F32) for i in range(4)]
            tmpl = T(P, st, "tmpl", [128, 512], F32)
            xsl = T(P, st, "xsl", [128, 512], F32)
            groups = [(0, 64, txw, AF.Tanh), (64, 64, xaT, AF.Copy), (128, 128, sxa, AF.Sigmoid), (256, 32, sxb, AF.Sigmoid)]
            for tb in range(4):
                for gi, (c0, M, dst, fn_) in enumerate(groups):
                    pst = ps[2 + (gi % 2)]
                    proj_fm(wl, c0, M, tb, pst)
                    shiftmix(pst, M, tb, Rl[gi], mul_[0:M, gi:gi + 1], mul_[0:M, 4 + gi:5 + gi], mul_.B, xsl[0:M, :], xsl.B, tmpl)
                    A_(lambda e, dst=dst, M=M, fn_=fn_, tb=tb: e.activation(out=dst[0:M, tb * 512:(tb + 1) * 512], in_=xsl[0:M, :], func=fn_),
                       [xsl.B], [dst.B])
            dump(st, 0, hT0[:, 3, 0:1024], hT0.b, 1024)
            dump(st, 1, txw[0:64, 1024:2048], [txw.B], 1024, 64)
            if stop == 1:
                P.wait_all("sync", dbg_bufs)
            P.flush()
            if stop == 1:
                P.disabled = True

        with ExitStack() as st:
            rb = T(P, st, "rb", [33, 8], F32)
            oh = T(P, st, "oh", [33, 383], F32)
            P.dma("sync", lambda e: e.dma_start(out=rb[0:32, :], in_=rel_bias), rb.B, writes=[rb.B])
            V_(lambda e: e.memset(rb[32:33, :], NEG), [], [rb.B])
            P.dma("sync", lambda e: e.dma_start(out=oh[:], in_=oh_d), oh.B, writes=[oh.B])
            rbh = T(P, st, "rbh", [33, 128], F32)
            treps = T(P, st, "treps", [128, 383], F32)
            BN = T(P, st, "BN", [128, 8, 256], F32, nb=8)
            farb = T(P, st, "farb", [128, 16], F32)
            drb = [P.buf("drb%d" % h) for h in range(8)]
            for h in range(8):
                V_(lambda e, h=h: e.tensor_copy(out=rbh[:], in_=rb[:, h:h + 1].to_broadcast([33, 128])), [rb.B], [rbh.B])
                mm(ps[0][:, 0:383], rbh[:], oh[:], True, True, [rbh.B, oh.B], ps[0].B)
                V_(lambda e: e.tensor_copy(out=treps[:], in_=ps[0][:, 0:383]), [ps[0].B], [treps.B])
                V_(lambda e, h=h: e.tensor_copy(out=farb[:, h:h + 1], in_=treps[:, 382:383]), [treps.B], [farb.B])
                P.dma("sync", lambda e, h=h: e.dma_start(out=trep_d[h], in_=treps[:]), treps.B, reads=[treps.B], writes=[drb[h]])
                skew = bass.AP(tensor=trep_d.tensor, offset=h * 128 * 383 + 127, ap=[[382, 128], [1, 256]])
                P.dma("sync", lambda e, h=h, skew=skew: e.dma_start(out=BN[:, h, :], in_=skew), BN.b[h], reads=[drb[h]], writes=[BN.b[h]])
            V_(lambda e: e.tensor_scalar(out=farb[:, 8:16], in0=farb[:, 0:8], scalar1=pbias[:, 0:1], scalar2=None, op0=ALU.add), [farb.B, pbias.B], [farb.B])
            if CUT == 1:
                P.disabled = True
            lq = T(P, st, "lq", [1, 4, 64], F32)
            for j, src in enumerate((lamq1, lamk1, lamq2, lamk2)):
                P.dma("sync", lambda e, j=j, src=src: e.dma_start(out=lq[0:1, j, :], in_=src.rearrange("(a n) -> a n", a=1)), lq.B, writes=[lq.B])
            lt = T(P, st, "lt", [1, 8], F32)
            lpr = T(P, st, "lpr", [1, 2, 64], F32)
            V_(lambda e: e.tensor_tensor(out=lpr[0:1, 0, :], in0=lq[0:1, 0, :], in1=lq[0:1, 1, :], op=ALU.mult), [lq.B], [lpr.B])
            V_(lambda e: e.tensor_tensor(out=lpr[0:1, 1, :], in0=lq[0:1, 2, :], in1=lq[0:1, 3, :], op=ALU.mult), [lq.B], [lpr.B])
            V_(lambda e: e.reduce_sum(out=lt[0:1, 0:2], in_=lpr[0:1, :, :], axis=AX.X), [lpr.B], [lt.B])
            A_(lambda e: e.activation(out=lt[0:1, 2:4], in_=lt[0:1, 0:2], func=AF.Exp), [lt.B], [lt.B])
            V_(lambda e: e.tensor_tensor(out=lt[0:1, 4:5], in0=lt[0:1, 3:4], in1=lt[0:1, 2:3], op=ALU.subtract), [lt.B], [lt.B])
            V_(lambda e: e.tensor_scalar(out=lt[0:1, 5:6], in0=lt[0:1, 4:5], scalar1=-LAMBDA_INIT, scalar2=None, op0=ALU.add), [lt.B], [lt.B])
            ones1 = T(P, st, "ones1", [1, 128], F32)
            V_(lambda e: e.memset(ones1[:], 1.0), [], [ones1.B])
            nlam = T(P, st, "nlam", [128, 1], F32)
            mm(ps[1][:, 0:1], ones1[0:1, :], lt[0:1, 5:6], True, True, [ones1.B, lt.B], ps[1].B)
            V_(lambda e: e.tensor_copy(out=nlam[:], in_=ps[1][:, 0:1]), [ps[1].B], [nlam.B])
            gsub = T(P, st, "gsub", [128, 128], F32)
            P.dma("sync", lambda e: e.dma_start(out=gsub[:], in_=subln_g.partition_broadcast(128)), gsub.B, writes=[gsub.B])
            V_(lambda e: e.tensor_scalar(out=gsub[:], in0=gsub[:], scalar1=1.0 - LAMBDA_INIT, scalar2=None, op0=ALU.mult), [gsub.B], [gsub.B])
            if CUT == 2:
                P.disabled = True

            WQ = [T(P, st, "WQ%d" % i, [128, 16, 128], BF16) for i in range(2)]
            WK = [T(P, st, "WK%d" % i, [128, 16, 128], BF16) for i in range(2)]
            WV = [T(P, st, "WV%d" % i, [128, 16, 128], BF16) for i in range(2)]
            qT = T(P, st, "qT", [128, 1024], BF16)
            kT = T(P, st, "kT", [128, 2048], BF16)
            Va = T(P, st, "Va", [128, 16, 129], BF16)
            V_(lambda e: e.memset(Va[:, :, 128:129], 1.0), [], [Va.B])
            E = [[T(P, st, "E%d%d" % (m, j), [128, 512], BF16) for j in range(2)] for m in range(2)]
            TMP = [[T(P, st, "TMPa%d%d" % (m, j), [128, 256], F32) for j in range(2)] for m in range(2)]
            rc = T(P, st, "rc", [128, 4], F32)
            o2 = T(P, st, "o2", [128, 128], F32)
            oo4 = [T(P, st, "oo%d" % i, [128, 128], F32) for i in range(4)]
            yb4 = T(P, st, "yb4", [128, 4, 128], BF16)
            pending = []
            junk = T(P, st, "junk", [128, 128], F32)
            ss = T(P, st, "ss", [128, 2], F32)
            yb = T(P, st, "yb", [128, 128], BF16)

            def load_head(h):
                s = h % 2
                for W_, c0 in ((WQ[s], h * 128), (WK[s], 1024 + h * 128), (WV[s], 2048 + h * 128)):
                    P.dma("gpsimd", lambda e, W_=W_, c0=c0: e.dma_start(out=W_[:], in_=w_in_v[:, :, c0:c0 + 128]), W_.B, writes=[W_.B])

            def accap(a, lo, hi):
                return ps[5 + a // 3][:, (a % 3) * 129 + lo:(a % 3) * 129 + hi]

            load_head(0)
            it = 0
            for h in range(NHEADS):
                if h + 1 < NHEADS:
                    load_head(h + 1)
                s = h % 2
                for tb in (2, 3):
                    proj_fm(WQ[s], 0, 128, tb, ps[tb % 2])
                    A_(lambda e, tb=tb: e.activation(out=qT[:, (tb - 2) * 512:(tb - 1) * 512], in_=ps[tb % 2][:, :], func=AF.Copy), [ps[tb % 2].B], [qT.B])
                for tb in range(4):
                    proj_fm(WK[s], 0, 128, tb, ps[2 + tb % 2])
                    V_(lambda e, tb=tb: e.tensor_copy(out=kT[:, tb * 512:(tb + 1) * 512], in_=ps[2 + tb % 2][:, :]), [ps[2 + tb % 2].B], [kT.B])
                for g4 in range(4):
                    pst = ps[g4 % 2]
                    for j in range(4):
                        i = g4 * 4 + j
                        for kc in range(16):
                            mm(pst[:, j * 128:(j + 1) * 128], hT0[:, kc, i * 128:(i + 1) * 128], WV[s][:, kc, :], kc == 0, kc == 15,
                               [WV[s].B, hT0.b[i]], pst.B)
                    V_(lambda e, g4=g4, pst=pst: e.tensor_copy(out=Va[:, g4 * 4:(g4 + 1) * 4, 0:128], in_=pst[:, :].rearrange("p (j t) -> p j t", t=128)),
                       [pst.B], [Va.B])
                while pending:
                    pending.pop(0)()
                if CUT == 3:
                    P.disabled = True
                for g in range(2):
                    nkb = 8 + 4 * g + 4
                    steps = []
                    for kb in range(nkb):
                        t = kb - 8 - 4 * g
                        i0 = max(0, t)
                        N = (4 - i0) * 128
                        q0 = g * 512 + i0 * 128
                        if t >= 0:
                            wn = min(256, N); b0 = 0
                        elif t == -1:
                            wn = 128; b0 = 128
                        else:
                            wn = 0; b0 = 0
                        steps.append((kb, i0, N, q0, wn, b0, kb < 8, it))
                        it += 1

                    def emit_scores(stp, h=h):
                        kb, i0, N, q0, wn, b0, pref, it_ = stp
                        for m in range(2):
                            pS = ps[m * 2 + (it_ % 2)]
                            Em = E[m][it_ % 2]
                            Tm = TMP[m][it_ % 2]
                            mm(pS[:, 0:N], kT[m * 64:(m + 1) * 64, kb * 128:(kb + 1) * 128], qT[m * 64:(m + 1) * 64, q0:q0 + N], True, True,
                               [kT.B, qT.B], pS.B)
                            if wn > 0:
                                V_(lambda e, pS=pS, Tm=Tm, wn=wn, b0=b0, h=h: e.scalar_tensor_tensor(out=Tm[:, 0:wn], in0=pS[:, 0:wn], scalar=0.125,
                                   in1=BN[:, h, b0:b0 + wn], op0=ALU.mult, op1=ALU.add), [pS.B, BN.b[h]], [Tm.B])
                                bcol = pbias[:, 0:1] if pref else EPSC[:, 3:4]
                                A_(lambda e, Em=Em, Tm=Tm, wn=wn, bcol=bcol: e.activation(out=Em[:, 0:wn], in_=Tm[:, 0:wn], func=AF.Exp, bias=bcol, scale=1.0),
                                   [Tm.B, pbias.B, EPSC.B], [Em.B])
                            if N > wn:
                                fcol = farb[:, 8 + h:9 + h] if pref else farb[:, h:h + 1]
                                A_(lambda e, Em=Em, pS=pS, wn=wn, N=N, fcol=fcol: e.activation(out=Em[:, wn:N], in_=pS[:, wn:N], func=AF.Exp, bias=fcol, scale=0.125),
                                   [pS.B, farb.B], [Em.B])

                    def emit_pv(stp):
                        kb, i0, N, q0, wn, b0, pref, it_ = stp
                        for m in range(2):
                            Em = E[m][it_ % 2]
                            for li in range(4 - i0):
                                i = i0 + li
                                a = m * 4 + i
                                mm(accap(a, 0, 129), Em[:, li * 128:(li + 1) * 128], Va[:, kb, :], (kb == 0 and a % 3 == 0), False,
                                   [Em.B, Va.B], ps[5 + a // 3].B, sgc=True)

                    emit_scores(steps[0])
                    for si, stp in enumerate(steps):
                        if si + 1 < len(steps):
                            emit_scores(steps[si + 1])
                        emit_pv(stp)
                        if si == 1:
                            while pending:
                                pending.pop(0)()
                    if CUT == 4:
                        P.disabled = True
                    for i in range(4):
                        a1, a2 = i, 4 + i
                        B1, B2 = ps[5 + a1 // 3].B, ps[5 + a2 // 3].B
                        oo = oo4[i]
                        V_(lambda e, a1=a1: e.reciprocal(out=rc[:, 0:1], in_=accap(a1, 128, 129)), [B1], [rc.B])
                        V_(lambda e, a2=a2: e.reciprocal(out=rc[:, 1:2], in_=accap(a2, 128, 129)), [B2], [rc.B])
                        V_(lambda e: e.tensor_tensor(out=rc[:, 2:3], in0=rc[:, 1:2], in1=nlam[:, 0:1], op=ALU.mult), [rc.B, nlam.B], [rc.B])
                        V_(lambda e, a2=a2: e.tensor_scalar(out=o2[:], in0=accap(a2, 0, 128), scalar1=rc[:, 2:3], scalar2=None, op0=ALU.mult), [B2, rc.B], [o2.B])
                        V_(lambda e, a1=a1, oo=oo: e.scalar_tensor_tensor(out=oo[:], in0=accap(a1, 0, 128), scalar=rc[:, 0:1], in1=o2[:], op0=ALU.mult, op1=ALU.add),
                           [B1, rc.B, o2.B], [oo.B])
                    for i in range(4):
                        oo = oo4[i]
                        A_(lambda e, oo=oo: e.activation(out=junk[:], in_=oo[:], func=AF.Square, accum_out=ss[:, 0:1]), [oo.B], [junk.B, ss.B])
                        A_(lambda e: e.activation(out=ss[:, 1:2], in_=ss[:, 0:1], func=AF.Sqrt, bias=EPSC[:, 0:1], scale=1.0 / 128), [ss.B, EPSC.B], [ss.B])
                        V_(lambda e: e.reciprocal(out=ss[:, 1:2], in_=ss[:, 1:2]), [ss.B], [ss.B])
                        V_(lambda e, oo=oo, i=i: e.scalar_tensor_tensor(out=yb4[:, i, :], in0=oo[:], scalar=ss[:, 1:2], in1=gsub[:], op0=ALU.mult, op1=ALU.mult),
                           [oo.B, ss.B, gsub.B], [yb4.B])

                    def fin_pe(h=h, g=g):
                        pv = ps[4].t[:].bitcast(BF16)
                        for i in range(4):
                            P.op("tensor", lambda e, pv=pv, i=i: e.transpose(out=pv[:, i * 128:(i + 1) * 128], in_=yb4[:, i, :], identity=identb), reads=[yb4.B, cstb.B], writes=[ps[4].B])
                        A_(lambda e, pv=pv: e.activation(out=yT[:, h, g * 512:(g + 1) * 512], in_=pv[:, 0:512], func=AF.Copy), [ps[4].B], [yT.b[h]])
                    pending.append(fin_pe)
            while pending:
                pending.pop(0)()
            dump(st, 2, yT[:, 0, :], [yT.b[0]], 1024)
            dump(st, 3, yT[:, NHEADS - 1, :], [yT.b[NHEADS - 1]], 1024)
            if stop == 2:
                P.wait_all("sync", dbg_bufs)
            P.flush()
            if stop == 2:
                P.disabled = True

        with ExitStack() as st:
            WR = [T(P, st, "WR0", [128, 16, 128], BF16)] * 2
            WKr = [T(P, st, "WKr0", [128, 16, 128], BF16)] * 2
            WVr = [T(P, st, "WVr0", [128, 16, 128], BF16)] * 2
            prm = T(P, st, "prm", [128, 8, 8], F32)
            for j, src in enumerate((rwkv_k_k, rwkv_k_a, rwkv_a0)):
                small_load(prm[:, :, j], prm.B, src.rearrange("(a p) -> p a", p=128))
            V_(lambda e: e.tensor_scalar(out=prm[:, :, 3], in0=prm[:, :, 1], scalar1=-1.0, scalar2=1.0, op0=ALU.mult, op1=ALU.add), [prm.B], [prm.B])
            w2b = T(P, st, "w2b", [64, 1024], BF16); a2b = T(P, st, "a2b", [64, 1024], BF16); g2b = T(P, st, "g2b", [128, 2, 1024], BF16)
            P.dma("gpsimd", lambda e: e.dma_start(out=w2b[:], in_=rwkv_w2), w2b.B, writes=[w2b.B])
            P.dma("gpsimd", lambda e: e.dma_start(out=a2b[:], in_=rwkv_a2), a2b.B, writes=[a2b.B])
            P.dma("gpsimd", lambda e: e.dma_start(out=g2b[:, 0, :], in_=rwkv_g2[0:128, :]), g2b.B, writes=[g2b.B])
            P.dma("gpsimd", lambda e: e.dma_start(out=g2b[0:32, 1, :], in_=rwkv_g2[128:160, :]), g2b.B, writes=[g2b.B])
            pp3 = T(P, st, "pp3", [128, 3, 128], F32)
            rkf = T(P, st, "rkf", [128, 8], F32)
            small_load(rkf[:], rkf.B, rwkv_r_k.rearrange("(a p) -> p a", p=128))
            RK = T(P, st, "RK", [128, 8, 2], BF16)
            V_(lambda e: e.memset(RK[:], 0.0), [], [RK.B])
            V_(lambda e: e.tensor_copy(out=RK[0:64, :, 0], in_=rkf[0:64, :]), [rkf.B], [RK.B])
            V_(lambda e: e.tensor_copy(out=RK[64:128, :, 1], in_=rkf[64:128, :]), [rkf.B], [RK.B])
            Rr = [T(P, st, "Rr%d" % i, [128, 513], F32) for i in range(3)]
            tmpr = T(P, st, "tmpr", [128, 512], F32)
            rs = T(P, st, "rs", [128, 512], F32); ks = T(P, st, "ks", [128, 512], F32); vsb = T(P, st, "vsb", [128, 512], BF16)
            af = T(P, st, "af", [128, 512], F32); kkk = T(P, st, "kkk", [128, 512], F32); sqb = T(P, st, "sqb", [128, 512], BF16)
            rinv = T(P, st, "rinv", [128, 512], F32); kk = T(P, st, "kk", [128, 512], F32); uu = tmpr
            k2 = T(P, st, "k2", [128, 512], F32); bb_ = T(P, st, "bb", [128, 512], F32)
            k2b = T(P, st, "k2b", [128, 512], BF16); bbb = T(P, st, "bbb", [128, 512], BF16); rk2 = T(P, st, "rk2", [128, 512], BF16)
            lwt2 = [T(P, st, "lwt%d" % i, [128, 128], F32) for i in range(2)]
            ex2 = [T(P, st, "ex%d" % i, [128, 4, 128], F32) for i in range(2)]
            ART2 = [T(P, st, "ART%d" % i, [128, 256], BF16) for i in range(2)]
            BhT2 = [T(P, st, "BhT%d" % i, [128, 128], BF16) for i in range(2)]; KhT2 = [T(P, st, "KhT%d" % i, [128, 128], BF16) for i in range(2)]
            TMt2 = [T(P, st, "TMt%d" % i, [128, 4, 128], BF16) for i in range(2)]
            lwt, ex, ART, BhT, KhT, TMt = lwt2[1], ex2[1], ART2[1], BhT2[1], KhT2[1], TMt2[1]
            MTh = [T(P, st, "MT%d" % h_, [128, 2, 256], BF16) for h_ in range(2)]
            NMh = [[T(P, st, "NM%d%d" % (h_, i), [128, 256], BF16) for i in range(2)] for h_ in range(2)]
            Xfh = [T(P, st, "Xf%d" % h_, [128, 128], F32) for h_ in range(2)]; Xbh = [T(P, st, "Xb%d" % h_, [128, 128], BF16) for h_ in range(2)]
            MT, Xf = MTh[1], Xfh[1]
            pBx = [P.buf("pBx%d" % h_) for h_ in range(2)]; pBn = [P.buf("pBn%d" % h_) for h_ in range(2)]; pBq = [P.buf("pBq%d" % h_) for h_ in range(2)]
            GTs = T(P, st, "GTs", [128, 16, 128], BF16); Hs = T(P, st, "Hs", [128, 16, 64], F32)
            QeTs = T(P, st, "QeTs", [128, 8, 128], BF16); Yls = T(P, st, "Yls", [128, 8, 128], F32)
            Vtm = T(P, st, "Vtm", [128, 8, 128], BF16); sbon = T(P, st, "sbon", [128, 8, 2], F32)
            gtm = T(P, st, "gtm", [128, 8, 128], F32)
            Sb = T(P, st, "Sb", [128, 128], BF16)
            Yt = T(P, st, "Yt", [128, 128], F32); yn = T(P, st, "yn", [128, 128], F32); ybr = T(P, st, "ybr", [128, 128], BF16)
            gst = T(P, st, "gst", [128, 2, 6], F32); gmv = T(P, st, "gmv", [128, 2, 2], F32); grs = T(P, st, "grs", [128, 2], F32)
            ident64 = cst[0:64, IDENT, 0:64]
            V_(lambda e: e.memset(GTs[:], 0.0), [], [GTs.B])

            def load_pair(c):
                s = c % 2
                for W_, c0 in ((WR[s], 3072 + c * 128), (WKr[s], 4096 + c * 128), (WVr[s], 5120 + c * 128)):
                    P.dma("gpsimd", lambda e, W_=W_, c0=c0: e.dma_start(out=W_[:], in_=w_in_v[:, :, c0:c0 + 128]), W_.B, writes=[W_.B])

            if CUT == 11:
                P.disabled = True
            load_pair(0)
            for c in range(NPAIRS):
                s = c % 2
                csl = slice(c * 128, (c + 1) * 128)
                for j3, src3 in enumerate((rwkv_w0, lnx_g, lnx_b)):
                    P.dma("sync", lambda e, j3=j3, src3=src3, c=c: e.dma_start(out=pp3[:, j3, :], in_=src3[c * 128:(c + 1) * 128].partition_broadcast(128)), pp3.B, writes=[pp3.B])
                for tb in range(4):
                    for j, (W_, dst, dstB) in enumerate(((WR[s], rs[:, :], rs.B), (WKr[s], ks[:, :], ks.B), (WVr[s], vsb[:, :], vsb.B))):
                        pst = ps[j % 2]
                        proj_fm(W_, 0, 128, tb, pst)
                        shiftmix(pst, 128, tb, Rr[j], murkv[:, j * 8 + c:j * 8 + c + 1], murkv[:, 24 + j * 8 + c:25 + j * 8 + c], murkv.B, dst, dstB, tmpr)
                    mm(ps[0][:, :], a2b[0:64, csl], xaT[0:64, tb * 512:(tb + 1) * 512], True, True, [a2b.B, xaT.B], ps[0].B)
                    A_(lambda e, c=c: e.activation(out=af[:], in_=ps[0][:, :], func=AF.Sigmoid, bias=prm[:, c, 2:3], scale=1.0), [ps[0].B, prm.B], [af.B])
                    V_(lambda e, c=c: e.tensor_scalar(out=kkk[:], in0=ks[:], scalar1=prm[:, c, 0:1], scalar2=None, op0=ALU.mult), [ks.B, prm.B], [kkk.B])
                    A_(lambda e: e.activation(out=sqb[:], in_=kkk[:], func=AF.Square), [kkk.B], [sqb.B])
                    mm(ps[1][:, :], cstb[:, BLK1, :], sqb[:], True, True, [cstb.B, sqb.B], ps[1].B)
                    A_(lambda e: e.activation(out=rinv[:], in_=ps[1][:, :], func=AF.Sqrt, bias=EPSC[:, 2:3], scale=1.0), [ps[1].B, EPSC.B], [rinv.B])
                    V_(lambda e: e.reciprocal(out=rinv[:], in_=rinv[:]), [rinv.B], [rinv.B])
                    V_(lambda e: e.tensor_tensor(out=kk[:], in0=kkk[:], in1=rinv[:], op=ALU.mult), [kkk.B, rinv.B], [kk.B])
                    V_(lambda e, c=c: e.tensor_scalar(out=uu[:], in0=af[:], scalar1=prm[:, c, 1:2], scalar2=prm[:, c, 3:4], op0=ALU.mult, op1=ALU.add), [af.B, prm.B], [uu.B])
                    V_(lambda e: e.tensor_tensor(out=k2[:], in0=ks[:], in1=uu[:], op=ALU.mult), [ks.B, uu.B], [k2.B])
                    V_(lambda e: e.tensor_tensor(out=bb_[:], in0=kk[:], in1=af[:], op=ALU.mult), [kk.B, af.B], [bb_.B])
                    A_(lambda e: e.activation(out=k2b[:], in_=k2[:], func=AF.Copy), [k2.B], [k2b.B])
                    A_(lambda e: e.activation(out=bbb[:], in_=bb_[:], func=AF.Copy), [bb_.B], [bbb.B])
                    V_(lambda e: e.tensor_tensor(out=rk2[:], in0=rs[:], in1=k2[:], op=ALU.mult), [rs.B, k2.B], [rk2.B])
                    if CUT == 12:
                        P.disabled = True
                    def prologue(ch, q4, bs):
                        own = ch >= 8
                        tsl = slice(q4 * 128, (q4 + 1) * 128)
                        gsl = slice(ch * 128, (ch + 1) * 128)
                        lwt, ex, ART, BhT, KhT, TMt = lwt2[bs], ex2[bs], ART2[bs], BhT2[bs], KhT2[bs], TMt2[bs]
                        pz = ps[2]
                        mm(pz[:, 0:128], txw[0:64, gsl], w2b[0:64, csl], True, True, [txw.B, w2b.B], pz.B)
                        yield
                        V_(lambda e: e.tensor_tensor(out=lwt[:], in0=pz[:, 0:128], in1=pp3[:, 0, :], op=ALU.add), [pz.B, pp3.B], [lwt.B])
                        A_(lambda e: e.activation(out=lwt[:], in_=lwt[:], func=AF.Sigmoid), [lwt.B], [lwt.B])
                        yield
                        mm(pz[:, 128:384], lwt[:], cst[:, TRI_I:TRI_E + 1, :].rearrange("p a b -> p (a b)"), True, True, [lwt.B, cst.B], pz.B)
                        mm(pz[:, 384:512], cst[:, TRI_R, :], lwt[:], True, True, [lwt.B, cst.B], pz.B)
                        yield
                        A_(lambda e: e.activation(out=ex[:, 0:2, :].rearrange("p a b -> p (a b)"), in_=pz[:, 128:384], func=AF.Exp), [pz.B], [ex.B])
                        A_(lambda e: e.activation(out=ex[:, 2, :], in_=pz[:, 128:256], func=AF.Exp, scale=-1.0), [pz.B], [ex.B])
                        A_(lambda e: e.activation(out=ex[:, 3, :], in_=pz[:, 384:512], func=AF.Exp), [pz.B], [ex.B])
                        yield
                        V_(lambda e: e.scalar_tensor_tensor(out=ART[:, 0:128], in0=kk[:, tsl], scalar=-1.0, in1=ex[:, 1, :], op0=ALU.mult, op1=ALU.mult), [kk.B, ex.B], [ART.B])
                        V_(lambda e: e.tensor_tensor(out=ART[:, 128:256], in0=rs[:, tsl], in1=ex[:, 0, :], op=ALU.mult), [rs.B, ex.B], [ART.B])
                        V_(lambda e: e.tensor_tensor(out=BhT[:], in0=bb_[:, tsl], in1=ex[:, 2, :], op=ALU.mult), [bb_.B, ex.B], [BhT.B])
                        V_(lambda e: e.tensor_tensor(out=KhT[:], in0=k2[:, tsl], in1=ex[:, 2, :], op=ALU.mult), [k2.B, ex.B], [KhT.B])
                        yield
                        pt = ps[3]
                        ptv = pt.t[:].bitcast(BF16)
                        for j, (src, sB) in enumerate(((bbb[:, tsl], bbb.B), (k2b[:, tsl], k2b.B), (vsb[:, tsl], vsb.B), (ART[:, 0:128], ART.B))):
                            P.op("tensor", lambda e, j=j, src=src, ptv=ptv: e.transpose(out=ptv[:, j * 128:(j + 1) * 128], in_=src, identity=identb), reads=[sB, cstb.B], writes=[pt.B])
                        yield
                        V_(lambda e: e.tensor_tensor(out=TMt[:, 0:2, :], in0=ptv[:, 0:256].rearrange("p (a b) -> p a b", b=128),
                                                     in1=ex[:, 3:4, :].to_broadcast([128, 2, 128]), op=ALU.mult), [pt.B, ex.B], [TMt.B])
                        A_(lambda e: e.activation(out=TMt[:, 2:4, :].rearrange("p a b -> p (a b)"), in_=ptv[:, 256:512], func=AF.Copy), [pt.B], [TMt.B])
                        if own:
                            oc = ch - 8
                            yield
                            A_(lambda e: e.activation(out=Vtm[:, oc, :], in_=TMt[:, 2, :], func=AF.Copy), [TMt.B], [Vtm.B])
                            mm(ps[3][:, 256:258], rk2[:, tsl], RK[:, c, :], True, True, [rk2.B, RK.B], ps[3].B)
                            mm(ps[3][:, 384:512], sxa[:, gsl], g2b[:, 0, csl], True, False, [sxa.B, g2b.B], ps[3].B)
                            mm(ps[3][:, 384:512], sxb[0:32, gsl], g2b[0:32, 1, csl], False, True, [sxb.B, g2b.B], ps[3].B)
                            yield
                            V_(lambda e: e.tensor_copy(out=sbon[:, oc, :], in_=ps[3][:, 256:258]), [ps[3].B], [sbon.B])
                            V_(lambda e: e.tensor_copy(out=gtm[:, oc, :], in_=ps[3][:, 384:512]), [ps[3].B], [gtm.B])

                    def chain(ch, hh, own, tsl, bs):
                        hp = slice(hh * 64, (hh + 1) * 64)
                        tpk = (hh * 64, 0)
                        tpo = (0, hh * 64)
                        ex, ART, BhT, KhT, TMt = ex2[bs], ART2[bs], BhT2[bs], KhT2[bs], TMt2[bs]
                        pA, pB = ps[4 + hh], ps[6 + hh]
                        bX = bN = bQ = pB.B
                        NM, Xf, Xb, MT = NMh[hh], Xfh[hh], Xbh[hh], MTh[hh]
                        mm(pA[:, 0:256], BhT[hp, :], ART[hp, :], True, True, [BhT.B, ART.B], pA.B, tpk)
                        mm(pA[:, 256:512], KhT[hp, :], ART[hp, :], True, True, [KhT.B, ART.B], pA.B, tpk)
                        mm(pB[:, 384:512], ART[hp, 0:128], BhT[hp, :], True, True, [BhT.B, ART.B], bQ, tpk)
                        yield
                        V_(lambda e: e.tensor_tensor(out=MT[:, :, :], in0=pA[:, :].rearrange("p (a b) -> p a b", b=256),
                                                     in1=cst[:, UT_S:UT_I + 1, :].rearrange("p a b -> p (a b)").unsqueeze(1).to_broadcast([128, 2, 256]), op=ALU.mult),
                           [pA.B, cst.B], [MT.B])
                        V_(lambda e: e.tensor_tensor(out=NM[0][:, 0:128], in0=pA[:, 0:128], in1=cst[:, UT_S, :], op=ALU.mult), [pA.B, cst.B], [NM[0].B])
                        V_(lambda e: e.tensor_tensor(out=NM[0][:, 128:256], in0=pB[:, 384:512], in1=cst[:, LT_S, :], op=ALU.mult), [bQ, cst.B], [NM[0].B])
                        A_(lambda e: e.activation(out=Xb[:, 0:64], in_=TMt[:, 3, hp], func=AF.Copy), [TMt.B], [Xb.B])
                        A_(lambda e: e.activation(out=Xf[:, 0:64], in_=TMt[:, 3, hp], func=AF.Copy), [TMt.B], [Xf.B])
                        yield
                        mm(pA[:, 0:64], MT[:, 1, 0:128], TMt[:, 2, hp], True, True, [MT.B, TMt.B], pA.B)
                        yield
                        V_(lambda e: e.tensor_copy(out=Xb[:, 64:128], in_=pA[:, 0:64]), [pA.B], [Xb.B])
                        V_(lambda e: e.tensor_copy(out=Xf[:, 64:128], in_=pA[:, 0:64]), [pA.B], [Xf.B])
                        yield
                        for lv in range(7):
                            cur, nxt = NM[lv % 2], NM[(lv + 1) % 2]
                            mm(pB[:, 0:128], cur[:, 0:128], Xb[:], True, True, [cur.B, Xb.B], bX)
                            if lv < 6:
                                mm(pB[:, 128:256], cur[:, 128:256], cur[:, 0:128], True, True, [cur.B], bN)
                                mm(pB[:, 256:384], cur[:, 0:128], cur[:, 128:256], True, True, [cur.B], bN)
                            yield
                            V_(lambda e: e.tensor_tensor(out=Xb[:], in0=Xf[:], in1=pB[:, 0:128], op=ALU.add), [Xf.B, bX], [Xb.B])
                            if lv < 6:
                                V_(lambda e, nxt=nxt: e.tensor_copy(out=nxt[:], in_=pB[:, 128:384]), [bN], [nxt.B])
                                V_(lambda e: e.tensor_tensor(out=Xf[:], in0=Xf[:], in1=pB[:, 0:128], op=ALU.add), [Xf.B, bX], [Xf.B])
                            yield
                        mm(pA[hp, 64:128], Xb[:, 0:64], TMt[:, 0, hp], True, True, [Xb.B, TMt.B], pA.B, tpo)
                        mm(pA[hp, 128:192], TMt[:, 0, hp], Xb[:, 64:128], True, False, [Xb.B, TMt.B], pA.B, tpo)
                        mm(pA[hp, 128:192], TMt[:, 1, hp], TMt[:, 2, hp], False, True, [TMt.B], pA.B, tpo)
                        if own:
                            mm(pB[hp, 384:512], Xb[:, 0:64], MT[:, 0, 128:256], True, True, [Xb.B, MT.B], bQ, tpo)
                            mm(pA[:, 192:256], MT[:, 0, 128:256], Xb[:, 64:128], True, False, [Xb.B, MT.B], pA.B)
                            mm(pA[:, 192:256], MT[:, 1, 128:256], TMt[:, 2, hp], False, True, [TMt.B, MT.B], pA.B)
                        yield
                        V_(lambda e: e.scalar_tensor_tensor(out=GTs[hp, ch, hh * 64:(hh + 1) * 64], in0=cst[hp, IDENT, hh * 64:(hh + 1) * 64], scalar=ex[hp, 0, 127:128],
                                                            in1=pA[hp, 64:128], op0=ALU.mult, op1=ALU.add), [cst.B, ex.B, pA.B], [GTs.B])
                        V_(lambda e: e.tensor_copy(out=Hs[hp, ch, :], in_=pA[hp, 128:192]), [pA.B], [Hs.B])
                        if own:
                            oc = ch - 8
                            V_(lambda e: e.tensor_tensor(out=QeTs[hp, oc, :], in0=pB[hp, 384:512], in1=ART[hp, 128:256], op=ALU.add),
                               [bQ, ART.B], [QeTs.B])
                            V_(lambda e: e.tensor_copy(out=Yls[:, oc, hp], in_=pA[:, 192:256]), [pA.B], [Yls.B])

                    def run_rr(gens):
                        while gens:
                            for g_ in list(gens):
                                try:
                                    next(g_)
                                except StopIteration:
                                    gens.remove(g_)

                    run_rr([prologue(tb * 4, 0, 0)])
                    for q4 in range(4):
                        ch = tb * 4 + q4
                        own = ch >= 8
                        tsl = slice(q4 * 128, (q4 + 1) * 128)
                        gens = [chain(ch, 0, own, tsl, q4 % 2), chain(ch, 1, own, tsl, q4 % 2)]
                        if q4 < 3:
                            gens.append(prologue(ch + 1, q4 + 1, (q4 + 1) % 2))
                        run_rr(gens)
                if c + 1 < NPAIRS:
                    load_pair(c + 1)
                if CUT == 17:
                    P.disabled = True
                V_(lambda e: e.memset(Sb[:], 0.0), [], [Sb.B])
                for ch in range(16):
                    if CUT == 19 and ch == 8:
                        P.disabled = True
                    if ch >= 8:
                        oc = ch - 8
                        mm(ps[0][:, 0:128], QeTs[:, oc, :], Sb[:, :], True, True, [QeTs.B, Sb.B], ps[0].B)
                        V_(lambda e, oc=oc: e.tensor_tensor(out=Yt[:], in0=ps[0][:, 0:128], in1=Yls[:, oc, :], op=ALU.add), [ps[0].B, Yls.B], [Yt.B])
                    if CUT == 20 and ch == 8:
                        P.disabled = True
                    if ch < 15:
                        mm(ps[1][:, 0:128], GTs[:, ch, :], Sb[:, :], True, True, [GTs.B, Sb.B], ps[1].B)
                        for hh in range(2):
                            hp = slice(hh * 64, (hh + 1) * 64)
                            V_(lambda e, ch=ch, hp=hp: e.tensor_tensor(out=Sb[hp, hp], in0=ps[1][hp, hp], in1=Hs[hp, ch, :], op=ALU.add), [ps[1].B, Hs.B], [Sb.B])
                    if CUT == 21 and ch == 8:
                        P.disabled = True
                    if ch >= 8:
                        for hh in range(2):
                            V_(lambda e, hh=hh: e.bn_stats(out=gst[:, hh, :], in_=Yt[:, hh * 64:(hh + 1) * 64]), [Yt.B], [gst.B])
                            V_(lambda e, hh=hh: e.bn_aggr(out=gmv[:, hh, :], in_=gst[:, hh, :]), [gst.B], [gmv.B])
                        A_(lambda e: e.activation(out=grs[:], in_=gmv[:, :, 1], func=AF.Sqrt, bias=EPSC[:, 1:2], scale=1.0), [gmv.B, EPSC.B], [grs.B])
                        V_(lambda e: e.reciprocal(out=grs[:], in_=grs[:]), [grs.B], [grs.B])
                        for hh in range(2):
                            hs_ = slice(hh * 64, (hh + 1) * 64)
                            V_(lambda e, hh=hh, hs_=hs_: e.tensor_scalar(out=yn[:, hs_], in0=Yt[:, hs_], scalar1=gmv[:, hh, 0:1], scalar2=grs[:, hh:hh + 1],
                                                                       op0=ALU.subtract, op1=ALU.mult), [Yt.B, gmv.B, grs.B], [yn.B])
                        V_(lambda e, c=c: e.tensor_tensor(out=yn[:], in0=yn[:], in1=pp3[:, 1, :], op=ALU.mult), [yn.B, pp3.B], [yn.B])
                        V_(lambda e, c=c: e.tensor_tensor(out=yn[:], in0=yn[:], in1=pp3[:, 2, :], op=ALU.add), [yn.B, pp3.B], [yn.B])
                        for hh in range(2):
                            hs_ = slice(hh * 64, (hh + 1) * 64)
                            V_(lambda e, hh=hh, hs_=hs_, oc=oc: e.scalar_tensor_tensor(out=yn[:, hs_], in0=Vtm[:, oc, hs_], scalar=sbon[:, oc, hh:hh + 1], in1=yn[:, hs_],
                                                                                      op0=ALU.mult, op1=ALU.add), [Vtm.B, sbon.B, yn.B], [yn.B])
                        V_(lambda e, oc=oc: e.tensor_tensor(out=ybr[:], in0=yn[:], in1=gtm[:, oc, :], op=ALU.mult), [yn.B, gtm.B], [ybr.B])
                        if CUT == 22 and ch == 8:
                            P.disabled = True
                        pv = ps[3].t[:].bitcast(BF16)
                        P.op("tensor", lambda e, pv=pv: e.transpose(out=pv[:, 0:128], in_=ybr[:], identity=identb), reads=[ybr.B, cstb.B], writes=[ps[3].B])
                        if CUT == 23 and ch == 8:
                            P.disabled = True
                        A_(lambda e, pv=pv, c=c, oc=oc: e.activation(out=yT[:, 8 + c, oc * 128:(oc + 1) * 128], in_=pv[:, 0:128], func=AF.Copy), [ps[3].B], [yT.b[8 + c]])
            if CUT == 25:
                P.disabled = True
            if dbg and stop == 3 and CUT == 77:
                d6 = T(P, st, "d6", [128, 1024], F32)
                def dcp(c0, w, ap, bufs):
                    V_(lambda e: e.tensor_copy(out=d6[:, c0:c0 + w], in_=ap), bufs, [d6.B])
                def dsend(sl_):
                    P.dma("sync", lambda e: e.dma_start(out=dbg_d[sl_], in_=d6[:]), d6.B, reads=[d6.B])
                V_(lambda e: e.memset(d6[:], 0.0), [], [d6.B])
                dcp(0, 512, ex[:].rearrange("p a b -> p (a b)"), [ex.B]); dcp(512, 128, Xf[:], [Xf.B]); dcp(640, 128, lwt[:], [lwt.B])
                dcp(768, 128, Yt[:], [Yt.B]); dcp(896, 128, yn[:], [yn.B])
                dsend(6)
                dcp(0, 256, ART[:], [ART.B]); dcp(256, 128, BhT[:], [BhT.B]); dcp(384, 128, KhT[:], [KhT.B])
                dcp(512, 512, TMt[:].rearrange("p a b -> p (a b)"), [TMt.B])
                dsend(7)
                dcp(0, 512, MT[:].rearrange("p a b -> p (a b)"), [MT.B]); dcp(512, 64, GTs[:, 15, 0:64], [GTs.B]); dcp(576, 64, Hs[:, 15, :], [Hs.B])
                dcp(640, 128, QeTs[:, 7, :], [QeTs.B]); dcp(768, 128, Yls[:, 7, :], [Yls.B]); dcp(896, 64, Sb[:, 0:64], [Sb.B])
                dcp(960, 2, sbon[:, 7, :], [sbon.B])
                dsend(8)
                dbg_bufs.append(d6.B)
            dump(st, 4, yT[:, 8, 0:512], [yT.b[8]], 512)
            if not (dbg and stop == 3):
                dump(st, 5, yT[:, 7 + NPAIRS, :], [yT.b[7 + NPAIRS]], 1024)
            if stop == 3:
                P.wait_all("sync", dbg_bufs)
            P.flush()
            if stop == 3:
                P.disabled = True

        stA.close()
        with ExitStack() as st:
            acc = T(P, st, "acc", [128, 8, D], F32, nb=8)
            wt_ = T(P, st, "wt", [128, 8, 32], F32)
            accB = acc.b
            gb2 = [None, None]
            ws = [None, None, None]

            class AccT:
                def __init__(self, i):
                    self.i = i; self.B = accB[i]

                def __getitem__(self, k):
                    return acc[:, self.i, :] if k == slice(None) else acc[:, self.i, k[1]]

            def load_gb(gs, bs):
                P.dma("sync", lambda e: e.dma_start(out=gb2[0][:], in_=gs.partition_broadcast(128)), gb2[0].B, writes=[gb2[0].B])
                if bs is not None:
                    P.dma("sync", lambda e: e.dma_start(out=gb2[1][:], in_=bs.partition_broadcast(128)), gb2[1].B, writes=[gb2[1].B])

            with ExitStack() as st2:
                gb2[0] = T(P, st2, "gb2a0", [128, D], F32); gb2[1] = T(P, st2, "gb2a1", [128, D], F32)
                for i in range(3):
                    ws[i] = T(P, st2, "wsa%d" % i, [128, 16, 512], BF16)
                xt = [T(P, st2, "xt4%d" % i, [128, D], F32) for i in range(2)]
                xn = T(P, st2, "xn4", [128, D], F32)
                stt4 = T(P, st2, "stt4", [128, 4, 6], F32); mv4 = T(P, st2, "mv4", [128, 4], F32)
                load_gb(ln_in_g, ln_in_b)
                for i in range(8):
                    X = xt[i % 2]
                    P.dma("sync", lambda e, X=X, i=i: e.dma_start(out=X[:], in_=xs[(8 + i) * 128:(9 + i) * 128, :]), X.B, writes=[X.B])
                    ln_tile(P, X, xn, stt4, mv4, gb2[0], gb2[1], None, AccT(i))
                    A_(lambda e, i=i: e.activation(out=acc[:, i, :], in_=acc[:, i, :], func=AF.Copy, scale=ALPHA), [accB[i]], [accB[i]])
                w_out_v = w_out.rearrange("(kc p) n -> p kc n", p=128)
                for n in range(4):
                    W_ = ws[n % 3]
                    P.dma("gpsimd", lambda e, W_=W_, n=n: e.dma_start(out=W_[:], in_=w_out_v[:, :, n * 512:(n + 1) * 512]), W_.B, writes=[W_.B])
                    for i in range(8):
                        pst = ps[i % 2]
                        for kc in range(16):
                            mm(pst[:, :], yT[:, kc, i * 128:(i + 1) * 128], W_[:, kc, :], kc == 0, kc == 15, [yT.b[kc], W_.B], pst.B)
                        V_(lambda e, i=i, n=n, pst=pst: e.tensor_tensor(out=acc[:, i, n * 512:(n + 1) * 512], in0=acc[:, i, n * 512:(n + 1) * 512], in1=pst[:, :], op=ALU.add),
                           [accB[i], pst.B], [accB[i]])
                dump(st2, 6, acc[:, 0, 0:1024], [accB[0]], 1024)
                if stop == 4:
                    P.wait_all("sync", dbg_bufs)
                P.flush()
                if stop == 4:
                    P.disabled = True
            hT1 = T(P, st, "hT1", [128, 16, 1024], BF16, nb=8)
            with ExitStack() as st2:
                gb2[0] = T(P, st2, "gb2b0", [128, D], F32); gb2[1] = T(P, st2, "gb2b1", [128, D], F32)
                xn = T(P, st2, "xn4b", [128, D], F32)
                hb4 = T(P, st2, "hb4", [128, D], BF16)
                hlo = T(P, st2, "hlo", [128, D], BF16)
                stt4 = T(P, st2, "stt4b", [128, 4, 6], F32); mv4 = T(P, st2, "mv4b", [128, 4], F32)
                hT1l = T(P, st2, "hT1l", [128, 16, 128], BF16)
                load_gb(ln1_g, ln1_b)
                rwf = T(P, st2, "rwf", [128, 16, 36], F32); rwh = T(P, st2, "rwh", [128, 16, 36], BF16); rwl = T(P, st2, "rwl", [128, 16, 36], BF16)
                rbias = T(P, st2, "rbias", [128, 36], F32)
                P.dma("sync", lambda e: e.dma_start(out=rwf[:, :, 0:4], in_=rgw.rearrange("(kc p) n -> p kc n", p=128)), rwf.B, writes=[rwf.B])
                P.dma("sync", lambda e: e.dma_start(out=rwf[:, :, 4:36], in_=rew.rearrange("(kc p) n -> p kc n", p=128)), rwf.B, writes=[rwf.B])
                P.dma("sync", lambda e: e.dma_start(out=rbias[:, 0:4], in_=rgb.partition_broadcast(128)), rbias.B, writes=[rbias.B])
                P.dma("sync", lambda e: e.dma_start(out=rbias[:, 4:36], in_=reb.partition_broadcast(128)), rbias.B, writes=[rbias.B])
                V_(lambda e: e.tensor_copy(out=rwh[:], in_=rwf[:]), [rwf.B], [rwh.B])
                V_(lambda e: e.tensor_tensor(out=rwf[:], in0=rwf[:], in1=rwh[:], op=ALU.subtract), [rwf.B, rwh.B], [rwf.B])
                V_(lambda e: e.tensor_copy(out=rwl[:], in_=rwf[:]), [rwf.B], [rwl.B])
                L = T(P, st2, "L", [128, 36], F32); r1 = T(P, st2, "r1", [128, 8], F32)
                gm = T(P, st2, "gm", [128, 4], F32); elm = T(P, st2, "elm", [128, 32], F32); ee = T(P, st2, "ee", [128, 32], F32)
                m1 = T(P, st2, "m1", [128, 32], F32); e2 = T(P, st2, "e2", [128, 32], F32); m2 = T(P, st2, "m2", [128, 32], F32)
                for i in range(8):
                    A = AccT(i)
                    ln_tile(P, A, xn, stt4, mv4, gb2[0], gb2[1], hb4, A)
                    transpose_to_fm(P, hb4, hT1, i, ps, identb, cstb.B, pbase=2)
                    V_(lambda e, i=i: e.tensor_tensor(out=xn[:], in0=acc[:, i, :], in1=hb4[:], op=ALU.subtract), [accB[i], hb4.B], [xn.B])
                    A_(lambda e: e.activation(out=hlo[:], in_=xn[:], func=AF.Copy), [xn.B], [hlo.B])
                    transpose_to_fm(P, hlo, hT1l, 0, ps, identb, cstb.B, pbase=4)
                    tsl = slice(i * 128, (i + 1) * 128)
                    n_mm = 0
                    for (hT_, w_, sl_, bi_) in ((hT1, rwh, tsl, i), (hT1l, rwh, slice(0, 128), 0), (hT1, rwl, tsl, i)):
                        for kc in range(16):
                            mm(ps[6][:, 0:36], hT_[:, kc, sl_], w_[:, kc, :], n_mm == 0, n_mm == 47, [hT_.b[bi_], w_.B], ps[6].B)
                            n_mm += 1
                    V_(lambda e: e.tensor_tensor(out=L[:], in0=ps[6][:, 0:36], in1=rbias[:], op=ALU.add), [ps[6].B, rbias.B], [L.B])
                    V_(lambda e: e.reduce_max(out=r1[:, 0:1], in_=L[:, 0:4], axis=AX.X), [L.B], [r1.B])
                    V_(lambda e: e.tensor_scalar(out=gm[:], in0=L[:, 0:4], scalar1=r1[:, 0:1], scalar2=None, op0=ALU.is_ge), [L.B, r1.B], [gm.B])
                    V_(lambda e: e.tensor_scalar(out=r1[:, 1:2], in0=r1[:, 0:1], scalar1=-1.0, scalar2=None, op0=ALU.mult), [r1.B], [r1.B])
                    A_(lambda e: e.activation(out=ee[:, 0:4], in_=L[:, 0:4], func=AF.Exp, bias=r1[:, 1:2], scale=1.0, accum_out=r1[:, 2:3]), [L.B, r1.B], [ee.B, r1.B])
                    V_(lambda e: e.reciprocal(out=r1[:, 3:4], in_=r1[:, 2:3]), [r1.B], [r1.B])
                    V_(lambda e: e.tensor_scalar(out=gm[:], in0=gm[:], scalar1=-1.0, scalar2=-NEG, op0=ALU.add, op1=ALU.mult), [gm.B], [gm.B])
                    V_(lambda e: e.tensor_tensor(out=elm[:].rearrange("p (g e) -> p g e", e=8), in0=L[:, 4:36].rearrange("p (g e) -> p g e", e=8),
                                                 in1=gm[:].unsqueeze(2).to_broadcast([128, 4, 8]), op=ALU.add), [L.B, gm.B], [elm.B])
                    V_(lambda e: e.reduce_max(out=r1[:, 4:5], in_=elm[:], axis=AX.X), [elm.B], [r1.B])
                    V_(lambda e: e.tensor_scalar(out=r1[:, 5:6], in0=r1[:, 4:5], scalar1=-1.0, scalar2=None, op0=ALU.mult), [r1.B], [r1.B])
                    A_(lambda e: e.activation(out=ee[:], in_=elm[:], func=AF.Exp, bias=r1[:, 5:6], scale=1.0), [elm.B, r1.B], [ee.B])
                    V_(lambda e: e.tensor_scalar(out=m1[:], in0=elm[:], scalar1=r1[:, 4:5], scalar2=None, op0=ALU.is_ge), [elm.B, r1.B], [m1.B])
                    V_(lambda e: e.tensor_tensor(out=e2[:], in0=ee[:], in1=m1[:], op=ALU.mult), [ee.B, m1.B], [e2.B])
                    V_(lambda e: e.tensor_tensor(out=e2[:], in0=ee[:], in1=e2[:], op=ALU.subtract), [ee.B, e2.B], [e2.B])
                    V_(lambda e: e.reduce_max(out=r1[:, 6:7], in_=e2[:], axis=AX.X), [e2.B], [r1.B])
                    V_(lambda e: e.tensor_scalar(out=m2[:], in0=e2[:], scalar1=r1[:, 6:7], scalar2=None, op0=ALU.is_ge), [e2.B, r1.B], [m2.B])
                    V_(lambda e: e.tensor_tensor(out=m2[:], in0=m2[:], in1=e2[:], op=ALU.mult), [m2.B, e2.B], [m2.B])
                    V_(lambda e: e.tensor_tensor(out=m1[:], in0=m1[:], in1=m2[:], op=ALU.add), [m1.B, m2.B], [m1.B])
                    V_(lambda e: e.tensor_scalar(out=r1[:, 7:8], in0=r1[:, 6:7], scalar1=1.0, scalar2=None, op0=ALU.add), [r1.B], [r1.B])
                    V_(lambda e: e.reciprocal(out=r1[:, 7:8], in_=r1[:, 7:8]), [r1.B], [r1.B])
                    V_(lambda e, i=i: e.tensor_scalar(out=wt_[:, i, :], in0=m1[:], scalar1=r1[:, 7:8], scalar2=r1[:, 3:4], op0=ALU.mult, op1=ALU.mult), [m1.B, r1.B], [wt_.B])
                    A_(lambda e, i=i: e.activation(out=acc[:, i, :], in_=acc[:, i, :], func=AF.Copy, scale=ALPHA), [accB[i]], [accB[i]])
                dump(st2, 7, wt_[:, 0, :], [wt_.B], 32)
                if stop == 5:
                    P.wait_all("sync", dbg_bufs)
                P.flush()
                if stop == 5:
                    P.disabled = True
            with ExitStack() as st2:
                for i in range(3):
                    ws[i] = T(P, st2, "wsc%d" % i, [128, 16, 512], BF16)
                hid = [T(P, st2, "hid0", [128, 4, 1024], BF16)] * 2
                sg = [T(P, st2, "sg%d" % i, [128, 512], BF16) for i in range(2)]
                nld = 0
                for ex_ in range(NEXP):
                    Wg, Wu, Wd = ws[0], ws[1], ws[2]
                    P.dma("gpsimd", lambda e, ex_=ex_: e.dma_start(out=ws[0][:], in_=wg_d[ex_].rearrange("(kc p) n -> p kc n", p=128)), ws[0].B, writes=[ws[0].B])
                    P.dma("gpsimd", lambda e, ex_=ex_: e.dma_start(out=ws[1][:], in_=wu_d[ex_].rearrange("(kc p) n -> p kc n", p=128)), ws[1].B, writes=[ws[1].B])
                    P.dma("gpsimd", lambda e, ex_=ex_: e.dma_start(out=ws[2][:].rearrange("p (a b) n -> p a b n", b=4), in_=wd_d[ex_].rearrange("(fc p) (b n) -> p fc b n", p=128, n=512)),
                          ws[2].B, writes=[ws[2].B])
                    H_ = hid[ex_ % 2]
                    k_ = 0
                    for tb in range(2):
                        for fc in range(4):
                            pg, pu = ps[(k_ % 2) * 2], ps[(k_ % 2) * 2 + 1]
                            S_ = sg[k_ % 2]
                            for kc in range(16):
                                mm(pg[:, :], Wg[:, kc, fc * 128:(fc + 1) * 128], hT1[:, kc, tb * 512:(tb + 1) * 512], kc == 0, kc == 15, [Wg.B] + hT1.b[tb * 4:(tb + 1) * 4], pg.B)
                            for kc in range(16):
                                mm(pu[:, :], Wu[:, kc, fc * 128:(fc + 1) * 128], hT1[:, kc, tb * 512:(tb + 1) * 512], kc == 0, kc == 15, [Wu.B] + hT1.b[tb * 4:(tb + 1) * 4], pu.B)
                            A_(lambda e, S_=S_, pg=pg: e.activation(out=S_[:], in_=pg[:, :], func=AF.Silu), [pg.B], [S_.B])
                            V_(lambda e, S_=S_, pu=pu, H_=H_, fc=fc, tb=tb: e.tensor_tensor(out=H_[:, fc, tb * 512:(tb + 1) * 512], in0=S_[:], in1=pu[:, :], op=ALU.mult),
                               [S_.B, pu.B], [H_.B])
                            k_ += 1
                    for i in range(8):
                        for n in range(4):
                            po = ps[4 + (i * 4 + n) % 4]
                            for fc in range(4):
                                mm(po[:, :], H_[:, fc, i * 128:(i + 1) * 128], Wd[:, fc * 4 + n, :], fc == 0, fc == 3, [H_.B, Wd.B], po.B)
                            V_(lambda e, i=i, n=n, po=po, ex_=ex_: e.scalar_tensor_tensor(out=acc[:, i, n * 512:(n + 1) * 512], in0=po[:, :], scalar=wt_[:, i, ex_:ex_ + 1],
                                                                                  in1=acc[:, i, n * 512:(n + 1) * 512], op0=ALU.mult, op1=ALU.add), [po.B, wt_.B, accB[i]], [accB[i]])
                dump(st2, 8, acc[:, 0, 0:1024], [accB[0]], 1024)
                if stop == 6:
                    P.wait_all("sync", dbg_bufs)
                P.flush()
                if stop == 6:
                    P.disabled = True
            with ExitStack() as st2:
                gb2[0] = T(P, st2, "gb2d0", [128, D], F32); gb2[1] = T(P, st2, "gb2d1", [128, D], F32)
                ws[0] = T(P, st2, "wsd0", [128, 16, 512], BF16)
                xn = T(P, st2, "xn5", [128, D], F32); hb4 = T(P, st2, "hb5", [128, D], BF16)
                stt4 = T(P, st2, "stt5", [128, 4, 6], F32); mv4 = T(P, st2, "mv5", [128, 4], F32)
                pf = T(P, st2, "pf", [128, 256], F32); pb = T(P, st2, "pb", [128, 256], BF16)
                pT = T(P, st2, "pT", [128, 2, 1024], BF16, nb=8)
                plw = T(P, st2, "plw", [128, 2, D], BF16)
                ssq = T(P, st2, "ssq", [128, 8, 4], F32); rsp = T(P, st2, "rsp", [128, 8], F32)
                sgt = T(P, st2, "sgt", [128, 512], F32); t1_ = T(P, st2, "t1", [128, 512], F32)
                load_gb(ln2_g, ln2_b)
                P.dma("gpsimd", lambda e: e.dma_start(out=plw[:], in_=ple_w.rearrange("(kc p) n -> p kc n", p=128)), plw.B, writes=[plw.B])
                if CUT == 31:
                    P.disabled = True
                for i in range(8):
                    A = AccT(i)
                    ln_tile(P, A, xn, stt4, mv4, gb2[0], gb2[1], hb4, A)
                    transpose_to_fm(P, hb4, hT1, i, ps, identb, cstb.B, pbase=2)
                    P.dma("sync", lambda e, i=i: e.dma_start(out=pf[:], in_=p_in[i * 128:(i + 1) * 128, :]), pf.B, writes=[pf.B])
                    V_(lambda e: e.tensor_copy(out=pb[:], in_=pf[:]), [pf.B], [pb.B])
                    pv = ps[4].t[:].bitcast(BF16)
                    for k in range(2):
                        P.op("tensor", lambda e, k=k, pv=pv: e.transpose(out=pv[:, k * 128:(k + 1) * 128], in_=pb[:, k * 128:(k + 1) * 128], identity=identb), reads=[pb.B, cstb.B], writes=[ps[4].B])
                    V_(lambda e, i=i, pv=pv: e.tensor_copy(out=pT[:, :, i * 128:(i + 1) * 128], in_=pv[:, 0:256].rearrange("p (k t) -> p k t", t=128)), [ps[4].B], [pT.b[i]])
                    for n in range(4):
                        pp = ps[5 + n % 2]
                        for k in range(2):
                            mm(pp[:, :], pT[:, k, i * 128:(i + 1) * 128], plw[:, k, n * 512:(n + 1) * 512], k == 0, k == 1, [pT.b[i], plw.B], pp.B)
                        A_(lambda e, i=i, n=n, pp=pp: e.activation(out=t1_[:], in_=pp[:, :], func=AF.Square, accum_out=ssq[:, i, n:n + 1]), [pp.B], [t1_.B, ssq.B])
                if CUT == 32:
                    P.disabled = True
                V_(lambda e: e.reduce_sum(out=rsp[:], in_=ssq[:], axis=AX.X), [ssq.B], [rsp.B])
                A_(lambda e: e.activation(out=rsp[:], in_=rsp[:], func=AF.Sqrt, bias=EPSC[:, 0:1], scale=1.0 / D), [rsp.B, EPSC.B], [rsp.B])
                V_(lambda e: e.reciprocal(out=rsp[:], in_=rsp[:]), [rsp.B], [rsp.B])
                P.dma("sync", lambda e: e.dma_start(out=gb2[0][:], in_=ple_ng.partition_broadcast(128)), gb2[0].B, writes=[gb2[0].B])
                if CUT == 33:
                    P.disabled = True
                pgw_v = ple_gw.rearrange("(kc p) n -> p kc n", p=128)
                for n in range(4):
                    W_ = ws[0]
                    P.dma("gpsimd", lambda e, W_=W_, n=n: e.dma_start(out=W_[:], in_=pgw_v[:, :, n * 512:(n + 1) * 512]), W_.B, writes=[W_.B])
                    nsl = slice(n * 512, (n + 1) * 512)
                    for i in range(8):
                        pgt, pp = ps[(i % 2) * 2], ps[(i % 2) * 2 + 1]
                        for kc in range(16):
                            mm(pgt[:, :], hT1[:, kc, i * 128:(i + 1) * 128], W_[:, kc, :], kc == 0, kc == 15, [hT1.b[i], W_.B], pgt.B)
                        for k in range(2):
                            mm(pp[:, :], pT[:, k, i * 128:(i + 1) * 128], plw[:, k, nsl], k == 0, k == 1, [pT.b[i], plw.B], pp.B)
                        A_(lambda e, pgt=pgt: e.activation(out=sgt[:], in_=pgt[:, :], func=AF.Sigmoid), [pgt.B], [sgt.B])
                        V_(lambda e, i=i, pp=pp, nsl=nsl: e.scalar_tensor_tensor(out=t1_[:], in0=pp[:, :], scalar=rsp[:, i:i + 1], in1=gb2[0][:, nsl], op0=ALU.mult, op1=ALU.mult),
                           [pp.B, rsp.B, gb2[0].B], [t1_.B])
                        V_(lambda e: e.tensor_tensor(out=t1_[:], in0=t1_[:], in1=sgt[:], op=ALU.mult), [t1_.B, sgt.B], [t1_.B])
                        V_(lambda e, i=i, nsl=nsl: e.tensor_tensor(out=acc[:, i, nsl], in0=acc[:, i, nsl], in1=t1_[:], op=ALU.add), [accB[i], t1_.B], [accB[i]])
                        P.dma("sync", lambda e, i=i, nsl=nsl: e.dma_start(out=out_d[i * 128:(i + 1) * 128, nsl], in_=acc[:, i, nsl]), accB[i], reads=[accB[i]])
                if CUT == 34:
                    P.disabled = True
                P.wait_all("sync", accB + dbg_bufs)
                if stop == 7:
                    P.wait_all("sync", dbg_bufs)
                P.flush()
      except _Stop:
        pass
    return nc


EPSC = None


def rsqrt(P, out, in_, outB, inB, eps_ap):
    P.op("scalar", lambda e: e.activation(out=out, in_=in_, func=AF.Sqrt, bias=eps_ap, scale=1.0), reads=[inB, EPSC.B], writes=[outB])
    P.op("vector", lambda e: e.reciprocal(out=out, in_=out), reads=[outB], writes=[outB])


def ln_tile(P, X, XN, ST, MV, gbc, bbc, out_bf, out_f32, eps=1e-5):
    for j in range(4):
        P.op("vector", lambda e, j=j: e.bn_stats(out=ST[:, j, :], in_=X[:, j * 512:(j + 1) * 512]), reads=[X.B], writes=[ST.B])
    P.op("vector", lambda e: e.bn_aggr(out=MV[:, 0:2], in_=ST[:]), reads=[ST.B], writes=[MV.B])
    rsqrt(P, MV[:, 2:3], MV[:, 1:2], MV.B, MV.B, EPSC[:, 0:1])
    P.op("vector", lambda e: e.scalar_tensor_tensor(out=MV[:, 3:4], in0=MV[:, 0:1], scalar=-1.0, in1=MV[:, 2:3], op0=ALU.mult, op1=ALU.mult),
         reads=[MV.B], writes=[MV.B])
    P.op("scalar", lambda e: e.activation(out=XN[:], in_=X[:], func=AF.Identity, bias=MV[:, 3:4], scale=MV[:, 2:3]),
         reads=[X.B, MV.B], writes=[XN.B])
    P.op("vector", lambda e: e.tensor_tensor(out=XN[:], in0=XN[:], in1=gbc[:], op=ALU.mult), reads=[XN.B, gbc.B], writes=[XN.B])
    if out_f32 is not None:
        P.op("vector", lambda e: e.tensor_tensor(out=out_f32[:], in0=XN[:], in1=bbc[:], op=ALU.add), reads=[XN.B, bbc.B], writes=[out_f32.B])
        if out_bf is not None:
            P.op("scalar", lambda e: e.activation(out=out_bf[:], in_=out_f32[:], func=AF.Copy), reads=[out_f32.B], writes=[out_bf.B])
    else:
        P.op("vector", lambda e: e.tensor_tensor(out=out_bf[:], in0=XN[:], in1=bbc[:], op=ALU.add), reads=[XN.B, bbc.B], writes=[out_bf.B])


def transpose_to_fm(P, HB, dstT, ti, ps, identb, identB, pbase=0):
    for half in range(2):
        pst = ps[pbase + half]
        pv = pst.t[:].bitcast(BF16)
        for j in range(8):
            kc = half * 8 + j
            P.op("tensor", lambda e, kc=kc, j=j, pv=pv: e.transpose(out=pv[:, j * 128:(j + 1) * 128], in_=HB[:, kc * 128:(kc + 1) * 128], identity=identb),
                 reads=[HB.B, identB], writes=[pst.B])
        eng = "scalar" if half == 0 else "vector"
        dst = dstT[:, half * 8:(half + 1) * 8, ti * 128:(ti + 1) * 128]
        src = pv.rearrange("p (j t) -> p j t", t=128)
        if eng == "scalar":
            P.op("scalar", lambda e, dst=dst, src=src: e.activation(out=dst, in_=src, func=AF.Copy), reads=[pst.B], writes=[dstT.b[ti]])
        else:
            P.op("vector", lambda e, dst=dst, src=src: e.tensor_copy(out=dst, in_=src), reads=[pst.B], writes=[dstT.b[ti]])


def make_consts():
    c = np.zeros((8, 128, 128), np.float32)
    i = np.arange(128)
    c[0] = np.eye(128)
    c[1] = (i[:, None] <= i[None, :]) * -EM05
    c[2] = (i[:, None] < i[None, :]) * -EM05
    c[3] = (i[:, None] > i[None, :]) * -EM05
    c[4] = (i[None, :] > i[:, None])
    c[5] = (i[None, :] >= i[:, None])
    c[6] = (i[:, None] > i[None, :])
    c[7] = (i[:, None] // 64 == i[None, :] // 64)
    n = np.arange(-127, 256)
    nn = np.maximum(n, 0)
    nf = np.maximum(nn, 1).astype(np.float32)
    large = 16 + (np.log(nf / 16) / math.log(128 / 16) * 16).astype(np.int32)
    large = np.minimum(large, 31)
    bucket = np.where(nn < 16, nn, large)
    oh = np.zeros((33, 383), np.float32)
    oh[bucket, np.arange(383)] = 1.0
    oh[:32, n < 0] = 0.0
    oh[32, n < 0] = 1.0
    return c, oh


_CACHE = {}


def kernel(**inputs):
    dbg = inputs.pop("_dbg", None)
    stop = inputs.pop("_stop", 99)
    key = ("nc", dbg, stop)
    if key not in _CACHE:
        _CACHE[key] = build(dbg, stop)
    nc = _CACHE[key]
    x = np.asarray(inputs["x"], np.float32)
    p = np.asarray(inputs["p"], np.float32)
    cm, oh = make_consts()
    shared = {"cmat": cm, "onehot": oh}
    for k, v in inputs.items():
        if k in ("x", "p"):
            continue
        a = np.asarray(v, np.float32)
        if a.shape[0] == 1 and k not in ("ln_in_g", "ln_in_b"):
            a = a[0]
        if k in ("moe_w_gate", "moe_w_up", "moe_w_down"):
            a = a.reshape(32, a.shape[-2], a.shape[-1])
            if stop < 6:
                a = a[:1]
        if k in ("rwkv_r_k",):
            a = a.reshape(-1)
        shared[k] = np.ascontiguousarray(a)
    in_maps = []
    for c in range(8):
        b, s = c // 2, c % 2
        seq = np.zeros((2048, 2048), np.float32)
        if s == 1:
            seq[:] = x[b]
        else:
            seq[1024:] = x[b, :1024]
        m = dict(shared)
        m["xs"] = seq
        m["p"] = np.ascontiguousarray(p[0, b, s * 1024:(s + 1) * 1024])
        m["flag"] = np.full((128, 1), float(s), np.float32)
        in_maps.append(m)
    res = run_bass_kernel_spmd(nc, in_maps, core_ids=list(range(8)))
    if dbg:
        return [(r["dbg"], r["out"]) for r in res.results]
    out = np.zeros((4, 2048, 2048), np.float32)
    for c in range(8):
        b, s = c // 2, c % 2
        out[b, s * 1024:(s + 1) * 1024] = res.results[c]["out"]
    return out
```

```python
import math
from contextlib import ExitStack
import numpy as np
import concourse.bass as bass
import concourse.mybir as mybir
from concourse.bass_utils import run_bass_kernel_spmd

F32 = mybir.dt.float32
BF16 = mybir.dt.bfloat16
AF = mybir.ActivationFunctionType
ALU = mybir.AluOpType
AX = mybir.AxisListType
ENGS = ("sync", "scalar", "vector", "gpsimd", "tensor")
ALPHA = 2.0 ** 0.25
LAMBDA_INIT = 0.8 - 0.6
NEG = -1.0e30
EM05 = math.exp(-0.5)
NHEADS = 8
NPAIRS = 8
CUT = 0
NEXP = 32
OUTSEL = tuple(range(8))


class Buf:
    __slots__ = ("name", "w", "r", "dsem", "dcnt")

    def __init__(self, name):
        self.name = name
        self.w = None
        self.r = []
        self.dsem = None
        self.dcnt = 0


class Prog:
    def __init__(self, nc, stack):
        self.nc = nc
        self.stack = stack
        self.ops = {e: [] for e in ENGS}
        self.sem = {e: stack.enter_context(nc.semaphore("c_" + e)) for e in ("scalar", "vector", "gpsimd", "tensor")}
        self.cnt = {e: 0 for e in ENGS}
        self.seen = {e: {} for e in ENGS}
        self.semobj = {}
        self.nbuf = 0
        self.disabled = False
        self.dbufs = []

    def buf(self, name=None):
        self.nbuf += 1
        return Buf(name or ("b%d" % self.nbuf))

    def _deps(self, eng, reads, writes):
        deps = []
        for b in reads:
            if b.w is not None:
                deps.append(b.w)
        for b in writes:
            if b.w is not None:
                deps.append(b.w)
            deps.extend(b.r)
        waits = {}
        for (sid, val, src) in deps:
            if src == "tensor" and eng == "tensor":
                continue
            if self.seen[eng].get(sid, 0) >= val:
                continue
            if waits.get(sid, 0) < val:
                waits[sid] = val
        for sid, val in waits.items():
            self.seen[eng][sid] = val
        return [(self.semobj[sid], val) for sid, val in waits.items()]

    def op(self, eng, fn, reads=(), writes=()):
        if self.disabled:
            return None
        waits = self._deps(eng, reads, writes)
        self.cnt[eng] += 1
        sem = self.sem[eng]
        self.semobj[id(sem)] = sem
        tok = (id(sem), self.cnt[eng], eng)
        self.ops[eng].append((waits, fn, (sem, 1)))
        for b in reads:
            b.r.append(tok)
        for b in writes:
            b.w = tok
            b.r = []
        return tok

    def dma(self, eng, fn, sbuf_buf, reads=(), writes=()):
        if self.disabled:
            return None
        waits = self._deps(eng, reads, writes)
        if sbuf_buf.dsem is None:
            sbuf_buf.dsem = self.stack.enter_context(self.nc.semaphore("d_" + sbuf_buf.name))
            self.semobj[id(sbuf_buf.dsem)] = sbuf_buf.dsem
            self.dbufs.append(sbuf_buf)
        sbuf_buf.dcnt += 16
        tok = (id(sbuf_buf.dsem), sbuf_buf.dcnt, "dma")
        self.ops[eng].append((waits, fn, (sbuf_buf.dsem, 16)))
        for b in reads:
            b.r.append(tok)
        for b in writes:
            b.w = tok
            b.r = []
        return tok

    def wait_all(self, eng, bufs):
        if self.disabled:
            return
        waits = self._deps(eng, bufs, bufs)
        self.ops[eng].append((waits, None, None))

    def barrier(self):
        toks = []
        for x in ("scalar", "vector", "gpsimd", "tensor"):
            if self.cnt[x] > 0:
                self.semobj[id(self.sem[x])] = self.sem[x]
                toks.append((id(self.sem[x]), self.cnt[x]))
        for b in self.dbufs:
            toks.append((id(b.dsem), b.dcnt))
        for eng in ENGS:
            waits = []
            for sid, val in toks:
                if self.seen[eng].get(sid, 0) >= val:
                    continue
                self.seen[eng][sid] = val
                waits.append((self.semobj[sid], val))
            if waits:
                self.ops[eng].append((waits, None, None))

    def flush(self):
        self.barrier()
        nc = self.nc
        ops = self.ops
        self.ops = {e: [] for e in ENGS}

        def replay(lst, e):
            for waits, fn, inc in lst:
                for sem, val in waits:
                    e.wait_ge(sem, val)
                if fn is not None:
                    ins = fn(e)
                    ins.then_inc(inc[0], inc[1])

        with nc.Block() as block:
            @block.sync
            def _(e):
                replay(ops["sync"], e)

            @block.scalar
            def _(e):
                replay(ops["scalar"], e)

            @block.vector
            def _(e):
                replay(ops["vector"], e)

            @block.gpsimd
            def _(e):
                replay(ops["gpsimd"], e)

            @block.tensor
            def _(e):
                replay(ops["tensor"], e)


class T:
    def __init__(self, P, stack, name, shape, dtype, psum=False, nb=1):
        nc = P.nc
        if psum:
            self.t = stack.enter_context(nc.psum_tensor("t_" + name, shape, dtype))
        else:
            self.t = stack.enter_context(nc.sbuf_tensor("t_" + name, shape, dtype))
        self.b = [P.buf(name + "_%d" % i) for i in range(nb)]
        self.B = self.b[0]

    def __getitem__(self, k):
        return self.t[k]


class _Stop(Exception):
    pass


def build(dbg=None, stop=99):
    nc = bass.Bass("TRN2", target_bir_lowering=False)
    D = 2048
    dram = {}

    def din(name, shape):
        dram[name] = nc.dram_tensor(name, list(shape), F32, kind="ExternalInput").ap()
        return dram[name]

    xs = din("xs", [2048, D])
    p_in = din("p", [1024, 256])
    flag_d = din("flag", [128, 1])
    ln_in_g = din("ln_in_g", [D]); ln_in_b = din("ln_in_b", [D])
    rel_bias = din("rel_bias", [32, 8])
    w_in = din("w_in", [D, 6432])
    lamq1 = din("diff_lam_q1", [64]); lamk1 = din("diff_lam_k1", [64])
    lamq2 = din("diff_lam_q2", [64]); lamk2 = din("diff_lam_k2", [64])
    subln_g = din("diff_subln_g", [128])
    rwkv_mu = din("rwkv_mu", [3360])
    rwkv_w0 = din("rwkv_w0", [1024]); rwkv_w2 = din("rwkv_w2", [64, 1024])
    rwkv_a0 = din("rwkv_a0", [1024]); rwkv_a2 = din("rwkv_a2", [64, 1024])
    rwkv_g2 = din("rwkv_g2", [160, 1024])
    rwkv_k_k = din("rwkv_k_k", [1024]); rwkv_k_a = din("rwkv_k_a", [1024])
    rwkv_r_k = din("rwkv_r_k", [1024])
    lnx_g = din("rwkv_lnx_g", [1024]); lnx_b = din("rwkv_lnx_b", [1024])
    w_out = din("w_out", [D, D])
    ln1_g = din("ln1_g", [D]); ln1_b = din("ln1_b", [D])
    rgw = din("router_group_w", [D, 4]); rgb = din("router_group_b", [4])
    rew = din("router_expert_w", [D, 32]); reb = din("router_expert_b", [32])
    nE = 32 if stop >= 6 else 1
    wg_d = din("moe_w_gate", [nE, D, 512]); wu_d = din("moe_w_up", [nE, D, 512]); wd_d = din("moe_w_down", [nE, 512, D])
    ln2_g = din("ln2_g", [D]); ln2_b = din("ln2_b", [D])
    ple_w = din("ple_w", [256, D]); ple_ng = din("ple_norm_g", [D]); ple_gw = din("ple_gate_w", [D, D])
    cmat = din("cmat", [8, 128, 128])
    oh_d = din("onehot", [33, 383])
    out_d = nc.dram_tensor("out", [1024, D], F32, kind="ExternalOutput").ap()
    dbg_d = None
    if dbg:
        dbg_d = nc.dram_tensor("dbg", list(dbg), F32, kind="ExternalOutput").ap()
    trep_d = nc.dram_tensor("trep", [8, 128, 383], F32, kind="Internal").ap()

    stA = ExitStack()
    with ExitStack() as st0:
      try:
        P = Prog(nc, st0)
        stB = st0
        yT = T(P, stB, "yT", [128, 16, 1024], BF16, nb=16)
        cst = T(P, st0, "cst", [128, 8, 128], F32)
        cstb = T(P, st0, "cstb", [128, 8, 128], BF16)
        flag = T(P, st0, "flag", [128, 1], F32)
        pbias = T(P, st0, "pbias", [128, 1], F32)
        ps = [T(P, st0, "ps%d" % i, [128, 512], F32, psum=True) for i in range(8)]
        IDENT, TRI_I, TRI_E, TRI_R, UT_S, UT_I, LT_S, BLK1 = range(8)
        identb = cstb[:, IDENT, :]

        global EPSC
        EPSC = T(P, st0, "epsc", [128, 4], F32)
        P.op("vector", lambda e: e.memset(EPSC[:, 0:1], 1e-5), writes=[EPSC.B])
        P.op("vector", lambda e: e.memset(EPSC[:, 1:2], 64e-5), writes=[EPSC.B])
        P.op("vector", lambda e: e.memset(EPSC[:, 2:3], 1e-24), writes=[EPSC.B])
        P.op("vector", lambda e: e.memset(EPSC[:, 3:4], 0.0), writes=[EPSC.B])
        P.dma("sync", lambda e: e.dma_start(out=cst[:], in_=cmat.rearrange("m p n -> p m n")), cst.B, writes=[cst.B])
        P.dma("sync", lambda e: e.dma_start(out=flag[:], in_=flag_d), flag.B, writes=[flag.B])
        P.op("vector", lambda e: e.tensor_copy(out=cstb[:], in_=cst[:]), reads=[cst.B], writes=[cstb.B])
        P.op("vector", lambda e: e.tensor_scalar(out=pbias[:], in0=flag[:], scalar1=-1.0, scalar2=-NEG, op0=ALU.add, op1=ALU.mult),
             reads=[flag.B], writes=[pbias.B])

        dbg_bufs = []

        def dump(stk, slot, ap, bufs, width, parts=128):
            if not dbg or P.disabled:
                return
            tmp = T(P, stk, "dbg%d" % slot, [128, width], F32)
            P.op("vector", lambda e: e.tensor_copy(out=tmp[0:parts, :], in_=ap), reads=bufs, writes=[tmp.B])
            P.dma("sync", lambda e: e.dma_start(out=dbg_d[slot, 0:parts, 0:width], in_=tmp[0:parts, :]), tmp.B, reads=[tmp.B])
            dbg_bufs.append(tmp.B)

        def mm(out, lhsT, rhs, start, stop, reads, wB, tp=None, sgc=False):
            kw = {}
            if tp is not None:
                kw["tile_position"] = tp
            if sgc:
                kw["skip_group_check"] = True
            P.op("tensor", lambda e: e.matmul(out=out, lhsT=lhsT, rhs=rhs, start=start, stop=stop, **kw), reads=reads, writes=[wB])

        def V_(fn, reads, writes):
            P.op("vector", fn, reads=reads, writes=writes)

        def A_(fn, reads, writes):
            P.op("scalar", fn, reads=reads, writes=writes)

        def small_load(dst_ap, dstB, src_ap):
            P.dma("sync", lambda e: e.dma_start(out=dst_ap, in_=src_ap, allow_slow_non_contiguous=True), dstB, writes=[dstB])

        w_in_v = w_in.rearrange("(kc p) n -> p kc n", p=128)

        def proj_fm(wt, c0, M, tb, pst):
            for kc in range(16):
                mm(pst[0:M, 0:512], wt[:, kc, c0:c0 + M], hT0[:, kc, tb * 512:(tb + 1) * 512], kc == 0, kc == 15,
                   [wt.B] + hT0.b[tb * 4:(tb + 1) * 4], pst.B)

        def shiftmix(pst, M, tb, R, mucol, omucol, muB, out_ap, outB, tmp):
            if tb == 0:
                V_(lambda e: e.memset(R[:], 0.0), [], [R.B])
            else:
                V_(lambda e: e.tensor_copy(out=R[:, 0:1], in_=R[:, 512:513]), [R.B], [R.B])
            if tb < 2:
                A_(lambda e: e.activation(out=R[0:M, 1:513], in_=pst[0:M, 0:512], func=AF.Copy, scale=flag[0:M, 0:1]), [pst.B, flag.B, R.B], [R.B])
            else:
                A_(lambda e: e.activation(out=R[0:M, 1:513], in_=pst[0:M, 0:512], func=AF.Copy), [pst.B, R.B], [R.B])
            V_(lambda e: e.tensor_scalar(out=tmp[0:M, :], in0=R[0:M, 1:513], scalar1=omucol, scalar2=None, op0=ALU.mult), [R.B, muB], [tmp.B])
            V_(lambda e: e.scalar_tensor_tensor(out=out_ap, in0=R[0:M, 0:512], scalar=mucol, in1=tmp[0:M, :], op0=ALU.mult, op1=ALU.add),
               [R.B, muB, tmp.B], [outB])

        mul_ = T(P, st0, "mul", [128, 8], F32)
        murkv = T(P, st0, "murkv", [128, 48], F32)
        st0.enter_context(stA)
        hT0 = T(P, stA, "hT0", [128, 16, 2048], BF16, nb=16)
        txw = T(P, stA, "txw", [64, 2048], BF16)
        xaT = T(P, stA, "xaT", [64, 2048], BF16)
        sxa = T(P, stA, "sxa", [128, 2048], BF16)
        sxb = T(P, stA, "sxb", [32, 2048], BF16)
        V_(lambda e: e.memset(mul_[:], 0.0), [], [mul_.B])
        small_load(mul_[0:64, 0:1], mul_.B, rwkv_mu[3072:3136].rearrange("(p a) -> p a", a=1))
        small_load(mul_[0:64, 1:2], mul_.B, rwkv_mu[3136:3200].rearrange("(p a) -> p a", a=1))
        small_load(mul_[0:128, 2:3], mul_.B, rwkv_mu[3200:3328].rearrange("(p a) -> p a", a=1))
        small_load(mul_[0:32, 3:4], mul_.B, rwkv_mu[3328:3360].rearrange("(p a) -> p a", a=1))
        V_(lambda e: e.tensor_scalar(out=mul_[:, 4:8], in0=mul_[:, 0:4], scalar1=-1.0, scalar2=1.0, op0=ALU.mult, op1=ALU.add), [mul_.B], [mul_.B])
        small_load(murkv[:, 0:24], murkv.B, rwkv_mu[0:3072].rearrange("(a p) -> p a", p=128))
        V_(lambda e: e.tensor_scalar(out=murkv[:, 24:48], in0=murkv[:, 0:24], scalar1=-1.0, scalar2=1.0, op0=ALU.mult, op1=ALU.add), [murkv.B], [murkv.B])

        with ExitStack() as st:
            gbc = T(P, st, "gbc", [128, D], F32); bbc = T(P, st, "bbc", [128, D], F32)
            P.dma("sync", lambda e: e.dma_start(out=gbc[:], in_=ln_in_g.partition_broadcast(128)), gbc.B, writes=[gbc.B])
            P.dma("sync", lambda e: e.dma_start(out=bbc[:], in_=ln_in_b.partition_broadcast(128)), bbc.B, writes=[bbc.B])
            wl = T(P, st, "wl", [128, 16, 288], BF16)
            P.dma("gpsimd", lambda e: e.dma_start(out=wl[:], in_=w_in_v[:, :, 6144:6432]), wl.B, writes=[wl.B])
            xt = [T(P, st, "xt%d" % i, [128, D], F32) for i in range(2)]
            xn = [T(P, st, "xn%d" % i, [128, D], F32) for i in range(2)]
            hb = [T(P, st, "hb%d" % i, [128, D], BF16) for i in range(2)]
            stt = [T(P, st, "stt%d" % i, [128, 4, 6], F32) for i in range(2)]
            mv = [T(P, st, "mv%d" % i, [128, 4], F32) for i in range(2)]
            for i in range(16):
                s = i % 2
                X = xt[s]
                P.dma("sync", lambda e, X=X, i=i: e.dma_start(out=X[:], in_=xs[i * 128:(i + 1) * 128, :]), X.B, writes=[X.B])
                ln_tile(P, X, xn[s], stt[s], mv[s], gbc, bbc, hb[s], None)
                transpose_to_fm(P, hb[s], hT0, i, ps, identb, cstb.B)
            Rl = [T(P, st, "Rl%d" % i, [128, 513], F32) for i in range(4)]
            tmpl = T(P, st, "tmpl", [128, 512], F32)
            xsl = T(P, st, "xsl", [128, 512], F32)
            groups = [(0, 64, txw, AF.Tanh), (64, 64, xaT, AF.Copy), (128, 128, sxa, AF.Sigmoid), (256, 32, sxb, AF.Sigmoid)]
            for tb in range(4):
                for gi, (c0, M, dst, fn_) in enumerate(groups):
                    pst = ps[2 + (gi % 2)]
                    proj_fm(wl, c0, M, tb, pst)
                    shiftmix(pst, M, tb, Rl[gi], mul_[0:M, gi:gi + 1], mul_[0:M, 4 + gi:5 + gi], mul_.B, xsl[0:M, :], xsl.B, tmpl)
                    A_(lambda e, dst=dst, M=M, fn_=fn_, tb=tb: e.activation(out=dst[0:M, tb * 512:(tb + 1) * 512], in_=xsl[0:M, :], func=fn_),
                       [xsl.B], [dst.B])
            dump(st, 0, hT0[:, 3, 0:1024], hT0.b, 1024)
            dump(st, 1, txw[0:64, 1024:2048], [txw.B], 1024, 64)
            if stop == 1:
                P.wait_all("sync", dbg_bufs)
            P.flush()
            if stop == 1:
                P.disabled = True

        with ExitStack() as st:
            rb = T(P, st, "rb", [33, 8], F32)
            oh = T(P, st, "oh", [33, 383], F32)
            P.dma("sync", lambda e: e.dma_start(out=rb[0:32, :], in_=rel_bias), rb.B, writes=[rb.B])
            V_(lambda e: e.memset(rb[32:33, :], NEG), [], [rb.B])
            P.dma("sync", lambda e: e.dma_start(out=oh[:], in_=oh_d), oh.B, writes=[oh.B])
            rbh = T(P, st, "rbh", [33, 128], F32)
            treps = T(P, st, "treps", [128, 383], F32)
            BN = T(P, st, "BN", [128, 8, 256], F32, nb=8)
            farb = T(P, st, "farb", [128, 16], F32)
            drb = [P.buf("drb%d" % h) for h in range(8)]
            for h in range(8):
                V_(lambda e, h=h: e.tensor_copy(out=rbh[:], in_=rb[:, h:h + 1].to_broadcast([33, 128])), [rb.B], [rbh.B])
                mm(ps[0][:, 0:383], rbh[:], oh[:], True, True, [rbh.B, oh.B], ps[0].B)
                V_(lambda e: e.tensor_copy(out=treps[:], in_=ps[0][:, 0:383]), [ps[0].B], [treps.B])
                V_(lambda e, h=h: e.tensor_copy(out=farb[:, h:h + 1], in_=treps[:, 382:383]), [treps.B], [farb.B])
                P.dma("sync", lambda e, h=h: e.dma_start(out=trep_d[h], in_=treps[:]), treps.B, reads=[treps.B], writes=[drb[h]])
                skew = bass.AP(tensor=trep_d.tensor, offset=h * 128 * 383 + 127, ap=[[382, 128], [1, 256]])
                P.dma("sync", lambda e, h=h, skew=skew: e.dma_start(out=BN[:, h, :], in_=skew), BN.b[h], reads=[drb[h]], writes=[BN.b[h]])
            V_(lambda e: e.tensor_scalar(out=farb[:, 8:16], in0=farb[:, 0:8], scalar1=pbias[:, 0:1], scalar2=None, op0=ALU.add), [farb.B, pbias.B], [farb.B])
            if CUT == 1:
                P.disabled = True
            lq = T(P, st, "lq", [1, 4, 64], F32)
            for j, src in enumerate((lamq1, lamk1, lamq2, lamk2)):
                P.dma("sync", lambda e, j=j, src=src: e.dma_start(out=lq[0:1, j, :], in_=src.rearrange("(a n) -> a n", a=1)), lq.B, writes=[lq.B])
            lt = T(P, st, "lt", [1, 8], F32)
            lpr = T(P, st, "lpr", [1, 2, 64], F32)
            V_(lambda e: e.tensor_tensor(out=lpr[0:1, 0, :], in0=lq[0:1, 0, :], in1=lq[0:1, 1, :], op=ALU.mult), [lq.B], [lpr.B])
            V_(lambda e: e.tensor_tensor(out=lpr[0:1, 1, :], in0=lq[0:1, 2, :], in1=lq[0:1, 3, :], op=ALU.mult), [lq.B], [lpr.B])
            V_(lambda e: e.reduce_sum(out=lt[0:1, 0:2], in_=lpr[0:1, :, :], axis=AX.X), [lpr.B], [lt.B])
            A_(lambda e: e.activation(out=lt[0:1, 2:4], in_=lt[0:1, 0:2], func=AF.Exp), [lt.B], [lt.B])
            V_(lambda e: e.tensor_tensor(out=lt[0:1, 4:5], in0=lt[0:1, 3:4], in1=lt[0:1, 2:3], op=ALU.subtract), [lt.B], [lt.B])
            V_(lambda e: e.tensor_scalar(out=lt[0:1, 5:6], in0=lt[0:1, 4:5], scalar1=-LAMBDA_INIT, scalar2=None, op0=ALU.add), [lt.B], [lt.B])
            ones1 = T(P, st, "ones1", [1, 128], F32)
            V_(lambda e: e.memset(ones1[:], 1.0), [], [ones1.B])
            nlam = T(P, st, "nlam", [128, 1], F32)
            mm(ps[1][:, 0:1], ones1[0:1, :], lt[0:1, 5:6], True, True, [ones1.B, lt.B], ps[1].B)
            V_(lambda e: e.tensor_copy(out=nlam[:], in_=ps[1][:, 0:1]), [ps[1].B], [nlam.B])
            gsub = T(P, st, "gsub", [128, 128], F32)
            P.dma("sync", lambda e: e.dma_start(out=gsub[:], in_=subln_g.partition_broadcast(128)), gsub.B, writes=[gsub.B])
            V_(lambda e: e.tensor_scalar(out=gsub[:], in0=gsub[:], scalar1=1.0 - LAMBDA_INIT, scalar2=None, op0=ALU.mult), [gsub.B], [gsub.B])
            if CUT == 2:
                P.disabled = True

            WQ = [T(P, st, "WQ%d" % i, [128, 16, 128], BF16) for i in range(2)]
            WK = [T(P, st, "WK%d" % i, [128, 16, 128], BF16) for i in range(2)]
            WV = [T(P, st, "WV%d" % i, [128, 16, 128], BF16) for i in range(2)]
            qT = T(P, st, "qT", [128, 1024], BF16)
            kT = T(P, st, "kT", [128, 2048], BF16)
            Va = T(P, st, "Va", [128, 16, 129], BF16)
            V_(lambda e: e.memset(Va[:, :, 128:129], 1.0), [], [Va.B])
            E = [[T(P, st, "E%d%d" % (m, j), [128, 512], BF16) for j in range(2)] for m in range(2)]
            TMP = [[T(P, st, "TMPa%d%d" % (m, j), [128, 256], F32) for j in range(2)] for m in range(2)]
            rc = T(P, st, "rc", [128, 4], F32)
            o2 = T(P, st, "o2", [128, 128], F32)
            oo4 = [T(P, st, "oo%d" % i, [128, 128], F32) for i in range(4)]
            yb4 = T(P, st, "yb4", [128, 4, 128], BF16)
            pending = []
            junk = T(P, st, "junk", [128, 128], F32)
            ss = T(P, st, "ss", [128, 2], F32)
            yb = T(P, st, "yb", [128, 128], BF16)

            def load_head(h):
                s = h % 2
                for W_, c0 in ((WQ[s], h * 128), (WK[s], 1024 + h * 128), (WV[s], 2048 + h * 128)):
                    P.dma("gpsimd", lambda e, W_=W_, c0=c0: e.dma_start(out=W_[:], in_=w_in_v[:, :, c0:c0 + 128]), W_.B, writes=[W_.B])

            def accap(a, lo, hi):
                return ps[5 + a // 3][:, (a % 3) * 129 + lo:(a % 3) * 129 + hi]

            load_head(0)
            it = 0
            for h in range(NHEADS):
                if h + 1 < NHEADS:
                    load_head(h + 1)
                s = h % 2
                for tb in (2, 3):
                    proj_fm(WQ[s], 0, 128, tb, ps[tb % 2])
                    A_(lambda e, tb=tb: e.activation(out=qT[:, (tb - 2) * 512:(tb - 1) * 512], in_=ps[tb % 2][:, :], func=AF.Copy), [ps[tb % 2].B], [qT.B])
                for tb in range(4):
                    proj_fm(WK[s], 0, 128, tb, ps[2 + tb % 2])
                    V_(lambda e, tb=tb: e.tensor_copy(out=kT[:, tb * 512:(tb + 1) * 512], in_=ps[2 + tb % 2][:, :]), [ps[2 + tb % 2].B], [kT.B])
                for g4 in range(4):
                    pst = ps[g4 % 2]
                    for j in range(4):
                        i = g4 * 4 + j
                        for kc in range(16):
                            mm(pst[:, j * 128:(j + 1) * 128], hT0[:, kc, i * 128:(i + 1) * 128], WV[s][:, kc, :], kc == 0, kc == 15,
                               [WV[s].B, hT0.b[i]], pst.B)
                    V_(lambda e, g4=g4, pst=pst: e.tensor_copy(out=Va[:, g4 * 4:(g4 + 1) * 4, 0:128], in_=pst[:, :].rearrange("p (j t) -> p j t", t=128)),
                       [pst.B], [Va.B])
                while pending:
                    pending.pop(0)()
                if CUT == 3:
                    P.disabled = True
                for g in range(2):
                    nkb = 8 + 4 * g + 4
                    steps = []
                    for kb in range(nkb):
                        t = kb - 8 - 4 * g
                        i0 = max(0, t)
                        N = (4 - i0) * 128
                        q0 = g * 512 + i0 * 128
                        if t >= 0:
                            wn = min(256, N); b0 = 0
                        elif t == -1:
                            wn = 128; b0 = 128
                        else:
                            wn = 0; b0 = 0
                        steps.append((kb, i0, N, q0, wn, b0, kb < 8, it))
                        it += 1

                    def emit_scores(stp, h=h):
                        kb, i0, N, q0, wn, b0, pref, it_ = stp
                        for m in range(2):
                            pS = ps[m * 2 + (it_ % 2)]
                            Em = E[m][it_ % 2]
                            Tm = TMP[m][it_ % 2]
                            mm(pS[:, 0:N], kT[m * 64:(m + 1) * 64, kb * 128:(kb + 1) * 128], qT[m * 64:(m + 1) * 64, q0:q0 + N], True, True,
                               [kT.B, qT.B], pS.B)
                            if wn > 0:
                                V_(lambda e, pS=pS, Tm=Tm, wn=wn, b0=b0, h=h: e.scalar_tensor_tensor(out=Tm[:, 0:wn], in0=pS[:, 0:wn], scalar=0.125,
                                   in1=BN[:, h, b0:b0 + wn], op0=ALU.mult, op1=ALU.add), [pS.B, BN.b[h]], [Tm.B])
                                bcol = pbias[:, 0:1] if pref else EPSC[:, 3:4]
                                A_(lambda e, Em=Em, Tm=Tm, wn=wn, bcol=bcol: e.activation(out=Em[:, 0:wn], in_=Tm[:, 0:wn], func=AF.Exp, bias=bcol, scale=1.0),
                                   [Tm.B, pbias.B, EPSC.B], [Em.B])
                            if N > wn:
                                fcol = farb[:, 8 + h:9 + h] if pref else farb[:, h:h + 1]
                                A_(lambda e, Em=Em, pS=pS, wn=wn, N=N, fcol=fcol: e.activation(out=Em[:, wn:N], in_=pS[:, wn:N], func=AF.Exp, bias=fcol, scale=0.125),
                                   [pS.B, farb.B], [Em.B])

                    def emit_pv(stp):
                        kb, i0, N, q0, wn, b0, pref, it_ = stp
                        for m in range(2):
                            Em = E[m][it_ % 2]
                            for li in range(4 - i0):
                                i = i0 + li
                                a = m * 4 + i
                                mm(accap(a, 0, 129), Em[:, li * 128:(li + 1) * 128], Va[:, kb, :], (kb == 0 and a % 3 == 0), False,
                                   [Em.B, Va.B], ps[5 + a // 3].B, sgc=True)

                    emit_scores(steps[0])
                    for si, stp in enumerate(steps):
                        if si + 1 < len(steps):
                            emit_scores(steps[si + 1])
                        emit_pv(stp)
                        if si == 1:
                            while pending:
                                pending.pop(0)()
                    if CUT == 4:
                        P.disabled = True
                    for i in range(4):
                        a1, a2 = i, 4 + i
                        B1, B2 = ps[5 + a1 // 3].B, ps[5 + a2 // 3].B
                        oo = oo4[i]
                        V_(lambda e, a1=a1: e.reciprocal(out=rc[:, 0:1], in_=accap(a1, 128, 129)), [B1], [rc.B])
                        V_(lambda e, a2=a2: e.reciprocal(out=rc[:, 1:2], in_=accap(a2, 128, 129)), [B2], [rc.B])
                        V_(lambda e: e.tensor_tensor(out=rc[:, 2:3], in0=rc[:, 1:2], in1=nlam[:, 0:1], op=ALU.mult), [rc.B, nlam.B], [rc.B])
                        V_(lambda e, a2=a2: e.tensor_scalar(out=o2[:], in0=accap(a2, 0, 128), scalar1=rc[:, 2:3], scalar2=None, op0=ALU.mult), [B2, rc.B], [o2.B])
                        V_(lambda e, a1=a1, oo=oo: e.scalar_tensor_tensor(out=oo[:], in0=accap(a1, 0, 128), scalar=rc[:, 0:1], in1=o2[:], op0=ALU.mult, op1=ALU.add),
                           [B1, rc.B, o2.B], [oo.B])
                    for i in range(4):
                        oo = oo4[i]
                        A_(lambda e, oo=oo: e.activation(out=junk[:], in_=oo[:], func=AF.Square, accum_out=ss[:, 0:1]), [oo.B], [junk.B, ss.B])
                        A_(lambda e: e.activation(out=ss[:, 1:2], in_=ss[:, 0:1], func=AF.Sqrt, bias=EPSC[:, 0:1], scale=1.0 / 128), [ss.B, EPSC.B], [ss.B])
                        V_(lambda e: e.reciprocal(out=ss[:, 1:2], in_=ss[:, 1:2]), [ss.B], [ss.B])
                        V_(lambda e, oo=oo, i=i: e.scalar_tensor_tensor(out=yb4[:, i, :], in0=oo[:], scalar=ss[:, 1:2], in1=gsub[:], op0=ALU.mult, op1=ALU.mult),
                           [oo.B, ss.B, gsub.B], [yb4.B])

                    def fin_pe(h=h, g=g):
                        pv = ps[4].t[:].bitcast(BF16)
                        for i in range(4):
                            P.op("tensor", lambda e, pv=pv, i=i: e.transpose(out=pv[:, i * 128:(i + 1) * 128], in_=yb4[:, i, :], identity=identb), reads=[yb4.B, cstb.B], writes=[ps[4].B])
                        A_(lambda e, pv=pv: e.activation(out=yT[:, h, g * 512:(g + 1) * 512], in_=pv[:, 0:512], func=AF.Copy), [ps[4].B], [yT.b[h]])
                    pending.append(fin_pe)
            while pending:
                pending.pop(0)()
            dump(st, 2, yT[:, 0, :], [yT.b[0]], 1024)
            dump(st, 3, yT[:, NHEADS - 1, :], [yT.b[NHEADS - 1]], 1024)
            if stop == 2:
                P.wait_all("sync", dbg_bufs)
            P.flush()
            if stop == 2:
                P.disabled = True

        with ExitStack() as st:
            WR = [T(P, st, "WR0", [128, 16, 128], BF16)] * 2
            WKr = [T(P, st, "WKr0", [128, 16, 128], BF16)] * 2
            WVr = [T(P, st, "WVr0", [128, 16, 128], BF16)] * 2
            prm = T(P, st, "prm", [128, 8, 8], F32)
            for j, src in enumerate((rwkv_k_k, rwkv_k_a, rwkv_a0)):
                small_load(prm[:, :, j], prm.B, src.rearrange("(a p) -> p a", p=128))
            V_(lambda e: e.tensor_scalar(out=prm[:, :, 3], in0=prm[:, :, 1], scalar1=-1.0, scalar2=1.0, op0=ALU.mult, op1=ALU.add), [prm.B], [prm.B])
            w2b = T(P, st, "w2b", [64, 1024], BF16); a2b = T(P, st, "a2b", [64, 1024], BF16); g2b = T(P, st, "g2b", [128, 2, 1024], BF16)
            P.dma("gpsimd", lambda e: e.dma_start(out=w2b[:], in_=rwkv_w2), w2b.B, writes=[w2b.B])
            P.dma("gpsimd", lambda e: e.dma_start(out=a2b[:], in_=rwkv_a2), a2b.B, writes=[a2b.B])
            P.dma("gpsimd", lambda e: e.dma_start(out=g2b[:, 0, :], in_=rwkv_g2[0:128, :]), g2b.B, writes=[g2b.B])
            P.dma("gpsimd", lambda e: e.dma_start(out=g2b[0:32, 1, :], in_=rwkv_g2[128:160, :]), g2b.B, writes=[g2b.B])
            pp3 = T(P, st, "pp3", [128, 3, 128], F32)
            rkf = T(P, st, "rkf", [128, 8], F32)
            small_load(rkf[:], rkf.B, rwkv_r_k.rearrange("(a p) -> p a", p=128))
            RK = T(P, st, "RK", [128, 8, 2], BF16)
            V_(lambda e: e.memset(RK[:], 0.0), [], [RK.B])
            V_(lambda e: e.tensor_copy(out=RK[0:64, :, 0], in_=rkf[0:64, :]), [rkf.B], [RK.B])
            V_(lambda e: e.tensor_copy(out=RK[64:128, :, 1], in_=rkf[64:128, :]), [rkf.B], [RK.B])
            Rr = [T(P, st, "Rr%d" % i, [128, 513], F32) for i in range(3)]
            tmpr = T(P, st, "tmpr", [128, 512], F32)
            rs = T(P, st, "rs", [128, 512], F32); ks = T(P, st, "ks", [128, 512], F32); vsb = T(P, st, "vsb", [128, 512], BF16)
            af = T(P, st, "af", [128, 512], F32); kkk = T(P, st, "kkk", [128, 512], F32); sqb = T(P, st, "sqb", [128, 512], BF16)
            rinv = T(P, st, "rinv", [128, 512], F32); kk = T(P, st, "kk", [128, 512], F32); uu = tmpr
            k2 = T(P, st, "k2", [128, 512], F32); bb_ = T(P, st, "bb", [128, 512], F32)
            k2b = T(P, st, "k2b", [128, 512], BF16); bbb = T(P, st, "bbb", [128, 512], BF16); rk2 = T(P, st, "rk2", [128, 512], BF16)
            lwt2 = [T(P, st, "lwt%d" % i, [128, 128], F32) for i in range(2)]
            ex2 = [T(P, st, "ex%d" % i, [128, 4, 128], F32) for i in range(2)]
            ART2 = [T(P, st, "ART%d" % i, [128, 256], BF16) for i in range(2)]
            BhT2 = [T(P, st, "BhT%d" % i, [128, 128], BF16) for i in range(2)]; KhT2 = [T(P, st, "KhT%d" % i, [128, 128], BF16) for i in range(2)]
            TMt2 = [T(P, st, "TMt%d" % i, [128, 4, 128], BF16) for i in range(2)]
            lwt, ex, ART, BhT, KhT, TMt = lwt2[1], ex2[1], ART2[1], BhT2[1], KhT2[1], TMt2[1]
            MTh = [T(P, st, "MT%d" % h_, [128, 2, 256], BF16) for h_ in range(2)]
            NMh = [[T(P, st, "NM%d%d" % (h_, i), [128, 256], BF16) for i in range(2)] for h_ in range(2)]
            Xfh = [T(P, st, "Xf%d" % h_, [128, 128], F32) for h_ in range(2)]; Xbh = [T(P, st, "Xb%d" % h_, [128, 128], BF16) for h_ in range(2)]
            MT, Xf = MTh[1], Xfh[1]
            pBx = [P.buf("pBx%d" % h_) for h_ in range(2)]; pBn = [P.buf("pBn%d" % h_) for h_ in range(2)]; pBq = [P.buf("pBq%d" % h_) for h_ in range(2)]
            GTs = T(P, st, "GTs", [128, 16, 128], BF16); Hs = T(P, st, "Hs", [128, 16, 64], F32)
            QeTs = T(P, st, "QeTs", [128, 8, 128], BF16); Yls = T(P, st, "Yls", [128, 8, 128], F32)
            Vtm = T(P, st, "Vtm", [128, 8, 128], BF16); sbon = T(P, st, "sbon", [128, 8, 2], F32)
            gtm = T(P, st, "gtm", [128, 8, 128], F32)
            Sb = T(P, st, "Sb", [128, 128], BF16)
            Yt3 = [T(P, st, "Yt%d" % i, [128, 128], F32) for i in range(3)]; Yt = Yt3[0]; yn = T(P, st, "yn", [128, 128], F32); ybr = T(P, st, "ybr", [128, 128], BF16)
            gst = T(P, st, "gst", [128, 2, 6], F32); gmv = T(P, st, "gmv", [128, 2, 2], F32); grs = T(P, st, "grs", [128, 2], F32)
            ident64 = cst[0:64, IDENT, 0:64]
            V_(lambda e: e.memset(GTs[:], 0.0), [], [GTs.B])

            def load_pair(c):
                s = c % 2
                for W_, c0 in ((WR[s], 3072 + c * 128), (WKr[s], 4096 + c * 128), (WVr[s], 5120 + c * 128)):
                    P.dma("gpsimd", lambda e, W_=W_, c0=c0: e.dma_start(out=W_[:], in_=w_in_v[:, :, c0:c0 + 128]), W_.B, writes=[W_.B])

            if CUT == 11:
                P.disabled = True
            load_pair(0)
            for c in range(NPAIRS):
                s = c % 2
                csl = slice(c * 128, (c + 1) * 128)
                for j3, src3 in enumerate((rwkv_w0, lnx_g, lnx_b)):
                    P.dma("sync", lambda e, j3=j3, src3=src3, c=c: e.dma_start(out=pp3[:, j3, :], in_=src3[c * 128:(c + 1) * 128].partition_broadcast(128)), pp3.B, writes=[pp3.B])
                for tb in range(4):
                    for j, (W_, dst, dstB) in enumerate(((WR[s], rs[:, :], rs.B), (WKr[s], ks[:, :], ks.B), (WVr[s], vsb[:, :], vsb.B))):
                        pst = ps[j % 2]
                        proj_fm(W_, 0, 128, tb, pst)
                        shiftmix(pst, 128, tb, Rr[j], murkv[:, j * 8 + c:j * 8 + c + 1], murkv[:, 24 + j * 8 + c:25 + j * 8 + c], murkv.B, dst, dstB, tmpr)
                    mm(ps[0][:, :], a2b[0:64, csl], xaT[0:64, tb * 512:(tb + 1) * 512], True, True, [a2b.B, xaT.B], ps[0].B)
                    A_(lambda e, c=c: e.activation(out=af[:], in_=ps[0][:, :], func=AF.Sigmoid, bias=prm[:, c, 2:3], scale=1.0), [ps[0].B, prm.B], [af.B])
                    V_(lambda e, c=c: e.tensor_scalar(out=kkk[:], in0=ks[:], scalar1=prm[:, c, 0:1], scalar2=None, op0=ALU.mult), [ks.B, prm.B], [kkk.B])
                    A_(lambda e: e.activation(out=sqb[:], in_=kkk[:], func=AF.Square), [kkk.B], [sqb.B])
                    mm(ps[1][:, :], cstb[:, BLK1, :], sqb[:], True, True, [cstb.B, sqb.B], ps[1].B)
                    A_(lambda e: e.activation(out=rinv[:], in_=ps[1][:, :], func=AF.Sqrt, bias=EPSC[:, 2:3], scale=1.0), [ps[1].B, EPSC.B], [rinv.B])
                    V_(lambda e: e.reciprocal(out=rinv[:], in_=rinv[:]), [rinv.B], [rinv.B])
                    V_(lambda e: e.tensor_tensor(out=kk[:], in0=kkk[:], in1=rinv[:], op=ALU.mult), [kkk.B, rinv.B], [kk.B])
                    V_(lambda e, c=c: e.tensor_scalar(out=uu[:], in0=af[:], scalar1=prm[:, c, 1:2], scalar2=prm[:, c, 3:4], op0=ALU.mult, op1=ALU.add), [af.B, prm.B], [uu.B])
                    V_(lambda e: e.tensor_tensor(out=k2[:], in0=ks[:], in1=uu[:], op=ALU.mult), [ks.B, uu.B], [k2.B])
                    V_(lambda e: e.tensor_tensor(out=bb_[:], in0=kk[:], in1=af[:], op=ALU.mult), [kk.B, af.B], [bb_.B])
                    A_(lambda e: e.activation(out=k2b[:], in_=k2[:], func=AF.Copy), [k2.B], [k2b.B])
                    A_(lambda e: e.activation(out=bbb[:], in_=bb_[:], func=AF.Copy), [bb_.B], [bbb.B])
                    V_(lambda e: e.tensor_tensor(out=rk2[:], in0=rs[:], in1=k2[:], op=ALU.mult), [rs.B, k2.B], [rk2.B])
                    if CUT == 12:
                        P.disabled = True
                    def prologue(ch, q4, bs):
                        own = ch >= 8
                        tsl = slice(q4 * 128, (q4 + 1) * 128)
                        gsl = slice(ch * 128, (ch + 1) * 128)
                        lwt, ex, ART, BhT, KhT, TMt = lwt2[bs], ex2[bs], ART2[bs], BhT2[bs], KhT2[bs], TMt2[bs]
                        pz = ps[2]
                        mm(pz[:, 0:128], txw[0:64, gsl], w2b[0:64, csl], True, True, [txw.B, w2b.B], pz.B)
                        yield
                        V_(lambda e: e.tensor_tensor(out=lwt[:], in0=pz[:, 0:128], in1=pp3[:, 0, :], op=ALU.add), [pz.B, pp3.B], [lwt.B])
                        A_(lambda e: e.activation(out=lwt[:], in_=lwt[:], func=AF.Sigmoid), [lwt.B], [lwt.B])
                        yield
                        mm(pz[:, 128:384], lwt[:], cst[:, TRI_I:TRI_E + 1, :].rearrange("p a b -> p (a b)"), True, True, [lwt.B, cst.B], pz.B)
                        mm(pz[:, 384:512], cst[:, TRI_R, :], lwt[:], True, True, [lwt.B, cst.B], pz.B)
                        yield
                        A_(lambda e: e.activation(out=ex[:, 0:2, :].rearrange("p a b -> p (a b)"), in_=pz[:, 128:384], func=AF.Exp), [pz.B], [ex.B])
                        A_(lambda e: e.activation(out=ex[:, 2, :], in_=pz[:, 128:256], func=AF.Exp, scale=-1.0), [pz.B], [ex.B])
                        A_(lambda e: e.activation(out=ex[:, 3, :], in_=pz[:, 384:512], func=AF.Exp), [pz.B], [ex.B])
                        yield
                        V_(lambda e: e.scalar_tensor_tensor(out=ART[:, 0:128], in0=kk[:, tsl], scalar=-1.0, in1=ex[:, 1, :], op0=ALU.mult, op1=ALU.mult), [kk.B, ex.B], [ART.B])
                        V_(lambda e: e.tensor_tensor(out=ART[:, 128:256], in0=rs[:, tsl], in1=ex[:, 0, :], op=ALU.mult), [rs.B, ex.B], [ART.B])
                        V_(lambda e: e.tensor_tensor(out=BhT[:], in0=bb_[:, tsl], in1=ex[:, 2, :], op=ALU.mult), [bb_.B, ex.B], [BhT.B])
                        V_(lambda e: e.tensor_tensor(out=KhT[:], in0=k2[:, tsl], in1=ex[:, 2, :], op=ALU.mult), [k2.B, ex.B], [KhT.B])
                        yield
                        pt = ps[3]
                        ptv = pt.t[:].bitcast(BF16)
                        for j, (src, sB) in enumerate(((bbb[:, tsl], bbb.B), (k2b[:, tsl], k2b.B), (vsb[:, tsl], vsb.B), (ART[:, 0:128], ART.B))):
                            P.op("tensor", lambda e, j=j, src=src, ptv=ptv: e.transpose(out=ptv[:, j * 128:(j + 1) * 128], in_=src, identity=identb), reads=[sB, cstb.B], writes=[pt.B])
                        yield
                        V_(lambda e: e.tensor_tensor(out=TMt[:, 0:2, :], in0=ptv[:, 0:256].rearrange("p (a b) -> p a b", b=128),
                                                     in1=ex[:, 3:4, :].to_broadcast([128, 2, 128]), op=ALU.mult), [pt.B, ex.B], [TMt.B])
                        A_(lambda e: e.activation(out=TMt[:, 2:4, :].rearrange("p a b -> p (a b)"), in_=ptv[:, 256:512], func=AF.Copy), [pt.B], [TMt.B])
                        if own:
                            oc = ch - 8
                            yield
                            A_(lambda e: e.activation(out=Vtm[:, oc, :], in_=TMt[:, 2, :], func=AF.Copy), [TMt.B], [Vtm.B])
                            mm(ps[3][:, 256:258], rk2[:, tsl], RK[:, c, :], True, True, [rk2.B, RK.B], ps[3].B)
                            mm(ps[3][:, 384:512], sxa[:, gsl], g2b[:, 0, csl], True, False, [sxa.B, g2b.B], ps[3].B)
                            mm(ps[3][:, 384:512], sxb[0:32, gsl], g2b[0:32, 1, csl], False, True, [sxb.B, g2b.B], ps[3].B)
                            yield
                            V_(lambda e: e.tensor_copy(out=sbon[:, oc, :], in_=ps[3][:, 256:258]), [ps[3].B], [sbon.B])
                            V_(lambda e: e.tensor_copy(out=gtm[:, oc, :], in_=ps[3][:, 384:512]), [ps[3].B], [gtm.B])

                    def chain(ch, hh, own, tsl, bs):
                        hp = slice(hh * 64, (hh + 1) * 64)
                        tpk = (hh * 64, 0)
                        tpo = (0, hh * 64)
                        ex, ART, BhT, KhT, TMt = ex2[bs], ART2[bs], BhT2[bs], KhT2[bs], TMt2[bs]
                        pA, pB = ps[4 + hh], ps[6 + hh]
                        bX = bN = bQ = pB.B
                        NM, Xf, Xb, MT = NMh[hh], Xfh[hh], Xbh[hh], MTh[hh]
                        mm(pA[:, 0:256], BhT[hp, :], ART[hp, :], True, True, [BhT.B, ART.B], pA.B, tpk)
                        mm(pA[:, 256:512], KhT[hp, :], ART[hp, :], True, True, [KhT.B, ART.B], pA.B, tpk)
                        mm(pB[:, 384:512], ART[hp, 0:128], BhT[hp, :], True, True, [BhT.B, ART.B], bQ, tpk)
                        yield
                        V_(lambda e: e.tensor_tensor(out=MT[:, :, :], in0=pA[:, :].rearrange("p (a b) -> p a b", b=256),
                                                     in1=cst[:, UT_S:UT_I + 1, :].rearrange("p a b -> p (a b)").unsqueeze(1).to_broadcast([128, 2, 256]), op=ALU.mult),
                           [pA.B, cst.B], [MT.B])
                        V_(lambda e: e.tensor_tensor(out=NM[0][:, 0:128], in0=pA[:, 0:128], in1=cst[:, UT_S, :], op=ALU.mult), [pA.B, cst.B], [NM[0].B])
                        V_(lambda e: e.tensor_tensor(out=NM[0][:, 128:256], in0=pB[:, 384:512], in1=cst[:, LT_S, :], op=ALU.mult), [bQ, cst.B], [NM[0].B])
                        A_(lambda e: e.activation(out=Xb[:, 0:64], in_=TMt[:, 3, hp], func=AF.Copy), [TMt.B], [Xb.B])
                        A_(lambda e: e.activation(out=Xf[:, 0:64], in_=TMt[:, 3, hp], func=AF.Copy), [TMt.B], [Xf.B])
                        yield
                        mm(pA[:, 0:64], MT[:, 1, 0:128], TMt[:, 2, hp], True, True, [MT.B, TMt.B], pA.B)
                        yield
                        V_(lambda e: e.tensor_copy(out=Xb[:, 64:128], in_=pA[:, 0:64]), [pA.B], [Xb.B])
                        V_(lambda e: e.tensor_copy(out=Xf[:, 64:128], in_=pA[:, 0:64]), [pA.B], [Xf.B])
                        yield
                        for lv in range(7):
                            cur, nxt = NM[lv % 2], NM[(lv + 1) % 2]
                            mm(pB[:, 0:128], cur[:, 0:128], Xb[:], True, True, [cur.B, Xb.B], bX)
                            if lv < 6:
                                mm(pB[:, 128:256], cur[:, 128:256], cur[:, 0:128], True, True, [cur.B], bN)
                                mm(pB[:, 256:384], cur[:, 0:128], cur[:, 128:256], True, True, [cur.B], bN)
                            yield
                            V_(lambda e: e.tensor_tensor(out=Xb[:], in0=Xf[:], in1=pB[:, 0:128], op=ALU.add), [Xf.B, bX], [Xb.B])
                            if lv < 6:
                                V_(lambda e, nxt=nxt: e.tensor_copy(out=nxt[:], in_=pB[:, 128:384]), [bN], [nxt.B])
                                V_(lambda e: e.tensor_tensor(out=Xf[:], in0=Xf[:], in1=pB[:, 0:128], op=ALU.add), [Xf.B, bX], [Xf.B])
                            yield
                        mm(pA[hp, 64:128], Xb[:, 0:64], TMt[:, 0, hp], True, True, [Xb.B, TMt.B], pA.B, tpo)
                        mm(pA[hp, 128:192], TMt[:, 0, hp], Xb[:, 64:128], True, False, [Xb.B, TMt.B], pA.B, tpo)
                        mm(pA[hp, 128:192], TMt[:, 1, hp], TMt[:, 2, hp], False, True, [TMt.B], pA.B, tpo)
                        if own:
                            mm(pB[hp, 384:512], Xb[:, 0:64], MT[:, 0, 128:256], True, True, [Xb.B, MT.B], bQ, tpo)
                            mm(pA[:, 192:256], MT[:, 0, 128:256], Xb[:, 64:128], True, False, [Xb.B, MT.B], pA.B)
                            mm(pA[:, 192:256], MT[:, 1, 128:256], TMt[:, 2, hp], False, True, [TMt.B, MT.B], pA.B)
                        yield
                        V_(lambda e: e.scalar_tensor_tensor(out=GTs[hp, ch, hh * 64:(hh + 1) * 64], in0=cst[hp, IDENT, hh * 64:(hh + 1) * 64], scalar=ex[hp, 0, 127:128],
                                                            in1=pA[hp, 64:128], op0=ALU.mult, op1=ALU.add), [cst.B, ex.B, pA.B], [GTs.B])
                        V_(lambda e: e.tensor_copy(out=Hs[hp, ch, :], in_=pA[hp, 128:192]), [pA.B], [Hs.B])
                        if own:
                            oc = ch - 8
                            V_(lambda e: e.tensor_tensor(out=QeTs[hp, oc, :], in0=pB[hp, 384:512], in1=ART[hp, 128:256], op=ALU.add),
                               [bQ, ART.B], [QeTs.B])
                            V_(lambda e: e.tensor_copy(out=Yls[:, oc, hp], in_=pA[:, 192:256]), [pA.B], [Yls.B])

                    def run_rr(gens):
                        while gens:
                            for g_ in list(gens):
                                try:
                                    next(g_)
                                except StopIteration:
                                    gens.remove(g_)

                    run_rr([prologue(tb * 4, 0, 0)])
                    for q4 in range(4):
                        ch = tb * 4 + q4
                        own = ch >= 8
                        tsl = slice(q4 * 128, (q4 + 1) * 128)
                        gens = [chain(ch, 0, own, tsl, q4 % 2), chain(ch, 1, own, tsl, q4 % 2)]
                        if q4 < 3:
                            gens.append(prologue(ch + 1, q4 + 1, (q4 + 1) % 2))
                        run_rr(gens)
                if c + 1 < NPAIRS:
                    load_pair(c + 1)
                if CUT == 17:
                    P.disabled = True
                V_(lambda e: e.memset(Sb[:], 0.0), [], [Sb.B])
                q1, q2 = [], []
                for ch in range(16):
                    if CUT == 19 and ch == 8:
                        P.disabled = True
                    if ch >= 8:
                        oc = ch - 8
                        mm(ps[0][:, 0:128], QeTs[:, oc, :], Sb[:, :], True, True, [QeTs.B, Sb.B], ps[0].B)
                        V_(lambda e, oc=oc: e.tensor_tensor(out=Yt3[oc % 3][:], in0=ps[0][:, 0:128], in1=Yls[:, oc, :], op=ALU.add), [ps[0].B, Yls.B], [Yt3[oc % 3].B])
                    if CUT == 20 and ch == 8:
                        P.disabled = True
                    if ch < 15:
                        mm(ps[1][:, 0:128], GTs[:, ch, :], Sb[:, :], True, True, [GTs.B, Sb.B], ps[1].B)
                        for hh in range(2):
                            hp = slice(hh * 64, (hh + 1) * 64)
                            V_(lambda e, ch=ch, hp=hp: e.tensor_tensor(out=Sb[hp, hp], in0=ps[1][hp, hp], in1=Hs[hp, ch, :], op=ALU.add), [ps[1].B, Hs.B], [Sb.B])
                    if CUT == 21 and ch == 8:
                        P.disabled = True
                    run2, q2[:] = list(q2), []
                    for fn_ in run2:
                        fn_()
                    run1, q1[:] = list(q1), []
                    for fn_ in run1:
                        fn_()
                    if ch >= 8:
                        def gn_apply(oc=oc, c=c):
                            Yt = Yt3[oc % 3]
                            V_(lambda e: e.reciprocal(out=grs[:], in_=grs[:]), [grs.B], [grs.B])
                            for hh in range(2):
                                hs_ = slice(hh * 64, (hh + 1) * 64)
                                V_(lambda e, hh=hh, hs_=hs_: e.tensor_scalar(out=yn[:, hs_], in0=Yt[:, hs_], scalar1=gmv[:, hh, 0:1], scalar2=grs[:, hh:hh + 1],
                                                                           op0=ALU.subtract, op1=ALU.mult), [Yt.B, gmv.B, grs.B], [yn.B])
                            V_(lambda e: e.tensor_tensor(out=yn[:], in0=yn[:], in1=pp3[:, 1, :], op=ALU.mult), [yn.B, pp3.B], [yn.B])
                            V_(lambda e: e.tensor_tensor(out=yn[:], in0=yn[:], in1=pp3[:, 2, :], op=ALU.add), [yn.B, pp3.B], [yn.B])
                            for hh in range(2):
                                hs_ = slice(hh * 64, (hh + 1) * 64)
                                V_(lambda e, hh=hh, hs_=hs_: e.scalar_tensor_tensor(out=yn[:, hs_], in0=Vtm[:, oc, hs_], scalar=sbon[:, oc, hh:hh + 1], in1=yn[:, hs_],
                                                                                  op0=ALU.mult, op1=ALU.add), [Vtm.B, sbon.B, yn.B], [yn.B])
                            V_(lambda e: e.tensor_tensor(out=ybr[:], in0=yn[:], in1=gtm[:, oc, :], op=ALU.mult), [yn.B, gtm.B], [ybr.B])
                            pv = ps[3].t[:].bitcast(BF16)
                            P.op("tensor", lambda e, pv=pv: e.transpose(out=pv[:, 0:128], in_=ybr[:], identity=identb), reads=[ybr.B, cstb.B], writes=[ps[3].B])
                            A_(lambda e, pv=pv: e.activation(out=yT[:, 8 + c, oc * 128:(oc + 1) * 128], in_=pv[:, 0:128], func=AF.Copy), [ps[3].B], [yT.b[8 + c]])

                        def gn_stats(oc=oc, gn_apply=gn_apply):
                            Yt = Yt3[oc % 3]
                            for hh in range(2):
                                V_(lambda e, hh=hh: e.bn_stats(out=gst[:, hh, :], in_=Yt[:, hh * 64:(hh + 1) * 64]), [Yt.B], [gst.B])
                                V_(lambda e, hh=hh: e.bn_aggr(out=gmv[:, hh, :], in_=gst[:, hh, :]), [gst.B], [gmv.B])
                            A_(lambda e: e.activation(out=grs[:], in_=gmv[:, :, 1], func=AF.Sqrt, bias=EPSC[:, 1:2], scale=1.0), [gmv.B, EPSC.B], [grs.B])
                            q2.append(gn_apply)
                        q1.append(gn_stats)
                while q1 or q2:
                    run2, q2[:] = list(q2), []
                    for fn_ in run2:
                        fn_()
                    run1, q1[:] = list(q1), []
                    for fn_ in run1:
                        fn_()
            if CUT == 25:
                P.disabled = True
            if dbg and stop == 3 and CUT == 77:
                d6 = T(P, st, "d6", [128, 1024], F32)
                def dcp(c0, w, ap, bufs):
                    V_(lambda e: e.tensor_copy(out=d6[:, c0:c0 + w], in_=ap), bufs, [d6.B])
                def dsend(sl_):
                    P.dma("sync", lambda e: e.dma_start(out=dbg_d[sl_], in_=d6[:]), d6.B, reads=[d6.B])
                V_(lambda e: e.memset(d6[:], 0.0), [], [d6.B])
                dcp(0, 512, ex[:].rearrange("p a b -> p (a b)"), [ex.B]); dcp(512, 128, Xf[:], [Xf.B]); dcp(640, 128, lwt[:], [lwt.B])
                dcp(768, 128, Yt[:], [Yt.B]); dcp(896, 128, yn[:], [yn.B])
                dsend(6)
                dcp(0, 256, ART[:], [ART.B]); dcp(256, 128, BhT[:], [BhT.B]); dcp(384, 128, KhT[:], [KhT.B])
                dcp(512, 512, TMt[:].rearrange("p a b -> p (a b)"), [TMt.B])
                dsend(7)
                dcp(0, 512, MT[:].rearrange("p a b -> p (a b)"), [MT.B]); dcp(512, 64, GTs[:, 15, 0:64], [GTs.B]); dcp(576, 64, Hs[:, 15, :], [Hs.B])
                dcp(640, 128, QeTs[:, 7, :], [QeTs.B]); dcp(768, 128, Yls[:, 7, :], [Yls.B]); dcp(896, 64, Sb[:, 0:64], [Sb.B])
                dcp(960, 2, sbon[:, 7, :], [sbon.B])
                dsend(8)
                dbg_bufs.append(d6.B)
            dump(st, 4, yT[:, 8, 0:256], [yT.b[8]], 256)
            if not (dbg and stop == 3):
                dump(st, 5, yT[:, 7 + NPAIRS, :], [yT.b[7 + NPAIRS]], 1024)
            if stop == 3:
                P.wait_all("sync", dbg_bufs)
            P.flush()
            if stop == 3:
                P.disabled = True

        stA.close()
        with ExitStack() as st:
            acc = T(P, st, "acc", [128, 8, D], F32, nb=8)
            wt_ = T(P, st, "wt", [128, 8, 32], F32)
            accB = acc.b
            gb2 = [None, None]
            ws = [None, None, None]

            class AccT:
                def __init__(self, i):
                    self.i = i; self.B = accB[i]

                def __getitem__(self, k):
                    return acc[:, self.i, :] if k == slice(None) else acc[:, self.i, k[1]]

            def load_gb(gs, bs):
                P.dma("sync", lambda e: e.dma_start(out=gb2[0][:], in_=gs.partition_broadcast(128)), gb2[0].B, writes=[gb2[0].B])
                if bs is not None:
                    P.dma("sync", lambda e: e.dma_start(out=gb2[1][:], in_=bs.partition_broadcast(128)), gb2[1].B, writes=[gb2[1].B])

            with ExitStack() as st2:
                gb2[0] = T(P, st2, "gb2a0", [128, D], F32); gb2[1] = T(P, st2, "gb2a1", [128, D], F32)
                for i in range(3):
                    ws[i] = T(P, st2, "wsa%d" % i, [128, 16, 512], BF16)
                xt = [T(P, st2, "xt4%d" % i, [128, D], F32) for i in range(2)]
                xn = T(P, st2, "xn4", [128, D], F32)
                stt4 = T(P, st2, "stt4", [128, 4, 6], F32); mv4 = T(P, st2, "mv4", [128, 4], F32)
                load_gb(ln_in_g, ln_in_b)
                for i in range(8):
                    X = xt[i % 2]
                    P.dma("sync", lambda e, X=X, i=i: e.dma_start(out=X[:], in_=xs[(8 + i) * 128:(9 + i) * 128, :]), X.B, writes=[X.B])
                    ln_tile(P, X, xn, stt4, mv4, gb2[0], gb2[1], None, AccT(i))
                    A_(lambda e, i=i: e.activation(out=acc[:, i, :], in_=acc[:, i, :], func=AF.Copy, scale=ALPHA), [accB[i]], [accB[i]])
                w_out_v = w_out.rearrange("(kc p) n -> p kc n", p=128)
                for n in range(4):
                    W_ = ws[n % 3]
                    P.dma("gpsimd", lambda e, W_=W_, n=n: e.dma_start(out=W_[:], in_=w_out_v[:, :, n * 512:(n + 1) * 512]), W_.B, writes=[W_.B])
                    for i in range(8):
                        pst = ps[i % 2]
                        for kc in range(16):
                            mm(pst[:, :], yT[:, kc, i * 128:(i + 1) * 128], W_[:, kc, :], kc == 0, kc == 15, [yT.b[kc], W_.B], pst.B)
                        V_(lambda e, i=i, n=n, pst=pst: e.tensor_tensor(out=acc[:, i, n * 512:(n + 1) * 512], in0=acc[:, i, n * 512:(n + 1) * 512], in1=pst[:, :], op=ALU.add),
                           [accB[i], pst.B], [accB[i]])
                dump(st2, 6, acc[:, 0, 0:1024], [accB[0]], 1024)
                if stop == 4:
                    P.wait_all("sync", dbg_bufs)
                P.flush()
                if stop == 4:
                    P.disabled = True
            hT1 = T(P, st, "hT1", [128, 16, 1024], BF16, nb=8)
            with ExitStack() as st2:
                gb2[0] = T(P, st2, "gb2b0", [128, D], F32); gb2[1] = T(P, st2, "gb2b1", [128, D], F32)
                xn = T(P, st2, "xn4b", [128, D], F32)
                hb4 = T(P, st2, "hb4", [128, D], BF16)
                hlo = T(P, st2, "hlo", [128, D], BF16)
                stt4 = T(P, st2, "stt4b", [128, 4, 6], F32); mv4 = T(P, st2, "mv4b", [128, 4], F32)
                hT1l = T(P, st2, "hT1l", [128, 16, 128], BF16)
                load_gb(ln1_g, ln1_b)
                rwf = T(P, st2, "rwf", [128, 16, 36], F32); rwh = T(P, st2, "rwh", [128, 16, 36], BF16); rwl = T(P, st2, "rwl", [128, 16, 36], BF16)
                rbias = T(P, st2, "rbias", [128, 36], F32)
                P.dma("sync", lambda e: e.dma_start(out=rwf[:, :, 0:4], in_=rgw.rearrange("(kc p) n -> p kc n", p=128)), rwf.B, writes=[rwf.B])
                P.dma("sync", lambda e: e.dma_start(out=rwf[:, :, 4:36], in_=rew.rearrange("(kc p) n -> p kc n", p=128)), rwf.B, writes=[rwf.B])
                P.dma("sync", lambda e: e.dma_start(out=rbias[:, 0:4], in_=rgb.partition_broadcast(128)), rbias.B, writes=[rbias.B])
                P.dma("sync", lambda e: e.dma_start(out=rbias[:, 4:36], in_=reb.partition_broadcast(128)), rbias.B, writes=[rbias.B])
                V_(lambda e: e.tensor_copy(out=rwh[:], in_=rwf[:]), [rwf.B], [rwh.B])
                V_(lambda e: e.tensor_tensor(out=rwf[:], in0=rwf[:], in1=rwh[:], op=ALU.subtract), [rwf.B, rwh.B], [rwf.B])
                V_(lambda e: e.tensor_copy(out=rwl[:], in_=rwf[:]), [rwf.B], [rwl.B])
                L = T(P, st2, "L", [128, 36], F32); r1 = T(P, st2, "r1", [128, 8], F32)
                gm = T(P, st2, "gm", [128, 4], F32); elm = T(P, st2, "elm", [128, 32], F32); ee = T(P, st2, "ee", [128, 32], F32)
                m1 = T(P, st2, "m1", [128, 32], F32); e2 = T(P, st2, "e2", [128, 32], F32); m2 = T(P, st2, "m2", [128, 32], F32)
                for i in range(8):
                    A = AccT(i)
                    ln_tile(P, A, xn, stt4, mv4, gb2[0], gb2[1], hb4, A)
                    transpose_to_fm(P, hb4, hT1, i, ps, identb, cstb.B, pbase=2)
                    V_(lambda e, i=i: e.tensor_tensor(out=xn[:], in0=acc[:, i, :], in1=hb4[:], op=ALU.subtract), [accB[i], hb4.B], [xn.B])
                    A_(lambda e: e.activation(out=hlo[:], in_=xn[:], func=AF.Copy), [xn.B], [hlo.B])
                    transpose_to_fm(P, hlo, hT1l, 0, ps, identb, cstb.B, pbase=4)
                    tsl = slice(i * 128, (i + 1) * 128)
                    n_mm = 0
                    for (hT_, w_, sl_, bi_) in ((hT1, rwh, tsl, i), (hT1l, rwh, slice(0, 128), 0), (hT1, rwl, tsl, i)):
                        for kc in range(16):
                            mm(ps[6][:, 0:36], hT_[:, kc, sl_], w_[:, kc, :], n_mm == 0, n_mm == 47, [hT_.b[bi_], w_.B], ps[6].B)
                            n_mm += 1
                    V_(lambda e: e.tensor_tensor(out=L[:], in0=ps[6][:, 0:36], in1=rbias[:], op=ALU.add), [ps[6].B, rbias.B], [L.B])
                    V_(lambda e: e.reduce_max(out=r1[:, 0:1], in_=L[:, 0:4], axis=AX.X), [L.B], [r1.B])
                    V_(lambda e: e.tensor_scalar(out=gm[:], in0=L[:, 0:4], scalar1=r1[:, 0:1], scalar2=None, op0=ALU.is_ge), [L.B, r1.B], [gm.B])
                    V_(lambda e: e.tensor_scalar(out=r1[:, 1:2], in0=r1[:, 0:1], scalar1=-1.0, scalar2=None, op0=ALU.mult), [r1.B], [r1.B])
                    A_(lambda e: e.activation(out=ee[:, 0:4], in_=L[:, 0:4], func=AF.Exp, bias=r1[:, 1:2], scale=1.0, accum_out=r1[:, 2:3]), [L.B, r1.B], [ee.B, r1.B])
                    V_(lambda e: e.reciprocal(out=r1[:, 3:4], in_=r1[:, 2:3]), [r1.B], [r1.B])
                    V_(lambda e: e.tensor_scalar(out=gm[:], in0=gm[:], scalar1=-1.0, scalar2=-NEG, op0=ALU.add, op1=ALU.mult), [gm.B], [gm.B])
                    V_(lambda e: e.tensor_tensor(out=elm[:].rearrange("p (g e) -> p g e", e=8), in0=L[:, 4:36].rearrange("p (g e) -> p g e", e=8),
                                                 in1=gm[:].unsqueeze(2).to_broadcast([128, 4, 8]), op=ALU.add), [L.B, gm.B], [elm.B])
                    V_(lambda e: e.reduce_max(out=r1[:, 4:5], in_=elm[:], axis=AX.X), [elm.B], [r1.B])
                    V_(lambda e: e.tensor_scalar(out=r1[:, 5:6], in0=r1[:, 4:5], scalar1=-1.0, scalar2=None, op0=ALU.mult), [r1.B], [r1.B])
                    A_(lambda e: e.activation(out=ee[:], in_=elm[:], func=AF.Exp, bias=r1[:, 5:6], scale=1.0), [elm.B, r1.B], [ee.B])
                    V_(lambda e: e.tensor_scalar(out=m1[:], in0=elm[:], scalar1=r1[:, 4:5], scalar2=None, op0=ALU.is_ge), [elm.B, r1.B], [m1.B])
                    V_(lambda e: e.tensor_tensor(out=e2[:], in0=ee[:], in1=m1[:], op=ALU.mult), [ee.B, m1.B], [e2.B])
                    V_(lambda e: e.tensor_tensor(out=e2[:], in0=ee[:], in1=e2[:], op=ALU.subtract), [ee.B, e2.B], [e2.B])
                    V_(lambda e: e.reduce_max(out=r1[:, 6:7], in_=e2[:], axis=AX.X), [e2.B], [r1.B])
                    V_(lambda e: e.tensor_scalar(out=m2[:], in0=e2[:], scalar1=r1[:, 6:7], scalar2=None, op0=ALU.is_ge), [e2.B, r1.B], [m2.B])
                    V_(lambda e: e.tensor_tensor(out=m2[:], in0=m2[:], in1=e2[:], op=ALU.mult), [m2.B, e2.B], [m2.B])
                    V_(lambda e: e.tensor_tensor(out=m1[:], in0=m1[:], in1=m2[:], op=ALU.add), [m1.B, m2.B], [m1.B])
                    V_(lambda e: e.tensor_scalar(out=r1[:, 7:8], in0=r1[:, 6:7], scalar1=1.0, scalar2=None, op0=ALU.add), [r1.B], [r1.B])
                    V_(lambda e: e.reciprocal(out=r1[:, 7:8], in_=r1[:, 7:8]), [r1.B], [r1.B])
                    V_(lambda e, i=i: e.tensor_scalar(out=wt_[:, i, :], in0=m1[:], scalar1=r1[:, 7:8], scalar2=r1[:, 3:4], op0=ALU.mult, op1=ALU.mult), [m1.B, r1.B], [wt_.B])
                    A_(lambda e, i=i: e.activation(out=acc[:, i, :], in_=acc[:, i, :], func=AF.Copy, scale=ALPHA), [accB[i]], [accB[i]])
                dump(st2, 7, wt_[:, 0, :], [wt_.B], 32)
                if stop == 5:
                    P.wait_all("sync", dbg_bufs)
                P.flush()
                if stop == 5:
                    P.disabled = True
            with ExitStack() as st2:
                for i in range(3):
                    ws[i] = T(P, st2, "wsc%d" % i, [128, 16, 512], BF16)
                hid = [T(P, st2, "hid0", [128, 4, 1024], BF16)] * 2
                sg = [T(P, st2, "sg%d" % i, [128, 512], BF16) for i in range(2)]
                nld = 0
                for ex_ in range(NEXP):
                    Wg, Wu, Wd = ws[0], ws[1], ws[2]
                    P.dma("gpsimd", lambda e, ex_=ex_: e.dma_start(out=ws[0][:], in_=wg_d[ex_].rearrange("(kc p) n -> p kc n", p=128)), ws[0].B, writes=[ws[0].B])
                    P.dma("gpsimd", lambda e, ex_=ex_: e.dma_start(out=ws[1][:], in_=wu_d[ex_].rearrange("(kc p) n -> p kc n", p=128)), ws[1].B, writes=[ws[1].B])
                    P.dma("gpsimd", lambda e, ex_=ex_: e.dma_start(out=ws[2][:].rearrange("p (a b) n -> p a b n", b=4), in_=wd_d[ex_].rearrange("(fc p) (b n) -> p fc b n", p=128, n=512)),
                          ws[2].B, writes=[ws[2].B])
                    H_ = hid[ex_ % 2]
                    k_ = 0
                    for tb in range(2):
                        for fc in range(4):
                            pg, pu = ps[(k_ % 2) * 2], ps[(k_ % 2) * 2 + 1]
                            S_ = sg[k_ % 2]
                            for kc in range(16):
                                mm(pg[:, :], Wg[:, kc, fc * 128:(fc + 1) * 128], hT1[:, kc, tb * 512:(tb + 1) * 512], kc == 0, kc == 15, [Wg.B] + hT1.b[tb * 4:(tb + 1) * 4], pg.B)
                            for kc in range(16):
                                mm(pu[:, :], Wu[:, kc, fc * 128:(fc + 1) * 128], hT1[:, kc, tb * 512:(tb + 1) * 512], kc == 0, kc == 15, [Wu.B] + hT1.b[tb * 4:(tb + 1) * 4], pu.B)
                            A_(lambda e, S_=S_, pg=pg: e.activation(out=S_[:], in_=pg[:, :], func=AF.Silu), [pg.B], [S_.B])
                            V_(lambda e, S_=S_, pu=pu, H_=H_, fc=fc, tb=tb: e.tensor_tensor(out=H_[:, fc, tb * 512:(tb + 1) * 512], in0=S_[:], in1=pu[:, :], op=ALU.mult),
                               [S_.B, pu.B], [H_.B])
                            k_ += 1
                    for i in range(8):
                        for n in range(4):
                            po = ps[4 + (i * 4 + n) % 4]
                            for fc in range(4):
                                mm(po[:, :], H_[:, fc, i * 128:(i + 1) * 128], Wd[:, fc * 4 + n, :], fc == 0, fc == 3, [H_.B, Wd.B], po.B)
                            V_(lambda e, i=i, n=n, po=po, ex_=ex_: e.scalar_tensor_tensor(out=acc[:, i, n * 512:(n + 1) * 512], in0=po[:, :], scalar=wt_[:, i, ex_:ex_ + 1],
                                                                                  in1=acc[:, i, n * 512:(n + 1) * 512], op0=ALU.mult, op1=ALU.add), [po.B, wt_.B, accB[i]], [accB[i]])
                dump(st2, 8, acc[:, 0, 0:1024], [accB[0]], 1024)
                if stop == 6:
                    P.wait_all("sync", dbg_bufs)
                P.flush()
                if stop == 6:
                    P.disabled = True
            with ExitStack() as st2:
                gb2[0] = T(P, st2, "gb2d0", [128, D], F32); gb2[1] = T(P, st2, "gb2d1", [128, D], F32)
                ws[0] = T(P, st2, "wsd0", [128, 16, 512], BF16)
                xn = T(P, st2, "xn5", [128, D], F32); hb4 = T(P, st2, "hb5", [128, D], BF16)
                stt4 = T(P, st2, "stt5", [128, 4, 6], F32); mv4 = T(P, st2, "mv5", [128, 4], F32)
                pf = T(P, st2, "pf", [128, 256], F32); pb = T(P, st2, "pb", [128, 256], BF16)
                pT = T(P, st2, "pT", [128, 2, 1024], BF16, nb=8)
                plw = T(P, st2, "plw", [128, 2, D], BF16)
                ssq = T(P, st2, "ssq", [128, 8, 4], F32); rsp = T(P, st2, "rsp", [128, 8], F32)
                sgt = T(P, st2, "sgt", [128, 512], F32); t1_ = T(P, st2, "t1", [128, 512], F32)
                load_gb(ln2_g, ln2_b)
                P.dma("gpsimd", lambda e: e.dma_start(out=plw[:], in_=ple_w.rearrange("(kc p) n -> p kc n", p=128)), plw.B, writes=[plw.B])
                if CUT == 31:
                    P.disabled = True
                for i in range(8):
                    A = AccT(i)
                    ln_tile(P, A, xn, stt4, mv4, gb2[0], gb2[1], hb4, A)
                    transpose_to_fm(P, hb4, hT1, i, ps, identb, cstb.B, pbase=2)
                    P.dma("sync", lambda e, i=i: e.dma_start(out=pf[:], in_=p_in[i * 128:(i + 1) * 128, :]), pf.B, writes=[pf.B])
                    V_(lambda e: e.tensor_copy(out=pb[:], in_=pf[:]), [pf.B], [pb.B])
                    pv = ps[4].t[:].bitcast(BF16)
                    for k in range(2):
                        P.op("tensor", lambda e, k=k, pv=pv: e.transpose(out=pv[:, k * 128:(k + 1) * 128], in_=pb[:, k * 128:(k + 1) * 128], identity=identb), reads=[pb.B, cstb.B], writes=[ps[4].B])
                    V_(lambda e, i=i, pv=pv: e.tensor_copy(out=pT[:, :, i * 128:(i + 1) * 128], in_=pv[:, 0:256].rearrange("p (k t) -> p k t", t=128)), [ps[4].B], [pT.b[i]])
                    for n in range(4):
                        pp = ps[5 + n % 2]
                        for k in range(2):
                            mm(pp[:, :], pT[:, k, i * 128:(i + 1) * 128], plw[:, k, n * 512:(n + 1) * 512], k == 0, k == 1, [pT.b[i], plw.B], pp.B)
                        A_(lambda e, i=i, n=n, pp=pp: e.activation(out=t1_[:], in_=pp[:, :], func=AF.Square, accum_out=ssq[:, i, n:n + 1]), [pp.B], [t1_.B, ssq.B])
                if CUT == 32:
                    P.disabled = True
                V_(lambda e: e.reduce_sum(out=rsp[:], in_=ssq[:], axis=AX.X), [ssq.B], [rsp.B])
                A_(lambda e: e.activation(out=rsp[:], in_=rsp[:], func=AF.Sqrt, bias=EPSC[:, 0:1], scale=1.0 / D), [rsp.B, EPSC.B], [rsp.B])
                V_(lambda e: e.reciprocal(out=rsp[:], in_=rsp[:]), [rsp.B], [rsp.B])
                P.dma("sync", lambda e: e.dma_start(out=gb2[0][:], in_=ple_ng.partition_broadcast(128)), gb2[0].B, writes=[gb2[0].B])
                if CUT == 33:
                    P.disabled = True
                pgw_v = ple_gw.rearrange("(kc p) n -> p kc n", p=128)
                for n in range(4):
                    W_ = ws[0]
                    P.dma("gpsimd", lambda e, W_=W_, n=n: e.dma_start(out=W_[:], in_=pgw_v[:, :, n * 512:(n + 1) * 512]), W_.B, writes=[W_.B])
                    nsl = slice(n * 512, (n + 1) * 512)
                    for i in range(8):
                        pgt, pp = ps[(i % 2) * 2], ps[(i % 2) * 2 + 1]
                        for kc in range(16):
                            mm(pgt[:, :], hT1[:, kc, i * 128:(i + 1) * 128], W_[:, kc, :], kc == 0, kc == 15, [hT1.b[i], W_.B], pgt.B)
                        for k in range(2):
                            mm(pp[:, :], pT[:, k, i * 128:(i + 1) * 128], plw[:, k, nsl], k == 0, k == 1, [pT.b[i], plw.B], pp.B)
                        A_(lambda e, pgt=pgt: e.activation(out=sgt[:], in_=pgt[:, :], func=AF.Sigmoid), [pgt.B], [sgt.B])
                        V_(lambda e, i=i, pp=pp, nsl=nsl: e.scalar_tensor_tensor(out=t1_[:], in0=pp[:, :], scalar=rsp[:, i:i + 1], in1=gb2[0][:, nsl], op0=ALU.mult, op1=ALU.mult),
                           [pp.B, rsp.B, gb2[0].B], [t1_.B])
                        V_(lambda e: e.tensor_tensor(out=t1_[:], in0=t1_[:], in1=sgt[:], op=ALU.mult), [t1_.B, sgt.B], [t1_.B])
                        V_(lambda e, i=i, nsl=nsl: e.tensor_tensor(out=acc[:, i, nsl], in0=acc[:, i, nsl], in1=t1_[:], op=ALU.add), [accB[i], t1_.B], [accB[i]])
                        P.dma("sync", lambda e, i=i, nsl=nsl: e.dma_start(out=out_d[i * 128:(i + 1) * 128, nsl], in_=acc[:, i, nsl]), accB[i], reads=[accB[i]])
                if CUT == 34:
                    P.disabled = True
                P.wait_all("sync", accB + dbg_bufs)
                if stop == 7:
                    P.wait_all("sync", dbg_bufs)
                P.flush()
      except _Stop:
        pass
    return nc


EPSC = None


def rsqrt(P, out, in_, outB, inB, eps_ap):
    P.op("scalar", lambda e: e.activation(out=out, in_=in_, func=AF.Sqrt, bias=eps_ap, scale=1.0), reads=[inB, EPSC.B], writes=[outB])
    P.op("vector", lambda e: e.reciprocal(out=out, in_=out), reads=[outB], writes=[outB])


def ln_tile(P, X, XN, ST, MV, gbc, bbc, out_bf, out_f32, eps=1e-5):
    for j in range(4):
        P.op("vector", lambda e, j=j: e.bn_stats(out=ST[:, j, :], in_=X[:, j * 512:(j + 1) * 512]), reads=[X.B], writes=[ST.B])
    P.op("vector", lambda e: e.bn_aggr(out=MV[:, 0:2], in_=ST[:]), reads=[ST.B], writes=[MV.B])
    rsqrt(P, MV[:, 2:3], MV[:, 1:2], MV.B, MV.B, EPSC[:, 0:1])
    P.op("vector", lambda e: e.scalar_tensor_tensor(out=MV[:, 3:4], in0=MV[:, 0:1], scalar=-1.0, in1=MV[:, 2:3], op0=ALU.mult, op1=ALU.mult),
         reads=[MV.B], writes=[MV.B])
    P.op("scalar", lambda e: e.activation(out=XN[:], in_=X[:], func=AF.Identity, bias=MV[:, 3:4], scale=MV[:, 2:3]),
         reads=[X.B, MV.B], writes=[XN.B])
    P.op("vector", lambda e: e.tensor_tensor(out=XN[:], in0=XN[:], in1=gbc[:], op=ALU.mult), reads=[XN.B, gbc.B], writes=[XN.B])
    if out_f32 is not None:
        P.op("vector", lambda e: e.tensor_tensor(out=out_f32[:], in0=XN[:], in1=bbc[:], op=ALU.add), reads=[XN.B, bbc.B], writes=[out_f32.B])
        if out_bf is not None:
            P.op("scalar", lambda e: e.activation(out=out_bf[:], in_=out_f32[:], func=AF.Copy), reads=[out_f32.B], writes=[out_bf.B])
    else:
        P.op("vector", lambda e: e.tensor_tensor(out=out_bf[:], in0=XN[:], in1=bbc[:], op=ALU.add), reads=[XN.B, bbc.B], writes=[out_bf.B])


def transpose_to_fm(P, HB, dstT, ti, ps, identb, identB, pbase=0):
    for half in range(2):
        pst = ps[pbase + half]
        pv = pst.t[:].bitcast(BF16)
        for j in range(8):
            kc = half * 8 + j
            P.op("tensor", lambda e, kc=kc, j=j, pv=pv: e.transpose(out=pv[:, j * 128:(j + 1) * 128], in_=HB[:, kc * 128:(kc + 1) * 128], identity=identb),
                 reads=[HB.B, identB], writes=[pst.B])
        eng = "scalar" if half == 0 else "vector"
        dst = dstT[:, half * 8:(half + 1) * 8, ti * 128:(ti + 1) * 128]
        src = pv.rearrange("p (j t) -> p j t", t=128)
        if eng == "scalar":
            P.op("scalar", lambda e, dst=dst, src=src: e.activation(out=dst, in_=src, func=AF.Copy), reads=[pst.B], writes=[dstT.b[ti]])
        else:
            P.op("vector", lambda e, dst=dst, src=src: e.tensor_copy(out=dst, in_=src), reads=[pst.B], writes=[dstT.b[ti]])


def make_consts():
    c = np.zeros((8, 128, 128), np.float32)
    i = np.arange(128)
    c[0] = np.eye(128)
    c[1] = (i[:, None] <= i[None, :]) * -EM05
    c[2] = (i[:, None] < i[None, :]) * -EM05
    c[3] = (i[:, None] > i[None, :]) * -EM05
    c[4] = (i[None, :] > i[:, None])
    c[5] = (i[None, :] >= i[:, None])
    c[6] = (i[:, None] > i[None, :])
    c[7] = (i[:, None] // 64 == i[None, :] // 64)
    n = np.arange(-127, 256)
    nn = np.maximum(n, 0)
    nf = np.maximum(nn, 1).astype(np.float32)
    large = 16 + (np.log(nf / 16) / math.log(128 / 16) * 16).astype(np.int32)
    large = np.minimum(large, 31)
    bucket = np.where(nn < 16, nn, large)
    oh = np.zeros((33, 383), np.float32)
    oh[bucket, np.arange(383)] = 1.0
    oh[:32, n < 0] = 0.0
    oh[32, n < 0] = 1.0
    return c, oh


_CACHE = {}


def kernel(**inputs):
    dbg = inputs.pop("_dbg", None)
    stop = inputs.pop("_stop", 99)
    key = ("nc", dbg, stop)
    if key not in _CACHE:
        _CACHE[key] = build(dbg, stop)
    nc = _CACHE[key]
    x = np.asarray(inputs["x"], np.float32)
    p = np.asarray(inputs["p"], np.float32)
    cm, oh = make_consts()
    shared = {"cmat": cm, "onehot": oh}
    for k, v in inputs.items():
        if k in ("x", "p"):
            continue
        a = np.asarray(v, np.float32)
        if a.shape[0] == 1 and k not in ("ln_in_g", "ln_in_b"):
            a = a[0]
        if k in ("moe_w_gate", "moe_w_up", "moe_w_down"):
            a = a.reshape(32, a.shape[-2], a.shape[-1])
            if stop < 6:
                a = a[:1]
        if k in ("rwkv_r_k",):
            a = a.reshape(-1)
        shared[k] = np.ascontiguousarray(a)
    in_maps = []
    for c in range(8):
        b, s = c // 2, c % 2
        seq = np.zeros((2048, 2048), np.float32)
        if s == 1:
            seq[:] = x[b]
        else:
            seq[1024:] = x[b, :1024]
        m = dict(shared)
        m["xs"] = seq
        m["p"] = np.ascontiguousarray(p[0, b, s * 1024:(s + 1) * 1024])
        m["flag"] = np.full((128, 1), float(s), np.float32)
        in_maps.append(m)
    res = run_bass_kernel_spmd(nc, in_maps, core_ids=list(range(8)))
    if dbg:
        return [(r["dbg"], r["out"]) for r in res.results]
    out = np.zeros((4, 2048, 2048), np.float32)
    for c in range(8):
        b, s = c // 2, c % 2
        out[b, s * 1024:(s + 1) * 1024] = res.results[c]["out"]
    return out
```
